# Optimizing a Trainium2 kernel written in Bass

```python
import jax
import jax.numpy as jnp
from jax import lax
import numpy as np

D_MODEL = 1024
BATCH = 8
SEQ = 2048
DEPTH = 4

GRID_W = 64
CTX_LEN = 256
N_EVEN = (DEPTH + 1) // 2
N_ODD = DEPTH // 2
MIX_W = D_MODEL
CHUNK = 128
A_GROUPS = 8
A_GROUP_DIM = MIX_W // A_GROUPS
CONV_W = 31
N_HEADS = 16
N_KV_HEADS = 4
HEAD_DIM = D_MODEL // N_HEADS
KV_GROUP = N_HEADS // N_KV_HEADS
WINDOW = 128
BLOCK = 128
ROPE_THETA = 10000.0
ROPE_AXIS_DIM = HEAD_DIM // 2
D_FF = 3584
N_EXPERTS = 8
TOP_K = 2
EPS = 1e-6
NEG_INF = -1e30

kernel_name = 'hybrid_dit_gmlp_conformer_swa_moe'


def rms_norm(x, g):
    xf = x.astype(jnp.float32)
    y = xf * lax.rsqrt(jnp.mean(xf * xf, axis=-1, keepdims=True) + EPS)
    return (y * g.astype(jnp.float32)).astype(x.dtype)


def layer_norm(x, g, b):
    xf = x.astype(jnp.float32)
    mu = jnp.mean(xf, axis=-1, keepdims=True)
    xc = xf - mu
    y = xc * lax.rsqrt(jnp.mean(xc * xc, axis=-1, keepdims=True) + EPS)
    return (y * g.astype(jnp.float32) + b.astype(jnp.float32)).astype(x.dtype)


def ada_mod(cvec, w, b):
    m = jax.nn.silu(cvec) @ w + b
    m = m.reshape(-1, 1, 6 * D_MODEL)
    return jnp.split(m, 6, axis=-1)


def rope_1d(x, ang):
    x1, x2 = jnp.split(x, 2, axis=-1)
    shp = (1, ang.shape[0]) + (1,) * (x.ndim - 3) + (ang.shape[1],)
    cos = jnp.cos(ang).reshape(shp).astype(x.dtype)
    sin = jnp.sin(ang).reshape(shp).astype(x.dtype)
    return jnp.concatenate([x1 * cos - x2 * sin, x2 * cos + x1 * sin], axis=-1)


def rope_2d(x, ang_row, ang_col):
    xr, xc = jnp.split(x, 2, axis=-1)
    return jnp.concatenate([rope_1d(xr, ang_row), rope_1d(xc, ang_col)], axis=-1)


def even_mixer(h, w_in, ln_g, ln_b, ws, bs, conv_w, conv_b, cn_g, w_out):
    bsz, length, _ = h.shape
    z = h @ w_in
    za, zb = z[..., :2 * MIX_W], z[..., 2 * MIX_W:]
    za = jax.nn.gelu(za)
    u, v = jnp.split(za, 2, axis=-1)
    v = layer_norm(v, ln_g, ln_b)
    v = v.reshape(bsz, length // CHUNK, CHUNK, A_GROUPS, A_GROUP_DIM)
    v = jnp.einsum('gpq,bnqgc->bnpgc', ws, v) + bs.T[None, None, :, :, None]
    y_a = u * v.reshape(bsz, length, MIX_W)
    a, gt = jnp.split(zb, 2, axis=-1)
    g = a * jax.nn.sigmoid(gt)
    g = lax.conv_general_dilated(
        g, conv_w[:, None, :], window_strides=(1,),
        padding=[(CONV_W // 2, CONV_W // 2)],
        dimension_numbers=('NWC', 'WIO', 'NWC'),
        feature_group_count=MIX_W) + conv_b
    y_b = jax.nn.silu(rms_norm(g, cn_g))
    return jnp.concatenate([y_a, y_b], axis=-1) @ w_out


def sink_softmax(scores, sink_g):
    lead = scores[0].shape[:-1]
    sink_col = jnp.broadcast_to(sink_g[None, :, :, None, None], lead + (1,))
    p = jax.nn.softmax(jnp.concatenate(list(scores) + [sink_col], axis=-1), axis=-1)
    cuts, acc = [], 0
    for s in scores[:-1]:
        acc += s.shape[-1]
        cuts.append(acc)
    return jnp.split(p[..., :-1], cuts, axis=-1)


def window_attention(hl, hc, w_qkv, q_g, k_g, sink, w_o, ang_row, ang_col, need_ctx):
    bsz, s_len, _ = hl.shape
    c_len = hc.shape[1]
    n_q = N_HEADS * HEAD_DIM
    n_kv = N_KV_HEADS * HEAD_DIM
    scale = HEAD_DIM ** -0.5
    sink_g = sink.astype(jnp.float32).reshape(N_KV_HEADS, KV_GROUP)

    def split_q(z, length):
        return rms_norm(z[..., :n_q].reshape(bsz, length, N_KV_HEADS, KV_GROUP, HEAD_DIM), q_g)

    def split_kv(z, length):
        k = rms_norm(z[..., n_q:n_q + n_kv].reshape(bsz, length, N_KV_HEADS, HEAD_DIM), k_g)
        v = z[..., n_q + n_kv:].reshape(bsz, length, N_KV_HEADS, HEAD_DIM)
        return k, v

    zl = hl @ w_qkv
    zc = hc @ w_qkv
    ql = rope_2d(split_q(zl, s_len), ang_row, ang_col)
    kl, vl = split_kv(zl, s_len)
    kl = rope_2d(kl, ang_row, ang_col)
    kc, vc = split_kv(zc, c_len)

    kpad = jnp.pad(kl, ((0, 0), (BLOCK, BLOCK), (0, 0), (0, 0)))
    vpad = jnp.pad(vl, ((0, 0), (BLOCK, BLOCK), (0, 0), (0, 0)))
    n_blocks = s_len // BLOCK

    def block(i):
        q0 = i * BLOCK
        qb = lax.dynamic_slice_in_dim(ql, q0, BLOCK, axis=1)
        kb = lax.dynamic_slice_in_dim(kpad, q0, 3 * BLOCK, axis=1)
        vb = lax.dynamic_slice_in_dim(vpad, q0, 3 * BLOCK, axis=1)
        s_win = jnp.einsum('bqkgd,bskd->bkgqs', qb, kb).astype(jnp.float32) * scale
        qpos = q0 + jnp.arange(BLOCK)
        kpos = q0 - BLOCK + jnp.arange(3 * BLOCK)
        valid = (kpos[None, :] >= 0) & (kpos[None, :] < s_len) & (jnp.abs(qpos[:, None] - kpos[None, :]) <= WINDOW)
        s_win = jnp.where(valid, s_win, NEG_INF)
        s_ctx = jnp.einsum('bqkgd,bskd->bkgqs', qb, kc).astype(jnp.float32) * scale
        p_win, p_ctx = sink_softmax([s_win, s_ctx], sink_g)
        o = (jnp.einsum('bkgqs,bskd->bqkgd', p_win.astype(vb.dtype), vb)
             + jnp.einsum('bkgqs,bskd->bqkgd', p_ctx.astype(vc.dtype), vc))
        return o.reshape(bsz, BLOCK, n_q)

    o_lat = lax.map(block, jnp.arange(n_blocks))
    y_lat = jnp.moveaxis(o_lat, 0, 1).reshape(bsz, s_len, n_q) @ w_o

    y_ctx = None
    if need_ctx:
        qc = split_q(zc, c_len)
        s_c = jnp.einsum('bqkgd,bskd->bkgqs', qc, kc).astype(jnp.float32) * scale
        (p_c,) = sink_softmax([s_c], sink_g)
        o_c = jnp.einsum('bkgqs,bskd->bqkgd', p_c.astype(vc.dtype), vc)
        y_ctx = o_c.reshape(bsz, c_len, n_q) @ w_o
    return y_lat, y_ctx


def swiglu(h, w1, w3, w2):
    return (jax.nn.silu(h @ w1) * (h @ w3)) @ w2


def moe_swiglu(h, router, w1, w3, w2):
    logits = (h @ router).astype(jnp.float32)
    vals, idx = lax.top_k(logits, TOP_K)
    wts = jax.nn.softmax(vals, axis=-1)
    combine = jnp.sum(jax.nn.one_hot(idx, N_EXPERTS, dtype=jnp.float32) * wts[..., None], axis=-2).astype(h.dtype)
    y = jnp.zeros_like(h)
    for e in range(N_EXPERTS):
        y = y + combine[..., e:e + 1] * swiglu(h, w1[e], w3[e], w2[e])
    return y


def _normal(key, shape, scale):
    return jax.random.normal(key, shape, dtype=jnp.float32) * scale


def setup_inputs(seed: int = 0) -> dict:
    key = jax.random.key(seed)
    ks = jax.random.split(key, 29)
    D = D_MODEL
    return {
        'x': _normal(ks[0], (BATCH, SEQ, D), 1.0),
        'c': _normal(ks[1], (BATCH, D), 1.0),
        'ctx': _normal(ks[2], (BATCH, CTX_LEN, D), 1.0),
        'c_ctx': _normal(ks[3], (D,), 1.0),
        'ada_w': _normal(ks[4], (DEPTH, D, 6 * D), 0.5 * D ** -0.5),
        'ada_b': _normal(ks[5], (DEPTH, 6 * D), 0.02),
        'norm_mix_g': 1.0 + _normal(ks[6], (DEPTH, D), 0.05),
        'norm_ffn_g': 1.0 + _normal(ks[7], (DEPTH, D), 0.05),
        'ev_w_in': _normal(ks[8], (N_EVEN, D, 4 * MIX_W), D ** -0.5),
        'ev_ln_g': 1.0 + _normal(ks[9], (N_EVEN, MIX_W), 0.05),
        'ev_ln_b': _normal(ks[10], (N_EVEN, MIX_W), 0.02),
        'ev_ws': _normal(ks[11], (N_EVEN, A_GROUPS, CHUNK, CHUNK), CHUNK ** -0.5),
        'ev_bs': 1.0 + _normal(ks[12], (N_EVEN, A_GROUPS, CHUNK), 0.05),
        'ev_conv_w': _normal(ks[13], (N_EVEN, CONV_W, MIX_W), CONV_W ** -0.5),
        'ev_conv_b': _normal(ks[14], (N_EVEN, MIX_W), 0.02),
        'ev_cnorm_g': 1.0 + _normal(ks[15], (N_EVEN, MIX_W), 0.05),
        'ev_w_out': _normal(ks[16], (N_EVEN, 2 * MIX_W, D), (2 * MIX_W) ** -0.5),
        'od_w_qkv': _normal(ks[17], (N_ODD, D, (N_HEADS + 2 * N_KV_HEADS) * HEAD_DIM), D ** -0.5),
        'od_q_g': 1.0 + _normal(ks[18], (N_ODD, HEAD_DIM), 0.05),
        'od_k_g': 1.0 + _normal(ks[19], (N_ODD, HEAD_DIM), 0.05),
        'od_sink': _normal(ks[20], (N_ODD, N_HEADS), 0.5),
        'od_w_o': _normal(ks[21], (N_ODD, N_HEADS * HEAD_DIM, D), (N_HEADS * HEAD_DIM) ** -0.5),
        'ff_w1': _normal(ks[22], (N_EVEN, D, D_FF), D ** -0.5),
        'ff_w3': _normal(ks[23], (N_EVEN, D, D_FF), D ** -0.5),
        'ff_w2': _normal(ks[24], (N_EVEN, D_FF, D), D_FF ** -0.5),
        'moe_router': _normal(ks[25], (N_ODD, D, N_EXPERTS), D ** -0.5),
        'moe_w1': _normal(ks[26], (N_ODD, N_EXPERTS, D, D_FF), D ** -0.5),
        'moe_w3': _normal(ks[27], (N_ODD, N_EXPERTS, D, D_FF), D ** -0.5),
        'moe_w2': _normal(ks[28], (N_ODD, N_EXPERTS, D_FF, D), D_FF ** -0.5),
    }


def reference(x, c, ctx, c_ctx, ada_w, ada_b, norm_mix_g, norm_ffn_g,
              ev_w_in, ev_ln_g, ev_ln_b, ev_ws, ev_bs, ev_conv_w, ev_conv_b, ev_cnorm_g, ev_w_out,
              od_w_qkv, od_q_g, od_k_g, od_sink, od_w_o,
              ff_w1, ff_w3, ff_w2, moe_router, moe_w1, moe_w3, moe_w2):
    s_len = x.shape[1]
    ROWS = s_len // GRID_W
    pos_row = jnp.repeat(jnp.arange(ROWS), GRID_W).astype(jnp.float32)
    pos_col = jnp.tile(jnp.arange(GRID_W), ROWS).astype(jnp.float32)
    inv_freq = ROPE_THETA ** (-jnp.arange(0, ROPE_AXIS_DIM, 2, dtype=jnp.float32) / ROPE_AXIS_DIM)
    ang_row = pos_row[:, None] * inv_freq[None, :]
    ang_col = pos_col[:, None] * inv_freq[None, :]

    xl, xc = x, ctx
    for li in range(DEPTH):
        need_ctx = li < DEPTH - 1
        j = li // 2
        sh1, sc1, g1, sh2, sc2, g2 = ada_mod(c, ada_w[li], ada_b[li])
        csh1, csc1, cg1, csh2, csc2, cg2 = ada_mod(c_ctx, ada_w[li], ada_b[li])

        hl = rms_norm(xl, norm_mix_g[li]) * (1 + sc1) + sh1
        hc = rms_norm(xc, norm_mix_g[li]) * (1 + csc1) + csh1
        if li % 2 == 0:
            p = (ev_w_in[j], ev_ln_g[j], ev_ln_b[j], ev_ws[j], ev_bs[j],
                 ev_conv_w[j], ev_conv_b[j], ev_cnorm_g[j], ev_w_out[j])
            yl = even_mixer(hl, *p)
            yc = even_mixer(hc, *p) if need_ctx else None
        else:
            yl, yc = window_attention(hl, hc, od_w_qkv[j], od_q_g[j], od_k_g[j], od_sink[j], od_w_o[j],
                                      ang_row, ang_col, need_ctx)
        xl = xl + g1 * yl
        if need_ctx:
            xc = xc + cg1 * yc

        hl = rms_norm(xl, norm_ffn_g[li]) * (1 + sc2) + sh2
        if li % 2 == 0:
            xl = xl + g2 * swiglu(hl, ff_w1[j], ff_w3[j], ff_w2[j])
        else:
            xl = xl + g2 * moe_swiglu(hl, moe_router[j], moe_w1[j], moe_w3[j], moe_w2[j])
        if need_ctx:
            hc = rms_norm(xc, norm_ffn_g[li]) * (1 + csc2) + csh2
            if li % 2 == 0:
                xc = xc + cg2 * swiglu(hc, ff_w1[j], ff_w3[j], ff_w2[j])
            else:
                xc = xc + cg2 * moe_swiglu(hc, moe_router[j], moe_w1[j], moe_w3[j], moe_w2[j])
    return xl
```

```python
import numpy as np
import concourse.bass as bass
import concourse.mybir as mybir
from concourse.bass_utils import run_bass_kernel_spmd
from contextlib import ExitStack

F32 = mybir.dt.float32
BF16 = mybir.dt.bfloat16
AF = mybir.ActivationFunctionType
ALU = mybir.AluOpType
AX = mybir.AxisListType

NL, NCX, NT = 2048, 256, 2304
D, DFF, NE = 1024, 3584, 8
EPS = 1e-6
BLK_L = [(0, 512, 0), (512, 512, 0), (1024, 512, 0), (1536, 512, 0)]
BLK_C = [(2048, 256, 1)]
SEMLIM = 10 ** 9


class Tl:
    __slots__ = ("name", "w", "r", "dsem", "dcnt")

    def __init__(s, name):
        s.name = name
        s.w = None
        s.r = {}
        s.dsem = None
        s.dcnt = 0


class Eng:
    def __init__(s, name, h):
        s.name, s.h, s.sem, s.cnt, s.waited = name, h, None, 0, {}


class K:
    def __init__(s, nc, es):
        s.nc, s.es = nc, es
        s.E = {"pe": Eng("pe", nc.tensor), "act": Eng("act", nc.scalar), "dve": Eng("dve", nc.vector),
               "pool": Eng("pool", nc.gpsimd), "sp": Eng("sp", nc.sync)}
        s.nsem = 0
        for e in s.E.values():
            e.sem = s.newsem(e.name)
        s.dsems = {}
        s.psum = []
        s.psi = 0
        s.npool = 8

    def newsem(s, name):
        s.nsem += 1
        return s.es.enter_context(s.nc.semaphore(f"{name}_{s.nsem}"))

    def _need(s, E, deps, embed=False):
        todo = []
        for num, (h, v) in deps.items():
            if E.waited.get(num, 0) < v:
                todo.append((h, v))
                E.waited[num] = v
        last = None
        if embed and todo:
            last = todo.pop()
        for (h, v) in todo:
            E.h.wait_ge(h, v)
        return last

    @staticmethod
    def _deps(reads, writes):
        d = {}

        def add(rec):
            h, v = rec
            if h.num not in d or d[h.num][1] < v:
                d[h.num] = rec
        for t in reads:
            if t.w:
                add(t.w)
        for t in writes:
            if t.w:
                add(t.w)
            for rec in t.r.values():
                add(rec)
        return d

    @staticmethod
    def _mark(rec, reads, writes):
        for t in reads:
            t.r[rec[0].num] = rec
        for t in writes:
            t.w = rec
            t.r = {}

    def _rot(s, E):
        if E.cnt >= SEMLIM:
            E.sem = s.newsem(E.name)
            E.cnt = 0

    def op(s, en, fn, reads=(), writes=()):
        E = s.E[en]
        last = s._need(E, s._deps(reads, writes), embed=True)
        ins = fn(E.h)
        if last is not None:
            ins._wait_ge(last[0], last[1])
        E.cnt += 1
        ins.then_inc(E.sem, 1)
        s._mark((E.sem, E.cnt), reads, writes)
        s._rot(E)

    def mm(s, reads, writes, items):
        E = s.E["pe"]
        last = s._need(E, s._deps(reads, writes), embed=True)
        ins = None
        for it in items:
            ins = E.h.matmul(**it)
            if last is not None:
                ins._wait_ge(last[0], last[1])
                last = None
        E.cnt += 1
        ins.then_inc(E.sem, 1)
        s._mark((E.sem, E.cnt), reads, writes)
        s._rot(E)

    def dma(s, q, out, in_, reads=(), writes=(), semt=None):
        E = s.E[q]
        s._need(E, s._deps(reads, writes))
        ins = E.h.dma_start(out=out, in_=in_)
        t = semt if semt is not None else writes[0]
        if t.dsem is None:
            t.dsem = s.newsem("d" + t.name)
        t.dcnt += 16
        ins.then_inc(t.dsem, 16)
        s.dsems[t.dsem.num] = (t.dsem, t.dcnt)
        s._mark((t.dsem, t.dcnt), reads, writes)

    def barrier(s, engines=("pe", "act", "dve", "pool", "sp"), own=False):
        for en in engines:
            E = s.E[en]
            d = {}
            for F in s.E.values():
                if (own or F is not E) and F.cnt > 0:
                    d[F.sem.num] = (F.sem, F.cnt)
            d.update(s.dsems)
            s._need(E, d)

    def region_begin(s):
        for n, E in s.E.items():
            d = {}
            if E.cnt > 0:
                d[E.sem.num] = (E.sem, E.cnt)
            if n == "sp":
                d.update(s.dsems)
            s._need(E, d)
        s._snap = ({n: (E.sem, E.cnt) for n, E in s.E.items()}, dict(s.dsems), {n: dict(E.waited) for n, E in s.E.items()})

    def region_end(s):
        e0, d0, w0 = s._snap
        deltas = []
        for n, E in s.E.items():
            assert E.sem.num == e0[n][0].num, "semaphore rotated inside region"
            if E.cnt > e0[n][1]:
                deltas.append((n, E.sem, E.cnt - e0[n][1]))
        for num, (h, v) in s.dsems.items():
            v0 = d0[num][1] if num in d0 else 0
            if v > v0:
                deltas.append(("sp", h, v - v0))
        for n, E in s.E.items():
            E.waited = w0[n]
        return deltas

    def ps(s):
        p = s.psum[s.psi % s.npool]
        s.psi += 1
        return p


def build():
    nc = bass.Bass("TRN2", target_bir_lowering=False)

    def din(name, shape):
        return nc.dram_tensor(name, list(shape), F32, kind="ExternalInput").ap()

    xin = din("xin", [1024, NT])
    cv_d = din("cv", [128, 16])
    ada_w = din("ada_w", [4, 1024, 6144])
    ada_b = din("ada_b", [4, 128, 48])
    ng1_d = din("ng1", [4, 128, 8])
    ng2_d = din("ng2", [4, 128, 8])
    w_in = din("ev_w_in", [2, 1024, 4096])
    lngf_d = din("ev_ln_g", [2, 128, 8])
    lnbf_d = din("ev_ln_b", [2, 128, 8])
    wsT_d = din("ev_wsT", [2, 8, 128, 128])
    bs_d = din("ev_bs", [2, 1024])
    convw_d = din("ev_conv_w", [2, 128, 248])
    convb_d = din("ev_conv_b", [2, 128, 8])
    cng_d = din("ev_cnorm_g", [2, 128, 8])
    w_out = din("ev_w_out", [2, 2048, 1024])
    wqkv = din("od_w_qkv", [2, 1024, 1536])
    qg_d = din("od_q_g", [2, 128, 1])
    kg_d = din("od_k_g", [2, 128, 1])
    sink_d = din("od_sink", [2, 16])
    wo_d = din("od_w_o", [2, 1024, 1024])
    ffw1 = din("ff_w1", [2, 1024, DFF])
    ffw3 = din("ff_w3", [2, 1024, DFF])
    ffw2 = din("ff_w2", [2, DFF, 1024])
    router_d = din("moe_router", [2, 1024, 8])
    mw1 = din("moe_w1", [2, 8, 1024, DFF])
    mw3 = din("moe_w3", [2, 8, 1024, DFF])
    mw2 = din("moe_w2", [2, 8, DFF, 1024])
    ident_d = din("c_ident", [128, 128])
    perm_d = din("c_perm", [128, 128])
    bd_d = din("c_bd", [128, 128])
    mask_d = din("c_mask", [128, 384])
    pos_d = din("c_pos", [128, 2048])
    fidx_d = din("c_fidx", [128, 1])
    triu_d = din("c_triu", [128, 128])
    iota_d = din("c_iota", [128, 384])
    slotid_d = din("c_slotid", [128, 18])
    h_tm_d = nc.dram_tensor("h_tm_scratch", [18, 128, 1024], BF16, kind="Internal").ap()
    out_d = nc.dram_tensor("out", [1024, NT], F32, kind="ExternalOutput").ap()

    with ExitStack() as es:
        k = K(nc, es)

        def sb(name, shape, dt):
            return es.enter_context(nc.sbuf_tensor("sb_" + name, list(shape), dt))

        RX = sb("RX", [128, 8 * NT], F32)
        RH = sb("RH", [128, 8 * NT], BF16)
        RA = sb("RA", [128, 8 * NT], BF16)
        RW = sb("RW", [128, 12288], BF16)
        xT = RX[:, :].rearrange("p (c t) -> p c t", c=8)
        hT = RH[:, :].rearrange("p (c t) -> p c t", c=8)
        for i in range(8):
            k.psum.append((es.enter_context(nc.psum_tensor(f"ps{i}", [128, 512], F32)), Tl(f"ps{i}")))

        ident = sb("ident", [128, 128], F32)
        ones_bf = sb("ones_bf", [128, 128], BF16)
        ones_f = sb("ones_f", [16, 128], F32)
        triu_bf = sb("triu_bf", [128, 128], BF16)
        ident_bf = sb("ident_bf", [128, 128], BF16)
        perm_bf = sb("perm_bf", [128, 128], BF16)
        bd_bf = sb("bd_bf", [128, 128], BF16)
        mask_bf = sb("mask_bf", [128, 384], BF16)
        cv = sb("cv", [128, 16], F32)
        scv = sb("scv", [128, 16], BF16)
        adab = sb("adab", [128, 48], F32)
        mT = sb("mT", [128, 96], F32)
        mT3 = mT[:, :].rearrange("p (j v) -> p j v", v=2)
        A1 = sb("A1", [128, 16], F32)
        A2 = sb("A2", [128, 16], F32)
        A1v = A1[:, :].rearrange("p (c v) -> p c v", v=2)
        A2v = A2[:, :].rearrange("p (c v) -> p c v", v=2)
        ng1 = sb("ng1", [128, 8], F32)
        ng2 = sb("ng2", [128, 8], F32)
        tmp16 = sb("tmp16", [128, 16], F32)
        convw = sb("convw", [128, 248], F32)
        convb = sb("convb", [128, 8], F32)
        cng = sb("cng", [128, 8], F32)
        qg = sb("qg", [128, 1], F32)
        kg = sb("kg", [128, 1], F32)
        sinke = sb("sinke", [128, 16], F32)
        router_f = sb("router_f", [128, 64], F32)
        cossin = sb("cossin", [128, 2 * 2048], BF16)
        cosT = cossin[:, 0:2048]
        sinT = cossin[:, 2048:4096]
        sq = [sb(f"sq{i}", [128, 512], BF16) for i in range(2)]
        rs = [sb(f"rs{i}", [128, 512], F32) for i in range(2)]
        t32 = [sb(f"t32_{i}", [128, 512], F32) for i in range(2)]
        gt = [sb(f"gt{i}", [128, 512], BF16) for i in range(2)]
        tt = [sb(f"tt{i}", [128, 512], BF16) for i in range(2)]
        TMP = sb("TMP", [128, 2560], F32)
        sml = sb("sml", [128, 64], F32)

        t_sq = [Tl(f"sq{i}") for i in range(2)]
        t_rs = [Tl(f"rs{i}") for i in range(2)]
        t_t32 = [Tl(f"t32{i}") for i in range(2)]
        t_gt = [Tl(f"gt{i}") for i in range(2)]
        t_tt = [Tl(f"tt{i}") for i in range(2)]
        t_par = Tl("par")
        t_lpar = Tl("lpar")
        t_mT = Tl("mT")
        t_X = [Tl(f"X{i}") for i in range(5)]
        t_H = [Tl(f"H{i}") for i in range(5)]
        TW = [[Tl(f"w{a}_{i}") for i in range(3)] for a in range(2)]
        t_p2 = Tl("m2p")
        TU = [[Tl(f"u{a}_{b}") for b in range(5)] for a in range(2)]
        rot = {"sq": 0, "rs": 0, "t32": 0, "gt": 0, "tt": 0}

        def nxt(name, arr, tls):
            i = rot[name] % len(arr)
            rot[name] += 1
            return arr[i], tls[i]

        k.dma("sp", ident[:, :], ident_d[:, :], writes=[t_par])
        k.dma("sp", cv[:, :], cv_d[:, :], writes=[t_par])
        k.dma("pool", perm_bf[:, :], perm_d[:, :], writes=[t_par])
        k.dma("pool", bd_bf[:, :], bd_d[:, :], writes=[t_par])
        k.dma("pool", mask_bf[:, :], mask_d[:, :], writes=[t_par])
        k.dma("pool", triu_bf[:, :], triu_d[:, :], writes=[t_par])
        k.dma("pool", ident_bf[:, :], ident_d[:, :], writes=[t_par])
        t_one = Tl("ones")
        k.op("dve", lambda e: e.memset(ones_bf[:, :], 1.0), writes=[t_one])
        k.op("dve", lambda e: e.memset(ones_f[:, :], 1.0), writes=[t_one])
        for b in range(5):
            t0, n, _ = (BLK_L + BLK_C)[b]
            k.dma("sp", xT[:, :, t0:t0 + n], xin[:, t0:t0 + n].rearrange("(c p) t -> p c t", p=128), writes=[t_X[b]])
        k.op("act", lambda e: e.activation(out=scv[:, :], in_=cv[:, :], func=AF.Silu), reads=[t_par], writes=[t_one])
        scv3 = scv[:, :].rearrange("p (c v) -> p c v", v=2)

        W0 = RW[:, 0:6144]
        W1s = RW[:, 6144:12288]
        slots = [W0, W1s]

        def layer_params(li):
            t_w = [TW[0][0], TW[1][0]]
            k.dma("sp", adab[:, :], ada_b[li], writes=[t_lpar])
            k.dma("sp", ng1[:, :], ng1_d[li], writes=[t_lpar])
            k.dma("sp", ng2[:, :], ng2_d[li], writes=[t_lpar])
            psm, t_psm = k.ps()
            src = ada_w[li].rearrange("(c p) n -> p c n", p=128)
            for i in range(12):
                wv = slots[i % 2][:, 0:4096].rearrange("p (c n) -> p c n", c=8)
                k.dma("pool", wv, src[:, :, i * 512:(i + 1) * 512], writes=[t_w[i % 2]])
                for jj in range(4):
                    j = 4 * i + jj
                    k.mm([t_w[i % 2], t_one], [t_psm],
                         [dict(out=psm[:, 2 * j:2 * j + 2], lhsT=wv[:, c, jj * 128:(jj + 1) * 128], rhs=scv3[:, c, :],
                               start=(c == 0), stop=(c == 7)) for c in range(8)])
            ps3 = psm[:, 0:96].rearrange("p (j v) -> p j v", v=2)
            for v in range(2):
                k.op("dve", lambda e, v=v: e.tensor_tensor(out=mT3[:, :, v], in0=ps3[:, :, v], in1=adab[:, :], op=ALU.add),
                     reads=[t_psm, t_lpar], writes=[t_mT])
            t16 = tmp16[:, :].rearrange("p (c v) -> p c v", v=2)
            for (Av, ng, off) in ((A1v, ng1, 8), (A2v, ng2, 32)):
                k.op("dve", lambda e, off=off: e.tensor_scalar(out=t16, in0=mT3[:, off:off + 8, :], scalar1=1.0, scalar2=None, op0=ALU.add),
                     reads=[t_mT], writes=[t_lpar])
                for v in range(2):
                    k.op("dve", lambda e, v=v, Av=Av, ng=ng: e.tensor_tensor(out=Av[:, :, v], in0=t16[:, :, v], in1=ng[:, :], op=ALU.mult),
                         reads=[t_lpar], writes=[t_lpar])
            k.barrier()

        SH1, G1, SH2, G2 = mT3[:, 0:8, :], mT3[:, 16:24, :], mT3[:, 24:32, :], mT3[:, 40:48, :]

        def make_h(Av, SHv, blks, moe_j=None, combT=None, t_comb=None):
            for (t0, n, v) in blks:
                b = t0 // 512
                rsb, t_r = nxt("rs", rs, t_rs)
                psm, t_ps = k.ps()
                for c in range(8):
                    sqb, t_s = nxt("sq", sq, t_sq)
                    k.op("act", lambda e, c=c, sqb=sqb: e.activation(out=sqb[:, :n], in_=xT[:, c, t0:t0 + n], func=AF.Square),
                         reads=[t_X[b]], writes=[t_s])
                    k.mm([t_s, t_one], [t_ps], [dict(out=psm[:, :n], lhsT=ones_bf[:, :], rhs=sqb[:, :n], start=(c == 0), stop=(c == 7))])
                k.op("act", lambda e: e.activation(out=rsb[:, :n], in_=psm[:, :n], func=AF.Sqrt, scale=1.0 / D, bias=EPS),
                     reads=[t_ps], writes=[t_r])
                k.op("dve", lambda e: e.reciprocal(out=rsb[:, :n], in_=rsb[:, :n]), reads=[t_r], writes=[t_r])
                if moe_j is not None:
                    pslg, t_pslg = k.ps()
                    nsb = n // 128
                    k.mm([t_one], [t_pslg], [dict(out=pslg[:, 0:8 * nsb], lhsT=zer_bf[:, :], rhs=zer_bf[:, 0:8 * nsb], start=True, stop=False,
                                                skip_group_check=True)])
                for c in range(8):
                    tb, t_t = nxt("t32", t32, t_t32)
                    k.op("dve", lambda e, c=c, tb=tb: e.scalar_tensor_tensor(out=tb[:, :n], in0=xT[:, c, t0:t0 + n], scalar=Av[:, c, v:v + 1],
                                                                           in1=rsb[:, :n], op0=ALU.mult, op1=ALU.mult),
                         reads=[t_X[b], t_r, t_lpar], writes=[t_t])
                    if moe_j is None:
                        k.op("act", lambda e, c=c, tb=tb: e.activation(out=hT[:, c, t0:t0 + n], in_=tb[:, :n], func=AF.Identity,
                                                                     bias=SHv[:, c, v:v + 1]),
                             reads=[t_t, t_mT], writes=[t_H[b]])
                    else:
                        k.op("act", lambda e, c=c, tb=tb: e.activation(out=tb[:, :n], in_=tb[:, :n], func=AF.Identity,
                                                                     bias=SHv[:, c, v:v + 1]),
                             reads=[t_mT], writes=[t_t])
                        k.op("pool", lambda e, c=c, tb=tb: e.tensor_copy(out=hT[:, c, t0:t0 + n], in_=tb[:, :n]),
                             reads=[t_t], writes=[t_H[b]])
                        k.mm([t_t, t_lpar], [t_pslg],
                             [dict(out=pslg[:, 8 * s_:8 * s_ + 8], lhsT=tb[:, s_ * 128:(s_ + 1) * 128], rhs=router_f[:, 8 * c:8 * c + 8],
                                   start=False, stop=(c == 7), skip_group_check=True) for s_ in range(nsb)])
                if moe_j is not None:
                    for s_ in range(nsb):
                        lg = sml[:, 0:8]
                        mx = sml[:, 8:16]
                        ex = sml[:, 16:24]
                        msk = sml[:, 24:32]
                        nm1 = sml[:, 32:33]
                        den = sml[:, 33:34]
                        k.op("dve", lambda e: e.tensor_copy(out=lg, in_=pslg[:, 8 * s_:8 * s_ + 8]), reads=[t_pslg], writes=[t_sml])
                        k.op("dve", lambda e: e.max(out=mx, in_=lg), reads=[t_sml], writes=[t_sml])
                        k.op("dve", lambda e: e.tensor_scalar(out=nm1, in0=mx[:, 0:1], scalar1=-1.0, scalar2=None, op0=ALU.mult),
                             reads=[t_sml], writes=[t_sml])
                        k.op("act", lambda e: e.activation(out=ex, in_=lg, func=AF.Exp, bias=nm1), reads=[t_sml], writes=[t_sml])
                        k.op("dve", lambda e: e.tensor_scalar(out=msk, in0=lg, scalar1=mx[:, 1:2], scalar2=None, op0=ALU.is_ge),
                             reads=[t_sml], writes=[t_sml])
                        k.op("dve", lambda e: e.tensor_tensor(out=ex, in0=ex, in1=msk, op=ALU.mult), reads=[t_sml], writes=[t_sml])
                        k.op("dve", lambda e: e.reduce_sum(out=den, in_=ex, axis=AX.X), reads=[t_sml], writes=[t_sml])
                        k.op("dve", lambda e: e.reciprocal(out=den, in_=den), reads=[t_sml], writes=[t_sml])
                        k.op("dve", lambda e: e.tensor_scalar(out=ex, in0=ex, scalar1=den, scalar2=None, op0=ALU.mult),
                             reads=[t_sml], writes=[t_sml])
                        tbi = t0 // 128 + s_
                        k.op("dve", lambda e: e.tensor_copy(out=moe_j["comb_tm"][:, tbi, :], in_=ex), reads=[t_sml], writes=[moe_j["t_rt"]])
                        k.op("dve", lambda e: e.tensor_copy(out=moe_j["mask_tm"][:, tbi, :], in_=msk), reads=[t_sml], writes=[moe_j["t_rt"]])

        zer_bf = sb("zer_bf", [128, 128], BF16)
        k.op("dve", lambda e: e.memset(zer_bf[:, :], 0.0), writes=[t_one])
        t_sml = Tl("sml")

        def ffn(W1d, W3d, W2d, Gv, blks, tag, cb=None, t_cb=None, bar=True):
            NP = 14
            t_w = TW
            t_u = TU
            w1s = W1d.rearrange("(c p) n -> p c n", p=128)
            w3s = W3d.rearrange("(c p) n -> p c n", p=128)
            w2s = W2d.rearrange("(f p) n -> p f n", p=128)

            def views(s_):
                sl = slots[s_]
                return (sl[:, 0:2048].rearrange("p (c n) -> p c n", c=8), sl[:, 2048:4096].rearrange("p (c n) -> p c n", c=8),
                        sl[:, 4096:6144].rearrange("p (f n) -> p f n", f=2))

            def load(i):
                a, b_, c_ = views(i % 2)
                tw = t_w[i % 2]
                k.dma("pool", a, w1s[:, :, i * 256:(i + 1) * 256], writes=[tw[0]])
                k.dma("pool", b_, w3s[:, :, i * 256:(i + 1) * 256], writes=[tw[1]])
                k.dma("pool", c_, w2s[:, 2 * i:2 * i + 2, :], writes=[tw[2]])

            load(0)
            for i in range(NP):
                if i + 1 < NP:
                    load(i + 1)
                s_ = i % 2
                w1v, w3v, w2v = views(s_)
                tw = t_w[s_]
                uv = RA[:, s_ * 2 * NT:(s_ + 1) * 2 * NT].rearrange("p (f t) -> p f t", f=2)
                for fc in range(2):
                    for (t0, n, v) in blks:
                        b = t0 // 512
                        p1, t_p1 = k.ps()
                        p3, t_p3 = k.ps()
                        k.mm([tw[0], t_H[b]], [t_p1], [dict(out=p1[:, :n], lhsT=w1v[:, c, fc * 128:(fc + 1) * 128], rhs=hT[:, c, t0:t0 + n],
                                                          start=(c == 0), stop=(c == 7)) for c in range(8)])
                        k.mm([tw[1], t_H[b]], [t_p3], [dict(out=p3[:, :n], lhsT=w3v[:, c, fc * 128:(fc + 1) * 128], rhs=hT[:, c, t0:t0 + n],
                                                          start=(c == 0), stop=(c == 7)) for c in range(8)])
                        gb, t_g = nxt("gt", gt, t_gt)
                        k.op("act", lambda e: e.activation(out=gb[:, :n], in_=p1[:, :n], func=AF.Silu), reads=[t_p1], writes=[t_g])
                        if cb is None:
                            k.op("dve", lambda e: e.tensor_tensor(out=uv[:, fc, t0:t0 + n], in0=gb[:, :n], in1=p3[:, :n], op=ALU.mult),
                                 reads=[t_g, t_p3], writes=[t_u[s_][b]])
                        else:
                            tb_, t_t = nxt("tt", tt, t_tt)
                            k.op("dve", lambda e: e.tensor_tensor(out=tb_[:, :n], in0=gb[:, :n], in1=p3[:, :n], op=ALU.mult),
                                 reads=[t_g, t_p3], writes=[t_t])
                            k.op("pool", lambda e: e.tensor_tensor(out=uv[:, fc, t0:t0 + n], in0=tb_[:, :n], in1=cb[:, t0:t0 + n], op=ALU.mult),
                                 reads=[t_t, t_cb], writes=[t_u[s_][b]])
                for d in range(8):
                    for (t0, n, v) in blks:
                        b = t0 // 512
                        po, t_po = k.ps()
                        k.mm([tw[2], t_u[s_][b]], [t_po], [dict(out=po[:, :n], lhsT=w2v[:, fc, d * 128:(d + 1) * 128], rhs=uv[:, fc, t0:t0 + n],
                                                              start=(fc == 0), stop=(fc == 1)) for fc in range(2)])
                        k.op("dve", lambda e: e.scalar_tensor_tensor(out=xT[:, d, t0:t0 + n], in0=po[:, :n], scalar=Gv[:, d, v:v + 1],
                                                                    in1=xT[:, d, t0:t0 + n], op0=ALU.mult, op1=ALU.add),
                             reads=[t_po, t_mT, t_X[b]], writes=[t_X[b]])
            if bar:
                k.barrier()

        def proj_out(Wd_rows, yv, t_y, Gv, blks, tag):
            t_w = [TW[0][0], TW[1][0]]
            ws = Wd_rows.rearrange("(c p) n -> p c n", p=128)
            wv = [slots[i][:, 0:4096].rearrange("p (c n) -> p c n", c=4) for i in range(2)]
            for i in range(2):
                k.dma("pool", wv[i], ws[:, 4 * i:4 * i + 4, :], writes=[t_w[i]])
            for d in range(8):
                for (t0, n, v) in blks:
                    b = t0 // 512
                    po, t_po = k.ps()
                    k.mm([t_w[0], t_w[1], t_y[b]], [t_po],
                         [dict(out=po[:, :n], lhsT=wv[c // 4][:, c % 4, d * 128:(d + 1) * 128], rhs=yv[:, c, t0:t0 + n],
                               start=(c == 0), stop=(c == 7)) for c in range(8)])
                    k.op("dve", lambda e: e.scalar_tensor_tensor(out=xT[:, d, t0:t0 + n], in0=po[:, :n], scalar=Gv[:, d, v:v + 1],
                                                                in1=xT[:, d, t0:t0 + n], op0=ALU.mult, op1=ALU.add),
                         reads=[t_po, t_mT, t_X[b]], writes=[t_X[b]])
            k.barrier()

        def even_mixer(j, blks):
            yv = RA[:, :].rearrange("p (c t) -> p c t", c=8)
            t_y = [Tl(f"ya{j}_{b}") for b in range(5)]
            win = w_in[j].rearrange("(c p) n -> p c n", p=128)
            t_w = [TW[0][0], TW[1][0]]

            def wv1(s_):
                return slots[s_][:, 0:2048].rearrange("p (c n) -> p c n", c=8)
            k.dma("pool", wv1(0), win[:, :, 0:256], writes=[t_w[0]])
            for i in range(4):
                if i + 1 < 4:
                    k.dma("pool", wv1((i + 1) % 2), win[:, :, (i + 1) * 256:(i + 2) * 256], writes=[t_w[(i + 1) % 2]])
                for fc in range(2):
                    jc = 2 * i + fc
                    for (t0, n, v) in blks:
                        b = t0 // 512
                        p1, t_p1 = k.ps()
                        k.mm([t_w[i % 2], t_H[b]], [t_p1], [dict(out=p1[:, :n], lhsT=wv1(i % 2)[:, c, fc * 128:(fc + 1) * 128], rhs=hT[:, c, t0:t0 + n],
                                                              start=(c == 0), stop=(c == 7)) for c in range(8)])
                        k.op("act", lambda e: e.activation(out=yv[:, jc, t0:t0 + n], in_=p1[:, :n], func=AF.Gelu_apprx_tanh),
                             reads=[t_p1], writes=[t_y[b]])
            k.barrier()
            t_wv = [TW[0][0], TW[1][0]]
            wvv = [slots[i][:, 0:4096].rearrange("p (c n) -> p c n", c=8) for i in range(2)]
            for i in range(2):
                k.dma("pool", wvv[i], win[:, :, 1024 + i * 512:1024 + (i + 1) * 512], writes=[t_wv[i]])
            vgs = [slots[0][:, 4096:6144].bitcast(F32), slots[1][:, 4096:6144].bitcast(F32)]
            T2 = TMP[:, 0:1024]
            wsTv = TMP[:, 1024:1536].bitcast(BF16).rearrange("p (g q) -> p g q", g=8)
            bsb = TMP[:, 1536:2560]
            lgf = sml[:, 32:40]
            lbf = sml[:, 40:48]
            k.dma("sp", lgf, lngf_d[j], writes=[t_p2])
            k.dma("sp", lbf, lnbf_d[j], writes=[t_p2])
            k.dma("sp", bsb, bs_d[j:j + 1, :].partition_broadcast(128), writes=[t_p2])
            k.dma("pool", wsTv, wsT_d[j].rearrange("g q p -> q g p"), writes=[t_p2])
            t_T2 = Tl("T2")
            for gb_ in range(2):
                pw_, t_pw_ = k.ps()
                for gg in range(4):
                    g_ = gb_ * 4 + gg
                    k.mm([t_p2, t_one], [t_pw_], [dict(out=pw_[:, gg * 128:(gg + 1) * 128], lhsT=ones_bf[:, :], rhs=wsTv[:, g_, :], start=True, stop=True)])
                for gg in range(4):
                    g_ = gb_ * 4 + gg
                    k.op("dve", lambda e: e.scalar_tensor_tensor(out=T2[:, g_ * 128:(g_ + 1) * 128], in0=pw_[:, gg * 128:(gg + 1) * 128], scalar=lbf[:, g_:g_ + 1],
                                                                in1=bsb[:, g_ * 128:(g_ + 1) * 128], op0=ALU.mult, op1=ALU.add), reads=[t_pw_, t_p2], writes=[t_T2])
            t_vgs = [Tl("vg0"), Tl("vg1")]
            t_st = [Tl("st0"), Tl("st1")]
            vb2 = [t32[0][:, :].bitcast(BF16), t32[1][:, :].bitcast(BF16)]
            ntb = [tb for (t0, n, v) in blks for tb in range(t0 // 128, (t0 + n) // 128)]
            for it_, tb in enumerate(ntb):
                b = min(tb // 4, 4)
                tk = tb * 128
                par = it_ % 2
                vg, t_vg = vgs[par], t_vgs[par]
                pv = []
                for h_ in range(2):
                    p_, t_p = k.ps()
                    k.mm([t_wv[h_], t_H[b]], [t_p], [dict(out=p_[:, :], lhsT=hT[:, c, tk:tk + 128], rhs=wvv[h_][:, c, :],
                                                        start=(c == 0), stop=(c == 7)) for c in range(8)])
                    pv.append((p_, t_p))
                for h_ in range(2):
                    k.op("act", lambda e, h_=h_: e.activation(out=vg[:, h_ * 512:(h_ + 1) * 512], in_=pv[h_][0][:, :], func=AF.Gelu_apprx_tanh),
                         reads=[pv[h_][1]], writes=[t_vg])
                so = par * 16
                st = sml[:, so:so + 12].rearrange("p (a b) -> p a b", a=2)
                mv = sml[:, so + 12:so + 14]
                rstd = sml[:, so + 14:so + 15]
                nmr = sml[:, so + 15:so + 16]
                t_s_ = t_st[par]
                for h_ in range(2):
                    k.op("dve", lambda e, h_=h_: e.bn_stats(out=st[:, h_, :], in_=vg[:, h_ * 512:(h_ + 1) * 512]), reads=[t_vg], writes=[t_s_])
                k.op("dve", lambda e: e.bn_aggr(out=mv, in_=sml[:, so:so + 12]), reads=[t_s_], writes=[t_s_])
                k.op("act", lambda e: e.activation(out=rstd, in_=mv[:, 1:2], func=AF.Sqrt, bias=EPS), reads=[t_s_], writes=[t_s_])
                k.op("dve", lambda e: e.reciprocal(out=rstd, in_=rstd), reads=[t_s_], writes=[t_s_])
                k.op("dve", lambda e: e.scalar_tensor_tensor(out=nmr, in0=mv[:, 0:1], scalar=-1.0, in1=rstd, op0=ALU.mult, op1=ALU.mult),
                     reads=[t_s_], writes=[t_s_])
                vbf = vb2[par]
                t_v = t_t32[par]
                k.op("act", lambda e: e.activation(out=vbf, in_=vg, func=AF.Identity, scale=rstd, bias=nmr), reads=[t_s_, t_vg], writes=[t_v])
                for gb_ in range(2):
                    pg, t_pg = k.ps()
                    for gg in range(4):
                        g_ = gb_ * 4 + gg
                        k.mm([t_v, t_p2], [t_pg], [dict(out=pg[:, gg * 128:(gg + 1) * 128], lhsT=vbf[:, g_ * 128:(g_ + 1) * 128], rhs=wsTv[:, g_, :],
                                                      start=True, stop=True)])
                    tb_, t_t = nxt("rs", rs, t_rs)
                    for gg in range(4):
                        g_ = gb_ * 4 + gg
                        k.op("dve", lambda e: e.scalar_tensor_tensor(out=tb_[:, gg * 128:(gg + 1) * 128], in0=pg[:, gg * 128:(gg + 1) * 128], scalar=lgf[:, g_:g_ + 1],
                                                                    in1=T2[:, g_ * 128:(g_ + 1) * 128], op0=ALU.mult, op1=ALU.add),
                             reads=[t_pg, t_p2, t_T2], writes=[t_t])
                    yslice = yv[:, gb_ * 4:gb_ * 4 + 4, tk:tk + 128]
                    k.op("pool", lambda e: e.tensor_tensor(out=yslice, in0=yslice, in1=tb_[:, :].rearrange("p (g q) -> p g q", g=4), op=ALU.mult),
                         reads=[t_t], writes=[t_y[b]])
            k.barrier()
            proj_out(w_out[j, 0:1024, :], yv, t_y, G1, blks, f"m4a{j}")
            t_yb = [Tl(f"yb{j}_{b}") for b in range(5)]
            k.dma("sp", convw[:, :], convw_d[j], writes=[t_lpar])
            k.dma("sp", convb[:, :], convb_d[j], writes=[t_lpar])
            k.dma("sp", cng[:, :], cng_d[j], writes=[t_lpar])
            cw3 = convw[:, :].rearrange("p (c k) -> p c k", c=8)
            GW = 2078 + 286
            gbufs = [(slots[1][:, a_ * GW:a_ * GW + 2078], slots[1][:, a_ * GW + 2078:(a_ + 1) * GW]) for a_ in range(2)]
            t_gs = [Tl("g0"), Tl("g1")]
            Dg = TMP[:, 0:1984].bitcast(BF16).rearrange("p (k m) -> p k m", k=31)
            t_dg = Tl("dg")
            for a_ in range(2):
                k.op("pool", lambda e: e.memset(slots[1][:, a_ * GW:(a_ + 1) * GW], 0.0), writes=[t_gs[a_]])
            t_w3 = [TW[0][0], TW[0][1]]

            def wv3(s_):
                base = s_ * 2048
                return (slots[0][:, base:base + 1024].rearrange("p (c n) -> p c n", c=8),
                        slots[0][:, base + 1024:base + 2048].rearrange("p (c n) -> p c n", c=8))

            def load3(i):
                a_, g_ = wv3(i % 2)
                k.dma("pool", a_, win[:, :, 2048 + i * 128:2048 + (i + 1) * 128], writes=[t_w3[i % 2]])
                k.dma("pool", g_, win[:, :, 3072 + i * 128:3072 + (i + 1) * 128], writes=[t_w3[i % 2]], semt=t_w3[i % 2])

            def stage_proj(i):
                if i + 1 < 8:
                    load3(i + 1)
                a_, g_ = wv3(i % 2)
                gL, gC = gbufs[i % 2]
                for (t0, n, v) in blks:
                    b = t0 // 512
                    pa, t_pa = k.ps()
                    pg, t_pg = k.ps()
                    k.mm([t_w3[i % 2], t_H[b]], [t_pa], [dict(out=pa[:, :n], lhsT=a_[:, c, :], rhs=hT[:, c, t0:t0 + n], start=(c == 0), stop=(c == 7)) for c in range(8)])
                    k.mm([t_w3[i % 2], t_H[b]], [t_pg], [dict(out=pg[:, :n], lhsT=g_[:, c, :], rhs=hT[:, c, t0:t0 + n], start=(c == 0), stop=(c == 7)) for c in range(8)])
                    sg, t_sg = nxt("rs", rs, t_rs)
                    k.op("act", lambda e: e.activation(out=sg[:, :n], in_=pg[:, :n], func=AF.Sigmoid), reads=[t_pg], writes=[t_sg])
                    dst = gL[:, 15 + t0:15 + t0 + n] if v == 0 else gC[:, 15:15 + n]
                    k.op("dve", lambda e: e.tensor_tensor(out=dst, in0=sg[:, :n], in1=pa[:, :n], op=ALU.mult), reads=[t_sg, t_pa], writes=[t_gs[i % 2]])

            def stage_conv(i):
                gL, gC = gbufs[i % 2]
                for tap in range(31):
                    k.op("dve", lambda e: e.tensor_scalar(out=Dg[:, tap, :], in0=ident_bf[:, :], scalar1=cw3[:, i, tap:tap + 1], scalar2=None, op0=ALU.mult),
                         reads=[t_par, t_lpar], writes=[t_dg])
                for (t0, n, v) in blks:
                    b = t0 // 512
                    gbuf, o0 = (gL, t0) if v == 0 else (gC, 0)
                    pa_, t_pa_ = k.ps()
                    k.mm([t_dg, t_gs[i % 2]], [t_pa_], [dict(out=pa_[:, :n], lhsT=Dg[:, tap, :], rhs=gbuf[:, o0 + tap:o0 + tap + n], start=(tap == 0), stop=(tap == 30))
                                                        for tap in range(31)])
                    k.op("act", lambda e: e.activation(out=yv[:, i, t0:t0 + n], in_=pa_[:, :n], func=AF.Identity, bias=convb[:, i:i + 1]),
                         reads=[t_pa_, t_lpar], writes=[t_yb[b]])

            load3(0)
            stage_proj(0)
            for i in range(8):
                if i + 1 < 8:
                    stage_proj(i + 1)
                stage_conv(i)
            for (t0, n, v) in blks:
                b = t0 // 512
                rsb, t_r = nxt("rs", rs, t_rs)
                psm, t_ps = k.ps()
                for c in range(8):
                    sqb, t_s = nxt("sq", sq, t_sq)
                    k.op("act", lambda e: e.activation(out=sqb[:, :n], in_=yv[:, c, t0:t0 + n], func=AF.Square), reads=[t_yb[b]], writes=[t_s])
                    k.mm([t_s, t_one], [t_ps], [dict(out=psm[:, :n], lhsT=ones_bf[:, :], rhs=sqb[:, :n], start=(c == 0), stop=(c == 7))])
                k.op("act", lambda e: e.activation(out=rsb[:, :n], in_=psm[:, :n], func=AF.Sqrt, scale=1.0 / D, bias=EPS), reads=[t_ps], writes=[t_r])
                k.op("dve", lambda e: e.reciprocal(out=rsb[:, :n], in_=rsb[:, :n]), reads=[t_r], writes=[t_r])
                for c in range(8):
                    tb_, t_t = nxt("t32", t32, t_t32)
                    k.op("dve", lambda e: e.scalar_tensor_tensor(out=tb_[:, :n], in0=yv[:, c, t0:t0 + n], scalar=cng[:, c:c + 1], in1=rsb[:, :n],
                                                                op0=ALU.mult, op1=ALU.mult), reads=[t_yb[b], t_r, t_lpar], writes=[t_t])
                    k.op("act", lambda e: e.activation(out=yv[:, c, t0:t0 + n], in_=tb_[:, :n], func=AF.Silu), reads=[t_t], writes=[t_yb[b]])
            k.barrier()
            proj_out(w_out[j, 1024:2048, :], yv, t_yb, G1, blks, f"m4b{j}")


        I32 = mybir.dt.int32
        TWO_PI = 6.283185307179586

        def rope_tables():
            RAf = RA[:, 0:16384].bitcast(F32)
            y = RAf[:, 0:2048]
            yy = RAf[:, 2048:4096]
            kf = RAf[:, 4096:6144]
            ki = RAf[:, 6144:8192].bitcast(I32)
            fidx = sml[:, 40:41]
            invf = sml[:, 41:42]
            t_r = Tl("ropetmp")
            k.dma("sp", y, pos_d[:, :], writes=[t_r])
            k.dma("sp", fidx, fidx_d[:, :], writes=[t_r])
            k.op("act", lambda e: e.activation(out=invf, in_=fidx, func=AF.Exp, scale=-float(np.log(10000.0)) / 16.0), reads=[t_r], writes=[t_r])
            k.op("dve", lambda e: e.tensor_scalar(out=y, in0=y, scalar1=invf, scalar2=1.0 / TWO_PI, op0=ALU.mult, op1=ALU.mult), reads=[t_r], writes=[t_r])
            for shift, dst in ((0.0, sinT), (0.25, cosT)):
                k.op("dve", lambda e: e.tensor_scalar(out=yy, in0=y, scalar1=shift, scalar2=None, op0=ALU.add), reads=[t_r], writes=[t_r])
                k.op("dve", lambda e: e.tensor_copy(out=ki, in_=yy), reads=[t_r], writes=[t_r])
                k.op("dve", lambda e: e.tensor_copy(out=kf, in_=ki), reads=[t_r], writes=[t_r])
                k.op("dve", lambda e: e.tensor_tensor(out=yy, in0=yy, in1=kf, op=ALU.subtract), reads=[t_r], writes=[t_r])
                k.op("dve", lambda e: e.tensor_single_scalar(out=kf, in_=yy, scalar=0.5, op=ALU.is_gt), reads=[t_r], writes=[t_r])
                k.op("dve", lambda e: e.tensor_tensor(out=yy, in0=yy, in1=kf, op=ALU.subtract), reads=[t_r], writes=[t_r])
                k.op("dve", lambda e: e.tensor_single_scalar(out=kf, in_=yy, scalar=-0.5, op=ALU.is_lt), reads=[t_r], writes=[t_r])
                k.op("dve", lambda e: e.tensor_tensor(out=yy, in0=yy, in1=kf, op=ALU.add), reads=[t_r], writes=[t_r])
                k.op("act", lambda e: e.activation(out=dst, in_=yy, func=AF.Sin, scale=TWO_PI * (1.0 - 1e-6)), reads=[t_r], writes=[t_par])
            k.barrier()

        def attention(j, need_ctx):
            blks_q = BLK_L + (BLK_C if need_ctx else [])
            blks_a = BLK_L + BLK_C
            k.dma("sp", qg[:, :], qg_d[j], writes=[t_lpar])
            k.dma("sp", kg[:, :], kg_d[j], writes=[t_lpar])
            k.dma("sp", sinke[:, :], sink_d[j:j + 1, :].partition_broadcast(128), writes=[t_lpar])
            k.op("act", lambda e: e.activation(out=sinke[:, :], in_=sinke[:, :], func=AF.Exp), reads=[], writes=[t_lpar])
            k.npool = 6
            po, t_po = k.psum[6]
            pd, t_pd = k.psum[7]
            qT = RA[:, 0:2 * NT].rearrange("p (h t) -> p h t", h=2)
            kT = RA[:, 2 * NT:3 * NT]
            Vg = RA[:, 3 * NT:3 * NT + 1152].rearrange("p (b d) -> p b d", b=18)
            Pc = RA[:, 3 * NT + 1152:3 * NT + 1152 + 2 * NT].rearrange("p (b t) -> p b t", b=2)
            Pring = TMP[:, :].bitcast(BF16)
            NR = 8
            t_q = [[Tl(f"q{h}_{b}") for b in range(5)] for h in range(4)]
            t_k = [Tl(f"k{b}") for b in range(5)]
            t_v = [Tl(f"v{b}") for b in range(3)]
            t_pc = Tl("pc")
            t_pr = [Tl(f"pr{i}") for i in range(NR)]
            wsrc = wqkv[j].rearrange("(c p) n -> p c n", p=128)
            tq = [TW[0][0], TW[0][1]]
            tv = [TW[1][1], TW[1][2]]

            def wviews(s_):
                base = s_ * 3072
                return (slots[0][:, base:base + 2048].rearrange("p (c n) -> p c n", c=8),
                        slots[0][:, base + 2048:base + 3072].rearrange("p (c n) -> p c n", c=8),
                        slots[1][:, 2048 + s_ * 512:2048 + (s_ + 1) * 512].rearrange("p (c n) -> p c n", c=8))

            def loadw(g):
                a_, b_, c_ = wviews(g % 2)
                t_ = tq[g % 2]
                k.dma("pool", a_, wsrc[:, :, g * 256:(g + 1) * 256], writes=[t_])
                k.dma("pool", b_[:, :, 0:64], wsrc[:, :, 1024 + g * 64:1024 + (g + 1) * 64], writes=[t_])
                k.dma("pool", b_[:, :, 64:128], wsrc[:, :, 1024 + g * 64:1024 + (g + 1) * 64], writes=[t_])
                k.dma("pool", c_, wsrc[:, :, 1280 + g * 64:1280 + (g + 1) * 64], writes=[tv[g % 2]])
            wov = slots[1][:, 0:2048].rearrange("p (h n) -> p h n", h=2)
            t_wo = TW[1][0]

            def qk_chain(projitems, rd, n, gvec, dst, t_dsts, rope, t0):
                ps_ = k.ps()
                k.mm(rd, [ps_[1]], projitems(ps_[0]))
                yield
                sqb, t_s = nxt("sq", sq, t_sq)
                k.op("act", lambda e: e.activation(out=sqb[:, :n], in_=ps_[0][:, :n], func=AF.Square), reads=[ps_[1]], writes=[t_s])
                yield
                pss, t_pss = k.ps()
                k.mm([t_s, t_par], [t_pss], [dict(out=pss[:, :n], lhsT=bd_bf[:, :], rhs=sqb[:, :n], start=True, stop=True)])
                yield
                rsb, t_r = nxt("rs", rs, t_rs)
                k.op("act", lambda e: e.activation(out=rsb[:, :n], in_=pss[:, :n], func=AF.Sqrt, scale=1.0 / 64, bias=EPS), reads=[t_pss], writes=[t_r])
                yield
                k.op("dve", lambda e: e.reciprocal(out=rsb[:, :n], in_=rsb[:, :n]), reads=[t_r], writes=[t_r])
                yield
                qn, t_qn = nxt("t32", t32, t_t32)
                k.op("dve", lambda e: e.scalar_tensor_tensor(out=qn[:, :n], in0=ps_[0][:, :n], scalar=gvec[:, 0:1], in1=rsb[:, :n],
                                                            op0=ALU.mult, op1=ALU.mult), reads=[ps_[1], t_r, t_lpar], writes=[t_qn])
                yield
                if not rope:
                    k.op("act", lambda e: e.copy(out=dst, in_=qn[:, :n]), reads=[t_qn], writes=t_dsts)
                    return
                qb, t_qb = nxt("tt", tt, t_tt)
                k.op("pool", lambda e: e.tensor_copy(out=qb[:, :n], in_=qn[:, :n]), reads=[t_qn], writes=[t_qb])
                yield
                psr, t_psr = k.ps()
                k.mm([t_qb, t_par], [t_psr], [dict(out=psr[:, :n], lhsT=perm_bf[:, :], rhs=qb[:, :n], start=True, stop=True)])
                yield
                bb, t_bb = nxt("rs", rs, t_rs)
                k.op("dve", lambda e: e.tensor_tensor(out=bb[:, :n], in0=psr[:, :n], in1=sinT[:, t0:t0 + n], op=ALU.mult), reads=[t_psr, t_par], writes=[t_bb])
                k.op("dve", lambda e: e.tensor_tensor(out=qn[:, :n], in0=qn[:, :n], in1=cosT[:, t0:t0 + n], op=ALU.mult), reads=[t_par], writes=[t_qn])
                yield
                k.op("pool", lambda e: e.tensor_tensor(out=dst, in0=qn[:, :n], in1=bb[:, :n], op=ALU.add), reads=[t_qn, t_bb], writes=t_dsts)

            def lockstep(gens, width=2):
                for i0 in range(0, len(gens), width):
                    active = gens[i0:i0 + width]
                    while active:
                        alive = []
                        for g_ in active:
                            try:
                                next(g_)
                                alive.append(g_)
                            except StopIteration:
                                pass
                        active = alive

            loadw(0)
            for g in range(4):
                if g + 1 < 4:
                    loadw(g + 1)
                k.dma("pool", wov, wo_d[j][g * 256:(g + 1) * 256, :].rearrange("(h p) n -> p h n", p=128), writes=[t_wo])
                wq_, wk_, wv_ = wviews(g % 2)
                t_w = tq[g % 2]
                chains = []
                for (t0, n, v) in blks_a:
                    b = t0 // 512
                    chains.append(qk_chain(lambda pst, t0=t0, n=n: [dict(out=pst[:, :n], lhsT=wk_[:, c, :], rhs=hT[:, c, t0:t0 + n], start=(c == 0), stop=(c == 7)) for c in range(8)],
                                           [t_w, t_H[b]], n, kg, kT[:, t0:t0 + n], [t_k[b]], v == 0, t0))
                for pr in range(2):
                    for (t0, n, v) in blks_q:
                        b = t0 // 512
                        chains.append(qk_chain(lambda pst, t0=t0, n=n, pr=pr: [dict(out=pst[:, :n], lhsT=wq_[:, c, pr * 128:(pr + 1) * 128], rhs=hT[:, c, t0:t0 + n],
                                                                                 start=(c == 0), stop=(c == 7)) for c in range(8)],
                                               [t_w, t_H[b]], n, qg, qT[:, pr, t0:t0 + n], [t_q[2 * pr][b], t_q[2 * pr + 1][b]], v == 0, t0))
                lockstep(chains)
                for vb in range(3):
                    tbs = list(range(vb * 8, min(18, vb * 8 + 8)))
                    ps_ = k.ps()
                    for ii, tb in enumerate(tbs):
                        b = min(tb // 4, 4)
                        k.mm([tv[g % 2], t_H[b]], [ps_[1]], [dict(out=ps_[0][:, ii * 64:(ii + 1) * 64], lhsT=hT[:, c, tb * 128:(tb + 1) * 128], rhs=wv_[:, c, :],
                                                              start=(c == 0), stop=(c == 7)) for c in range(8)])
                    nb = len(tbs)
                    k.op("act", lambda e: e.copy(out=Vg[:, tbs[0]:tbs[0] + nb, :], in_=ps_[0][:, 0:nb * 64].rearrange("p (b d) -> p b d", b=nb)),
                         reads=[ps_[1]], writes=[t_v[vb]])
                for hh in range(4):
                    h = 4 * g + hh
                    pr = hh // 2
                    P0 = (hh % 2) * 64
                    P1 = P0 + 64
                    for kb in range(2):
                        for (t0, n, v) in blks_q:
                            b = t0 // 512
                            ps_ = k.ps()
                            k.mm([t_k[4], t_q[hh][b]], [ps_[1]], [dict(out=ps_[0][:, :n], lhsT=kT[P0:P1, 2048 + kb * 128:2048 + (kb + 1) * 128], rhs=qT[P0:P1, pr, t0:t0 + n],
                                                                    start=True, stop=True)])
                            k.op("act", lambda e: e.activation(out=Pc[:, kb, t0:t0 + n], in_=ps_[0][:, :n], func=AF.Exp, scale=0.125), reads=[ps_[1]], writes=[t_pc])
                    pinfo = {}

                    def pv(i):
                        col = (i % 4) * 128
                        srcs = [(Vg[:, 16 + kb, :], Pc[:, kb, i * 128:(i + 1) * 128], t_pc) for kb in range(2)]
                        for jb in (i - 1, i, i + 1):
                            if 0 <= jb <= 15:
                                pr_, q0_, t_ = pinfo[jb]
                                srcs.append((Vg[:, jb, :], pr_[:, i * 128 - q0_:i * 128 - q0_ + 128], t_))
                        rd = [t_v[0], t_v[1], t_v[2]] + [s_[2] for s_ in srcs]
                        k.mm(rd, [t_po], [dict(out=po[P0:P1, col:col + 128], lhsT=va, rhs=pa, start=(ii == 0), stop=(ii == len(srcs) - 1))
                                          for ii, (va, pa, _) in enumerate(srcs)])
                        k.mm(rd + [t_one], [t_pd], [dict(out=pd[P0:P1, col:col + 128], lhsT=ones_bf[:, 0:64], rhs=pa, start=(ii == 0), stop=(ii == len(srcs) - 1))
                                                    for ii, (va, pa, _) in enumerate(srcs)])
                        if i % 4 == 3:
                            m_ = i // 4
                            finish(m_ * 512, 512, m_)

                    def finish(t0, n, b):
                        dn, t_dn = nxt("rs", rs, t_rs)
                        k.op("dve", lambda e: e.tensor_scalar(out=dn[P0:P1, :n], in0=pd[P0:P1, :n], scalar1=sinke[P0:P1, h:h + 1], scalar2=None, op0=ALU.add),
                             reads=[t_pd, t_lpar], writes=[t_dn])
                        k.op("dve", lambda e: e.reciprocal(out=dn[P0:P1, :n], in_=dn[P0:P1, :n]), reads=[t_dn], writes=[t_dn])
                        k.op("dve", lambda e: e.tensor_tensor(out=qT[P0:P1, pr, t0:t0 + n], in0=po[P0:P1, :n], in1=dn[P0:P1, :n], op=ALU.mult),
                             reads=[t_po, t_dn], writes=[t_q[hh][b]])

                    for jb in range(16):
                        q0 = max(0, 128 * (jb - 1))
                        q1 = min(NL, 128 * (jb + 2))
                        n = q1 - q0
                        mo = q0 - 128 * (jb - 1)
                        ps_ = k.ps()
                        qb_ = sorted(set([q0 // 512, (q1 - 1) // 512]))
                        k.mm([t_k[jb // 4], t_par] + [t_q[hh][b] for b in qb_], [ps_[1]],
                             [dict(out=ps_[0][:, :n], lhsT=kT[P0:P1, jb * 128:(jb + 1) * 128], rhs=qT[P0:P1, pr, q0:q1], start=True, stop=False),
                              dict(out=ps_[0][:, :n], lhsT=ident_bf[:, :], rhs=mask_bf[:, mo:mo + n], start=False, stop=True)])
                        ri = jb % NR
                        prt = Pring[:, ri * 384:(ri + 1) * 384]
                        k.op("act", lambda e: e.activation(out=prt[:, :n], in_=ps_[0][:, :n], func=AF.Exp, scale=0.125), reads=[ps_[1]], writes=[t_pr[ri]])
                        pinfo[jb] = (prt, q0, t_pr[ri])
                        if jb >= 4:
                            pv(jb - 4)
                    for i_ in range(12, 16):
                        pv(i_)
                    if need_ctx:
                        srcs = [(Vg[:, 16 + kb, :], Pc[:, kb, 2048:2304]) for kb in range(2)]
                        k.mm([t_v[2], t_pc], [t_po], [dict(out=po[P0:P1, 0:256], lhsT=va, rhs=pa, start=(ii == 0), stop=(ii == 1)) for ii, (va, pa) in enumerate(srcs)])
                        k.mm([t_pc, t_one], [t_pd], [dict(out=pd[P0:P1, 0:256], lhsT=ones_bf[:, 0:64], rhs=pa, start=(ii == 0), stop=(ii == 1)) for ii, (va, pa) in enumerate(srcs)])
                        finish(2048, 256, 4)
                for d in range(8):
                    for (t0, n, v) in blks_q:
                        b = t0 // 512
                        pw, t_pw = k.ps()
                        k.mm([t_wo] + [t_q[hh][b] for hh in range(4)], [t_pw],
                             [dict(out=pw[:, :n], lhsT=wov[:, pr, d * 128:(d + 1) * 128], rhs=qT[:, pr, t0:t0 + n], start=(pr == 0), stop=(pr == 1)) for pr in range(2)])
                        k.op("dve", lambda e: e.scalar_tensor_tensor(out=xT[:, d, t0:t0 + n], in0=pw[:, :n], scalar=G1[:, d, v:v + 1],
                                                                    in1=xT[:, d, t0:t0 + n], op0=ALU.mult, op1=ALU.add),
                             reads=[t_pw, t_mT, t_X[b]], writes=[t_X[b]])
                k.barrier()
            k.npool = 8

        def moe(j, blks):
            combT = RA[:, 4 * NT:6 * NT].bitcast(F32)
            cbs = [RA[:, 6 * NT:7 * NT], RA[:, 7 * NT:8 * NT]]
            t_comb = Tl("comb")
            t_cbs = [Tl("cb0"), Tl("cb1")]
            t_cm = Tl("cm")
            cm = TMP[0:8, 0:NT]
            k.dma("sp", router_f[:, :].rearrange("p (c e) -> p c e", c=8), router_d[j].rearrange("(c p) e -> p c e", p=128), writes=[t_lpar])
            make_h(A2v, SH2, blks, moe_j=j, combT=combT, t_comb=t_comb)
            k.barrier()
            for e_ in range(NE):
                k.op("dve", lambda e: e.tensor_scalar(out=cm, in0=combT[0:8, :], scalar1=ident[0:8, e_:e_ + 1], scalar2=None, op0=ALU.mult),
                     reads=[t_comb, t_par], writes=[t_cm])
                for (t0, n, v) in blks:
                    pc_, t_pc_ = k.ps()
                    k.mm([t_cm, t_one], [t_pc_], [dict(out=pc_[:, :n], lhsT=ones_f[0:8, :], rhs=cm[:, t0:t0 + n], start=True, stop=True)])
                    k.op("act", lambda e: e.copy(out=cbs[e_ % 2][:, t0:t0 + n], in_=pc_[:, :n]), reads=[t_pc_], writes=[t_cbs[e_ % 2]])
                ffn(mw1[j, e_], mw3[j, e_], mw2[j, e_], G2, blks, f"moe{j}_{e_}", cb=cbs[e_ % 2], t_cb=t_cbs[e_ % 2], bar=False)
            k.barrier()


        def moe_sparse(j, blks):
            ntok = sum(n for (_, n, _) in blks)
            ntb = ntok // 128
            NS = 768
            hg = RA[:, 0:6144].rearrange("p (c t) -> p c t", c=8)
            uvs = [RA[:, 6144 + a * 1536:6144 + (a + 1) * 1536].rearrange("p (f t) -> p f t", f=2) for a in range(2)]
            hbuf = [RA[:, 9216 + a * 1024:9216 + (a + 1) * 1024] for a in range(3)]
            Sg = [RA[:, 12288 + a * 384:12288 + (a + 1) * 384] for a in range(3)]
            STw = RA[:, 13440:16512].rearrange("p (a t) -> p a t", a=6)
            iota_f = RA[:, 16512:17280].bitcast(F32)
            pos_tm = RA[:, 17280:17568].bitcast(F32).rearrange("p (b e) -> p b e", e=8)
            comb_tm = RA[:, 17568:17856].bitcast(F32).rearrange("p (b e) -> p b e", e=8)
            mask_tm = RA[:, 17856:18000].rearrange("p (b e) -> p b e", e=8)
            cnt_i = RA[:, 18000:18016].bitcast(I32)
            slotid = RA[:, 18016:18052].bitcast(F32)
            CP = TMP[0:16, 0:NT]
            acc = RH[:, 0:12288].bitcast(F32).rearrange("p (c t) -> p c t", c=8)
            otm = RH[:, 12288:18432].rearrange("p (a d) -> p a d", a=6)
            t_rt = Tl("rt")
            t_mc = Tl("mc")
            t_cp = Tl("cp")
            t_hb = [Tl(f"hb{a}") for a in range(3)]
            t_sg = [Tl(f"sg{a}") for a in range(3)]
            t_hg = [Tl("hg0"), Tl("hg1")]
            t_acc = [Tl("acc0"), Tl("acc1")]
            t_otm = [Tl(f"otm{a}") for a in range(6)]
            t_stw = Tl("stw")
            t_hd = Tl("hd")
            blocks = [(0, 384), (384, 384)]
            k.dma("sp", router_f[:, :].rearrange("p (c e) -> p c e", c=8), router_d[j].rearrange("(c p) e -> p c e", p=128), writes=[t_lpar])
            k.dma("sp", iota_f, iota_d[:, :], writes=[t_mc])
            k.dma("sp", slotid, slotid_d[:, :], writes=[t_mc])
            make_h(A2v, SH2, blks, moe_j=dict(comb_tm=comb_tm, mask_tm=mask_tm, t_rt=t_rt))
            for tbi in range(ntb):
                pp, t_pp = k.ps()
                items = [dict(out=pp[:, 0:8], lhsT=ones_bf[:, :], rhs=mask_tm[:, b_, :], start=(b_ == 0), stop=False) for b_ in range(tbi)]
                items.append(dict(out=pp[:, 0:8], lhsT=triu_bf[:, :], rhs=mask_tm[:, tbi, :], start=(tbi == 0), stop=True))
                k.mm([t_rt, t_one, t_par], [t_pp], items)
                k.op("dve", lambda e: e.scalar_tensor_tensor(out=pos_tm[:, tbi, :], in0=pp[:, 0:8], scalar=1.0, in1=mask_tm[:, tbi, :], op0=ALU.add, op1=ALU.mult),
                     reads=[t_pp], writes=[t_rt])
                k.op("dve", lambda e: e.tensor_scalar(out=pos_tm[:, tbi, :], in0=pos_tm[:, tbi, :], scalar1=-1.0, scalar2=None, op0=ALU.add), writes=[t_rt])
                cp16 = sml[:, 48:64]
                k.op("dve", lambda e: e.tensor_copy(out=cp16[:, 0:8], in_=comb_tm[:, tbi, :]), reads=[t_rt], writes=[t_sml])
                k.op("dve", lambda e: e.tensor_copy(out=cp16[:, 8:16], in_=pos_tm[:, tbi, :]), reads=[t_rt], writes=[t_sml])
                pst, t_pst = k.ps()
                k.mm([t_sml, t_par], [t_pst], [dict(out=pst[0:16, 0:128], lhsT=cp16, rhs=ident[:, :], start=True, stop=True, is_transpose=True)])
                k.op("act", lambda e: e.copy(out=CP[0:16, tbi * 128:(tbi + 1) * 128], in_=pst[0:16, 0:128]), reads=[t_pst], writes=[t_cp])
            pcn, t_pcn = k.ps()
            k.mm([t_rt, t_one], [t_pcn], [dict(out=pcn[:, 0:8], lhsT=ones_bf[:, :], rhs=mask_tm[:, b_, :], start=(b_ == 0), stop=(b_ == ntb - 1)) for b_ in range(ntb)])
            t_cnt = Tl("cnt")
            k.op("dve", lambda e: e.tensor_copy(out=cnt_i, in_=pcn[:, 0:8]), reads=[t_pcn], writes=[t_cnt])
            for tb in range(ntb):
                b = min(tb // 4, 4)
                pt, t_pt = k.ps()
                ptb = pt[:, :].bitcast(BF16)
                k.mm([t_H[b], t_par], [t_pt], [dict(out=ptb[:, c * 128:(c + 1) * 128], lhsT=hT[:, c, tb * 128:(tb + 1) * 128], rhs=ident_bf[:, :],
                                                     start=True, stop=True, is_transpose=True) for c in range(8)])
                hb, t_h = hbuf[tb % 3], t_hb[tb % 3]
                k.op("act" if tb % 2 == 0 else "dve", (lambda e: e.copy(out=hb, in_=ptb[:, 0:1024])) if tb % 2 == 0 else (lambda e: e.tensor_copy(out=hb, in_=ptb[:, 0:1024])),
                     reads=[t_pt], writes=[t_h])
                k.dma("sp", h_tm_d[tb], hb, reads=[t_h], semt=t_hd)
            k.barrier()

            w_srcs = None

            def pass_body(e_, p_):
                W1d, W3d, W2d = mw1[j, e_], mw3[j, e_], mw2[j, e_]
                w1s = W1d.rearrange("(c p) n -> p c n", p=128)
                w3s = W3d.rearrange("(c p) n -> p c n", p=128)
                w2s = W2d.rearrange("(f p) n -> p f n", p=128)

                def views(a):
                    sl = slots[a]
                    return (sl[:, 0:2048].rearrange("p (c n) -> p c n", c=8), sl[:, 2048:4096].rearrange("p (c n) -> p c n", c=8),
                            sl[:, 4096:6144].rearrange("p (f n) -> p f n", f=2))

                def load(i):
                    a, b_, c_ = views(i % 2)
                    tw = TW[i % 2]
                    k.dma("pool", a, w1s[:, :, i * 256:(i + 1) * 256], writes=[tw[0]])
                    k.dma("pool", b_, w3s[:, :, i * 256:(i + 1) * 256], writes=[tw[1]])
                    k.dma("pool", c_, w2s[:, 2 * i:2 * i + 2, :], writes=[tw[2]])
                load(0)
                hi = 0
                for bi, (s0, sn) in enumerate(blocks):
                    accs = [k.psum[c] for c in range(8)]
                    for tb in range(ntb):
                        hb, t_h = hbuf[hi % 3], t_hb[hi % 3]
                        sg, t_s = Sg[hi % 3], t_sg[hi % 3]
                        hi += 1
                        k.dma("sp", hb, h_tm_d[tb], writes=[t_h])
                        k.op("dve", lambda e: e.tensor_scalar(out=sg, in0=iota_f, scalar1=float(NS * p_ + s0), scalar2=pos_tm[:, tb, e_:e_ + 1],
                                                              op0=ALU.add, op1=ALU.is_equal), reads=[t_mc, t_rt], writes=[t_s])
                        for c in range(8):
                            k.mm([t_h, t_s], [accs[c][1]], [dict(out=accs[c][0][:, 0:sn], lhsT=hb[:, c * 128:(c + 1) * 128], rhs=sg, start=(tb == 0), stop=(tb == ntb - 1))])
                    for c in range(8):
                        if c % 2 == 0:
                            k.op("act", lambda e: e.copy(out=hg[:, c, s0:s0 + sn], in_=accs[c][0][:, 0:sn]), reads=[accs[c][1]], writes=[t_hg[bi]])
                        else:
                            k.op("dve", lambda e: e.tensor_copy(out=hg[:, c, s0:s0 + sn], in_=accs[c][0][:, 0:sn]), reads=[accs[c][1]], writes=[t_hg[bi]])
                for i in range(14):
                    if i + 1 < 14:
                        load(i + 1)
                    a = i % 2
                    w1v, w3v, w2v = views(a)
                    tw = TW[a]
                    uv = uvs[a]
                    for fc in range(2):
                        for bi, (s0, sn) in enumerate(blocks):
                            p1, t_p1 = k.ps()
                            p3, t_p3 = k.ps()
                            k.mm([tw[0], t_hg[bi]], [t_p1], [dict(out=p1[:, :sn], lhsT=w1v[:, c, fc * 128:(fc + 1) * 128], rhs=hg[:, c, s0:s0 + sn],
                                                                 start=(c == 0), stop=(c == 7)) for c in range(8)])
                            k.mm([tw[1], t_hg[bi]], [t_p3], [dict(out=p3[:, :sn], lhsT=w3v[:, c, fc * 128:(fc + 1) * 128], rhs=hg[:, c, s0:s0 + sn],
                                                                 start=(c == 0), stop=(c == 7)) for c in range(8)])
                            gb, t_g = nxt("gt", gt, t_gt)
                            k.op("act", lambda e: e.activation(out=gb[:, :sn], in_=p1[:, :sn], func=AF.Silu), reads=[t_p1], writes=[t_g])
                            k.op("dve", lambda e: e.tensor_tensor(out=uv[:, fc, s0:s0 + sn], in0=gb[:, :sn], in1=p3[:, :sn], op=ALU.mult),
                                 reads=[t_g, t_p3], writes=[TU[a][bi]])
                    for d in range(8):
                        for bi, (s0, sn) in enumerate(blocks):
                            po_, t_po_ = k.ps()
                            k.mm([tw[2], TU[a][bi]], [t_po_], [dict(out=po_[:, :sn], lhsT=w2v[:, fc, d * 128:(d + 1) * 128], rhs=uv[:, fc, s0:s0 + sn],
                                                                   start=(fc == 0), stop=(fc == 1)) for fc in range(2)])
                            if i == 0:
                                k.op("act", lambda e: e.copy(out=acc[:, d, s0:s0 + sn], in_=po_[:, :sn]), reads=[t_po_], writes=[t_acc[bi]])
                            else:
                                k.op("dve", lambda e: e.tensor_tensor(out=acc[:, d, s0:s0 + sn], in0=po_[:, :sn], in1=acc[:, d, s0:s0 + sn], op=ALU.add),
                                     reads=[t_po_], writes=[t_acc[bi]])
                for sb_ in range(6):
                    for half in range(2):
                        pt, t_pt = k.ps()
                        k.mm([t_acc[sb_ // 3], t_par], [t_pt],
                             [dict(out=pt[:, dd * 128:(dd + 1) * 128], lhsT=acc[:, half * 4 + dd, sb_ * 128:(sb_ + 1) * 128], rhs=ident[:, :],
                                   start=True, stop=True, is_transpose=True) for dd in range(4)])
                        if half == 0:
                            k.op("act", lambda e: e.copy(out=otm[:, sb_, 0:512], in_=pt[:, :]), reads=[t_pt], writes=[t_otm[sb_]])
                        else:
                            k.op("dve", lambda e: e.tensor_copy(out=otm[:, sb_, 512:1024], in_=pt[:, :]), reads=[t_pt], writes=[t_otm[sb_]])
                for (t0, n, v) in blks:
                    b = t0 // 512
                    cmt, t_c = nxt("t32", t32, t_t32)
                    k.op("dve", lambda e: e.tensor_scalar(out=cmt[0:16, :n], in0=CP[0:16, t0:t0 + n], scalar1=ident[0:16, e_:e_ + 1], scalar2=None, op0=ALU.mult),
                         reads=[t_cp, t_par], writes=[t_c])
                    pc_, t_pc_ = k.ps()
                    k.mm([t_c, t_one], [t_pc_], [dict(out=pc_[:, :n], lhsT=ones_f[0:16, :], rhs=cmt[0:16, :n], start=True, stop=True)])
                    cbb, t_cb = nxt("rs", rs, t_rs)
                    k.op("act", lambda e: e.copy(out=cbb[:, :n], in_=pc_[:, :n]), reads=[t_pc_], writes=[t_cb])
                    pmt, t_p = nxt("t32", t32, t_t32)
                    k.op("dve", lambda e: e.tensor_scalar(out=pmt[0:16, :n], in0=CP[0:16, t0:t0 + n], scalar1=ident[0:16, 8 + e_:9 + e_], scalar2=None, op0=ALU.mult),
                         reads=[t_cp, t_par], writes=[t_p])
                    pp_, t_pp_ = k.ps()
                    k.mm([t_p, t_one], [t_pp_], [dict(out=pp_[:, :n], lhsT=ones_f[0:16, :], rhs=pmt[0:16, :n], start=True, stop=True)])
                    for sb_ in range(6):
                        kk = 6 * p_ + sb_
                        k.op("dve", lambda e: e.scalar_tensor_tensor(out=STw[:, sb_, :n], in0=pp_[:, :n], scalar=slotid[:, kk:kk + 1], in1=cbb[:, :n],
                                                                    op0=ALU.is_equal, op1=ALU.mult), reads=[t_pp_, t_cb, t_mc], writes=[t_stw])
                    for d in range(8):
                        px, t_px = k.ps()
                        k.mm([t_stw] + t_otm, [t_px], [dict(out=px[:, :n], lhsT=otm[:, sb_, d * 128:(d + 1) * 128], rhs=STw[:, sb_, :n],
                                                           start=(sb_ == 0), stop=(sb_ == 5)) for sb_ in range(6)])
                        k.op("dve", lambda e: e.scalar_tensor_tensor(out=xT[:, d, t0:t0 + n], in0=px[:, :n], scalar=G2[:, d, v:v + 1],
                                                                    in1=xT[:, d, t0:t0 + n], op0=ALU.mult, op1=ALU.add),
                             reads=[t_px, t_mT, t_X[b]], writes=[t_X[b]])

            npass = (ntok + NS - 1) // NS
            for e_ in range(NE):
                regs = nc.alloc_registers(f"cnt{j}_{e_}")
                for r_ in regs:
                    en = {"Pool": "pool", "Activation": "act", "PE": "pe", "DVE": "dve", "SP": "sp"}[str(r_.engine).split(".")[-1]]
                    E = k.E[en]
                    k._need(E, k._deps([t_cnt], []))
                    E.h.load(r_, cnt_i[0:1, e_:e_ + 1])
                cval = nc.snap(regs, donate=True)
                for p_ in range(npass):
                    k.region_begin()
                    with nc.If(cval > NS * p_):
                        pass_body(e_, p_)
                        deltas = k.region_end()
                    with nc.Else():
                        for (en, h_, dv) in deltas:
                            k.E[en].h.sem_inc(h_, dv)
            k.barrier()

        moe_regs = nc.alloc_registers("cnt")
        import os
        stop = int(os.environ.get("KSTOP", "99"))
        phase = 0
        for li in range(4):
            j = li // 2
            need_ctx = li < 3
            blks = BLK_L + (BLK_C if need_ctx else [])
            layer_params(li)
            if li % 2 == 0:
                make_h(A1v, SH1, blks)
                k.barrier()
                even_mixer(j, blks)
                phase += 1
                if phase >= stop:
                    break
                make_h(A2v, SH2, blks)
                k.barrier()
                ffn(ffw1[j], ffw3[j], ffw2[j], G2, blks, f"ff{j}")
                phase += 1
                if phase >= stop:
                    break
            else:
                if li == 1:
                    rope_tables()
                make_h(A1v, SH1, BLK_L + BLK_C)
                k.barrier()
                attention(j, need_ctx)
                phase += 1
                if phase >= stop:
                    break
                moe_sparse(j, blks)
                phase += 1
                if phase >= stop:
                    break

        k.barrier()
        t_out = Tl("out")
        for b in range(5):
            t0, n, _ = (BLK_L + BLK_C)[b]
            k.dma("sp", out_d[:, t0:t0 + n].rearrange("(c p) t -> p c t", p=128), xT[:, :, t0:t0 + n], reads=[t_X[b]], semt=t_out)
        k.E["sp"].h.wait_ge(t_out.dsem, t_out.dcnt)
    return nc


def _prep(inputs, b):
    f = np.float32
    x, ctx, c, c_ctx = inputs["x"], inputs["ctx"], inputs["c"], inputs["c_ctx"]
    m = {}
    m["xin"] = np.ascontiguousarray(np.concatenate([x[b], ctx[b]], axis=0).T)
    cvv = np.stack([c[b].reshape(8, 128).T, c_ctx.reshape(8, 128).T], axis=-1)
    m["cv"] = np.ascontiguousarray(cvv.reshape(128, 16))
    return m


def _shared(inputs):
    m = {}
    g = lambda a: np.ascontiguousarray(a, dtype=np.float32)
    m["ada_w"] = g(inputs["ada_w"])
    m["ada_b"] = g(inputs["ada_b"].reshape(4, 48, 128).transpose(0, 2, 1))
    m["ng1"] = g(inputs["norm_mix_g"].reshape(4, 8, 128).transpose(0, 2, 1))
    m["ng2"] = g(inputs["norm_ffn_g"].reshape(4, 8, 128).transpose(0, 2, 1))
    m["ev_w_in"] = g(inputs["ev_w_in"])
    m["ev_ln_g"] = g(inputs["ev_ln_g"].reshape(2, 8, 128).transpose(0, 2, 1))
    m["ev_ln_b"] = g(inputs["ev_ln_b"].reshape(2, 8, 128).transpose(0, 2, 1))
    m["ev_wsT"] = g(inputs["ev_ws"].transpose(0, 1, 3, 2))
    m["ev_bs"] = g(inputs["ev_bs"].reshape(2, 1024))
    m["ev_conv_w"] = g(inputs["ev_conv_w"].transpose(0, 2, 1).reshape(2, 8, 128, 31).transpose(0, 2, 1, 3).reshape(2, 128, 248))
    m["ev_conv_b"] = g(inputs["ev_conv_b"].reshape(2, 8, 128).transpose(0, 2, 1))
    m["ev_cnorm_g"] = g(inputs["ev_cnorm_g"].reshape(2, 8, 128).transpose(0, 2, 1))
    m["ev_w_out"] = g(inputs["ev_w_out"])
    m["od_w_qkv"] = g(inputs["od_w_qkv"])
    m["od_q_g"] = g(np.concatenate([inputs["od_q_g"], inputs["od_q_g"]], axis=1).reshape(2, 128, 1))
    m["od_k_g"] = g(np.concatenate([inputs["od_k_g"], inputs["od_k_g"]], axis=1).reshape(2, 128, 1))
    m["od_sink"] = g(inputs["od_sink"])
    m["od_w_o"] = g(inputs["od_w_o"])
    m["ff_w1"] = g(inputs["ff_w1"])
    m["ff_w3"] = g(inputs["ff_w3"])
    m["ff_w2"] = g(inputs["ff_w2"])
    m["moe_router"] = g(inputs["moe_router"])
    m["moe_w1"] = g(inputs["moe_w1"])
    m["moe_w3"] = g(inputs["moe_w3"])
    m["moe_w2"] = g(inputs["moe_w2"])
    m["c_ident"] = np.eye(128, dtype=np.float32)
    pm = np.zeros((64, 64), np.float32)
    for p in range(64):
        r = p % 32
        if r < 16:
            pm[p + 16, p] = -1.0
        else:
            pm[p - 16, p] = 1.0
    pm2 = np.zeros((128, 128), np.float32)
    pm2[0:64, 0:64] = pm
    pm2[64:128, 64:128] = pm
    m["c_perm"] = pm2
    bd = np.zeros((128, 128), np.float32)
    bd[0:64, 0:64] = 1.0
    bd[64:128, 64:128] = 1.0
    m["c_bd"] = bd
    kk = np.arange(128)[:, None]
    qq = np.arange(384)[None, :] - 128
    m["c_mask"] = np.where(np.abs(qq - kk) <= 128, 0.0, -240000.0).astype(np.float32)
    t = np.arange(2048)
    pos = np.zeros((64, 2048), np.float32)
    pos[0:32, :] = (t // 64)[None, :]
    pos[32:64, :] = (t % 64)[None, :]
    m["c_pos"] = np.concatenate([pos, pos], axis=0)
    m["c_fidx"] = (np.arange(128) % 16).astype(np.float32).reshape(128, 1)
    m["c_triu"] = (np.arange(128)[:, None] < np.arange(128)[None, :]).astype(np.float32)
    m["c_iota"] = np.tile(np.arange(384, dtype=np.float32)[None, :], (128, 1))
    m["c_slotid"] = (np.arange(128, dtype=np.float32)[:, None] + 128.0 * np.arange(18, dtype=np.float32)[None, :])
    return m


_NC_CACHE = {}


def kernel(**inputs):
    inputs = {k_: np.asarray(v) for k_, v in inputs.items()}
    ncores = 8
    if "nc" not in _NC_CACHE:
        _NC_CACHE["nc"] = build()
    nc = _NC_CACHE["nc"]
    shared = _shared(inputs)
    in_maps = []
    for b in range(ncores):
        m = dict(shared)
        m.update(_prep(inputs, b))
        in_maps.append(m)
    res = run_bass_kernel_spmd(nc, in_maps, core_ids=list(range(ncores)))
    outs = [np.asarray(r["out"]) for r in res.results]
    return np.stack([o[:, :NL].T for o in outs], axis=0).astype(np.float32)
```

```python
import numpy as np
import concourse.bass as bass
import concourse.mybir as mybir
from concourse.bass_utils import run_bass_kernel_spmd
from contextlib import ExitStack

F32 = mybir.dt.float32
BF16 = mybir.dt.bfloat16
AF = mybir.ActivationFunctionType
ALU = mybir.AluOpType
AX = mybir.AxisListType

NL, NCX, NT = 2048, 256, 2304
D, DFF, NE = 1024, 3584, 8
EPS = 1e-6
BLK_L = [(0, 512, 0), (512, 512, 0), (1024, 512, 0), (1536, 512, 0)]
BLK_C = [(2048, 256, 1)]
SEMLIM = 10 ** 9


class Tl:
    __slots__ = ("name", "w", "r", "dsem", "dcnt")

    def __init__(s, name):
        s.name = name
        s.w = None
        s.r = {}
        s.dsem = None
        s.dcnt = 0


class Eng:
    def __init__(s, name, h):
        s.name, s.h, s.sem, s.cnt, s.waited = name, h, None, 0, {}


class K:
    def __init__(s, nc, es):
        s.nc, s.es = nc, es
        s.E = {"pe": Eng("pe", nc.tensor), "act": Eng("act", nc.scalar), "dve": Eng("dve", nc.vector),
               "pool": Eng("pool", nc.gpsimd), "sp": Eng("sp", nc.sync)}
        s.nsem = 0
        for e in s.E.values():
            e.sem = s.newsem(e.name)
        s.dsems = {}
        s.psum = []
        s.psi = 0
        s._snaps = []
        s.npool = 8

    def newsem(s, name):
        s.nsem += 1
        return s.es.enter_context(s.nc.semaphore(f"{name}_{s.nsem}"))

    def _need(s, E, deps, embed=False):
        todo = []
        for num, (h, v) in deps.items():
            if E.waited.get(num, 0) < v:
                todo.append((h, v))
                E.waited[num] = v
        last = None
        if embed and todo:
            last = todo.pop()
        for (h, v) in todo:
            E.h.wait_ge(h, v)
        return last

    @staticmethod
    def _deps(reads, writes):
        d = {}

        def add(rec):
            h, v = rec
            if h.num not in d or d[h.num][1] < v:
                d[h.num] = rec
        for t in reads:
            if t.w:
                add(t.w)
        for t in writes:
            if t.w:
                add(t.w)
            for rec in t.r.values():
                add(rec)
        return d

    @staticmethod
    def _mark(rec, reads, writes):
        for t in reads:
            t.r[rec[0].num] = rec
        for t in writes:
            t.w = rec
            t.r = {}

    def _rot(s, E):
        if E.cnt >= SEMLIM:
            E.sem = s.newsem(E.name)
            E.cnt = 0

    def op(s, en, fn, reads=(), writes=()):
        E = s.E[en]
        last = s._need(E, s._deps(reads, writes), embed=True)
        ins = fn(E.h)
        if last is not None:
            ins._wait_ge(last[0], last[1])
        E.cnt += 1
        ins.then_inc(E.sem, 1)
        s._mark((E.sem, E.cnt), reads, writes)
        s._rot(E)

    def mm(s, reads, writes, items):
        E = s.E["pe"]
        last = s._need(E, s._deps(reads, writes), embed=True)
        ins = None
        for it in items:
            ins = E.h.matmul(**it)
            if last is not None:
                ins._wait_ge(last[0], last[1])
                last = None
        E.cnt += 1
        ins.then_inc(E.sem, 1)
        s._mark((E.sem, E.cnt), reads, writes)
        s._rot(E)

    def dma(s, q, out, in_, reads=(), writes=(), semt=None):
        E = s.E[q]
        s._need(E, s._deps(reads, writes))
        ins = E.h.dma_start(out=out, in_=in_)
        t = semt if semt is not None else writes[0]
        if t.dsem is None:
            t.dsem = s.newsem("d" + t.name)
        t.dcnt += 16
        ins.then_inc(t.dsem, 16)
        s.dsems[t.dsem.num] = (t.dsem, t.dcnt)
        s._mark((t.dsem, t.dcnt), reads, writes)

    def barrier(s, engines=("pe", "act", "dve", "pool", "sp"), own=False):
        for en in engines:
            E = s.E[en]
            d = {}
            for F in s.E.values():
                if (own or F is not E) and F.cnt > 0:
                    d[F.sem.num] = (F.sem, F.cnt)
            d.update(s.dsems)
            s._need(E, d)

    def region_begin(s):
        for n, E in s.E.items():
            d = {}
            if E.cnt > 0:
                d[E.sem.num] = (E.sem, E.cnt)
            if n == "sp":
                d.update(s.dsems)
            s._need(E, d)
        s._snaps.append(({n: (E.sem, E.cnt) for n, E in s.E.items()}, dict(s.dsems), {n: dict(E.waited) for n, E in s.E.items()}))

    def region_end(s):
        e0, d0, w0 = s._snaps.pop()
        deltas = []
        for n, E in s.E.items():
            assert E.sem.num == e0[n][0].num, "semaphore rotated inside region"
            if E.cnt > e0[n][1]:
                deltas.append((n, E.sem, E.cnt - e0[n][1]))
        for num, (h, v) in s.dsems.items():
            v0 = d0[num][1] if num in d0 else 0
            if v > v0:
                deltas.append(("sp", h, v - v0))
        for n, E in s.E.items():
            E.waited = w0[n]
        return deltas

    def ps(s):
        p = s.psum[s.psi % s.npool]
        s.psi += 1
        return p


def build():
    nc = bass.Bass("TRN2", target_bir_lowering=False)

    def din(name, shape):
        return nc.dram_tensor(name, list(shape), F32, kind="ExternalInput").ap()

    xin = din("xin", [1024, NT])
    cv_d = din("cv", [128, 16])
    ada_w = din("ada_w", [4, 1024, 6144])
    ada_b = din("ada_b", [4, 128, 48])
    ng1_d = din("ng1", [4, 128, 8])
    ng2_d = din("ng2", [4, 128, 8])
    w_in = din("ev_w_in", [2, 1024, 4096])
    lngf_d = din("ev_ln_g", [2, 128, 8])
    lnbf_d = din("ev_ln_b", [2, 128, 8])
    wsT_d = din("ev_wsT", [2, 8, 128, 128])
    bs_d = din("ev_bs", [2, 1024])
    convw_d = din("ev_conv_w", [2, 128, 248])
    convb_d = din("ev_conv_b", [2, 128, 8])
    cng_d = din("ev_cnorm_g", [2, 128, 8])
    w_out = din("ev_w_out", [2, 2048, 1024])
    wqkv = din("od_w_qkv", [2, 1024, 1536])
    qg_d = din("od_q_g", [2, 128, 1])
    kg_d = din("od_k_g", [2, 128, 1])
    sink_d = din("od_sink", [2, 16])
    wo_d = din("od_w_o", [2, 1024, 1024])
    ffw1 = din("ff_w1", [2, 1024, DFF])
    ffw3 = din("ff_w3", [2, 1024, DFF])
    ffw2 = din("ff_w2", [2, DFF, 1024])
    router_d = din("moe_router", [2, 1024, 8])
    mw1 = din("moe_w1", [2, 8, 1024, DFF])
    mw3 = din("moe_w3", [2, 8, 1024, DFF])
    mw2 = din("moe_w2", [2, 8, DFF, 1024])
    ident_d = din("c_ident", [128, 128])
    perm_d = din("c_perm", [128, 128])
    bd_d = din("c_bd", [128, 128])
    mask_d = din("c_mask", [128, 384])
    pos_d = din("c_pos", [128, 2048])
    fidx_d = din("c_fidx", [128, 1])
    triu_d = din("c_triu", [128, 128])
    iota_d = din("c_iota", [128, 384])
    slotid_d = din("c_slotid", [128, 18])
    h_tm_d = nc.dram_tensor("h_tm_scratch", [18, 128, 1024], BF16, kind="Internal").ap()
    out_d = nc.dram_tensor("out", [1024, NT], F32, kind="ExternalOutput").ap()

    with ExitStack() as es:
        k = K(nc, es)

        def sb(name, shape, dt):
            return es.enter_context(nc.sbuf_tensor("sb_" + name, list(shape), dt))

        RX = sb("RX", [128, 8 * NT], F32)
        RH = sb("RH", [128, 8 * NT], BF16)
        RA = sb("RA", [128, 8 * NT], BF16)
        RW = sb("RW", [128, 12288], BF16)
        xT = RX[:, :].rearrange("p (c t) -> p c t", c=8)
        hT = RH[:, :].rearrange("p (c t) -> p c t", c=8)
        for i in range(8):
            k.psum.append((es.enter_context(nc.psum_tensor(f"ps{i}", [128, 512], F32)), Tl(f"ps{i}")))

        ident = sb("ident", [128, 128], F32)
        ones_bf = sb("ones_bf", [128, 128], BF16)
        ones_f = sb("ones_f", [16, 128], F32)
        triu_bf = sb("triu_bf", [128, 128], BF16)
        ident_bf = sb("ident_bf", [128, 128], BF16)
        perm_bf = sb("perm_bf", [128, 128], BF16)
        bd_bf = sb("bd_bf", [128, 128], BF16)
        mask_bf = sb("mask_bf", [128, 384], BF16)
        cv = sb("cv", [128, 16], F32)
        scv = sb("scv", [128, 16], BF16)
        adab = sb("adab", [128, 48], F32)
        mT = sb("mT", [128, 96], F32)
        mT3 = mT[:, :].rearrange("p (j v) -> p j v", v=2)
        A1 = sb("A1", [128, 16], F32)
        A2 = sb("A2", [128, 16], F32)
        A1v = A1[:, :].rearrange("p (c v) -> p c v", v=2)
        A2v = A2[:, :].rearrange("p (c v) -> p c v", v=2)
        ng1 = sb("ng1", [128, 8], F32)
        ng2 = sb("ng2", [128, 8], F32)
        tmp16 = sb("tmp16", [128, 16], F32)
        convw = sb("convw", [128, 248], F32)
        convb = sb("convb", [128, 8], F32)
        cng = sb("cng", [128, 8], F32)
        qg = sb("qg", [128, 1], F32)
        kg = sb("kg", [128, 1], F32)
        sinke = sb("sinke", [128, 16], F32)
        router_f = sb("router_f", [128, 64], F32)
        cossin = sb("cossin", [128, 2 * 2048], BF16)
        cosT = cossin[:, 0:2048]
        sinT = cossin[:, 2048:4096]
        sq = [sb(f"sq{i}", [128, 512], BF16) for i in range(2)]
        rs = [sb(f"rs{i}", [128, 512], F32) for i in range(2)]
        t32 = [sb(f"t32_{i}", [128, 512], F32) for i in range(2)]
        gt = [sb(f"gt{i}", [128, 512], BF16) for i in range(2)]
        tt = [sb(f"tt{i}", [128, 512], BF16) for i in range(2)]
        TMP = sb("TMP", [128, 2560], F32)
        sml = sb("sml", [128, 64], F32)

        t_sq = [Tl(f"sq{i}") for i in range(2)]
        t_rs = [Tl(f"rs{i}") for i in range(2)]
        t_t32 = [Tl(f"t32{i}") for i in range(2)]
        t_gt = [Tl(f"gt{i}") for i in range(2)]
        t_tt = [Tl(f"tt{i}") for i in range(2)]
        t_par = Tl("par")
        t_lpar = Tl("lpar")
        t_mT = Tl("mT")
        t_X = [Tl(f"X{i}") for i in range(5)]
        t_H = [Tl(f"H{i}") for i in range(5)]
        TW = [[Tl(f"w{a}_{i}") for i in range(3)] for a in range(2)]
        t_p2 = Tl("m2p")
        TU = [[Tl(f"u{a}_{b}") for b in range(5)] for a in range(2)]
        rot = {"sq": 0, "rs": 0, "t32": 0, "gt": 0, "tt": 0}

        def nxt(name, arr, tls):
            i = rot[name] % len(arr)
            rot[name] += 1
            return arr[i], tls[i]

        k.dma("sp", ident[:, :], ident_d[:, :], writes=[t_par])
        k.dma("sp", cv[:, :], cv_d[:, :], writes=[t_par])
        k.dma("pool", perm_bf[:, :], perm_d[:, :], writes=[t_par])
        k.dma("pool", bd_bf[:, :], bd_d[:, :], writes=[t_par])
        k.dma("pool", mask_bf[:, :], mask_d[:, :], writes=[t_par])
        k.dma("pool", triu_bf[:, :], triu_d[:, :], writes=[t_par])
        k.dma("pool", ident_bf[:, :], ident_d[:, :], writes=[t_par])
        t_one = Tl("ones")
        k.op("dve", lambda e: e.memset(ones_bf[:, :], 1.0), writes=[t_one])
        k.op("dve", lambda e: e.memset(ones_f[:, :], 1.0), writes=[t_one])
        for b in range(5):
            t0, n, _ = (BLK_L + BLK_C)[b]
            k.dma("sp", xT[:, :, t0:t0 + n], xin[:, t0:t0 + n].rearrange("(c p) t -> p c t", p=128), writes=[t_X[b]])
        k.op("act", lambda e: e.activation(out=scv[:, :], in_=cv[:, :], func=AF.Silu), reads=[t_par], writes=[t_one])
        scv3 = scv[:, :].rearrange("p (c v) -> p c v", v=2)

        W0 = RW[:, 0:6144]
        W1s = RW[:, 6144:12288]
        slots = [W0, W1s]

        def layer_params(li):
            t_w = [TW[0][0], TW[1][0]]
            k.dma("sp", adab[:, :], ada_b[li], writes=[t_lpar])
            k.dma("sp", ng1[:, :], ng1_d[li], writes=[t_lpar])
            k.dma("sp", ng2[:, :], ng2_d[li], writes=[t_lpar])
            psm, t_psm = k.ps()
            src = ada_w[li].rearrange("(c p) n -> p c n", p=128)
            for i in range(12):
                wv = slots[i % 2][:, 0:4096].rearrange("p (c n) -> p c n", c=8)
                k.dma("pool", wv, src[:, :, i * 512:(i + 1) * 512], writes=[t_w[i % 2]])
                for jj in range(4):
                    j = 4 * i + jj
                    k.mm([t_w[i % 2], t_one], [t_psm],
                         [dict(out=psm[:, 2 * j:2 * j + 2], lhsT=wv[:, c, jj * 128:(jj + 1) * 128], rhs=scv3[:, c, :],
                               start=(c == 0), stop=(c == 7)) for c in range(8)])
            ps3 = psm[:, 0:96].rearrange("p (j v) -> p j v", v=2)
            for v in range(2):
                k.op("dve", lambda e, v=v: e.tensor_tensor(out=mT3[:, :, v], in0=ps3[:, :, v], in1=adab[:, :], op=ALU.add),
                     reads=[t_psm, t_lpar], writes=[t_mT])
            t16 = tmp16[:, :].rearrange("p (c v) -> p c v", v=2)
            for (Av, ng, off) in ((A1v, ng1, 8), (A2v, ng2, 32)):
                k.op("dve", lambda e, off=off: e.tensor_scalar(out=t16, in0=mT3[:, off:off + 8, :], scalar1=1.0, scalar2=None, op0=ALU.add),
                     reads=[t_mT], writes=[t_lpar])
                for v in range(2):
                    k.op("dve", lambda e, v=v, Av=Av, ng=ng: e.tensor_tensor(out=Av[:, :, v], in0=t16[:, :, v], in1=ng[:, :], op=ALU.mult),
                         reads=[t_lpar], writes=[t_lpar])
            k.barrier()

        SH1, G1, SH2, G2 = mT3[:, 0:8, :], mT3[:, 16:24, :], mT3[:, 24:32, :], mT3[:, 40:48, :]

        def make_h(Av, SHv, blks, moe_j=None, combT=None, t_comb=None):
            for (t0, n, v) in blks:
                b = t0 // 512
                rsb, t_r = nxt("rs", rs, t_rs)
                psm, t_ps = k.ps()
                for c in range(8):
                    sqb, t_s = nxt("sq", sq, t_sq)
                    k.op("act", lambda e, c=c, sqb=sqb: e.activation(out=sqb[:, :n], in_=xT[:, c, t0:t0 + n], func=AF.Square),
                         reads=[t_X[b]], writes=[t_s])
                    k.mm([t_s, t_one], [t_ps], [dict(out=psm[:, :n], lhsT=ones_bf[:, :], rhs=sqb[:, :n], start=(c == 0), stop=(c == 7))])
                k.op("act", lambda e: e.activation(out=rsb[:, :n], in_=psm[:, :n], func=AF.Sqrt, scale=1.0 / D, bias=EPS),
                     reads=[t_ps], writes=[t_r])
                k.op("dve", lambda e: e.reciprocal(out=rsb[:, :n], in_=rsb[:, :n]), reads=[t_r], writes=[t_r])
                if moe_j is not None:
                    pslg, t_pslg = k.ps()
                    nsb = n // 128
                    k.mm([t_one], [t_pslg], [dict(out=pslg[:, 0:8 * nsb], lhsT=zer_bf[:, :], rhs=zer_bf[:, 0:8 * nsb], start=True, stop=False,
                                                skip_group_check=True)])
                for c in range(8):
                    tb, t_t = nxt("t32", t32, t_t32)
                    k.op("dve", lambda e, c=c, tb=tb: e.scalar_tensor_tensor(out=tb[:, :n], in0=xT[:, c, t0:t0 + n], scalar=Av[:, c, v:v + 1],
                                                                           in1=rsb[:, :n], op0=ALU.mult, op1=ALU.mult),
                         reads=[t_X[b], t_r, t_lpar], writes=[t_t])
                    if moe_j is None:
                        k.op("act", lambda e, c=c, tb=tb: e.activation(out=hT[:, c, t0:t0 + n], in_=tb[:, :n], func=AF.Identity,
                                                                     bias=SHv[:, c, v:v + 1]),
                             reads=[t_t, t_mT], writes=[t_H[b]])
                    else:
                        k.op("act", lambda e, c=c, tb=tb: e.activation(out=tb[:, :n], in_=tb[:, :n], func=AF.Identity,
                                                                     bias=SHv[:, c, v:v + 1]),
                             reads=[t_mT], writes=[t_t])
                        k.op("pool", lambda e, c=c, tb=tb: e.tensor_copy(out=hT[:, c, t0:t0 + n], in_=tb[:, :n]),
                             reads=[t_t], writes=[t_H[b]])
                        k.mm([t_t, t_lpar], [t_pslg],
                             [dict(out=pslg[:, 8 * s_:8 * s_ + 8], lhsT=tb[:, s_ * 128:(s_ + 1) * 128], rhs=router_f[:, 8 * c:8 * c + 8],
                                   start=False, stop=(c == 7), skip_group_check=True) for s_ in range(nsb)])
                if moe_j is not None:
                    for s_ in range(nsb):
                        lg = sml[:, 0:8]
                        mx = sml[:, 8:16]
                        ex = sml[:, 16:24]
                        msk = sml[:, 24:32]
                        nm1 = sml[:, 32:33]
                        den = sml[:, 33:34]
                        k.op("dve", lambda e: e.tensor_copy(out=lg, in_=pslg[:, 8 * s_:8 * s_ + 8]), reads=[t_pslg], writes=[t_sml])
                        k.op("dve", lambda e: e.max(out=mx, in_=lg), reads=[t_sml], writes=[t_sml])
                        k.op("dve", lambda e: e.tensor_scalar(out=nm1, in0=mx[:, 0:1], scalar1=-1.0, scalar2=None, op0=ALU.mult),
                             reads=[t_sml], writes=[t_sml])
                        k.op("act", lambda e: e.activation(out=ex, in_=lg, func=AF.Exp, bias=nm1), reads=[t_sml], writes=[t_sml])
                        k.op("dve", lambda e: e.tensor_scalar(out=msk, in0=lg, scalar1=mx[:, 1:2], scalar2=None, op0=ALU.is_ge),
                             reads=[t_sml], writes=[t_sml])
                        k.op("dve", lambda e: e.tensor_tensor(out=ex, in0=ex, in1=msk, op=ALU.mult), reads=[t_sml], writes=[t_sml])
                        k.op("dve", lambda e: e.reduce_sum(out=den, in_=ex, axis=AX.X), reads=[t_sml], writes=[t_sml])
                        k.op("dve", lambda e: e.reciprocal(out=den, in_=den), reads=[t_sml], writes=[t_sml])
                        k.op("dve", lambda e: e.tensor_scalar(out=ex, in0=ex, scalar1=den, scalar2=None, op0=ALU.mult),
                             reads=[t_sml], writes=[t_sml])
                        tbi = t0 // 128 + s_
                        k.op("dve", lambda e: e.tensor_copy(out=moe_j["comb_tm"][:, tbi, :], in_=ex), reads=[t_sml], writes=[moe_j["t_rt"]])
                        k.op("dve", lambda e: e.tensor_copy(out=moe_j["mask_tm"][:, tbi, :], in_=msk), reads=[t_sml], writes=[moe_j["t_rt"]])

        zer_bf = sb("zer_bf", [128, 128], BF16)
        k.op("dve", lambda e: e.memset(zer_bf[:, :], 0.0), writes=[t_one])
        t_sml = Tl("sml")

        def ffn(W1d, W3d, W2d, Gv, blks, tag, cb=None, t_cb=None, bar=True):
            NP = 14
            t_w = TW
            t_u = TU
            w1s = W1d.rearrange("(c p) n -> p c n", p=128)
            w3s = W3d.rearrange("(c p) n -> p c n", p=128)
            w2s = W2d.rearrange("(f p) n -> p f n", p=128)

            def views(s_):
                sl = slots[s_]
                return (sl[:, 0:2048].rearrange("p (c n) -> p c n", c=8), sl[:, 2048:4096].rearrange("p (c n) -> p c n", c=8),
                        sl[:, 4096:6144].rearrange("p (f n) -> p f n", f=2))

            def load(i):
                a, b_, c_ = views(i % 2)
                tw = t_w[i % 2]
                k.dma("pool", a, w1s[:, :, i * 256:(i + 1) * 256], writes=[tw[0]])
                k.dma("pool", b_, w3s[:, :, i * 256:(i + 1) * 256], writes=[tw[1]])
                k.dma("pool", c_, w2s[:, 2 * i:2 * i + 2, :], writes=[tw[2]])

            load(0)
            for i in range(NP):
                if i + 1 < NP:
                    load(i + 1)
                s_ = i % 2
                w1v, w3v, w2v = views(s_)
                tw = t_w[s_]
                uv = RA[:, s_ * 2 * NT:(s_ + 1) * 2 * NT].rearrange("p (f t) -> p f t", f=2)
                for fc in range(2):
                    for (t0, n, v) in blks:
                        b = t0 // 512
                        p1, t_p1 = k.ps()
                        p3, t_p3 = k.ps()
                        k.mm([tw[0], t_H[b]], [t_p1], [dict(out=p1[:, :n], lhsT=w1v[:, c, fc * 128:(fc + 1) * 128], rhs=hT[:, c, t0:t0 + n],
                                                          start=(c == 0), stop=(c == 7)) for c in range(8)])
                        k.mm([tw[1], t_H[b]], [t_p3], [dict(out=p3[:, :n], lhsT=w3v[:, c, fc * 128:(fc + 1) * 128], rhs=hT[:, c, t0:t0 + n],
                                                          start=(c == 0), stop=(c == 7)) for c in range(8)])
                        gb, t_g = nxt("gt", gt, t_gt)
                        k.op("act", lambda e: e.activation(out=gb[:, :n], in_=p1[:, :n], func=AF.Silu), reads=[t_p1], writes=[t_g])
                        if cb is None:
                            k.op("dve", lambda e: e.tensor_tensor(out=uv[:, fc, t0:t0 + n], in0=gb[:, :n], in1=p3[:, :n], op=ALU.mult),
                                 reads=[t_g, t_p3], writes=[t_u[s_][b]])
                        else:
                            tb_, t_t = nxt("tt", tt, t_tt)
                            k.op("dve", lambda e: e.tensor_tensor(out=tb_[:, :n], in0=gb[:, :n], in1=p3[:, :n], op=ALU.mult),
                                 reads=[t_g, t_p3], writes=[t_t])
                            k.op("pool", lambda e: e.tensor_tensor(out=uv[:, fc, t0:t0 + n], in0=tb_[:, :n], in1=cb[:, t0:t0 + n], op=ALU.mult),
                                 reads=[t_t, t_cb], writes=[t_u[s_][b]])
                for d in range(8):
                    for (t0, n, v) in blks:
                        b = t0 // 512
                        po, t_po = k.ps()
                        k.mm([tw[2], t_u[s_][b]], [t_po], [dict(out=po[:, :n], lhsT=w2v[:, fc, d * 128:(d + 1) * 128], rhs=uv[:, fc, t0:t0 + n],
                                                              start=(fc == 0), stop=(fc == 1)) for fc in range(2)])
                        k.op("dve", lambda e: e.scalar_tensor_tensor(out=xT[:, d, t0:t0 + n], in0=po[:, :n], scalar=Gv[:, d, v:v + 1],
                                                                    in1=xT[:, d, t0:t0 + n], op0=ALU.mult, op1=ALU.add),
                             reads=[t_po, t_mT, t_X[b]], writes=[t_X[b]])
            if bar:
                k.barrier()

        def proj_out(Wd_rows, yv, t_y, Gv, blks, tag):
            t_w = [TW[0][0], TW[1][0]]
            ws = Wd_rows.rearrange("(c p) n -> p c n", p=128)
            wv = [slots[i][:, 0:4096].rearrange("p (c n) -> p c n", c=4) for i in range(2)]
            for i in range(2):
                k.dma("pool", wv[i], ws[:, 4 * i:4 * i + 4, :], writes=[t_w[i]])
            for d in range(8):
                for (t0, n, v) in blks:
                    b = t0 // 512
                    po, t_po = k.ps()
                    k.mm([t_w[0], t_w[1], t_y[b]], [t_po],
                         [dict(out=po[:, :n], lhsT=wv[c // 4][:, c % 4, d * 128:(d + 1) * 128], rhs=yv[:, c, t0:t0 + n],
                               start=(c == 0), stop=(c == 7)) for c in range(8)])
                    k.op("dve", lambda e: e.scalar_tensor_tensor(out=xT[:, d, t0:t0 + n], in0=po[:, :n], scalar=Gv[:, d, v:v + 1],
                                                                in1=xT[:, d, t0:t0 + n], op0=ALU.mult, op1=ALU.add),
                         reads=[t_po, t_mT, t_X[b]], writes=[t_X[b]])
            k.barrier()

        def even_mixer(j, blks):
            yv = RA[:, :].rearrange("p (c t) -> p c t", c=8)
            t_y = [Tl(f"ya{j}_{b}") for b in range(5)]
            win = w_in[j].rearrange("(c p) n -> p c n", p=128)
            t_w = [TW[0][0], TW[1][0]]

            def wv1(s_):
                return slots[s_][:, 0:2048].rearrange("p (c n) -> p c n", c=8)
            k.dma("pool", wv1(0), win[:, :, 0:256], writes=[t_w[0]])
            for i in range(4):
                if i + 1 < 4:
                    k.dma("pool", wv1((i + 1) % 2), win[:, :, (i + 1) * 256:(i + 2) * 256], writes=[t_w[(i + 1) % 2]])
                for fc in range(2):
                    jc = 2 * i + fc
                    for (t0, n, v) in blks:
                        b = t0 // 512
                        p1, t_p1 = k.ps()
                        k.mm([t_w[i % 2], t_H[b]], [t_p1], [dict(out=p1[:, :n], lhsT=wv1(i % 2)[:, c, fc * 128:(fc + 1) * 128], rhs=hT[:, c, t0:t0 + n],
                                                              start=(c == 0), stop=(c == 7)) for c in range(8)])
                        k.op("act", lambda e: e.activation(out=yv[:, jc, t0:t0 + n], in_=p1[:, :n], func=AF.Gelu_apprx_tanh),
                             reads=[t_p1], writes=[t_y[b]])
            k.barrier()
            t_wv = [TW[0][0], TW[1][0]]
            wvv = [slots[i][:, 0:4096].rearrange("p (c n) -> p c n", c=8) for i in range(2)]
            for i in range(2):
                k.dma("pool", wvv[i], win[:, :, 1024 + i * 512:1024 + (i + 1) * 512], writes=[t_wv[i]])
            vgs = [slots[0][:, 4096:6144].bitcast(F32), slots[1][:, 4096:6144].bitcast(F32)]
            T2 = TMP[:, 0:1024]
            wsTv = TMP[:, 1024:1536].bitcast(BF16).rearrange("p (g q) -> p g q", g=8)
            bsb = TMP[:, 1536:2560]
            lgf = sml[:, 32:40]
            lbf = sml[:, 40:48]
            k.dma("sp", lgf, lngf_d[j], writes=[t_p2])
            k.dma("sp", lbf, lnbf_d[j], writes=[t_p2])
            k.dma("sp", bsb, bs_d[j:j + 1, :].partition_broadcast(128), writes=[t_p2])
            k.dma("pool", wsTv, wsT_d[j].rearrange("g q p -> q g p"), writes=[t_p2])
            t_T2 = Tl("T2")
            for gb_ in range(2):
                pw_, t_pw_ = k.ps()
                for gg in range(4):
                    g_ = gb_ * 4 + gg
                    k.mm([t_p2, t_one], [t_pw_], [dict(out=pw_[:, gg * 128:(gg + 1) * 128], lhsT=ones_bf[:, :], rhs=wsTv[:, g_, :], start=True, stop=True)])
                for gg in range(4):
                    g_ = gb_ * 4 + gg
                    k.op("dve", lambda e: e.scalar_tensor_tensor(out=T2[:, g_ * 128:(g_ + 1) * 128], in0=pw_[:, gg * 128:(gg + 1) * 128], scalar=lbf[:, g_:g_ + 1],
                                                                in1=bsb[:, g_ * 128:(g_ + 1) * 128], op0=ALU.mult, op1=ALU.add), reads=[t_pw_, t_p2], writes=[t_T2])
            t_vgs = [Tl("vg0"), Tl("vg1")]
            t_st = [Tl("st0"), Tl("st1")]
            vb2 = [t32[0][:, :].bitcast(BF16), t32[1][:, :].bitcast(BF16)]
            ntb = [tb for (t0, n, v) in blks for tb in range(t0 // 128, (t0 + n) // 128)]
            for it_, tb in enumerate(ntb):
                b = min(tb // 4, 4)
                tk = tb * 128
                par = it_ % 2
                vg, t_vg = vgs[par], t_vgs[par]
                pv = []
                for h_ in range(2):
                    p_, t_p = k.ps()
                    k.mm([t_wv[h_], t_H[b]], [t_p], [dict(out=p_[:, :], lhsT=hT[:, c, tk:tk + 128], rhs=wvv[h_][:, c, :],
                                                        start=(c == 0), stop=(c == 7)) for c in range(8)])
                    pv.append((p_, t_p))
                for h_ in range(2):
                    k.op("act", lambda e, h_=h_: e.activation(out=vg[:, h_ * 512:(h_ + 1) * 512], in_=pv[h_][0][:, :], func=AF.Gelu_apprx_tanh),
                         reads=[pv[h_][1]], writes=[t_vg])
                so = par * 16
                st = sml[:, so:so + 12].rearrange("p (a b) -> p a b", a=2)
                mv = sml[:, so + 12:so + 14]
                rstd = sml[:, so + 14:so + 15]
                nmr = sml[:, so + 15:so + 16]
                t_s_ = t_st[par]
                for h_ in range(2):
                    k.op("dve", lambda e, h_=h_: e.bn_stats(out=st[:, h_, :], in_=vg[:, h_ * 512:(h_ + 1) * 512]), reads=[t_vg], writes=[t_s_])
                k.op("dve", lambda e: e.bn_aggr(out=mv, in_=sml[:, so:so + 12]), reads=[t_s_], writes=[t_s_])
                k.op("act", lambda e: e.activation(out=rstd, in_=mv[:, 1:2], func=AF.Sqrt, bias=EPS), reads=[t_s_], writes=[t_s_])
                k.op("dve", lambda e: e.reciprocal(out=rstd, in_=rstd), reads=[t_s_], writes=[t_s_])
                k.op("dve", lambda e: e.scalar_tensor_tensor(out=nmr, in0=mv[:, 0:1], scalar=-1.0, in1=rstd, op0=ALU.mult, op1=ALU.mult),
                     reads=[t_s_], writes=[t_s_])
                vbf = vb2[par]
                t_v = t_t32[par]
                k.op("act", lambda e: e.activation(out=vbf, in_=vg, func=AF.Identity, scale=rstd, bias=nmr), reads=[t_s_, t_vg], writes=[t_v])
                for gb_ in range(2):
                    pg, t_pg = k.ps()
                    for gg in range(4):
                        g_ = gb_ * 4 + gg
                        k.mm([t_v, t_p2], [t_pg], [dict(out=pg[:, gg * 128:(gg + 1) * 128], lhsT=vbf[:, g_ * 128:(g_ + 1) * 128], rhs=wsTv[:, g_, :],
                                                      start=True, stop=True)])
                    tb_, t_t = nxt("rs", rs, t_rs)
                    for gg in range(4):
                        g_ = gb_ * 4 + gg
                        k.op("dve", lambda e: e.scalar_tensor_tensor(out=tb_[:, gg * 128:(gg + 1) * 128], in0=pg[:, gg * 128:(gg + 1) * 128], scalar=lgf[:, g_:g_ + 1],
                                                                    in1=T2[:, g_ * 128:(g_ + 1) * 128], op0=ALU.mult, op1=ALU.add),
                             reads=[t_pg, t_p2, t_T2], writes=[t_t])
                    yslice = yv[:, gb_ * 4:gb_ * 4 + 4, tk:tk + 128]
                    k.op("pool", lambda e: e.tensor_tensor(out=yslice, in0=yslice, in1=tb_[:, :].rearrange("p (g q) -> p g q", g=4), op=ALU.mult),
                         reads=[t_t], writes=[t_y[b]])
            k.barrier()
            proj_out(w_out[j, 0:1024, :], yv, t_y, G1, blks, f"m4a{j}")
            t_yb = [Tl(f"yb{j}_{b}") for b in range(5)]
            k.dma("sp", convw[:, :], convw_d[j], writes=[t_lpar])
            k.dma("sp", convb[:, :], convb_d[j], writes=[t_lpar])
            k.dma("sp", cng[:, :], cng_d[j], writes=[t_lpar])
            cw3 = convw[:, :].rearrange("p (c k) -> p c k", c=8)
            GW = 2078 + 286
            gbufs = [(slots[1][:, a_ * GW:a_ * GW + 2078], slots[1][:, a_ * GW + 2078:(a_ + 1) * GW]) for a_ in range(2)]
            t_gs = [Tl("g0"), Tl("g1")]
            Dg = TMP[:, 0:1984].bitcast(BF16).rearrange("p (k m) -> p k m", k=31)
            t_dg = Tl("dg")
            for a_ in range(2):
                k.op("pool", lambda e: e.memset(slots[1][:, a_ * GW:(a_ + 1) * GW], 0.0), writes=[t_gs[a_]])
            t_w3 = [TW[0][0], TW[0][1]]

            def wv3(s_):
                base = s_ * 2048
                return (slots[0][:, base:base + 1024].rearrange("p (c n) -> p c n", c=8),
                        slots[0][:, base + 1024:base + 2048].rearrange("p (c n) -> p c n", c=8))

            def load3(i):
                a_, g_ = wv3(i % 2)
                k.dma("pool", a_, win[:, :, 2048 + i * 128:2048 + (i + 1) * 128], writes=[t_w3[i % 2]])
                k.dma("pool", g_, win[:, :, 3072 + i * 128:3072 + (i + 1) * 128], writes=[t_w3[i % 2]], semt=t_w3[i % 2])

            def stage_proj(i):
                if i + 1 < 8:
                    load3(i + 1)
                a_, g_ = wv3(i % 2)
                gL, gC = gbufs[i % 2]
                for (t0, n, v) in blks:
                    b = t0 // 512
                    pa, t_pa = k.ps()
                    pg, t_pg = k.ps()
                    k.mm([t_w3[i % 2], t_H[b]], [t_pa], [dict(out=pa[:, :n], lhsT=a_[:, c, :], rhs=hT[:, c, t0:t0 + n], start=(c == 0), stop=(c == 7)) for c in range(8)])
                    k.mm([t_w3[i % 2], t_H[b]], [t_pg], [dict(out=pg[:, :n], lhsT=g_[:, c, :], rhs=hT[:, c, t0:t0 + n], start=(c == 0), stop=(c == 7)) for c in range(8)])
                    sg, t_sg = nxt("rs", rs, t_rs)
                    k.op("act", lambda e: e.activation(out=sg[:, :n], in_=pg[:, :n], func=AF.Sigmoid), reads=[t_pg], writes=[t_sg])
                    dst = gL[:, 15 + t0:15 + t0 + n] if v == 0 else gC[:, 15:15 + n]
                    k.op("dve", lambda e: e.tensor_tensor(out=dst, in0=sg[:, :n], in1=pa[:, :n], op=ALU.mult), reads=[t_sg, t_pa], writes=[t_gs[i % 2]])

            def stage_conv(i):
                gL, gC = gbufs[i % 2]
                for tap in range(31):
                    k.op("dve", lambda e: e.tensor_scalar(out=Dg[:, tap, :], in0=ident_bf[:, :], scalar1=cw3[:, i, tap:tap + 1], scalar2=None, op0=ALU.mult),
                         reads=[t_par, t_lpar], writes=[t_dg])
                for (t0, n, v) in blks:
                    b = t0 // 512
                    gbuf, o0 = (gL, t0) if v == 0 else (gC, 0)
                    pa_, t_pa_ = k.ps()
                    k.mm([t_dg, t_gs[i % 2]], [t_pa_], [dict(out=pa_[:, :n], lhsT=Dg[:, tap, :], rhs=gbuf[:, o0 + tap:o0 + tap + n], start=(tap == 0), stop=(tap == 30))
                                                        for tap in range(31)])
                    k.op("act", lambda e: e.activation(out=yv[:, i, t0:t0 + n], in_=pa_[:, :n], func=AF.Identity, bias=convb[:, i:i + 1]),
                         reads=[t_pa_, t_lpar], writes=[t_yb[b]])

            load3(0)
            stage_proj(0)
            for i in range(8):
                if i + 1 < 8:
                    stage_proj(i + 1)
                stage_conv(i)
            for (t0, n, v) in blks:
                b = t0 // 512
                rsb, t_r = nxt("rs", rs, t_rs)
                psm, t_ps = k.ps()
                for c in range(8):
                    sqb, t_s = nxt("sq", sq, t_sq)
                    k.op("act", lambda e: e.activation(out=sqb[:, :n], in_=yv[:, c, t0:t0 + n], func=AF.Square), reads=[t_yb[b]], writes=[t_s])
                    k.mm([t_s, t_one], [t_ps], [dict(out=psm[:, :n], lhsT=ones_bf[:, :], rhs=sqb[:, :n], start=(c == 0), stop=(c == 7))])
                k.op("act", lambda e: e.activation(out=rsb[:, :n], in_=psm[:, :n], func=AF.Sqrt, scale=1.0 / D, bias=EPS), reads=[t_ps], writes=[t_r])
                k.op("dve", lambda e: e.reciprocal(out=rsb[:, :n], in_=rsb[:, :n]), reads=[t_r], writes=[t_r])
                for c in range(8):
                    tb_, t_t = nxt("t32", t32, t_t32)
                    k.op("dve", lambda e: e.scalar_tensor_tensor(out=tb_[:, :n], in0=yv[:, c, t0:t0 + n], scalar=cng[:, c:c + 1], in1=rsb[:, :n],
                                                                op0=ALU.mult, op1=ALU.mult), reads=[t_yb[b], t_r, t_lpar], writes=[t_t])
                    k.op("act", lambda e: e.activation(out=yv[:, c, t0:t0 + n], in_=tb_[:, :n], func=AF.Silu), reads=[t_t], writes=[t_yb[b]])
            k.barrier()
            proj_out(w_out[j, 1024:2048, :], yv, t_yb, G1, blks, f"m4b{j}")


        I32 = mybir.dt.int32
        TWO_PI = 6.283185307179586

        def rope_tables():
            RAf = RA[:, 0:16384].bitcast(F32)
            y = RAf[:, 0:2048]
            yy = RAf[:, 2048:4096]
            kf = RAf[:, 4096:6144]
            ki = RAf[:, 6144:8192].bitcast(I32)
            fidx = sml[:, 40:41]
            invf = sml[:, 41:42]
            t_r = Tl("ropetmp")
            k.dma("sp", y, pos_d[:, :], writes=[t_r])
            k.dma("sp", fidx, fidx_d[:, :], writes=[t_r])
            k.op("act", lambda e: e.activation(out=invf, in_=fidx, func=AF.Exp, scale=-float(np.log(10000.0)) / 16.0), reads=[t_r], writes=[t_r])
            k.op("dve", lambda e: e.tensor_scalar(out=y, in0=y, scalar1=invf, scalar2=1.0 / TWO_PI, op0=ALU.mult, op1=ALU.mult), reads=[t_r], writes=[t_r])
            for shift, dst in ((0.0, sinT), (0.25, cosT)):
                k.op("dve", lambda e: e.tensor_scalar(out=yy, in0=y, scalar1=shift, scalar2=None, op0=ALU.add), reads=[t_r], writes=[t_r])
                k.op("dve", lambda e: e.tensor_copy(out=ki, in_=yy), reads=[t_r], writes=[t_r])
                k.op("dve", lambda e: e.tensor_copy(out=kf, in_=ki), reads=[t_r], writes=[t_r])
                k.op("dve", lambda e: e.tensor_tensor(out=yy, in0=yy, in1=kf, op=ALU.subtract), reads=[t_r], writes=[t_r])
                k.op("dve", lambda e: e.tensor_single_scalar(out=kf, in_=yy, scalar=0.5, op=ALU.is_gt), reads=[t_r], writes=[t_r])
                k.op("dve", lambda e: e.tensor_tensor(out=yy, in0=yy, in1=kf, op=ALU.subtract), reads=[t_r], writes=[t_r])
                k.op("dve", lambda e: e.tensor_single_scalar(out=kf, in_=yy, scalar=-0.5, op=ALU.is_lt), reads=[t_r], writes=[t_r])
                k.op("dve", lambda e: e.tensor_tensor(out=yy, in0=yy, in1=kf, op=ALU.add), reads=[t_r], writes=[t_r])
                k.op("act", lambda e: e.activation(out=dst, in_=yy, func=AF.Sin, scale=TWO_PI * (1.0 - 1e-6)), reads=[t_r], writes=[t_par])
            k.barrier()

        def attention(j, need_ctx):
            blks_q = BLK_L + (BLK_C if need_ctx else [])
            blks_a = BLK_L + BLK_C
            k.dma("sp", qg[:, :], qg_d[j], writes=[t_lpar])
            k.dma("sp", kg[:, :], kg_d[j], writes=[t_lpar])
            k.dma("sp", sinke[:, :], sink_d[j:j + 1, :].partition_broadcast(128), writes=[t_lpar])
            k.op("act", lambda e: e.activation(out=sinke[:, :], in_=sinke[:, :], func=AF.Exp), reads=[], writes=[t_lpar])
            k.npool = 6
            po, t_po = k.psum[6]
            pd, t_pd = k.psum[7]
            qT = RA[:, 0:2 * NT].rearrange("p (h t) -> p h t", h=2)
            kT = RA[:, 2 * NT:3 * NT]
            Vg = RA[:, 3 * NT:3 * NT + 1152].rearrange("p (b d) -> p b d", b=18)
            Pc = RA[:, 3 * NT + 1152:3 * NT + 1152 + 2 * NT].rearrange("p (b t) -> p b t", b=2)
            Pring = TMP[:, :].bitcast(BF16)
            NR = 8
            t_q = [[Tl(f"q{h}_{b}") for b in range(5)] for h in range(4)]
            t_k = [Tl(f"k{b}") for b in range(5)]
            t_v = [Tl(f"v{b}") for b in range(3)]
            t_pc = Tl("pc")
            t_pr = [Tl(f"pr{i}") for i in range(NR)]
            wsrc = wqkv[j].rearrange("(c p) n -> p c n", p=128)
            tq = [TW[0][0], TW[0][1]]
            tv = [TW[1][1], TW[1][2]]

            def wviews(s_):
                base = s_ * 3072
                return (slots[0][:, base:base + 2048].rearrange("p (c n) -> p c n", c=8),
                        slots[0][:, base + 2048:base + 3072].rearrange("p (c n) -> p c n", c=8),
                        slots[1][:, 2048 + s_ * 512:2048 + (s_ + 1) * 512].rearrange("p (c n) -> p c n", c=8))

            def loadw(g):
                a_, b_, c_ = wviews(g % 2)
                t_ = tq[g % 2]
                k.dma("pool", a_, wsrc[:, :, g * 256:(g + 1) * 256], writes=[t_])
                k.dma("pool", b_[:, :, 0:64], wsrc[:, :, 1024 + g * 64:1024 + (g + 1) * 64], writes=[t_])
                k.dma("pool", b_[:, :, 64:128], wsrc[:, :, 1024 + g * 64:1024 + (g + 1) * 64], writes=[t_])
                k.dma("pool", c_, wsrc[:, :, 1280 + g * 64:1280 + (g + 1) * 64], writes=[tv[g % 2]])
            wov = slots[1][:, 0:2048].rearrange("p (h n) -> p h n", h=2)
            t_wo = TW[1][0]

            def qk_chain(projitems, rd, n, gvec, dst, t_dsts, rope, t0):
                ps_ = k.ps()
                k.mm(rd, [ps_[1]], projitems(ps_[0]))
                yield
                sqb, t_s = nxt("sq", sq, t_sq)
                k.op("act", lambda e: e.activation(out=sqb[:, :n], in_=ps_[0][:, :n], func=AF.Square), reads=[ps_[1]], writes=[t_s])
                yield
                pss, t_pss = k.ps()
                k.mm([t_s, t_par], [t_pss], [dict(out=pss[:, :n], lhsT=bd_bf[:, :], rhs=sqb[:, :n], start=True, stop=True)])
                yield
                rsb, t_r = nxt("rs", rs, t_rs)
                k.op("act", lambda e: e.activation(out=rsb[:, :n], in_=pss[:, :n], func=AF.Sqrt, scale=1.0 / 64, bias=EPS), reads=[t_pss], writes=[t_r])
                yield
                k.op("dve", lambda e: e.reciprocal(out=rsb[:, :n], in_=rsb[:, :n]), reads=[t_r], writes=[t_r])
                yield
                qn, t_qn = nxt("t32", t32, t_t32)
                k.op("dve", lambda e: e.scalar_tensor_tensor(out=qn[:, :n], in0=ps_[0][:, :n], scalar=gvec[:, 0:1], in1=rsb[:, :n],
                                                            op0=ALU.mult, op1=ALU.mult), reads=[ps_[1], t_r, t_lpar], writes=[t_qn])
                yield
                if not rope:
                    k.op("act", lambda e: e.copy(out=dst, in_=qn[:, :n]), reads=[t_qn], writes=t_dsts)
                    return
                qb, t_qb = nxt("tt", tt, t_tt)
                k.op("pool", lambda e: e.tensor_copy(out=qb[:, :n], in_=qn[:, :n]), reads=[t_qn], writes=[t_qb])
                yield
                psr, t_psr = k.ps()
                k.mm([t_qb, t_par], [t_psr], [dict(out=psr[:, :n], lhsT=perm_bf[:, :], rhs=qb[:, :n], start=True, stop=True)])
                yield
                bb, t_bb = nxt("rs", rs, t_rs)
                k.op("dve", lambda e: e.tensor_tensor(out=bb[:, :n], in0=psr[:, :n], in1=sinT[:, t0:t0 + n], op=ALU.mult), reads=[t_psr, t_par], writes=[t_bb])
                k.op("dve", lambda e: e.tensor_tensor(out=qn[:, :n], in0=qn[:, :n], in1=cosT[:, t0:t0 + n], op=ALU.mult), reads=[t_par], writes=[t_qn])
                yield
                k.op("pool", lambda e: e.tensor_tensor(out=dst, in0=qn[:, :n], in1=bb[:, :n], op=ALU.add), reads=[t_qn, t_bb], writes=t_dsts)

            def lockstep(gens, width=2):
                for i0 in range(0, len(gens), width):
                    active = gens[i0:i0 + width]
                    while active:
                        alive = []
                        for g_ in active:
                            try:
                                next(g_)
                                alive.append(g_)
                            except StopIteration:
                                pass
                        active = alive

            loadw(0)
            for g in range(4):
                if g + 1 < 4:
                    loadw(g + 1)
                k.dma("pool", wov, wo_d[j][g * 256:(g + 1) * 256, :].rearrange("(h p) n -> p h n", p=128), writes=[t_wo])
                wq_, wk_, wv_ = wviews(g % 2)
                t_w = tq[g % 2]
                chains = []
                for (t0, n, v) in blks_a:
                    b = t0 // 512
                    chains.append(qk_chain(lambda pst, t0=t0, n=n: [dict(out=pst[:, :n], lhsT=wk_[:, c, :], rhs=hT[:, c, t0:t0 + n], start=(c == 0), stop=(c == 7)) for c in range(8)],
                                           [t_w, t_H[b]], n, kg, kT[:, t0:t0 + n], [t_k[b]], v == 0, t0))
                for pr in range(2):
                    for (t0, n, v) in blks_q:
                        b = t0 // 512
                        chains.append(qk_chain(lambda pst, t0=t0, n=n, pr=pr: [dict(out=pst[:, :n], lhsT=wq_[:, c, pr * 128:(pr + 1) * 128], rhs=hT[:, c, t0:t0 + n],
                                                                                 start=(c == 0), stop=(c == 7)) for c in range(8)],
                                               [t_w, t_H[b]], n, qg, qT[:, pr, t0:t0 + n], [t_q[2 * pr][b], t_q[2 * pr + 1][b]], v == 0, t0))
                lockstep(chains)
                for vb in range(3):
                    tbs = list(range(vb * 8, min(18, vb * 8 + 8)))
                    ps_ = k.ps()
                    for ii, tb in enumerate(tbs):
                        b = min(tb // 4, 4)
                        k.mm([tv[g % 2], t_H[b]], [ps_[1]], [dict(out=ps_[0][:, ii * 64:(ii + 1) * 64], lhsT=hT[:, c, tb * 128:(tb + 1) * 128], rhs=wv_[:, c, :],
                                                              start=(c == 0), stop=(c == 7)) for c in range(8)])
                    nb = len(tbs)
                    k.op("act", lambda e: e.copy(out=Vg[:, tbs[0]:tbs[0] + nb, :], in_=ps_[0][:, 0:nb * 64].rearrange("p (b d) -> p b d", b=nb)),
                         reads=[ps_[1]], writes=[t_v[vb]])
                for hh in range(4):
                    h = 4 * g + hh
                    pr = hh // 2
                    P0 = (hh % 2) * 64
                    P1 = P0 + 64
                    for kb in range(2):
                        for (t0, n, v) in blks_q:
                            b = t0 // 512
                            ps_ = k.ps()
                            k.mm([t_k[4], t_q[hh][b]], [ps_[1]], [dict(out=ps_[0][:, :n], lhsT=kT[P0:P1, 2048 + kb * 128:2048 + (kb + 1) * 128], rhs=qT[P0:P1, pr, t0:t0 + n],
                                                                    start=True, stop=True)])
                            k.op("act", lambda e: e.activation(out=Pc[:, kb, t0:t0 + n], in_=ps_[0][:, :n], func=AF.Exp, scale=0.125), reads=[ps_[1]], writes=[t_pc])
                    pinfo = {}

                    def pv(i):
                        col = (i % 4) * 128
                        srcs = [(Vg[:, 16 + kb, :], Pc[:, kb, i * 128:(i + 1) * 128], t_pc) for kb in range(2)]
                        for jb in (i - 1, i, i + 1):
                            if 0 <= jb <= 15:
                                pr_, q0_, t_ = pinfo[jb]
                                srcs.append((Vg[:, jb, :], pr_[:, i * 128 - q0_:i * 128 - q0_ + 128], t_))
                        rd = [t_v[0], t_v[1], t_v[2]] + [s_[2] for s_ in srcs]
                        k.mm(rd, [t_po], [dict(out=po[P0:P1, col:col + 128], lhsT=va, rhs=pa, start=(ii == 0), stop=(ii == len(srcs) - 1))
                                          for ii, (va, pa, _) in enumerate(srcs)])
                        k.mm(rd + [t_one], [t_pd], [dict(out=pd[P0:P1, col:col + 128], lhsT=ones_bf[:, 0:64], rhs=pa, start=(ii == 0), stop=(ii == len(srcs) - 1))
                                                    for ii, (va, pa, _) in enumerate(srcs)])
                        if i % 4 == 3:
                            m_ = i // 4
                            finish(m_ * 512, 512, m_)

                    def finish(t0, n, b):
                        dn, t_dn = nxt("rs", rs, t_rs)
                        k.op("dve", lambda e: e.tensor_scalar(out=dn[P0:P1, :n], in0=pd[P0:P1, :n], scalar1=sinke[P0:P1, h:h + 1], scalar2=None, op0=ALU.add),
                             reads=[t_pd, t_lpar], writes=[t_dn])
                        k.op("dve", lambda e: e.reciprocal(out=dn[P0:P1, :n], in_=dn[P0:P1, :n]), reads=[t_dn], writes=[t_dn])
                        k.op("dve", lambda e: e.tensor_tensor(out=qT[P0:P1, pr, t0:t0 + n], in0=po[P0:P1, :n], in1=dn[P0:P1, :n], op=ALU.mult),
                             reads=[t_po, t_dn], writes=[t_q[hh][b]])

                    for jb in range(16):
                        q0 = max(0, 128 * (jb - 1))
                        q1 = min(NL, 128 * (jb + 2))
                        n = q1 - q0
                        mo = q0 - 128 * (jb - 1)
                        ps_ = k.ps()
                        qb_ = sorted(set([q0 // 512, (q1 - 1) // 512]))
                        k.mm([t_k[jb // 4], t_par] + [t_q[hh][b] for b in qb_], [ps_[1]],
                             [dict(out=ps_[0][:, :n], lhsT=kT[P0:P1, jb * 128:(jb + 1) * 128], rhs=qT[P0:P1, pr, q0:q1], start=True, stop=False),
                              dict(out=ps_[0][:, :n], lhsT=ident_bf[:, :], rhs=mask_bf[:, mo:mo + n], start=False, stop=True)])
                        ri = jb % NR
                        prt = Pring[:, ri * 384:(ri + 1) * 384]
                        k.op("act", lambda e: e.activation(out=prt[:, :n], in_=ps_[0][:, :n], func=AF.Exp, scale=0.125), reads=[ps_[1]], writes=[t_pr[ri]])
                        pinfo[jb] = (prt, q0, t_pr[ri])
                        if jb >= 4:
                            pv(jb - 4)
                    for i_ in range(12, 16):
                        pv(i_)
                    if need_ctx:
                        srcs = [(Vg[:, 16 + kb, :], Pc[:, kb, 2048:2304]) for kb in range(2)]
                        k.mm([t_v[2], t_pc], [t_po], [dict(out=po[P0:P1, 0:256], lhsT=va, rhs=pa, start=(ii == 0), stop=(ii == 1)) for ii, (va, pa) in enumerate(srcs)])
                        k.mm([t_pc, t_one], [t_pd], [dict(out=pd[P0:P1, 0:256], lhsT=ones_bf[:, 0:64], rhs=pa, start=(ii == 0), stop=(ii == 1)) for ii, (va, pa) in enumerate(srcs)])
                        finish(2048, 256, 4)
                for d in range(8):
                    for (t0, n, v) in blks_q:
                        b = t0 // 512
                        pw, t_pw = k.ps()
                        k.mm([t_wo] + [t_q[hh][b] for hh in range(4)], [t_pw],
                             [dict(out=pw[:, :n], lhsT=wov[:, pr, d * 128:(d + 1) * 128], rhs=qT[:, pr, t0:t0 + n], start=(pr == 0), stop=(pr == 1)) for pr in range(2)])
                        k.op("dve", lambda e: e.scalar_tensor_tensor(out=xT[:, d, t0:t0 + n], in0=pw[:, :n], scalar=G1[:, d, v:v + 1],
                                                                    in1=xT[:, d, t0:t0 + n], op0=ALU.mult, op1=ALU.add),
                             reads=[t_pw, t_mT, t_X[b]], writes=[t_X[b]])
                k.barrier()
            k.npool = 8

        def moe(j, blks):
            combT = RA[:, 4 * NT:6 * NT].bitcast(F32)
            cbs = [RA[:, 6 * NT:7 * NT], RA[:, 7 * NT:8 * NT]]
            t_comb = Tl("comb")
            t_cbs = [Tl("cb0"), Tl("cb1")]
            t_cm = Tl("cm")
            cm = TMP[0:8, 0:NT]
            k.dma("sp", router_f[:, :].rearrange("p (c e) -> p c e", c=8), router_d[j].rearrange("(c p) e -> p c e", p=128), writes=[t_lpar])
            make_h(A2v, SH2, blks, moe_j=j, combT=combT, t_comb=t_comb)
            k.barrier()
            for e_ in range(NE):
                k.op("dve", lambda e: e.tensor_scalar(out=cm, in0=combT[0:8, :], scalar1=ident[0:8, e_:e_ + 1], scalar2=None, op0=ALU.mult),
                     reads=[t_comb, t_par], writes=[t_cm])
                for (t0, n, v) in blks:
                    pc_, t_pc_ = k.ps()
                    k.mm([t_cm, t_one], [t_pc_], [dict(out=pc_[:, :n], lhsT=ones_f[0:8, :], rhs=cm[:, t0:t0 + n], start=True, stop=True)])
                    k.op("act", lambda e: e.copy(out=cbs[e_ % 2][:, t0:t0 + n], in_=pc_[:, :n]), reads=[t_pc_], writes=[t_cbs[e_ % 2]])
                ffn(mw1[j, e_], mw3[j, e_], mw2[j, e_], G2, blks, f"moe{j}_{e_}", cb=cbs[e_ % 2], t_cb=t_cbs[e_ % 2], bar=False)
            k.barrier()


        def moe_sparse(j, blks):
            ntok = sum(n for (_, n, _) in blks)
            ntb = ntok // 128
            NS = 768
            hg = RA[:, 0:6144].rearrange("p (c t) -> p c t", c=8)
            uvs = [RA[:, 6144 + a * 1536:6144 + (a + 1) * 1536].rearrange("p (f t) -> p f t", f=2) for a in range(2)]
            hbuf = [RA[:, 9216 + a * 1024:9216 + (a + 1) * 1024] for a in range(3)]
            Sg = [RA[:, 12288 + a * 384:12288 + (a + 1) * 384] for a in range(3)]
            STw = RA[:, 13440:16512].rearrange("p (a t) -> p a t", a=6)
            iota_f = RA[:, 16512:17280].bitcast(F32)
            pos_tm = RA[:, 17280:17568].bitcast(F32).rearrange("p (b e) -> p b e", e=8)
            comb_tm = RA[:, 17568:17856].bitcast(F32).rearrange("p (b e) -> p b e", e=8)
            mask_tm = RA[:, 17856:18000].rearrange("p (b e) -> p b e", e=8)
            cnt_i = RA[:, 18000:18016].bitcast(I32)
            slotid = RA[:, 18016:18052].bitcast(F32)
            CP = TMP[0:16, 0:NT]
            acc = RH[:, 0:12288].bitcast(F32).rearrange("p (c t) -> p c t", c=8)
            otm = RH[:, 12288:18432].rearrange("p (a d) -> p a d", a=6)
            t_rt = Tl("rt")
            t_mc = Tl("mc")
            t_cp = Tl("cp")
            t_hb = [Tl(f"hb{a}") for a in range(3)]
            t_sg = [Tl(f"sg{a}") for a in range(3)]
            t_hg = [Tl("hg0"), Tl("hg1")]
            t_acc = [Tl("acc0"), Tl("acc1")]
            t_otm = [Tl(f"otm{a}") for a in range(6)]
            t_stw = Tl("stw")
            t_hd = Tl("hd")
            k.dma("sp", router_f[:, :].rearrange("p (c e) -> p c e", c=8), router_d[j].rearrange("(c p) e -> p c e", p=128), writes=[t_lpar])
            k.dma("sp", iota_f, iota_d[:, :], writes=[t_mc])
            k.dma("sp", slotid, slotid_d[:, :], writes=[t_mc])
            make_h(A2v, SH2, blks, moe_j=dict(comb_tm=comb_tm, mask_tm=mask_tm, t_rt=t_rt))
            for tbi in range(ntb):
                pp, t_pp = k.ps()
                items = [dict(out=pp[:, 0:8], lhsT=ones_bf[:, :], rhs=mask_tm[:, b_, :], start=(b_ == 0), stop=False) for b_ in range(tbi)]
                items.append(dict(out=pp[:, 0:8], lhsT=triu_bf[:, :], rhs=mask_tm[:, tbi, :], start=(tbi == 0), stop=True))
                k.mm([t_rt, t_one, t_par], [t_pp], items)
                k.op("dve", lambda e: e.scalar_tensor_tensor(out=pos_tm[:, tbi, :], in0=pp[:, 0:8], scalar=1.0, in1=mask_tm[:, tbi, :], op0=ALU.add, op1=ALU.mult),
                     reads=[t_pp], writes=[t_rt])
                k.op("dve", lambda e: e.tensor_scalar(out=pos_tm[:, tbi, :], in0=pos_tm[:, tbi, :], scalar1=-1.0, scalar2=None, op0=ALU.add), writes=[t_rt])
                cp16 = sml[:, 48:64]
                k.op("dve", lambda e: e.tensor_copy(out=cp16[:, 0:8], in_=comb_tm[:, tbi, :]), reads=[t_rt], writes=[t_sml])
                k.op("dve", lambda e: e.tensor_copy(out=cp16[:, 8:16], in_=pos_tm[:, tbi, :]), reads=[t_rt], writes=[t_sml])
                pst, t_pst = k.ps()
                k.mm([t_sml, t_par], [t_pst], [dict(out=pst[0:16, 0:128], lhsT=cp16, rhs=ident[:, :], start=True, stop=True, is_transpose=True)])
                k.op("act", lambda e: e.copy(out=CP[0:16, tbi * 128:(tbi + 1) * 128], in_=pst[0:16, 0:128]), reads=[t_pst], writes=[t_cp])
            pcn, t_pcn = k.ps()
            k.mm([t_rt, t_one], [t_pcn], [dict(out=pcn[:, 0:8], lhsT=ones_bf[:, :], rhs=mask_tm[:, b_, :], start=(b_ == 0), stop=(b_ == ntb - 1)) for b_ in range(ntb)])
            t_cnt = Tl("cnt")
            k.op("dve", lambda e: e.tensor_copy(out=cnt_i, in_=pcn[:, 0:8]), reads=[t_pcn], writes=[t_cnt])
            for tb in range(ntb):
                b = min(tb // 4, 4)
                pt, t_pt = k.ps()
                ptb = pt[:, :].bitcast(BF16)
                k.mm([t_H[b], t_par], [t_pt], [dict(out=ptb[:, c * 128:(c + 1) * 128], lhsT=hT[:, c, tb * 128:(tb + 1) * 128], rhs=ident_bf[:, :],
                                                     start=True, stop=True, is_transpose=True) for c in range(8)])
                hb, t_h = hbuf[tb % 3], t_hb[tb % 3]
                k.op("act" if tb % 2 == 0 else "dve", (lambda e: e.copy(out=hb, in_=ptb[:, 0:1024])) if tb % 2 == 0 else (lambda e: e.tensor_copy(out=hb, in_=ptb[:, 0:1024])),
                     reads=[t_pt], writes=[t_h])
                k.dma("sp", h_tm_d[tb], hb, reads=[t_h], semt=t_hd)
            k.barrier()

            w_srcs = None

            def pass_body(e_, p_, blocks, nsb):
                spb = nsb // 2
                W1d, W3d, W2d = mw1[j, e_], mw3[j, e_], mw2[j, e_]
                w1s = W1d.rearrange("(c p) n -> p c n", p=128)
                w3s = W3d.rearrange("(c p) n -> p c n", p=128)
                w2s = W2d.rearrange("(f p) n -> p f n", p=128)

                def views(a):
                    sl = slots[a]
                    return (sl[:, 0:2048].rearrange("p (c n) -> p c n", c=8), sl[:, 2048:4096].rearrange("p (c n) -> p c n", c=8),
                            sl[:, 4096:6144].rearrange("p (f n) -> p f n", f=2))

                def load(i):
                    a, b_, c_ = views(i % 2)
                    tw = TW[i % 2]
                    k.dma("pool", a, w1s[:, :, i * 256:(i + 1) * 256], writes=[tw[0]])
                    k.dma("pool", b_, w3s[:, :, i * 256:(i + 1) * 256], writes=[tw[1]])
                    k.dma("pool", c_, w2s[:, 2 * i:2 * i + 2, :], writes=[tw[2]])
                load(0)
                hi = 0
                for bi, (s0, sn) in enumerate(blocks):
                    accs = [k.psum[c] for c in range(8)]
                    for tb in range(ntb):
                        hb, t_h = hbuf[hi % 3], t_hb[hi % 3]
                        sg, t_s = Sg[hi % 3], t_sg[hi % 3]
                        hi += 1
                        k.dma("sp", hb, h_tm_d[tb], writes=[t_h])
                        k.op("dve", lambda e: e.tensor_scalar(out=sg[:, 0:sn], in0=iota_f[:, 0:sn], scalar1=float(NS * p_ + s0), scalar2=pos_tm[:, tb, e_:e_ + 1],
                                                              op0=ALU.add, op1=ALU.is_equal), reads=[t_mc, t_rt], writes=[t_s])
                        for c in range(8):
                            k.mm([t_h, t_s], [accs[c][1]], [dict(out=accs[c][0][:, 0:sn], lhsT=hb[:, c * 128:(c + 1) * 128], rhs=sg[:, 0:sn], start=(tb == 0), stop=(tb == ntb - 1))])
                    for c in range(8):
                        if c % 2 == 0:
                            k.op("act", lambda e: e.copy(out=hg[:, c, s0:s0 + sn], in_=accs[c][0][:, 0:sn]), reads=[accs[c][1]], writes=[t_hg[bi]])
                        else:
                            k.op("dve", lambda e: e.tensor_copy(out=hg[:, c, s0:s0 + sn], in_=accs[c][0][:, 0:sn]), reads=[accs[c][1]], writes=[t_hg[bi]])
                for i in range(14):
                    if i + 1 < 14:
                        load(i + 1)
                    a = i % 2
                    w1v, w3v, w2v = views(a)
                    tw = TW[a]
                    uv = uvs[a]
                    for fc in range(2):
                        for bi, (s0, sn) in enumerate(blocks):
                            p1, t_p1 = k.ps()
                            p3, t_p3 = k.ps()
                            k.mm([tw[0], t_hg[bi]], [t_p1], [dict(out=p1[:, :sn], lhsT=w1v[:, c, fc * 128:(fc + 1) * 128], rhs=hg[:, c, s0:s0 + sn],
                                                                 start=(c == 0), stop=(c == 7)) for c in range(8)])
                            k.mm([tw[1], t_hg[bi]], [t_p3], [dict(out=p3[:, :sn], lhsT=w3v[:, c, fc * 128:(fc + 1) * 128], rhs=hg[:, c, s0:s0 + sn],
                                                                 start=(c == 0), stop=(c == 7)) for c in range(8)])
                            gb, t_g = nxt("gt", gt, t_gt)
                            k.op("act", lambda e: e.activation(out=gb[:, :sn], in_=p1[:, :sn], func=AF.Silu), reads=[t_p1], writes=[t_g])
                            k.op("dve", lambda e: e.tensor_tensor(out=uv[:, fc, s0:s0 + sn], in0=gb[:, :sn], in1=p3[:, :sn], op=ALU.mult),
                                 reads=[t_g, t_p3], writes=[TU[a][bi]])
                    for d in range(8):
                        for bi, (s0, sn) in enumerate(blocks):
                            po_, t_po_ = k.ps()
                            k.mm([tw[2], TU[a][bi]], [t_po_], [dict(out=po_[:, :sn], lhsT=w2v[:, fc, d * 128:(d + 1) * 128], rhs=uv[:, fc, s0:s0 + sn],
                                                                   start=(fc == 0), stop=(fc == 1)) for fc in range(2)])
                            if i == 0:
                                k.op("act", lambda e: e.copy(out=acc[:, d, s0:s0 + sn], in_=po_[:, :sn]), reads=[t_po_], writes=[t_acc[bi]])
                            else:
                                k.op("dve", lambda e: e.tensor_tensor(out=acc[:, d, s0:s0 + sn], in0=po_[:, :sn], in1=acc[:, d, s0:s0 + sn], op=ALU.add),
                                     reads=[t_po_], writes=[t_acc[bi]])
                for sb_ in range(nsb):
                    for half in range(2):
                        pt, t_pt = k.ps()
                        k.mm([t_acc[sb_ // spb], t_par], [t_pt],
                             [dict(out=pt[:, dd * 128:(dd + 1) * 128], lhsT=acc[:, half * 4 + dd, sb_ * 128:(sb_ + 1) * 128], rhs=ident[:, :],
                                   start=True, stop=True, is_transpose=True) for dd in range(4)])
                        if half == 0:
                            k.op("act", lambda e: e.copy(out=otm[:, sb_, 0:512], in_=pt[:, :]), reads=[t_pt], writes=[t_otm[sb_]])
                        else:
                            k.op("dve", lambda e: e.tensor_copy(out=otm[:, sb_, 512:1024], in_=pt[:, :]), reads=[t_pt], writes=[t_otm[sb_]])
                for (t0, n, v) in blks:
                    b = t0 // 512
                    cmt, t_c = nxt("t32", t32, t_t32)
                    k.op("dve", lambda e: e.tensor_scalar(out=cmt[0:16, :n], in0=CP[0:16, t0:t0 + n], scalar1=ident[0:16, e_:e_ + 1], scalar2=None, op0=ALU.mult),
                         reads=[t_cp, t_par], writes=[t_c])
                    pc_, t_pc_ = k.ps()
                    k.mm([t_c, t_one], [t_pc_], [dict(out=pc_[:, :n], lhsT=ones_f[0:16, :], rhs=cmt[0:16, :n], start=True, stop=True)])
                    cbb, t_cb = nxt("rs", rs, t_rs)
                    k.op("act", lambda e: e.copy(out=cbb[:, :n], in_=pc_[:, :n]), reads=[t_pc_], writes=[t_cb])
                    pmt, t_p = nxt("t32", t32, t_t32)
                    k.op("dve", lambda e: e.tensor_scalar(out=pmt[0:16, :n], in0=CP[0:16, t0:t0 + n], scalar1=ident[0:16, 8 + e_:9 + e_], scalar2=None, op0=ALU.mult),
                         reads=[t_cp, t_par], writes=[t_p])
                    pp_, t_pp_ = k.ps()
                    k.mm([t_p, t_one], [t_pp_], [dict(out=pp_[:, :n], lhsT=ones_f[0:16, :], rhs=pmt[0:16, :n], start=True, stop=True)])
                    for sb_ in range(nsb):
                        kk = 6 * p_ + sb_
                        k.op("dve", lambda e: e.scalar_tensor_tensor(out=STw[:, sb_, :n], in0=pp_[:, :n], scalar=slotid[:, kk:kk + 1], in1=cbb[:, :n],
                                                                    op0=ALU.is_equal, op1=ALU.mult), reads=[t_pp_, t_cb, t_mc], writes=[t_stw])
                    for d in range(8):
                        px, t_px = k.ps()
                        k.mm([t_stw] + t_otm[:nsb], [t_px], [dict(out=px[:, :n], lhsT=otm[:, sb_, d * 128:(d + 1) * 128], rhs=STw[:, sb_, :n],
                                                           start=(sb_ == 0), stop=(sb_ == nsb - 1)) for sb_ in range(nsb)])
                        k.op("dve", lambda e: e.scalar_tensor_tensor(out=xT[:, d, t0:t0 + n], in0=px[:, :n], scalar=G2[:, d, v:v + 1],
                                                                    in1=xT[:, d, t0:t0 + n], op0=ALU.mult, op1=ALU.add),
                             reads=[t_px, t_mT, t_X[b]], writes=[t_X[b]])

            npass = (ntok + NS - 1) // NS
            cnt_f = sml[:, 0:8]
            flg_f2 = TMP[:, 2304:2376]
            flg_f = flg_f2.rearrange("p (a e) -> p a e", e=8)
            flg_i = TMP[:, 2376:2448].bitcast(I32)
            t_flg = Tl("flg")
            k.op("dve", lambda e: e.tensor_copy(out=cnt_f, in_=cnt_i), reads=[t_cnt], writes=[t_sml])
            for p_ in range(3):
                k.op("dve", lambda e: e.tensor_single_scalar(out=flg_f[:, 2 * p_, :], in_=cnt_f, scalar=float(NS * p_ + 512), op=ALU.is_gt), reads=[t_sml], writes=[t_flg])
                k.op("dve", lambda e: e.tensor_single_scalar(out=flg_f[:, 2 * p_ + 1, :], in_=cnt_f, scalar=float(NS * p_), op=ALU.is_gt), reads=[t_sml], writes=[t_flg])
                k.op("dve", lambda e: e.tensor_tensor(out=flg_f[:, 2 * p_ + 1, :], in0=flg_f[:, 2 * p_ + 1, :], in1=flg_f[:, 2 * p_, :], op=ALU.subtract), writes=[t_flg])
            k.op("dve", lambda e: e.tensor_single_scalar(out=flg_f[:, 6, :], in_=cnt_f, scalar=float(NS), op=ALU.is_gt), reads=[t_sml], writes=[t_flg])
            k.op("dve", lambda e: e.tensor_single_scalar(out=flg_f[:, 7, :], in_=cnt_f, scalar=float(2 * NS), op=ALU.is_gt), reads=[t_sml], writes=[t_flg])
            k.op("dve", lambda e: e.tensor_tensor(out=flg_f[:, 8, :], in0=flg_f[:, 6, :], in1=flg_f[:, 0, :], op=ALU.subtract), writes=[t_flg])
            k.op("dve", lambda e: e.tensor_scalar(out=flg_f[:, 8, :], in0=flg_f[:, 8, :], scalar1=1.0, scalar2=None, op0=ALU.add), writes=[t_flg])
            k.op("dve", lambda e: e.tensor_copy(out=flg_i, in_=flg_f2), writes=[t_flg])
            regs = moe_regs
            variants = [([(0, 384), (384, 384)], 6), ([(0, 256), (256, 256)], 4)]

            def load_flag(row, e_):
                col = row * 8 + e_
                for r_ in regs:
                    en = {"Pool": "pool", "Activation": "act", "PE": "pe", "DVE": "dve", "SP": "sp"}[str(r_.engine).split(".")[-1]]
                    E = k.E[en]
                    k._need(E, k._deps([t_flg], []))
                    E.h.load(r_, flg_i[0:1, col:col + 1])

            def bump(deltas):
                for (en, h_, dv) in deltas:
                    k.E[en].h.sem_inc(h_, dv)

            def region(row, e_, body):
                load_flag(row, e_)
                k.region_begin()
                with nc.If_ne(regs, 0):
                    body()
                    deltas = k.region_end()
                with nc.Else():
                    bump(deltas)

            def run_pass(e_, p_):
                for vi, (blocks_v, nsb_v) in enumerate(variants):
                    region(2 * p_ + vi, e_, lambda: pass_body(e_, p_, blocks_v, nsb_v))

            def run_from(e_, p_):
                def body():
                    run_pass(e_, p_)
                    if p_ + 1 < npass:
                        run_from(e_, p_ + 1)
                region(5 + p_, e_, body)

            for e_ in range(NE):
                region(0, e_, lambda: pass_body(e_, 0, variants[0][0], variants[0][1]))

                def rest():
                    region(1, e_, lambda: pass_body(e_, 0, variants[1][0], variants[1][1]))
                    if npass > 1:
                        run_from(e_, 1)
                region(8, e_, rest)
            k.barrier()

        moe_regs = nc.alloc_registers("cnt")
        import os
        stop = int(os.environ.get("KSTOP", "99"))
        phase = 0
        for li in range(4):
            j = li // 2
            need_ctx = li < 3
            blks = BLK_L + (BLK_C if need_ctx else [])
            layer_params(li)
            if li % 2 == 0:
                make_h(A1v, SH1, blks)
                k.barrier()
                even_mixer(j, blks)
                phase += 1
                if phase >= stop:
                    break
                make_h(A2v, SH2, blks)
                k.barrier()
                ffn(ffw1[j], ffw3[j], ffw2[j], G2, blks, f"ff{j}")
                phase += 1
                if phase >= stop:
                    break
            else:
                if li == 1:
                    rope_tables()
                make_h(A1v, SH1, BLK_L + BLK_C)
                k.barrier()
                attention(j, need_ctx)
                phase += 1
                if phase >= stop:
                    break
                moe_sparse(j, blks)
                phase += 1
                if phase >= stop:
                    break

        k.barrier()
        t_out = Tl("out")
        for b in range(5):
            t0, n, _ = (BLK_L + BLK_C)[b]
            k.dma("sp", out_d[:, t0:t0 + n].rearrange("(c p) t -> p c t", p=128), xT[:, :, t0:t0 + n], reads=[t_X[b]], semt=t_out)
        k.E["sp"].h.wait_ge(t_out.dsem, t_out.dcnt)
    return nc


def _prep(inputs, b):
    f = np.float32
    x, ctx, c, c_ctx = inputs["x"], inputs["ctx"], inputs["c"], inputs["c_ctx"]
    m = {}
    m["xin"] = np.ascontiguousarray(np.concatenate([x[b], ctx[b]], axis=0).T)
    cvv = np.stack([c[b].reshape(8, 128).T, c_ctx.reshape(8, 128).T], axis=-1)
    m["cv"] = np.ascontiguousarray(cvv.reshape(128, 16))
    return m


def _shared(inputs):
    m = {}
    g = lambda a: np.ascontiguousarray(a, dtype=np.float32)
    m["ada_w"] = g(inputs["ada_w"])
    m["ada_b"] = g(inputs["ada_b"].reshape(4, 48, 128).transpose(0, 2, 1))
    m["ng1"] = g(inputs["norm_mix_g"].reshape(4, 8, 128).transpose(0, 2, 1))
    m["ng2"] = g(inputs["norm_ffn_g"].reshape(4, 8, 128).transpose(0, 2, 1))
    m["ev_w_in"] = g(inputs["ev_w_in"])
    m["ev_ln_g"] = g(inputs["ev_ln_g"].reshape(2, 8, 128).transpose(0, 2, 1))
    m["ev_ln_b"] = g(inputs["ev_ln_b"].reshape(2, 8, 128).transpose(0, 2, 1))
    m["ev_wsT"] = g(inputs["ev_ws"].transpose(0, 1, 3, 2))
    m["ev_bs"] = g(inputs["ev_bs"].reshape(2, 1024))
    m["ev_conv_w"] = g(inputs["ev_conv_w"].transpose(0, 2, 1).reshape(2, 8, 128, 31).transpose(0, 2, 1, 3).reshape(2, 128, 248))
    m["ev_conv_b"] = g(inputs["ev_conv_b"].reshape(2, 8, 128).transpose(0, 2, 1))
    m["ev_cnorm_g"] = g(inputs["ev_cnorm_g"].reshape(2, 8, 128).transpose(0, 2, 1))
    m["ev_w_out"] = g(inputs["ev_w_out"])
    m["od_w_qkv"] = g(inputs["od_w_qkv"])
    m["od_q_g"] = g(np.concatenate([inputs["od_q_g"], inputs["od_q_g"]], axis=1).reshape(2, 128, 1))
    m["od_k_g"] = g(np.concatenate([inputs["od_k_g"], inputs["od_k_g"]], axis=1).reshape(2, 128, 1))
    m["od_sink"] = g(inputs["od_sink"])
    m["od_w_o"] = g(inputs["od_w_o"])
    m["ff_w1"] = g(inputs["ff_w1"])
    m["ff_w3"] = g(inputs["ff_w3"])
    m["ff_w2"] = g(inputs["ff_w2"])
    m["moe_router"] = g(inputs["moe_router"])
    m["moe_w1"] = g(inputs["moe_w1"])
    m["moe_w3"] = g(inputs["moe_w3"])
    m["moe_w2"] = g(inputs["moe_w2"])
    m["c_ident"] = np.eye(128, dtype=np.float32)
    pm = np.zeros((64, 64), np.float32)
    for p in range(64):
        r = p % 32
        if r < 16:
            pm[p + 16, p] = -1.0
        else:
            pm[p - 16, p] = 1.0
    pm2 = np.zeros((128, 128), np.float32)
    pm2[0:64, 0:64] = pm
    pm2[64:128, 64:128] = pm
    m["c_perm"] = pm2
    bd = np.zeros((128, 128), np.float32)
    bd[0:64, 0:64] = 1.0
    bd[64:128, 64:128] = 1.0
    m["c_bd"] = bd
    kk = np.arange(128)[:, None]
    qq = np.arange(384)[None, :] - 128
    m["c_mask"] = np.where(np.abs(qq - kk) <= 128, 0.0, -240000.0).astype(np.float32)
    t = np.arange(2048)
    pos = np.zeros((64, 2048), np.float32)
    pos[0:32, :] = (t // 64)[None, :]
    pos[32:64, :] = (t % 64)[None, :]
    m["c_pos"] = np.concatenate([pos, pos], axis=0)
    m["c_fidx"] = (np.arange(128) % 16).astype(np.float32).reshape(128, 1)
    m["c_triu"] = (np.arange(128)[:, None] < np.arange(128)[None, :]).astype(np.float32)
    m["c_iota"] = np.tile(np.arange(384, dtype=np.float32)[None, :], (128, 1))
    m["c_slotid"] = (np.arange(128, dtype=np.float32)[:, None] + 128.0 * np.arange(18, dtype=np.float32)[None, :])
    return m


_NC_CACHE = {}


def kernel(**inputs):
    inputs = {k_: np.asarray(v) for k_, v in inputs.items()}
    ncores = 8
    if "nc" not in _NC_CACHE:
        _NC_CACHE["nc"] = build()
    nc = _NC_CACHE["nc"]
    shared = _shared(inputs)
    in_maps = []
    for b in range(ncores):
        m = dict(shared)
        m.update(_prep(inputs, b))
        in_maps.append(m)
    res = run_bass_kernel_spmd(nc, in_maps, core_ids=list(range(ncores)))
    outs = [np.asarray(r["out"]) for r in res.results]
    return np.stack([o[:, :NL].T for o in outs], axis=0).astype(np.float32)
```

```python
import numpy as np
import concourse.bass as bass
import concourse.mybir as mybir
from concourse.bass_utils import run_bass_kernel_spmd
from contextlib import ExitStack

F32 = mybir.dt.float32
BF16 = mybir.dt.bfloat16
AF = mybir.ActivationFunctionType
ALU = mybir.AluOpType
AX = mybir.AxisListType

NL, NCX, NT = 2048, 256, 2304
D, DFF, NE = 1024, 3584, 8
EPS = 1e-6
BLK_L = [(0, 512, 0), (512, 512, 0), (1024, 512, 0), (1536, 512, 0)]
BLK_C = [(2048, 256, 1)]
SEMLIM = 10 ** 9


class Tl:
    __slots__ = ("name", "w", "r", "dsem", "dcnt")

    def __init__(s, name):
        s.name = name
        s.w = None
        s.r = {}
        s.dsem = None
        s.dcnt = 0


class Eng:
    def __init__(s, name, h):
        s.name, s.h, s.sem, s.cnt, s.waited = name, h, None, 0, {}


class K:
    def __init__(s, nc, es):
        s.nc, s.es = nc, es
        s.E = {"pe": Eng("pe", nc.tensor), "act": Eng("act", nc.scalar), "dve": Eng("dve", nc.vector),
               "pool": Eng("pool", nc.gpsimd), "sp": Eng("sp", nc.sync)}
        s.nsem = 0
        for e in s.E.values():
            e.sem = s.newsem(e.name)
        s.dsems = {}
        s.psum = []
        s.psi = 0
        s._snaps = []
        s.npool = 8

    def newsem(s, name):
        s.nsem += 1
        return s.es.enter_context(s.nc.semaphore(f"{name}_{s.nsem}"))

    def _need(s, E, deps, embed=False):
        todo = []
        for num, (h, v) in deps.items():
            if E.waited.get(num, 0) < v:
                todo.append((h, v))
                E.waited[num] = v
        last = None
        if embed and todo:
            last = todo.pop()
        for (h, v) in todo:
            E.h.wait_ge(h, v)
        return last

    @staticmethod
    def _deps(reads, writes):
        d = {}

        def add(rec):
            h, v = rec
            if h.num not in d or d[h.num][1] < v:
                d[h.num] = rec
        for t in reads:
            if t.w:
                add(t.w)
        for t in writes:
            if t.w:
                add(t.w)
            for rec in t.r.values():
                add(rec)
        return d

    @staticmethod
    def _mark(rec, reads, writes):
        for t in reads:
            t.r[rec[0].num] = rec
        for t in writes:
            t.w = rec
            t.r = {}

    def _rot(s, E):
        if E.cnt >= SEMLIM:
            E.sem = s.newsem(E.name)
            E.cnt = 0

    def op(s, en, fn, reads=(), writes=()):
        E = s.E[en]
        last = s._need(E, s._deps(reads, writes), embed=True)
        ins = fn(E.h)
        if last is not None:
            ins._wait_ge(last[0], last[1])
        E.cnt += 1
        ins.then_inc(E.sem, 1)
        s._mark((E.sem, E.cnt), reads, writes)
        s._rot(E)

    def mm(s, reads, writes, items):
        E = s.E["pe"]
        last = s._need(E, s._deps(reads, writes), embed=True)
        ins = None
        for it in items:
            ins = E.h.matmul(**it)
            if last is not None:
                ins._wait_ge(last[0], last[1])
                last = None
        E.cnt += 1
        ins.then_inc(E.sem, 1)
        s._mark((E.sem, E.cnt), reads, writes)
        s._rot(E)

    def dma(s, q, out, in_, reads=(), writes=(), semt=None):
        E = s.E[q]
        s._need(E, s._deps(reads, writes))
        ins = E.h.dma_start(out=out, in_=in_)
        t = semt if semt is not None else writes[0]
        if t.dsem is None:
            t.dsem = s.newsem("d" + t.name)
        t.dcnt += 16
        ins.then_inc(t.dsem, 16)
        s.dsems[t.dsem.num] = (t.dsem, t.dcnt)
        s._mark((t.dsem, t.dcnt), reads, writes)

    def barrier(s, engines=("pe", "act", "dve", "pool", "sp"), own=False):
        for en in engines:
            E = s.E[en]
            d = {}
            for F in s.E.values():
                if (own or F is not E) and F.cnt > 0:
                    d[F.sem.num] = (F.sem, F.cnt)
            d.update(s.dsems)
            s._need(E, d)

    def region_begin(s):
        for n, E in s.E.items():
            d = {}
            if E.cnt > 0:
                d[E.sem.num] = (E.sem, E.cnt)
            if n == "sp":
                d.update(s.dsems)
            s._need(E, d)
        s._snaps.append(({n: (E.sem, E.cnt) for n, E in s.E.items()}, dict(s.dsems), {n: dict(E.waited) for n, E in s.E.items()}))

    def region_end(s):
        e0, d0, w0 = s._snaps.pop()
        deltas = []
        for n, E in s.E.items():
            assert E.sem.num == e0[n][0].num, "semaphore rotated inside region"
            if E.cnt > e0[n][1]:
                deltas.append((n, E.sem, E.cnt - e0[n][1]))
        for num, (h, v) in s.dsems.items():
            v0 = d0[num][1] if num in d0 else 0
            if v > v0:
                deltas.append(("sp", h, v - v0))
        for n, E in s.E.items():
            E.waited = w0[n]
        return deltas

    def ps(s):
        p = s.psum[s.psi % s.npool]
        s.psi += 1
        return p


def build():
    nc = bass.Bass("TRN2", target_bir_lowering=False)

    def din(name, shape):
        return nc.dram_tensor(name, list(shape), F32, kind="ExternalInput").ap()

    xin = din("xin", [1024, NT])
    cv_d = din("cv", [128, 16])
    ada_w = din("ada_w", [4, 1024, 6144])
    ada_b = din("ada_b", [4, 128, 48])
    ng1_d = din("ng1", [4, 128, 8])
    ng2_d = din("ng2", [4, 128, 8])
    w_in = din("ev_w_in", [2, 1024, 4096])
    lngf_d = din("ev_ln_g", [2, 128, 8])
    lnbf_d = din("ev_ln_b", [2, 128, 8])
    wsT_d = din("ev_wsT", [2, 8, 128, 128])
    bs_d = din("ev_bs", [2, 1024])
    convw_d = din("ev_conv_w", [2, 128, 248])
    convb_d = din("ev_conv_b", [2, 128, 8])
    cng_d = din("ev_cnorm_g", [2, 128, 8])
    w_out = din("ev_w_out", [2, 2048, 1024])
    wqkv = din("od_w_qkv", [2, 1024, 1536])
    qg_d = din("od_q_g", [2, 128, 1])
    kg_d = din("od_k_g", [2, 128, 1])
    sink_d = din("od_sink", [2, 16])
    wo_d = din("od_w_o", [2, 1024, 1024])
    ffw1 = din("ff_w1", [2, 1024, DFF])
    ffw3 = din("ff_w3", [2, 1024, DFF])
    ffw2 = din("ff_w2", [2, DFF, 1024])
    router_d = din("moe_router", [2, 1024, 8])
    mw1 = din("moe_w1", [2, 8, 1024, DFF])
    mw3 = din("moe_w3", [2, 8, 1024, DFF])
    mw2 = din("moe_w2", [2, 8, DFF, 1024])
    ident_d = din("c_ident", [128, 128])
    perm_d = din("c_perm", [128, 128])
    bd_d = din("c_bd", [128, 128])
    mask_d = din("c_mask", [128, 384])
    pos_d = din("c_pos", [128, 2048])
    fidx_d = din("c_fidx", [128, 1])
    triu_d = din("c_triu", [128, 128])
    iota_d = din("c_iota", [128, 384])
    slotid_d = din("c_slotid", [128, 18])
    h_tm_d = nc.dram_tensor("h_tm_scratch", [18, 128, 1024], BF16, kind="Internal").ap()
    out_d = nc.dram_tensor("out", [1024, NT], F32, kind="ExternalOutput").ap()

    with ExitStack() as es:
        k = K(nc, es)

        def sb(name, shape, dt):
            return es.enter_context(nc.sbuf_tensor("sb_" + name, list(shape), dt))

        RX = sb("RX", [128, 8 * NT], F32)
        RH = sb("RH", [128, 8 * NT], BF16)
        RA = sb("RA", [128, 8 * NT], BF16)
        RW = sb("RW", [128, 12288], BF16)
        xT = RX[:, :].rearrange("p (c t) -> p c t", c=8)
        hT = RH[:, :].rearrange("p (c t) -> p c t", c=8)
        for i in range(8):
            k.psum.append((es.enter_context(nc.psum_tensor(f"ps{i}", [128, 512], F32)), Tl(f"ps{i}")))

        ident = sb("ident", [128, 128], F32)
        ones_bf = sb("ones_bf", [128, 128], BF16)
        ones_f = sb("ones_f", [16, 128], F32)
        triu_bf = sb("triu_bf", [128, 128], BF16)
        ident_bf = sb("ident_bf", [128, 128], BF16)
        perm_bf = sb("perm_bf", [128, 128], BF16)
        bd_bf = sb("bd_bf", [128, 128], BF16)
        mask_bf = sb("mask_bf", [128, 384], BF16)
        cv = sb("cv", [128, 16], F32)
        scv = sb("scv", [128, 16], BF16)
        adab = sb("adab", [128, 48], F32)
        mT = sb("mT", [128, 96], F32)
        mT3 = mT[:, :].rearrange("p (j v) -> p j v", v=2)
        A1 = sb("A1", [128, 16], F32)
        A2 = sb("A2", [128, 16], F32)
        A1v = A1[:, :].rearrange("p (c v) -> p c v", v=2)
        A2v = A2[:, :].rearrange("p (c v) -> p c v", v=2)
        ng1 = sb("ng1", [128, 8], F32)
        ng2 = sb("ng2", [128, 8], F32)
        tmp16 = sb("tmp16", [128, 16], F32)
        convw = sb("convw", [128, 248], F32)
        convb = sb("convb", [128, 8], F32)
        cng = sb("cng", [128, 8], F32)
        qg = sb("qg", [128, 1], F32)
        kg = sb("kg", [128, 1], F32)
        sinke = sb("sinke", [128, 16], F32)
        router_f = sb("router_f", [128, 64], F32)
        cossin = sb("cossin", [128, 2 * 2048], BF16)
        cosT = cossin[:, 0:2048]
        sinT = cossin[:, 2048:4096]
        sq = [sb(f"sq{i}", [128, 512], BF16) for i in range(2)]
        rs = [sb(f"rs{i}", [128, 512], F32) for i in range(2)]
        t32 = [sb(f"t32_{i}", [128, 512], F32) for i in range(2)]
        gt = [sb(f"gt{i}", [128, 512], BF16) for i in range(2)]
        tt = [sb(f"tt{i}", [128, 512], BF16) for i in range(2)]
        TMP = sb("TMP", [128, 2560], F32)
        sml = sb("sml", [128, 64], F32)

        t_sq = [Tl(f"sq{i}") for i in range(2)]
        t_rs = [Tl(f"rs{i}") for i in range(2)]
        t_t32 = [Tl(f"t32{i}") for i in range(2)]
        t_gt = [Tl(f"gt{i}") for i in range(2)]
        t_tt = [Tl(f"tt{i}") for i in range(2)]
        t_par = Tl("par")
        t_lpar = Tl("lpar")
        t_mT = Tl("mT")
        t_X = [Tl(f"X{i}") for i in range(5)]
        t_H = [Tl(f"H{i}") for i in range(5)]
        TW = [[Tl(f"w{a}_{i}") for i in range(3)] for a in range(2)]
        t_p2 = Tl("m2p")
        TU = [[Tl(f"u{a}_{b}") for b in range(5)] for a in range(2)]
        rot = {"sq": 0, "rs": 0, "t32": 0, "gt": 0, "tt": 0}

        def nxt(name, arr, tls):
            i = rot[name] % len(arr)
            rot[name] += 1
            return arr[i], tls[i]

        k.dma("sp", ident[:, :], ident_d[:, :], writes=[t_par])
        k.dma("sp", cv[:, :], cv_d[:, :], writes=[t_par])
        k.dma("pool", perm_bf[:, :], perm_d[:, :], writes=[t_par])
        k.dma("pool", bd_bf[:, :], bd_d[:, :], writes=[t_par])
        k.dma("pool", mask_bf[:, :], mask_d[:, :], writes=[t_par])
        k.dma("pool", triu_bf[:, :], triu_d[:, :], writes=[t_par])
        k.dma("pool", ident_bf[:, :], ident_d[:, :], writes=[t_par])
        t_one = Tl("ones")
        k.op("dve", lambda e: e.memset(ones_bf[:, :], 1.0), writes=[t_one])
        k.op("dve", lambda e: e.memset(ones_f[:, :], 1.0), writes=[t_one])
        for b in range(5):
            t0, n, _ = (BLK_L + BLK_C)[b]
            k.dma("sp", xT[:, :, t0:t0 + n], xin[:, t0:t0 + n].rearrange("(c p) t -> p c t", p=128), writes=[t_X[b]])
        k.op("act", lambda e: e.activation(out=scv[:, :], in_=cv[:, :], func=AF.Silu), reads=[t_par], writes=[t_one])
        scv3 = scv[:, :].rearrange("p (c v) -> p c v", v=2)

        W0 = RW[:, 0:6144]
        W1s = RW[:, 6144:12288]
        slots = [W0, W1s]

        def layer_params(li):
            t_w = [TW[0][0], TW[1][0]]
            k.dma("sp", adab[:, :], ada_b[li], writes=[t_lpar])
            k.dma("sp", ng1[:, :], ng1_d[li], writes=[t_lpar])
            k.dma("sp", ng2[:, :], ng2_d[li], writes=[t_lpar])
            psm, t_psm = k.ps()
            src = ada_w[li].rearrange("(c p) n -> p c n", p=128)
            for i in range(12):
                wv = slots[i % 2][:, 0:4096].rearrange("p (c n) -> p c n", c=8)
                k.dma("pool", wv, src[:, :, i * 512:(i + 1) * 512], writes=[t_w[i % 2]])
                for jj in range(4):
                    j = 4 * i + jj
                    k.mm([t_w[i % 2], t_one], [t_psm],
                         [dict(out=psm[:, 2 * j:2 * j + 2], lhsT=wv[:, c, jj * 128:(jj + 1) * 128], rhs=scv3[:, c, :],
                               start=(c == 0), stop=(c == 7)) for c in range(8)])
            ps3 = psm[:, 0:96].rearrange("p (j v) -> p j v", v=2)
            for v in range(2):
                k.op("dve", lambda e, v=v: e.tensor_tensor(out=mT3[:, :, v], in0=ps3[:, :, v], in1=adab[:, :], op=ALU.add),
                     reads=[t_psm, t_lpar], writes=[t_mT])
            t16 = tmp16[:, :].rearrange("p (c v) -> p c v", v=2)
            for (Av, ng, off) in ((A1v, ng1, 8), (A2v, ng2, 32)):
                k.op("dve", lambda e, off=off: e.tensor_scalar(out=t16, in0=mT3[:, off:off + 8, :], scalar1=1.0, scalar2=None, op0=ALU.add),
                     reads=[t_mT], writes=[t_lpar])
                for v in range(2):
                    k.op("dve", lambda e, v=v, Av=Av, ng=ng: e.tensor_tensor(out=Av[:, :, v], in0=t16[:, :, v], in1=ng[:, :], op=ALU.mult),
                         reads=[t_lpar], writes=[t_lpar])
            k.barrier()

        SH1, G1, SH2, G2 = mT3[:, 0:8, :], mT3[:, 16:24, :], mT3[:, 24:32, :], mT3[:, 40:48, :]

        def make_h(Av, SHv, blks, moe_j=None, combT=None, t_comb=None):
            for (t0, n, v) in blks:
                b = t0 // 512
                rsb, t_r = nxt("rs", rs, t_rs)
                psm, t_ps = k.ps()
                for c in range(8):
                    sqb, t_s = nxt("sq", sq, t_sq)
                    k.op("act", lambda e, c=c, sqb=sqb: e.activation(out=sqb[:, :n], in_=xT[:, c, t0:t0 + n], func=AF.Square),
                         reads=[t_X[b]], writes=[t_s])
                    k.mm([t_s, t_one], [t_ps], [dict(out=psm[:, :n], lhsT=ones_bf[:, :], rhs=sqb[:, :n], start=(c == 0), stop=(c == 7))])
                k.op("act", lambda e: e.activation(out=rsb[:, :n], in_=psm[:, :n], func=AF.Ln, scale=1.0 / D, bias=EPS),
                     reads=[t_ps], writes=[t_r])
                k.op("act", lambda e: e.activation(out=rsb[:, :n], in_=rsb[:, :n], func=AF.Exp, scale=-0.5), reads=[t_r], writes=[t_r])
                if moe_j is not None:
                    pslg, t_pslg = k.ps()
                    nsb = n // 128
                    k.mm([t_one], [t_pslg], [dict(out=pslg[:, 0:8 * nsb], lhsT=zer_bf[:, :], rhs=zer_bf[:, 0:8 * nsb], start=True, stop=False,
                                                skip_group_check=True)])
                for c in range(8):
                    tb, t_t = nxt("t32", t32, t_t32)
                    k.op("dve", lambda e, c=c, tb=tb: e.scalar_tensor_tensor(out=tb[:, :n], in0=xT[:, c, t0:t0 + n], scalar=Av[:, c, v:v + 1],
                                                                           in1=rsb[:, :n], op0=ALU.mult, op1=ALU.mult),
                         reads=[t_X[b], t_r, t_lpar], writes=[t_t])
                    if moe_j is None:
                        k.op("act", lambda e, c=c, tb=tb: e.activation(out=hT[:, c, t0:t0 + n], in_=tb[:, :n], func=AF.Identity,
                                                                     bias=SHv[:, c, v:v + 1]),
                             reads=[t_t, t_mT], writes=[t_H[b]])
                    else:
                        k.op("act", lambda e, c=c, tb=tb: e.activation(out=tb[:, :n], in_=tb[:, :n], func=AF.Identity,
                                                                     bias=SHv[:, c, v:v + 1]),
                             reads=[t_mT], writes=[t_t])
                        k.op("pool", lambda e, c=c, tb=tb: e.tensor_copy(out=hT[:, c, t0:t0 + n], in_=tb[:, :n]),
                             reads=[t_t], writes=[t_H[b]])
                        k.mm([t_t, t_lpar], [t_pslg],
                             [dict(out=pslg[:, 8 * s_:8 * s_ + 8], lhsT=tb[:, s_ * 128:(s_ + 1) * 128], rhs=router_f[:, 8 * c:8 * c + 8],
                                   start=False, stop=(c == 7), skip_group_check=True) for s_ in range(nsb)])
                if moe_j is not None:
                    for s_ in range(nsb):
                        lg = sml[:, 0:8]
                        mx = sml[:, 8:16]
                        ex = sml[:, 16:24]
                        msk = sml[:, 24:32]
                        nm1 = sml[:, 32:33]
                        den = sml[:, 33:34]
                        k.op("dve", lambda e: e.tensor_copy(out=lg, in_=pslg[:, 8 * s_:8 * s_ + 8]), reads=[t_pslg], writes=[t_sml])
                        k.op("dve", lambda e: e.max(out=mx, in_=lg), reads=[t_sml], writes=[t_sml])
                        k.op("dve", lambda e: e.tensor_scalar(out=nm1, in0=mx[:, 0:1], scalar1=-1.0, scalar2=None, op0=ALU.mult),
                             reads=[t_sml], writes=[t_sml])
                        k.op("act", lambda e: e.activation(out=ex, in_=lg, func=AF.Exp, bias=nm1), reads=[t_sml], writes=[t_sml])
                        k.op("dve", lambda e: e.tensor_scalar(out=msk, in0=lg, scalar1=mx[:, 1:2], scalar2=None, op0=ALU.is_ge),
                             reads=[t_sml], writes=[t_sml])
                        k.op("dve", lambda e: e.tensor_tensor(out=ex, in0=ex, in1=msk, op=ALU.mult), reads=[t_sml], writes=[t_sml])
                        k.op("dve", lambda e: e.reduce_sum(out=den, in_=ex, axis=AX.X), reads=[t_sml], writes=[t_sml])
                        k.op("dve", lambda e: e.reciprocal(out=den, in_=den), reads=[t_sml], writes=[t_sml])
                        k.op("dve", lambda e: e.tensor_scalar(out=ex, in0=ex, scalar1=den, scalar2=None, op0=ALU.mult),
                             reads=[t_sml], writes=[t_sml])
                        tbi = t0 // 128 + s_
                        k.op("dve", lambda e: e.tensor_copy(out=moe_j["comb_tm"][:, tbi, :], in_=ex), reads=[t_sml], writes=[moe_j["t_rt"]])
                        k.op("dve", lambda e: e.tensor_copy(out=moe_j["mask_tm"][:, tbi, :], in_=msk), reads=[t_sml], writes=[moe_j["t_rt"]])

        zer_bf = sb("zer_bf", [128, 128], BF16)
        k.op("dve", lambda e: e.memset(zer_bf[:, :], 0.0), writes=[t_one])
        t_sml = Tl("sml")

        def ffn(W1d, W3d, W2d, Gv, blks, tag, cb=None, t_cb=None, bar=True):
            NP = 14
            t_w = TW
            t_u = TU
            w1s = W1d.rearrange("(c p) n -> p c n", p=128)
            w3s = W3d.rearrange("(c p) n -> p c n", p=128)
            w2s = W2d.rearrange("(f p) n -> p f n", p=128)

            def views(s_):
                sl = slots[s_]
                return (sl[:, 0:2048].rearrange("p (c n) -> p c n", c=8), sl[:, 2048:4096].rearrange("p (c n) -> p c n", c=8),
                        sl[:, 4096:6144].rearrange("p (f n) -> p f n", f=2))

            def load(i):
                a, b_, c_ = views(i % 2)
                tw = t_w[i % 2]
                k.dma("pool", a, w1s[:, :, i * 256:(i + 1) * 256], writes=[tw[0]])
                k.dma("pool", b_, w3s[:, :, i * 256:(i + 1) * 256], writes=[tw[1]])
                k.dma("pool", c_, w2s[:, 2 * i:2 * i + 2, :], writes=[tw[2]])

            load(0)
            for i in range(NP):
                if i + 1 < NP:
                    load(i + 1)
                s_ = i % 2
                w1v, w3v, w2v = views(s_)
                tw = t_w[s_]
                uv = RA[:, s_ * 2 * NT:(s_ + 1) * 2 * NT].rearrange("p (f t) -> p f t", f=2)
                for fc in range(2):
                    for (t0, n, v) in blks:
                        b = t0 // 512
                        p1, t_p1 = k.ps()
                        p3, t_p3 = k.ps()
                        k.mm([tw[0], t_H[b]], [t_p1], [dict(out=p1[:, :n], lhsT=w1v[:, c, fc * 128:(fc + 1) * 128], rhs=hT[:, c, t0:t0 + n],
                                                          start=(c == 0), stop=(c == 7)) for c in range(8)])
                        k.mm([tw[1], t_H[b]], [t_p3], [dict(out=p3[:, :n], lhsT=w3v[:, c, fc * 128:(fc + 1) * 128], rhs=hT[:, c, t0:t0 + n],
                                                          start=(c == 0), stop=(c == 7)) for c in range(8)])
                        gb, t_g = nxt("gt", gt, t_gt)
                        k.op("act", lambda e: e.activation(out=gb[:, :n], in_=p1[:, :n], func=AF.Silu), reads=[t_p1], writes=[t_g])
                        if cb is None:
                            k.op("dve", lambda e: e.tensor_tensor(out=uv[:, fc, t0:t0 + n], in0=gb[:, :n], in1=p3[:, :n], op=ALU.mult),
                                 reads=[t_g, t_p3], writes=[t_u[s_][b]])
                        else:
                            tb_, t_t = nxt("tt", tt, t_tt)
                            k.op("dve", lambda e: e.tensor_tensor(out=tb_[:, :n], in0=gb[:, :n], in1=p3[:, :n], op=ALU.mult),
                                 reads=[t_g, t_p3], writes=[t_t])
                            k.op("pool", lambda e: e.tensor_tensor(out=uv[:, fc, t0:t0 + n], in0=tb_[:, :n], in1=cb[:, t0:t0 + n], op=ALU.mult),
                                 reads=[t_t, t_cb], writes=[t_u[s_][b]])
                for d in range(8):
                    for (t0, n, v) in blks:
                        b = t0 // 512
                        po, t_po = k.ps()
                        k.mm([tw[2], t_u[s_][b]], [t_po], [dict(out=po[:, :n], lhsT=w2v[:, fc, d * 128:(d + 1) * 128], rhs=uv[:, fc, t0:t0 + n],
                                                              start=(fc == 0), stop=(fc == 1)) for fc in range(2)])
                        k.op("dve", lambda e: e.scalar_tensor_tensor(out=xT[:, d, t0:t0 + n], in0=po[:, :n], scalar=Gv[:, d, v:v + 1],
                                                                    in1=xT[:, d, t0:t0 + n], op0=ALU.mult, op1=ALU.add),
                             reads=[t_po, t_mT, t_X[b]], writes=[t_X[b]])
            if bar:
                k.barrier()

        def proj_out(Wd_rows, yv, t_y, Gv, blks, tag):
            t_w = [TW[0][0], TW[1][0]]
            ws = Wd_rows.rearrange("(c p) n -> p c n", p=128)
            wv = [slots[i][:, 0:4096].rearrange("p (c n) -> p c n", c=4) for i in range(2)]
            for i in range(2):
                k.dma("pool", wv[i], ws[:, 4 * i:4 * i + 4, :], writes=[t_w[i]])
            for d in range(8):
                for (t0, n, v) in blks:
                    b = t0 // 512
                    po, t_po = k.ps()
                    k.mm([t_w[0], t_w[1], t_y[b]], [t_po],
                         [dict(out=po[:, :n], lhsT=wv[c // 4][:, c % 4, d * 128:(d + 1) * 128], rhs=yv[:, c, t0:t0 + n],
                               start=(c == 0), stop=(c == 7)) for c in range(8)])
                    k.op("dve", lambda e: e.scalar_tensor_tensor(out=xT[:, d, t0:t0 + n], in0=po[:, :n], scalar=Gv[:, d, v:v + 1],
                                                                in1=xT[:, d, t0:t0 + n], op0=ALU.mult, op1=ALU.add),
                         reads=[t_po, t_mT, t_X[b]], writes=[t_X[b]])
            k.barrier()

        def even_mixer(j, blks):
            yv = RA[:, :].rearrange("p (c t) -> p c t", c=8)
            t_y = [Tl(f"ya{j}_{b}") for b in range(5)]
            win = w_in[j].rearrange("(c p) n -> p c n", p=128)
            t_w = [TW[0][0], TW[1][0]]

            def wv1(s_):
                return slots[s_][:, 0:2048].rearrange("p (c n) -> p c n", c=8)
            k.dma("pool", wv1(0), win[:, :, 0:256], writes=[t_w[0]])
            for i in range(4):
                if i + 1 < 4:
                    k.dma("pool", wv1((i + 1) % 2), win[:, :, (i + 1) * 256:(i + 2) * 256], writes=[t_w[(i + 1) % 2]])
                for fc in range(2):
                    jc = 2 * i + fc
                    for (t0, n, v) in blks:
                        b = t0 // 512
                        p1, t_p1 = k.ps()
                        k.mm([t_w[i % 2], t_H[b]], [t_p1], [dict(out=p1[:, :n], lhsT=wv1(i % 2)[:, c, fc * 128:(fc + 1) * 128], rhs=hT[:, c, t0:t0 + n],
                                                              start=(c == 0), stop=(c == 7)) for c in range(8)])
                        k.op("act", lambda e: e.activation(out=yv[:, jc, t0:t0 + n], in_=p1[:, :n], func=AF.Gelu_apprx_tanh),
                             reads=[t_p1], writes=[t_y[b]])
            k.barrier()
            t_wv = [TW[0][0], TW[1][0]]
            wvv = [slots[i][:, 0:4096].rearrange("p (c n) -> p c n", c=8) for i in range(2)]
            for i in range(2):
                k.dma("pool", wvv[i], win[:, :, 1024 + i * 512:1024 + (i + 1) * 512], writes=[t_wv[i]])
            vgs = [slots[0][:, 4096:6144].bitcast(F32), slots[1][:, 4096:6144].bitcast(F32)]
            T2 = TMP[:, 0:1024]
            wsTv = TMP[:, 1024:1536].bitcast(BF16).rearrange("p (g q) -> p g q", g=8)
            bsb = TMP[:, 1536:2560]
            lgf = sml[:, 32:40]
            lbf = sml[:, 40:48]
            k.dma("sp", lgf, lngf_d[j], writes=[t_p2])
            k.dma("sp", lbf, lnbf_d[j], writes=[t_p2])
            k.dma("sp", bsb, bs_d[j:j + 1, :].partition_broadcast(128), writes=[t_p2])
            k.dma("pool", wsTv, wsT_d[j].rearrange("g q p -> q g p"), writes=[t_p2])
            t_T2 = Tl("T2")
            for gb_ in range(2):
                pw_, t_pw_ = k.ps()
                for gg in range(4):
                    g_ = gb_ * 4 + gg
                    k.mm([t_p2, t_one], [t_pw_], [dict(out=pw_[:, gg * 128:(gg + 1) * 128], lhsT=ones_bf[:, :], rhs=wsTv[:, g_, :], start=True, stop=True)])
                for gg in range(4):
                    g_ = gb_ * 4 + gg
                    k.op("dve", lambda e: e.scalar_tensor_tensor(out=T2[:, g_ * 128:(g_ + 1) * 128], in0=pw_[:, gg * 128:(gg + 1) * 128], scalar=lbf[:, g_:g_ + 1],
                                                                in1=bsb[:, g_ * 128:(g_ + 1) * 128], op0=ALU.mult, op1=ALU.add), reads=[t_pw_, t_p2], writes=[t_T2])
            t_vgs = [Tl("vg0"), Tl("vg1")]
            t_st = [Tl("st0"), Tl("st1")]
            vb2 = [t32[0][:, :].bitcast(BF16), t32[1][:, :].bitcast(BF16)]
            ntb = [tb for (t0, n, v) in blks for tb in range(t0 // 128, (t0 + n) // 128)]
            for it_, tb in enumerate(ntb):
                b = min(tb // 4, 4)
                tk = tb * 128
                par = it_ % 2
                vg, t_vg = vgs[par], t_vgs[par]
                pv = []
                for h_ in range(2):
                    p_, t_p = k.ps()
                    k.mm([t_wv[h_], t_H[b]], [t_p], [dict(out=p_[:, :], lhsT=hT[:, c, tk:tk + 128], rhs=wvv[h_][:, c, :],
                                                        start=(c == 0), stop=(c == 7)) for c in range(8)])
                    pv.append((p_, t_p))
                for h_ in range(2):
                    k.op("act", lambda e, h_=h_: e.activation(out=vg[:, h_ * 512:(h_ + 1) * 512], in_=pv[h_][0][:, :], func=AF.Gelu_apprx_tanh),
                         reads=[pv[h_][1]], writes=[t_vg])
                so = par * 16
                st = sml[:, so:so + 12].rearrange("p (a b) -> p a b", a=2)
                mv = sml[:, so + 12:so + 14]
                rstd = sml[:, so + 14:so + 15]
                nmr = sml[:, so + 15:so + 16]
                t_s_ = t_st[par]
                for h_ in range(2):
                    k.op("dve", lambda e, h_=h_: e.bn_stats(out=st[:, h_, :], in_=vg[:, h_ * 512:(h_ + 1) * 512]), reads=[t_vg], writes=[t_s_])
                k.op("dve", lambda e: e.bn_aggr(out=mv, in_=sml[:, so:so + 12]), reads=[t_s_], writes=[t_s_])
                k.op("act", lambda e: e.activation(out=rstd, in_=mv[:, 1:2], func=AF.Sqrt, bias=EPS), reads=[t_s_], writes=[t_s_])
                k.op("dve", lambda e: e.reciprocal(out=rstd, in_=rstd), reads=[t_s_], writes=[t_s_])
                k.op("dve", lambda e: e.scalar_tensor_tensor(out=nmr, in0=mv[:, 0:1], scalar=-1.0, in1=rstd, op0=ALU.mult, op1=ALU.mult),
                     reads=[t_s_], writes=[t_s_])
                vbf = vb2[par]
                t_v = t_t32[par]
                k.op("act", lambda e: e.activation(out=vbf, in_=vg, func=AF.Identity, scale=rstd, bias=nmr), reads=[t_s_, t_vg], writes=[t_v])
                for gb_ in range(2):
                    pg, t_pg = k.ps()
                    for gg in range(4):
                        g_ = gb_ * 4 + gg
                        k.mm([t_v, t_p2], [t_pg], [dict(out=pg[:, gg * 128:(gg + 1) * 128], lhsT=vbf[:, g_ * 128:(g_ + 1) * 128], rhs=wsTv[:, g_, :],
                                                      start=True, stop=True)])
                    tb_, t_t = nxt("rs", rs, t_rs)
                    for gg in range(4):
                        g_ = gb_ * 4 + gg
                        k.op("dve", lambda e: e.scalar_tensor_tensor(out=tb_[:, gg * 128:(gg + 1) * 128], in0=pg[:, gg * 128:(gg + 1) * 128], scalar=lgf[:, g_:g_ + 1],
                                                                    in1=T2[:, g_ * 128:(g_ + 1) * 128], op0=ALU.mult, op1=ALU.add),
                             reads=[t_pg, t_p2, t_T2], writes=[t_t])
                    yslice = yv[:, gb_ * 4:gb_ * 4 + 4, tk:tk + 128]
                    k.op("pool", lambda e: e.tensor_tensor(out=yslice, in0=yslice, in1=tb_[:, :].rearrange("p (g q) -> p g q", g=4), op=ALU.mult),
                         reads=[t_t], writes=[t_y[b]])
            k.barrier()
            proj_out(w_out[j, 0:1024, :], yv, t_y, G1, blks, f"m4a{j}")
            t_yb = [Tl(f"yb{j}_{b}") for b in range(5)]
            k.dma("sp", convw[:, :], convw_d[j], writes=[t_lpar])
            k.dma("sp", convb[:, :], convb_d[j], writes=[t_lpar])
            k.dma("sp", cng[:, :], cng_d[j], writes=[t_lpar])
            cw3 = convw[:, :].rearrange("p (c k) -> p c k", c=8)
            GW = 2078 + 286
            gbufs = [(slots[1][:, a_ * GW:a_ * GW + 2078], slots[1][:, a_ * GW + 2078:(a_ + 1) * GW]) for a_ in range(2)]
            t_gs = [Tl("g0"), Tl("g1")]
            Dg = TMP[:, 0:1984].bitcast(BF16).rearrange("p (k m) -> p k m", k=31)
            t_dg = Tl("dg")
            for a_ in range(2):
                k.op("pool", lambda e: e.memset(slots[1][:, a_ * GW:(a_ + 1) * GW], 0.0), writes=[t_gs[a_]])
            t_w3 = [TW[0][0], TW[0][1]]

            def wv3(s_):
                base = s_ * 2048
                return (slots[0][:, base:base + 1024].rearrange("p (c n) -> p c n", c=8),
                        slots[0][:, base + 1024:base + 2048].rearrange("p (c n) -> p c n", c=8))

            def load3(i):
                a_, g_ = wv3(i % 2)
                k.dma("pool", a_, win[:, :, 2048 + i * 128:2048 + (i + 1) * 128], writes=[t_w3[i % 2]])
                k.dma("pool", g_, win[:, :, 3072 + i * 128:3072 + (i + 1) * 128], writes=[t_w3[i % 2]], semt=t_w3[i % 2])

            def stage_proj(i):
                if i + 1 < 8:
                    load3(i + 1)
                a_, g_ = wv3(i % 2)
                gL, gC = gbufs[i % 2]
                for (t0, n, v) in blks:
                    b = t0 // 512
                    pa, t_pa = k.ps()
                    pg, t_pg = k.ps()
                    k.mm([t_w3[i % 2], t_H[b]], [t_pa], [dict(out=pa[:, :n], lhsT=a_[:, c, :], rhs=hT[:, c, t0:t0 + n], start=(c == 0), stop=(c == 7)) for c in range(8)])
                    k.mm([t_w3[i % 2], t_H[b]], [t_pg], [dict(out=pg[:, :n], lhsT=g_[:, c, :], rhs=hT[:, c, t0:t0 + n], start=(c == 0), stop=(c == 7)) for c in range(8)])
                    sg, t_sg = nxt("rs", rs, t_rs)
                    k.op("act", lambda e: e.activation(out=sg[:, :n], in_=pg[:, :n], func=AF.Sigmoid), reads=[t_pg], writes=[t_sg])
                    dst = gL[:, 15 + t0:15 + t0 + n] if v == 0 else gC[:, 15:15 + n]
                    k.op("dve", lambda e: e.tensor_tensor(out=dst, in0=sg[:, :n], in1=pa[:, :n], op=ALU.mult), reads=[t_sg, t_pa], writes=[t_gs[i % 2]])

            def stage_conv(i):
                gL, gC = gbufs[i % 2]
                for tap in range(31):
                    k.op("dve", lambda e: e.tensor_scalar(out=Dg[:, tap, :], in0=ident_bf[:, :], scalar1=cw3[:, i, tap:tap + 1], scalar2=None, op0=ALU.mult),
                         reads=[t_par, t_lpar], writes=[t_dg])
                for (t0, n, v) in blks:
                    b = t0 // 512
                    gbuf, o0 = (gL, t0) if v == 0 else (gC, 0)
                    pa_, t_pa_ = k.ps()
                    k.mm([t_dg, t_gs[i % 2]], [t_pa_], [dict(out=pa_[:, :n], lhsT=Dg[:, tap, :], rhs=gbuf[:, o0 + tap:o0 + tap + n], start=(tap == 0), stop=(tap == 30))
                                                        for tap in range(31)])
                    k.op("act", lambda e: e.activation(out=yv[:, i, t0:t0 + n], in_=pa_[:, :n], func=AF.Identity, bias=convb[:, i:i + 1]),
                         reads=[t_pa_, t_lpar], writes=[t_yb[b]])

            load3(0)
            stage_proj(0)
            for i in range(8):
                if i + 1 < 8:
                    stage_proj(i + 1)
                stage_conv(i)
            for (t0, n, v) in blks:
                b = t0 // 512
                rsb, t_r = nxt("rs", rs, t_rs)
                psm, t_ps = k.ps()
                for c in range(8):
                    sqb, t_s = nxt("sq", sq, t_sq)
                    k.op("act", lambda e: e.activation(out=sqb[:, :n], in_=yv[:, c, t0:t0 + n], func=AF.Square), reads=[t_yb[b]], writes=[t_s])
                    k.mm([t_s, t_one], [t_ps], [dict(out=psm[:, :n], lhsT=ones_bf[:, :], rhs=sqb[:, :n], start=(c == 0), stop=(c == 7))])
                k.op("act", lambda e: e.activation(out=rsb[:, :n], in_=psm[:, :n], func=AF.Ln, scale=1.0 / D, bias=EPS), reads=[t_ps], writes=[t_r])
                k.op("act", lambda e: e.activation(out=rsb[:, :n], in_=rsb[:, :n], func=AF.Exp, scale=-0.5), reads=[t_r], writes=[t_r])
                for c in range(8):
                    tb_, t_t = nxt("t32", t32, t_t32)
                    k.op("dve", lambda e: e.scalar_tensor_tensor(out=tb_[:, :n], in0=yv[:, c, t0:t0 + n], scalar=cng[:, c:c + 1], in1=rsb[:, :n],
                                                                op0=ALU.mult, op1=ALU.mult), reads=[t_yb[b], t_r, t_lpar], writes=[t_t])
                    k.op("act", lambda e: e.activation(out=yv[:, c, t0:t0 + n], in_=tb_[:, :n], func=AF.Silu), reads=[t_t], writes=[t_yb[b]])
            k.barrier()
            proj_out(w_out[j, 1024:2048, :], yv, t_yb, G1, blks, f"m4b{j}")


        I32 = mybir.dt.int32
        TWO_PI = 6.283185307179586

        def rope_tables():
            RAf = RA[:, 0:16384].bitcast(F32)
            y = RAf[:, 0:2048]
            yy = RAf[:, 2048:4096]
            kf = RAf[:, 4096:6144]
            ki = RAf[:, 6144:8192].bitcast(I32)
            fidx = sml[:, 40:41]
            invf = sml[:, 41:42]
            t_r = Tl("ropetmp")
            k.dma("sp", y, pos_d[:, :], writes=[t_r])
            k.dma("sp", fidx, fidx_d[:, :], writes=[t_r])
            k.op("act", lambda e: e.activation(out=invf, in_=fidx, func=AF.Exp, scale=-float(np.log(10000.0)) / 16.0), reads=[t_r], writes=[t_r])
            k.op("dve", lambda e: e.tensor_scalar(out=y, in0=y, scalar1=invf, scalar2=1.0 / TWO_PI, op0=ALU.mult, op1=ALU.mult), reads=[t_r], writes=[t_r])
            for shift, dst in ((0.0, sinT), (0.25, cosT)):
                k.op("dve", lambda e: e.tensor_scalar(out=yy, in0=y, scalar1=shift, scalar2=None, op0=ALU.add), reads=[t_r], writes=[t_r])
                k.op("dve", lambda e: e.tensor_copy(out=ki, in_=yy), reads=[t_r], writes=[t_r])
                k.op("dve", lambda e: e.tensor_copy(out=kf, in_=ki), reads=[t_r], writes=[t_r])
                k.op("dve", lambda e: e.tensor_tensor(out=yy, in0=yy, in1=kf, op=ALU.subtract), reads=[t_r], writes=[t_r])
                k.op("dve", lambda e: e.tensor_single_scalar(out=kf, in_=yy, scalar=0.5, op=ALU.is_gt), reads=[t_r], writes=[t_r])
                k.op("dve", lambda e: e.tensor_tensor(out=yy, in0=yy, in1=kf, op=ALU.subtract), reads=[t_r], writes=[t_r])
                k.op("dve", lambda e: e.tensor_single_scalar(out=kf, in_=yy, scalar=-0.5, op=ALU.is_lt), reads=[t_r], writes=[t_r])
                k.op("dve", lambda e: e.tensor_tensor(out=yy, in0=yy, in1=kf, op=ALU.add), reads=[t_r], writes=[t_r])
                k.op("act", lambda e: e.activation(out=dst, in_=yy, func=AF.Sin, scale=TWO_PI * (1.0 - 1e-6)), reads=[t_r], writes=[t_par])
            k.barrier()

        def attention(j, need_ctx):
            blks_q = BLK_L + (BLK_C if need_ctx else [])
            blks_a = BLK_L + BLK_C
            k.dma("sp", qg[:, :], qg_d[j], writes=[t_lpar])
            k.dma("sp", kg[:, :], kg_d[j], writes=[t_lpar])
            k.dma("sp", sinke[:, :], sink_d[j:j + 1, :].partition_broadcast(128), writes=[t_lpar])
            k.op("act", lambda e: e.activation(out=sinke[:, :], in_=sinke[:, :], func=AF.Exp), reads=[], writes=[t_lpar])
            k.npool = 6
            po, t_po = k.psum[6]
            pd, t_pd = k.psum[7]
            qT = RA[:, 0:2 * NT].rearrange("p (h t) -> p h t", h=2)
            kT = RA[:, 2 * NT:3 * NT]
            Vg = RA[:, 3 * NT:3 * NT + 1152].rearrange("p (b d) -> p b d", b=18)
            Pc = RA[:, 3 * NT + 1152:3 * NT + 1152 + 2 * NT].rearrange("p (b t) -> p b t", b=2)
            Pring = TMP[:, :].bitcast(BF16)
            NR = 8
            t_q = [[Tl(f"q{h}_{b}") for b in range(5)] for h in range(4)]
            t_k = [Tl(f"k{b}") for b in range(5)]
            t_v = [Tl(f"v{b}") for b in range(3)]
            t_pc = Tl("pc")
            t_pr = [Tl(f"pr{i}") for i in range(NR)]
            wsrc = wqkv[j].rearrange("(c p) n -> p c n", p=128)
            tq = [TW[0][0], TW[0][1]]
            tv = [TW[1][1], TW[1][2]]

            def wviews(s_):
                base = s_ * 3072
                return (slots[0][:, base:base + 2048].rearrange("p (c n) -> p c n", c=8),
                        slots[0][:, base + 2048:base + 3072].rearrange("p (c n) -> p c n", c=8),
                        slots[1][:, 2048 + s_ * 512:2048 + (s_ + 1) * 512].rearrange("p (c n) -> p c n", c=8))

            def loadw(g):
                a_, b_, c_ = wviews(g % 2)
                t_ = tq[g % 2]
                k.dma("pool", a_, wsrc[:, :, g * 256:(g + 1) * 256], writes=[t_])
                k.dma("pool", b_[:, :, 0:64], wsrc[:, :, 1024 + g * 64:1024 + (g + 1) * 64], writes=[t_])
                k.dma("pool", b_[:, :, 64:128], wsrc[:, :, 1024 + g * 64:1024 + (g + 1) * 64], writes=[t_])
                k.dma("pool", c_, wsrc[:, :, 1280 + g * 64:1280 + (g + 1) * 64], writes=[tv[g % 2]])
            wov = slots[1][:, 0:2048].rearrange("p (h n) -> p h n", h=2)
            t_wo = TW[1][0]

            def qk_chain(projitems, rd, n, gvec, dst, t_dsts, rope, t0):
                ps_ = k.ps()
                k.mm(rd, [ps_[1]], projitems(ps_[0]))
                yield
                sqb, t_s = nxt("sq", sq, t_sq)
                k.op("act", lambda e: e.activation(out=sqb[:, :n], in_=ps_[0][:, :n], func=AF.Square), reads=[ps_[1]], writes=[t_s])
                yield
                pss, t_pss = k.ps()
                k.mm([t_s, t_par], [t_pss], [dict(out=pss[:, :n], lhsT=bd_bf[:, :], rhs=sqb[:, :n], start=True, stop=True)])
                yield
                rsb, t_r = nxt("rs", rs, t_rs)
                k.op("act", lambda e: e.activation(out=rsb[:, :n], in_=pss[:, :n], func=AF.Ln, scale=1.0 / 64, bias=EPS), reads=[t_pss], writes=[t_r])
                yield
                k.op("act", lambda e: e.activation(out=rsb[:, :n], in_=rsb[:, :n], func=AF.Exp, scale=-0.5), reads=[t_r], writes=[t_r])
                yield
                qn, t_qn = nxt("t32", t32, t_t32)
                k.op("dve", lambda e: e.scalar_tensor_tensor(out=qn[:, :n], in0=ps_[0][:, :n], scalar=gvec[:, 0:1], in1=rsb[:, :n],
                                                            op0=ALU.mult, op1=ALU.mult), reads=[ps_[1], t_r, t_lpar], writes=[t_qn])
                yield
                if not rope:
                    k.op("act", lambda e: e.copy(out=dst, in_=qn[:, :n]), reads=[t_qn], writes=t_dsts)
                    return
                qb, t_qb = nxt("tt", tt, t_tt)
                k.op("pool", lambda e: e.tensor_copy(out=qb[:, :n], in_=qn[:, :n]), reads=[t_qn], writes=[t_qb])
                yield
                psr, t_psr = k.ps()
                k.mm([t_qb, t_par], [t_psr], [dict(out=psr[:, :n], lhsT=perm_bf[:, :], rhs=qb[:, :n], start=True, stop=True)])
                yield
                bb, t_bb = nxt("rs", rs, t_rs)
                k.op("dve", lambda e: e.tensor_tensor(out=bb[:, :n], in0=psr[:, :n], in1=sinT[:, t0:t0 + n], op=ALU.mult), reads=[t_psr, t_par], writes=[t_bb])
                k.op("dve", lambda e: e.tensor_tensor(out=qn[:, :n], in0=qn[:, :n], in1=cosT[:, t0:t0 + n], op=ALU.mult), reads=[t_par], writes=[t_qn])
                yield
                k.op("pool", lambda e: e.tensor_tensor(out=dst, in0=qn[:, :n], in1=bb[:, :n], op=ALU.add), reads=[t_qn, t_bb], writes=t_dsts)

            def lockstep(gens, width=2):
                for i0 in range(0, len(gens), width):
                    active = gens[i0:i0 + width]
                    while active:
                        alive = []
                        for g_ in active:
                            try:
                                next(g_)
                                alive.append(g_)
                            except StopIteration:
                                pass
                        active = alive

            loadw(0)
            for g in range(4):
                if g + 1 < 4:
                    loadw(g + 1)
                k.dma("pool", wov, wo_d[j][g * 256:(g + 1) * 256, :].rearrange("(h p) n -> p h n", p=128), writes=[t_wo])
                wq_, wk_, wv_ = wviews(g % 2)
                t_w = tq[g % 2]
                chains = []
                for (t0, n, v) in blks_a:
                    b = t0 // 512
                    chains.append(qk_chain(lambda pst, t0=t0, n=n: [dict(out=pst[:, :n], lhsT=wk_[:, c, :], rhs=hT[:, c, t0:t0 + n], start=(c == 0), stop=(c == 7)) for c in range(8)],
                                           [t_w, t_H[b]], n, kg, kT[:, t0:t0 + n], [t_k[b]], v == 0, t0))
                for pr in range(2):
                    for (t0, n, v) in blks_q:
                        b = t0 // 512
                        chains.append(qk_chain(lambda pst, t0=t0, n=n, pr=pr: [dict(out=pst[:, :n], lhsT=wq_[:, c, pr * 128:(pr + 1) * 128], rhs=hT[:, c, t0:t0 + n],
                                                                                 start=(c == 0), stop=(c == 7)) for c in range(8)],
                                               [t_w, t_H[b]], n, qg, qT[:, pr, t0:t0 + n], [t_q[2 * pr][b], t_q[2 * pr + 1][b]], v == 0, t0))
                lockstep(chains)
                for vb in range(3):
                    tbs = list(range(vb * 8, min(18, vb * 8 + 8)))
                    ps_ = k.ps()
                    for ii, tb in enumerate(tbs):
                        b = min(tb // 4, 4)
                        k.mm([tv[g % 2], t_H[b]], [ps_[1]], [dict(out=ps_[0][:, ii * 64:(ii + 1) * 64], lhsT=hT[:, c, tb * 128:(tb + 1) * 128], rhs=wv_[:, c, :],
                                                              start=(c == 0), stop=(c == 7)) for c in range(8)])
                    nb = len(tbs)
                    k.op("act", lambda e: e.copy(out=Vg[:, tbs[0]:tbs[0] + nb, :], in_=ps_[0][:, 0:nb * 64].rearrange("p (b d) -> p b d", b=nb)),
                         reads=[ps_[1]], writes=[t_v[vb]])
                for hh in range(4):
                    h = 4 * g + hh
                    pr = hh // 2
                    P0 = (hh % 2) * 64
                    P1 = P0 + 64
                    for kb in range(2):
                        for (t0, n, v) in blks_q:
                            b = t0 // 512
                            ps_ = k.ps()
                            k.mm([t_k[4], t_q[hh][b]], [ps_[1]], [dict(out=ps_[0][:, :n], lhsT=kT[P0:P1, 2048 + kb * 128:2048 + (kb + 1) * 128], rhs=qT[P0:P1, pr, t0:t0 + n],
                                                                    start=True, stop=True)])
                            k.op("act", lambda e: e.activation(out=Pc[:, kb, t0:t0 + n], in_=ps_[0][:, :n], func=AF.Exp, scale=0.125), reads=[ps_[1]], writes=[t_pc])
                    pinfo = {}

                    def pv(i):
                        col = (i % 4) * 128
                        srcs = [(Vg[:, 16 + kb, :], Pc[:, kb, i * 128:(i + 1) * 128], t_pc) for kb in range(2)]
                        for jb in (i - 1, i, i + 1):
                            if 0 <= jb <= 15:
                                pr_, q0_, t_ = pinfo[jb]
                                srcs.append((Vg[:, jb, :], pr_[:, i * 128 - q0_:i * 128 - q0_ + 128], t_))
                        rd = [t_v[0], t_v[1], t_v[2]] + [s_[2] for s_ in srcs]
                        k.mm(rd, [t_po], [dict(out=po[P0:P1, col:col + 128], lhsT=va, rhs=pa, start=(ii == 0), stop=(ii == len(srcs) - 1))
                                          for ii, (va, pa, _) in enumerate(srcs)])
                        k.mm(rd + [t_one], [t_pd], [dict(out=pd[P0:P1, col:col + 128], lhsT=ones_bf[:, 0:64], rhs=pa, start=(ii == 0), stop=(ii == len(srcs) - 1))
                                                    for ii, (va, pa, _) in enumerate(srcs)])
                        if i % 4 == 3:
                            m_ = i // 4
                            finish(m_ * 512, 512, m_)

                    def finish(t0, n, b):
                        dn, t_dn = nxt("rs", rs, t_rs)
                        k.op("dve", lambda e: e.tensor_scalar(out=dn[P0:P1, :n], in0=pd[P0:P1, :n], scalar1=sinke[P0:P1, h:h + 1], scalar2=None, op0=ALU.add),
                             reads=[t_pd, t_lpar], writes=[t_dn])
                        k.op("act", lambda e: e.activation(out=dn[P0:P1, :n], in_=dn[P0:P1, :n], func=AF.Ln), reads=[t_dn], writes=[t_dn])
                        k.op("act", lambda e: e.activation(out=dn[P0:P1, :n], in_=dn[P0:P1, :n], func=AF.Exp, scale=-1.0), reads=[t_dn], writes=[t_dn])
                        k.op("dve", lambda e: e.tensor_tensor(out=qT[P0:P1, pr, t0:t0 + n], in0=po[P0:P1, :n], in1=dn[P0:P1, :n], op=ALU.mult),
                             reads=[t_po, t_dn], writes=[t_q[hh][b]])

                    for jb in range(16):
                        q0 = max(0, 128 * (jb - 1))
                        q1 = min(NL, 128 * (jb + 2))
                        n = q1 - q0
                        mo = q0 - 128 * (jb - 1)
                        ps_ = k.ps()
                        qb_ = sorted(set([q0 // 512, (q1 - 1) // 512]))
                        k.mm([t_k[jb // 4], t_par] + [t_q[hh][b] for b in qb_], [ps_[1]],
                             [dict(out=ps_[0][:, :n], lhsT=kT[P0:P1, jb * 128:(jb + 1) * 128], rhs=qT[P0:P1, pr, q0:q1], start=True, stop=False),
                              dict(out=ps_[0][:, :n], lhsT=ident_bf[:, :], rhs=mask_bf[:, mo:mo + n], start=False, stop=True)])
                        ri = jb % NR
                        prt = Pring[:, ri * 384:(ri + 1) * 384]
                        k.op("act", lambda e: e.activation(out=prt[:, :n], in_=ps_[0][:, :n], func=AF.Exp, scale=0.125), reads=[ps_[1]], writes=[t_pr[ri]])
                        pinfo[jb] = (prt, q0, t_pr[ri])
                        if jb >= 4:
                            pv(jb - 4)
                    for i_ in range(12, 16):
                        pv(i_)
                    if need_ctx:
                        srcs = [(Vg[:, 16 + kb, :], Pc[:, kb, 2048:2304]) for kb in range(2)]
                        k.mm([t_v[2], t_pc], [t_po], [dict(out=po[P0:P1, 0:256], lhsT=va, rhs=pa, start=(ii == 0), stop=(ii == 1)) for ii, (va, pa) in enumerate(srcs)])
                        k.mm([t_pc, t_one], [t_pd], [dict(out=pd[P0:P1, 0:256], lhsT=ones_bf[:, 0:64], rhs=pa, start=(ii == 0), stop=(ii == 1)) for ii, (va, pa) in enumerate(srcs)])
                        finish(2048, 256, 4)
                for d in range(8):
                    for (t0, n, v) in blks_q:
                        b = t0 // 512
                        pw, t_pw = k.ps()
                        k.mm([t_wo] + [t_q[hh][b] for hh in range(4)], [t_pw],
                             [dict(out=pw[:, :n], lhsT=wov[:, pr, d * 128:(d + 1) * 128], rhs=qT[:, pr, t0:t0 + n], start=(pr == 0), stop=(pr == 1)) for pr in range(2)])
                        k.op("dve", lambda e: e.scalar_tensor_tensor(out=xT[:, d, t0:t0 + n], in0=pw[:, :n], scalar=G1[:, d, v:v + 1],
                                                                    in1=xT[:, d, t0:t0 + n], op0=ALU.mult, op1=ALU.add),
                             reads=[t_pw, t_mT, t_X[b]], writes=[t_X[b]])
                k.barrier()
            k.npool = 8

        def moe(j, blks):
            combT = RA[:, 4 * NT:6 * NT].bitcast(F32)
            cbs = [RA[:, 6 * NT:7 * NT], RA[:, 7 * NT:8 * NT]]
            t_comb = Tl("comb")
            t_cbs = [Tl("cb0"), Tl("cb1")]
            t_cm = Tl("cm")
            cm = TMP[0:8, 0:NT]
            k.dma("sp", router_f[:, :].rearrange("p (c e) -> p c e", c=8), router_d[j].rearrange("(c p) e -> p c e", p=128), writes=[t_lpar])
            make_h(A2v, SH2, blks, moe_j=j, combT=combT, t_comb=t_comb)
            k.barrier()
            for e_ in range(NE):
                k.op("dve", lambda e: e.tensor_scalar(out=cm, in0=combT[0:8, :], scalar1=ident[0:8, e_:e_ + 1], scalar2=None, op0=ALU.mult),
                     reads=[t_comb, t_par], writes=[t_cm])
                for (t0, n, v) in blks:
                    pc_, t_pc_ = k.ps()
                    k.mm([t_cm, t_one], [t_pc_], [dict(out=pc_[:, :n], lhsT=ones_f[0:8, :], rhs=cm[:, t0:t0 + n], start=True, stop=True)])
                    k.op("act", lambda e: e.copy(out=cbs[e_ % 2][:, t0:t0 + n], in_=pc_[:, :n]), reads=[t_pc_], writes=[t_cbs[e_ % 2]])
                ffn(mw1[j, e_], mw3[j, e_], mw2[j, e_], G2, blks, f"moe{j}_{e_}", cb=cbs[e_ % 2], t_cb=t_cbs[e_ % 2], bar=False)
            k.barrier()


        def moe_sparse(j, blks):
            ntok = sum(n for (_, n, _) in blks)
            ntb = ntok // 128
            NS = 768
            hg = RA[:, 0:6144].rearrange("p (c t) -> p c t", c=8)
            uvs = [RA[:, 6144 + a * 1536:6144 + (a + 1) * 1536].rearrange("p (f t) -> p f t", f=2) for a in range(2)]
            hbuf = [RA[:, 9216 + a * 1024:9216 + (a + 1) * 1024] for a in range(3)]
            Sg = [RA[:, 12288 + a * 384:12288 + (a + 1) * 384] for a in range(3)]
            STw = RA[:, 13440:16512].rearrange("p (a t) -> p a t", a=6)
            iota_f = RA[:, 16512:17280].bitcast(F32)
            pos_tm = RA[:, 17280:17568].bitcast(F32).rearrange("p (b e) -> p b e", e=8)
            comb_tm = RA[:, 17568:17856].bitcast(F32).rearrange("p (b e) -> p b e", e=8)
            mask_tm = RA[:, 17856:18000].rearrange("p (b e) -> p b e", e=8)
            cnt_i = RA[:, 18000:18016].bitcast(I32)
            slotid = RA[:, 18016:18052].bitcast(F32)
            CP = TMP[0:16, 0:NT]
            acc = RH[:, 0:12288].bitcast(F32).rearrange("p (c t) -> p c t", c=8)
            otm = RH[:, 12288:18432].rearrange("p (a d) -> p a d", a=6)
            t_rt = Tl("rt")
            t_mc = Tl("mc")
            t_cp = Tl("cp")
            t_hb = [Tl(f"hb{a}") for a in range(3)]
            t_sg = [Tl(f"sg{a}") for a in range(3)]
            t_hg = [Tl("hg0"), Tl("hg1")]
            t_acc = [Tl("acc0"), Tl("acc1")]
            t_otm = [Tl(f"otm{a}") for a in range(6)]
            t_stw = Tl("stw")
            t_hd = Tl("hd")
            k.dma("sp", router_f[:, :].rearrange("p (c e) -> p c e", c=8), router_d[j].rearrange("(c p) e -> p c e", p=128), writes=[t_lpar])
            k.dma("sp", iota_f, iota_d[:, :], writes=[t_mc])
            k.dma("sp", slotid, slotid_d[:, :], writes=[t_mc])
            make_h(A2v, SH2, blks, moe_j=dict(comb_tm=comb_tm, mask_tm=mask_tm, t_rt=t_rt))
            for tbi in range(ntb):
                pp, t_pp = k.ps()
                items = [dict(out=pp[:, 0:8], lhsT=ones_bf[:, :], rhs=mask_tm[:, b_, :], start=(b_ == 0), stop=False) for b_ in range(tbi)]
                items.append(dict(out=pp[:, 0:8], lhsT=triu_bf[:, :], rhs=mask_tm[:, tbi, :], start=(tbi == 0), stop=True))
                k.mm([t_rt, t_one, t_par], [t_pp], items)
                k.op("dve", lambda e: e.scalar_tensor_tensor(out=pos_tm[:, tbi, :], in0=pp[:, 0:8], scalar=1.0, in1=mask_tm[:, tbi, :], op0=ALU.add, op1=ALU.mult),
                     reads=[t_pp], writes=[t_rt])
                k.op("dve", lambda e: e.tensor_scalar(out=pos_tm[:, tbi, :], in0=pos_tm[:, tbi, :], scalar1=-1.0, scalar2=None, op0=ALU.add), writes=[t_rt])
                cp16 = sml[:, 48:64]
                k.op("dve", lambda e: e.tensor_copy(out=cp16[:, 0:8], in_=comb_tm[:, tbi, :]), reads=[t_rt], writes=[t_sml])
                k.op("dve", lambda e: e.tensor_copy(out=cp16[:, 8:16], in_=pos_tm[:, tbi, :]), reads=[t_rt], writes=[t_sml])
                pst, t_pst = k.ps()
                k.mm([t_sml, t_par], [t_pst], [dict(out=pst[0:16, 0:128], lhsT=cp16, rhs=ident[:, :], start=True, stop=True, is_transpose=True)])
                k.op("act", lambda e: e.copy(out=CP[0:16, tbi * 128:(tbi + 1) * 128], in_=pst[0:16, 0:128]), reads=[t_pst], writes=[t_cp])
            pcn, t_pcn = k.ps()
            k.mm([t_rt, t_one], [t_pcn], [dict(out=pcn[:, 0:8], lhsT=ones_bf[:, :], rhs=mask_tm[:, b_, :], start=(b_ == 0), stop=(b_ == ntb - 1)) for b_ in range(ntb)])
            t_cnt = Tl("cnt")
            k.op("dve", lambda e: e.tensor_copy(out=cnt_i, in_=pcn[:, 0:8]), reads=[t_pcn], writes=[t_cnt])
            for tb in range(ntb):
                b = min(tb // 4, 4)
                pt, t_pt = k.ps()
                ptb = pt[:, :].bitcast(BF16)
                k.mm([t_H[b], t_par], [t_pt], [dict(out=ptb[:, c * 128:(c + 1) * 128], lhsT=hT[:, c, tb * 128:(tb + 1) * 128], rhs=ident_bf[:, :],
                                                     start=True, stop=True, is_transpose=True) for c in range(8)])
                hb, t_h = hbuf[tb % 3], t_hb[tb % 3]
                k.op("act" if tb % 2 == 0 else "dve", (lambda e: e.copy(out=hb, in_=ptb[:, 0:1024])) if tb % 2 == 0 else (lambda e: e.tensor_copy(out=hb, in_=ptb[:, 0:1024])),
                     reads=[t_pt], writes=[t_h])
                k.dma("sp", h_tm_d[tb], hb, reads=[t_h], semt=t_hd)
            k.barrier()

            w_srcs = None

            def pass_body(e_, p_, blocks, nsb):
                spb = nsb // 2
                W1d, W3d, W2d = mw1[j, e_], mw3[j, e_], mw2[j, e_]
                w1s = W1d.rearrange("(c p) n -> p c n", p=128)
                w3s = W3d.rearrange("(c p) n -> p c n", p=128)
                w2s = W2d.rearrange("(f p) n -> p f n", p=128)

                def views(a):
                    sl = slots[a]
                    return (sl[:, 0:2048].rearrange("p (c n) -> p c n", c=8), sl[:, 2048:4096].rearrange("p (c n) -> p c n", c=8),
                            sl[:, 4096:6144].rearrange("p (f n) -> p f n", f=2))

                def load(i):
                    a, b_, c_ = views(i % 2)
                    tw = TW[i % 2]
                    k.dma("pool", a, w1s[:, :, i * 256:(i + 1) * 256], writes=[tw[0]])
                    k.dma("pool", b_, w3s[:, :, i * 256:(i + 1) * 256], writes=[tw[1]])
                    k.dma("pool", c_, w2s[:, 2 * i:2 * i + 2, :], writes=[tw[2]])
                load(0)
                hi = 0
                for bi, (s0, sn) in enumerate(blocks):
                    accs = [k.psum[c] for c in range(8)]
                    for tb in range(ntb):
                        hb, t_h = hbuf[hi % 3], t_hb[hi % 3]
                        sg, t_s = Sg[hi % 3], t_sg[hi % 3]
                        hi += 1
                        k.dma("sp", hb, h_tm_d[tb], writes=[t_h])
                        k.op("dve", lambda e: e.tensor_scalar(out=sg[:, 0:sn], in0=iota_f[:, 0:sn], scalar1=float(NS * p_ + s0), scalar2=pos_tm[:, tb, e_:e_ + 1],
                                                              op0=ALU.add, op1=ALU.is_equal), reads=[t_mc, t_rt], writes=[t_s])
                        for c in range(8):
                            k.mm([t_h, t_s], [accs[c][1]], [dict(out=accs[c][0][:, 0:sn], lhsT=hb[:, c * 128:(c + 1) * 128], rhs=sg[:, 0:sn], start=(tb == 0), stop=(tb == ntb - 1))])
                    for c in range(8):
                        if c % 2 == 0:
                            k.op("act", lambda e: e.copy(out=hg[:, c, s0:s0 + sn], in_=accs[c][0][:, 0:sn]), reads=[accs[c][1]], writes=[t_hg[bi]])
                        else:
                            k.op("dve", lambda e: e.tensor_copy(out=hg[:, c, s0:s0 + sn], in_=accs[c][0][:, 0:sn]), reads=[accs[c][1]], writes=[t_hg[bi]])
                for i in range(14):
                    if i + 1 < 14:
                        load(i + 1)
                    a = i % 2
                    w1v, w3v, w2v = views(a)
                    tw = TW[a]
                    uv = uvs[a]
                    for fc in range(2):
                        for bi, (s0, sn) in enumerate(blocks):
                            p1, t_p1 = k.ps()
                            p3, t_p3 = k.ps()
                            k.mm([tw[0], t_hg[bi]], [t_p1], [dict(out=p1[:, :sn], lhsT=w1v[:, c, fc * 128:(fc + 1) * 128], rhs=hg[:, c, s0:s0 + sn],
                                                                 start=(c == 0), stop=(c == 7)) for c in range(8)])
                            k.mm([tw[1], t_hg[bi]], [t_p3], [dict(out=p3[:, :sn], lhsT=w3v[:, c, fc * 128:(fc + 1) * 128], rhs=hg[:, c, s0:s0 + sn],
                                                                 start=(c == 0), stop=(c == 7)) for c in range(8)])
                            gb, t_g = nxt("gt", gt, t_gt)
                            k.op("act", lambda e: e.activation(out=gb[:, :sn], in_=p1[:, :sn], func=AF.Silu), reads=[t_p1], writes=[t_g])
                            k.op("dve", lambda e: e.tensor_tensor(out=uv[:, fc, s0:s0 + sn], in0=gb[:, :sn], in1=p3[:, :sn], op=ALU.mult),
                                 reads=[t_g, t_p3], writes=[TU[a][bi]])
                    for d in range(8):
                        for bi, (s0, sn) in enumerate(blocks):
                            po_, t_po_ = k.ps()
                            k.mm([tw[2], TU[a][bi]], [t_po_], [dict(out=po_[:, :sn], lhsT=w2v[:, fc, d * 128:(d + 1) * 128], rhs=uv[:, fc, s0:s0 + sn],
                                                                   start=(fc == 0), stop=(fc == 1)) for fc in range(2)])
                            if i == 0:
                                k.op("act", lambda e: e.copy(out=acc[:, d, s0:s0 + sn], in_=po_[:, :sn]), reads=[t_po_], writes=[t_acc[bi]])
                            else:
                                k.op("dve", lambda e: e.tensor_tensor(out=acc[:, d, s0:s0 + sn], in0=po_[:, :sn], in1=acc[:, d, s0:s0 + sn], op=ALU.add),
                                     reads=[t_po_], writes=[t_acc[bi]])
                for sb_ in range(nsb):
                    for half in range(2):
                        pt, t_pt = k.ps()
                        k.mm([t_acc[sb_ // spb], t_par], [t_pt],
                             [dict(out=pt[:, dd * 128:(dd + 1) * 128], lhsT=acc[:, half * 4 + dd, sb_ * 128:(sb_ + 1) * 128], rhs=ident[:, :],
                                   start=True, stop=True, is_transpose=True) for dd in range(4)])
                        if half == 0:
                            k.op("act", lambda e: e.copy(out=otm[:, sb_, 0:512], in_=pt[:, :]), reads=[t_pt], writes=[t_otm[sb_]])
                        else:
                            k.op("dve", lambda e: e.tensor_copy(out=otm[:, sb_, 512:1024], in_=pt[:, :]), reads=[t_pt], writes=[t_otm[sb_]])
                for (t0, n, v) in blks:
                    b = t0 // 512
                    cmt, t_c = nxt("t32", t32, t_t32)
                    k.op("dve", lambda e: e.tensor_scalar(out=cmt[0:16, :n], in0=CP[0:16, t0:t0 + n], scalar1=ident[0:16, e_:e_ + 1], scalar2=None, op0=ALU.mult),
                         reads=[t_cp, t_par], writes=[t_c])
                    pc_, t_pc_ = k.ps()
                    k.mm([t_c, t_one], [t_pc_], [dict(out=pc_[:, :n], lhsT=ones_f[0:16, :], rhs=cmt[0:16, :n], start=True, stop=True)])
                    cbb, t_cb = nxt("rs", rs, t_rs)
                    k.op("act", lambda e: e.copy(out=cbb[:, :n], in_=pc_[:, :n]), reads=[t_pc_], writes=[t_cb])
                    pmt, t_p = nxt("t32", t32, t_t32)
                    k.op("dve", lambda e: e.tensor_scalar(out=pmt[0:16, :n], in0=CP[0:16, t0:t0 + n], scalar1=ident[0:16, 8 + e_:9 + e_], scalar2=None, op0=ALU.mult),
                         reads=[t_cp, t_par], writes=[t_p])
                    pp_, t_pp_ = k.ps()
                    k.mm([t_p, t_one], [t_pp_], [dict(out=pp_[:, :n], lhsT=ones_f[0:16, :], rhs=pmt[0:16, :n], start=True, stop=True)])
                    for sb_ in range(nsb):
                        kk = 6 * p_ + sb_
                        k.op("dve", lambda e: e.scalar_tensor_tensor(out=STw[:, sb_, :n], in0=pp_[:, :n], scalar=slotid[:, kk:kk + 1], in1=cbb[:, :n],
                                                                    op0=ALU.is_equal, op1=ALU.mult), reads=[t_pp_, t_cb, t_mc], writes=[t_stw])
                    for d in range(8):
                        px, t_px = k.ps()
                        k.mm([t_stw] + t_otm[:nsb], [t_px], [dict(out=px[:, :n], lhsT=otm[:, sb_, d * 128:(d + 1) * 128], rhs=STw[:, sb_, :n],
                                                           start=(sb_ == 0), stop=(sb_ == nsb - 1)) for sb_ in range(nsb)])
                        k.op("dve", lambda e: e.scalar_tensor_tensor(out=xT[:, d, t0:t0 + n], in0=px[:, :n], scalar=G2[:, d, v:v + 1],
                                                                    in1=xT[:, d, t0:t0 + n], op0=ALU.mult, op1=ALU.add),
                             reads=[t_px, t_mT, t_X[b]], writes=[t_X[b]])

            npass = (ntok + NS - 1) // NS
            cnt_f = sml[:, 0:8]
            flg_f2 = TMP[:, 2304:2376]
            flg_f = flg_f2.rearrange("p (a e) -> p a e", e=8)
            flg_i = TMP[:, 2376:2448].bitcast(I32)
            t_flg = Tl("flg")
            k.op("dve", lambda e: e.tensor_copy(out=cnt_f, in_=cnt_i), reads=[t_cnt], writes=[t_sml])
            for p_ in range(3):
                k.op("dve", lambda e: e.tensor_single_scalar(out=flg_f[:, 2 * p_, :], in_=cnt_f, scalar=float(NS * p_ + 512), op=ALU.is_gt), reads=[t_sml], writes=[t_flg])
                k.op("dve", lambda e: e.tensor_single_scalar(out=flg_f[:, 2 * p_ + 1, :], in_=cnt_f, scalar=float(NS * p_), op=ALU.is_gt), reads=[t_sml], writes=[t_flg])
                k.op("dve", lambda e: e.tensor_tensor(out=flg_f[:, 2 * p_ + 1, :], in0=flg_f[:, 2 * p_ + 1, :], in1=flg_f[:, 2 * p_, :], op=ALU.subtract), writes=[t_flg])
            k.op("dve", lambda e: e.tensor_single_scalar(out=flg_f[:, 6, :], in_=cnt_f, scalar=float(NS), op=ALU.is_gt), reads=[t_sml], writes=[t_flg])
            k.op("dve", lambda e: e.tensor_single_scalar(out=flg_f[:, 7, :], in_=cnt_f, scalar=float(2 * NS), op=ALU.is_gt), reads=[t_sml], writes=[t_flg])
            k.op("dve", lambda e: e.tensor_tensor(out=flg_f[:, 8, :], in0=flg_f[:, 6, :], in1=flg_f[:, 0, :], op=ALU.subtract), writes=[t_flg])
            k.op("dve", lambda e: e.tensor_scalar(out=flg_f[:, 8, :], in0=flg_f[:, 8, :], scalar1=1.0, scalar2=None, op0=ALU.add), writes=[t_flg])
            k.op("dve", lambda e: e.tensor_copy(out=flg_i, in_=flg_f2), writes=[t_flg])
            regs = moe_regs
            variants = [([(0, 384), (384, 384)], 6), ([(0, 256), (256, 256)], 4)]

            def load_flag(row, e_):
                col = row * 8 + e_
                for r_ in regs:
                    en = {"Pool": "pool", "Activation": "act", "PE": "pe", "DVE": "dve", "SP": "sp"}[str(r_.engine).split(".")[-1]]
                    E = k.E[en]
                    k._need(E, k._deps([t_flg], []))
                    E.h.load(r_, flg_i[0:1, col:col + 1])

            def bump(deltas):
                for (en, h_, dv) in deltas:
                    k.E[en].h.sem_inc(h_, dv)

            def region(row, e_, body):
                load_flag(row, e_)
                k.region_begin()
                with nc.If_ne(regs, 0):
                    body()
                    deltas = k.region_end()
                with nc.Else():
                    bump(deltas)

            def run_pass(e_, p_):
                for vi, (blocks_v, nsb_v) in enumerate(variants):
                    region(2 * p_ + vi, e_, lambda: pass_body(e_, p_, blocks_v, nsb_v))

            def run_from(e_, p_):
                def body():
                    run_pass(e_, p_)
                    if p_ + 1 < npass:
                        run_from(e_, p_ + 1)
                region(5 + p_, e_, body)

            for e_ in range(NE):
                region(0, e_, lambda: pass_body(e_, 0, variants[0][0], variants[0][1]))

                def rest():
                    region(1, e_, lambda: pass_body(e_, 0, variants[1][0], variants[1][1]))
                    if npass > 1:
                        run_from(e_, 1)
                region(8, e_, rest)
            k.barrier()

        moe_regs = nc.alloc_registers("cnt")
        import os
        stop = int(os.environ.get("KSTOP", "99"))
        phase = 0
        for li in range(4):
            j = li // 2
            need_ctx = li < 3
            blks = BLK_L + (BLK_C if need_ctx else [])
            layer_params(li)
            if li % 2 == 0:
                make_h(A1v, SH1, blks)
                k.barrier()
                even_mixer(j, blks)
                phase += 1
                if phase >= stop:
                    break
                make_h(A2v, SH2, blks)
                k.barrier()
                ffn(ffw1[j], ffw3[j], ffw2[j], G2, blks, f"ff{j}")
                phase += 1
                if phase >= stop:
                    break
            else:
                if li == 1:
                    rope_tables()
                make_h(A1v, SH1, BLK_L + BLK_C)
                k.barrier()
                attention(j, need_ctx)
                phase += 1
                if phase >= stop:
                    break
                moe_sparse(j, blks)
                phase += 1
                if phase >= stop:
                    break

        k.barrier()
        t_out = Tl("out")
        for b in range(5):
            t0, n, _ = (BLK_L + BLK_C)[b]
            k.dma("sp", out_d[:, t0:t0 + n].rearrange("(c p) t -> p c t", p=128), xT[:, :, t0:t0 + n], reads=[t_X[b]], semt=t_out)
        k.E["sp"].h.wait_ge(t_out.dsem, t_out.dcnt)
    return nc


def _prep(inputs, b):
    f = np.float32
    x, ctx, c, c_ctx = inputs["x"], inputs["ctx"], inputs["c"], inputs["c_ctx"]
    m = {}
    m["xin"] = np.ascontiguousarray(np.concatenate([x[b], ctx[b]], axis=0).T)
    cvv = np.stack([c[b].reshape(8, 128).T, c_ctx.reshape(8, 128).T], axis=-1)
    m["cv"] = np.ascontiguousarray(cvv.reshape(128, 16))
    return m


def _shared(inputs):
    m = {}
    g = lambda a: np.ascontiguousarray(a, dtype=np.float32)
    m["ada_w"] = g(inputs["ada_w"])
    m["ada_b"] = g(inputs["ada_b"].reshape(4, 48, 128).transpose(0, 2, 1))
    m["ng1"] = g(inputs["norm_mix_g"].reshape(4, 8, 128).transpose(0, 2, 1))
    m["ng2"] = g(inputs["norm_ffn_g"].reshape(4, 8, 128).transpose(0, 2, 1))
    m["ev_w_in"] = g(inputs["ev_w_in"])
    m["ev_ln_g"] = g(inputs["ev_ln_g"].reshape(2, 8, 128).transpose(0, 2, 1))
    m["ev_ln_b"] = g(inputs["ev_ln_b"].reshape(2, 8, 128).transpose(0, 2, 1))
    m["ev_wsT"] = g(inputs["ev_ws"].transpose(0, 1, 3, 2))
    m["ev_bs"] = g(inputs["ev_bs"].reshape(2, 1024))
    m["ev_conv_w"] = g(inputs["ev_conv_w"].transpose(0, 2, 1).reshape(2, 8, 128, 31).transpose(0, 2, 1, 3).reshape(2, 128, 248))
    m["ev_conv_b"] = g(inputs["ev_conv_b"].reshape(2, 8, 128).transpose(0, 2, 1))
    m["ev_cnorm_g"] = g(inputs["ev_cnorm_g"].reshape(2, 8, 128).transpose(0, 2, 1))
    m["ev_w_out"] = g(inputs["ev_w_out"])
    m["od_w_qkv"] = g(inputs["od_w_qkv"])
    m["od_q_g"] = g(np.concatenate([inputs["od_q_g"], inputs["od_q_g"]], axis=1).reshape(2, 128, 1))
    m["od_k_g"] = g(np.concatenate([inputs["od_k_g"], inputs["od_k_g"]], axis=1).reshape(2, 128, 1))
    m["od_sink"] = g(inputs["od_sink"])
    m["od_w_o"] = g(inputs["od_w_o"])
    m["ff_w1"] = g(inputs["ff_w1"])
    m["ff_w3"] = g(inputs["ff_w3"])
    m["ff_w2"] = g(inputs["ff_w2"])
    m["moe_router"] = g(inputs["moe_router"])
    m["moe_w1"] = g(inputs["moe_w1"])
    m["moe_w3"] = g(inputs["moe_w3"])
    m["moe_w2"] = g(inputs["moe_w2"])
    m["c_ident"] = np.eye(128, dtype=np.float32)
    pm = np.zeros((64, 64), np.float32)
    for p in range(64):
        r = p % 32
        if r < 16:
            pm[p + 16, p] = -1.0
        else:
            pm[p - 16, p] = 1.0
    pm2 = np.zeros((128, 128), np.float32)
    pm2[0:64, 0:64] = pm
    pm2[64:128, 64:128] = pm
    m["c_perm"] = pm2
    bd = np.zeros((128, 128), np.float32)
    bd[0:64, 0:64] = 1.0
    bd[64:128, 64:128] = 1.0
    m["c_bd"] = bd
    kk = np.arange(128)[:, None]
    qq = np.arange(384)[None, :] - 128
    m["c_mask"] = np.where(np.abs(qq - kk) <= 128, 0.0, -240000.0).astype(np.float32)
    t = np.arange(2048)
    pos = np.zeros((64, 2048), np.float32)
    pos[0:32, :] = (t // 64)[None, :]
    pos[32:64, :] = (t % 64)[None, :]
    m["c_pos"] = np.concatenate([pos, pos], axis=0)
    m["c_fidx"] = (np.arange(128) % 16).astype(np.float32).reshape(128, 1)
    m["c_triu"] = (np.arange(128)[:, None] < np.arange(128)[None, :]).astype(np.float32)
    m["c_iota"] = np.tile(np.arange(384, dtype=np.float32)[None, :], (128, 1))
    m["c_slotid"] = (np.arange(128, dtype=np.float32)[:, None] + 128.0 * np.arange(18, dtype=np.float32)[None, :])
    return m


_NC_CACHE = {}


def kernel(**inputs):
    inputs = {k_: np.asarray(v) for k_, v in inputs.items()}
    ncores = 8
    if "nc" not in _NC_CACHE:
        _NC_CACHE["nc"] = build()
    nc = _NC_CACHE["nc"]
    shared = _shared(inputs)
    in_maps = []
    for b in range(ncores):
        m = dict(shared)
        m.update(_prep(inputs, b))
        in_maps.append(m)
    res = run_bass_kernel_spmd(nc, in_maps, core_ids=list(range(ncores)))
    outs = [np.asarray(r["out"]) for r in res.results]
    return np.stack([o[:, :NL].T for o in outs], axis=0).astype(np.float32)
```

```python
import numpy as np
import concourse.bass as bass
import concourse.mybir as mybir
from concourse.bass_utils import run_bass_kernel_spmd
from contextlib import ExitStack

F32 = mybir.dt.float32
BF16 = mybir.dt.bfloat16
AF = mybir.ActivationFunctionType
ALU = mybir.AluOpType
AX = mybir.AxisListType

NL, NCX, NT = 2048, 256, 2304
D, DFF, NE = 1024, 3584, 8
EPS = 1e-6
BLK_L = [(0, 512, 0), (512, 512, 0), (1024, 512, 0), (1536, 512, 0)]
BLK_C = [(2048, 256, 1)]
SEMLIM = 10 ** 9


class Tl:
    __slots__ = ("name", "w", "r", "dsem", "dcnt")

    def __init__(s, name):
        s.name = name
        s.w = None
        s.r = {}
        s.dsem = None
        s.dcnt = 0


class Eng:
    def __init__(s, name, h):
        s.name, s.h, s.sem, s.cnt, s.waited = name, h, None, 0, {}


class K:
    def __init__(s, nc, es):
        s.nc, s.es = nc, es
        s.E = {"pe": Eng("pe", nc.tensor), "act": Eng("act", nc.scalar), "dve": Eng("dve", nc.vector),
               "pool": Eng("pool", nc.gpsimd), "sp": Eng("sp", nc.sync)}
        s.nsem = 0
        for e in s.E.values():
            e.sem = s.newsem(e.name)
        s.dsems = {}
        s.psum = []
        s.psi = 0
        s._snaps = []
        s.npool = 8

    def newsem(s, name):
        s.nsem += 1
        return s.es.enter_context(s.nc.semaphore(f"{name}_{s.nsem}"))

    def _need(s, E, deps, embed=False):
        todo = []
        for num, (h, v) in deps.items():
            if E.waited.get(num, 0) < v:
                todo.append((h, v))
                E.waited[num] = v
        last = None
        if embed and todo:
            last = todo.pop()
        for (h, v) in todo:
            E.h.wait_ge(h, v)
        return last

    @staticmethod
    def _deps(reads, writes):
        d = {}

        def add(rec):
            h, v = rec
            if h.num not in d or d[h.num][1] < v:
                d[h.num] = rec
        for t in reads:
            if t.w:
                add(t.w)
        for t in writes:
            if t.w:
                add(t.w)
            for rec in t.r.values():
                add(rec)
        return d

    @staticmethod
    def _mark(rec, reads, writes):
        for t in reads:
            t.r[rec[0].num] = rec
        for t in writes:
            t.w = rec
            t.r = {}

    def _rot(s, E):
        if E.cnt >= SEMLIM:
            E.sem = s.newsem(E.name)
            E.cnt = 0

    def op(s, en, fn, reads=(), writes=()):
        E = s.E[en]
        last = s._need(E, s._deps(reads, writes), embed=True)
        ins = fn(E.h)
        if last is not None:
            ins._wait_ge(last[0], last[1])
        E.cnt += 1
        ins.then_inc(E.sem, 1)
        s._mark((E.sem, E.cnt), reads, writes)
        s._rot(E)

    def mm(s, reads, writes, items):
        E = s.E["pe"]
        last = s._need(E, s._deps(reads, writes), embed=True)
        ins = None
        for it in items:
            ins = E.h.matmul(**it)
            if last is not None:
                ins._wait_ge(last[0], last[1])
                last = None
        E.cnt += 1
        ins.then_inc(E.sem, 1)
        s._mark((E.sem, E.cnt), reads, writes)
        s._rot(E)

    def dma(s, q, out, in_, reads=(), writes=(), semt=None):
        E = s.E[q]
        s._need(E, s._deps(reads, writes))
        ins = E.h.dma_start(out=out, in_=in_)
        t = semt if semt is not None else writes[0]
        if t.dsem is None:
            t.dsem = s.newsem("d" + t.name)
        t.dcnt += 16
        ins.then_inc(t.dsem, 16)
        s.dsems[t.dsem.num] = (t.dsem, t.dcnt)
        s._mark((t.dsem, t.dcnt), reads, writes)

    def barrier(s, engines=("pe", "act", "dve", "pool", "sp"), own=False):
        for en in engines:
            E = s.E[en]
            d = {}
            for F in s.E.values():
                if (own or F is not E) and F.cnt > 0:
                    d[F.sem.num] = (F.sem, F.cnt)
            d.update(s.dsems)
            s._need(E, d)

    def region_begin(s):
        for n, E in s.E.items():
            d = {}
            if E.cnt > 0:
                d[E.sem.num] = (E.sem, E.cnt)
            if n == "sp":
                d.update(s.dsems)
            s._need(E, d)
        s._snaps.append(({n: (E.sem, E.cnt) for n, E in s.E.items()}, dict(s.dsems), {n: dict(E.waited) for n, E in s.E.items()}))

    def region_end(s):
        e0, d0, w0 = s._snaps.pop()
        deltas = []
        for n, E in s.E.items():
            assert E.sem.num == e0[n][0].num, "semaphore rotated inside region"
            if E.cnt > e0[n][1]:
                deltas.append((n, E.sem, E.cnt - e0[n][1]))
        for num, (h, v) in s.dsems.items():
            v0 = d0[num][1] if num in d0 else 0
            if v > v0:
                deltas.append(("sp", h, v - v0))
        for n, E in s.E.items():
            E.waited = w0[n]
        return deltas

    def ps(s):
        p = s.psum[s.psi % s.npool]
        s.psi += 1
        return p


def build():
    nc = bass.Bass("TRN2", target_bir_lowering=False)

    def din(name, shape):
        return nc.dram_tensor(name, list(shape), F32, kind="ExternalInput").ap()

    xin = din("xin", [1024, NT])
    cv_d = din("cv", [128, 16])
    ada_w = din("ada_w", [4, 1024, 6144])
    ada_b = din("ada_b", [4, 128, 48])
    ng1_d = din("ng1", [4, 128, 8])
    ng2_d = din("ng2", [4, 128, 8])
    w_in = din("ev_w_in", [2, 1024, 4096])
    lngf_d = din("ev_ln_g", [2, 128, 8])
    lnbf_d = din("ev_ln_b", [2, 128, 8])
    wsT_d = din("ev_wsT", [2, 8, 128, 128])
    bs_d = din("ev_bs", [2, 1024])
    convw_d = din("ev_conv_w", [2, 128, 248])
    convb_d = din("ev_conv_b", [2, 128, 8])
    cng_d = din("ev_cnorm_g", [2, 128, 8])
    w_out = din("ev_w_out", [2, 2048, 1024])
    wqkv = din("od_w_qkv", [2, 1024, 1536])
    qg_d = din("od_q_g", [2, 128, 1])
    kg_d = din("od_k_g", [2, 128, 1])
    sink_d = din("od_sink", [2, 16])
    wo_d = din("od_w_o", [2, 1024, 1024])
    ffw1 = din("ff_w1", [2, 1024, DFF])
    ffw3 = din("ff_w3", [2, 1024, DFF])
    ffw2 = din("ff_w2", [2, DFF, 1024])
    router_d = din("moe_router", [2, 1024, 8])
    mw1 = din("moe_w1", [2, 8, 1024, DFF])
    mw3 = din("moe_w3", [2, 8, 1024, DFF])
    mw2 = din("moe_w2", [2, 8, DFF, 1024])
    ident_d = din("c_ident", [128, 128])
    perm_d = din("c_perm", [128, 128])
    bd_d = din("c_bd", [128, 128])
    mask_d = din("c_mask", [128, 384])
    pos_d = din("c_pos", [128, 2048])
    fidx_d = din("c_fidx", [128, 1])
    triu_d = din("c_triu", [128, 128])
    iota_d = din("c_iota", [128, 384])
    slotid_d = din("c_slotid", [128, 18])
    h_tm_d = nc.dram_tensor("h_tm_scratch", [18, 128, 1024], BF16, kind="Internal").ap()
    out_d = nc.dram_tensor("out", [1024, NT], F32, kind="ExternalOutput").ap()

    with ExitStack() as es:
        k = K(nc, es)

        def sb(name, shape, dt):
            return es.enter_context(nc.sbuf_tensor("sb_" + name, list(shape), dt))

        RX = sb("RX", [128, 8 * NT], F32)
        RH = sb("RH", [128, 8 * NT], BF16)
        RA = sb("RA", [128, 8 * NT], BF16)
        RW = sb("RW", [128, 12288], BF16)
        xT = RX[:, :].rearrange("p (c t) -> p c t", c=8)
        hT = RH[:, :].rearrange("p (c t) -> p c t", c=8)
        for i in range(8):
            k.psum.append((es.enter_context(nc.psum_tensor(f"ps{i}", [128, 512], F32)), Tl(f"ps{i}")))

        ident = sb("ident", [128, 128], F32)
        ones_bf = sb("ones_bf", [128, 128], BF16)
        ones_f = sb("ones_f", [16, 128], F32)
        triu_bf = sb("triu_bf", [128, 128], BF16)
        ident_bf = sb("ident_bf", [128, 128], BF16)
        perm_bf = sb("perm_bf", [128, 128], BF16)
        bd_bf = sb("bd_bf", [128, 128], BF16)
        mask_bf = sb("mask_bf", [128, 384], BF16)
        cv = sb("cv", [128, 16], F32)
        scv = sb("scv", [128, 16], BF16)
        adab = sb("adab", [128, 48], F32)
        mT = sb("mT", [128, 96], F32)
        mT3 = mT[:, :].rearrange("p (j v) -> p j v", v=2)
        A1 = sb("A1", [128, 16], F32)
        A2 = sb("A2", [128, 16], F32)
        A1v = A1[:, :].rearrange("p (c v) -> p c v", v=2)
        A2v = A2[:, :].rearrange("p (c v) -> p c v", v=2)
        ng1 = sb("ng1", [128, 8], F32)
        ng2 = sb("ng2", [128, 8], F32)
        tmp16 = sb("tmp16", [128, 16], F32)
        convw = sb("convw", [128, 248], F32)
        convb = sb("convb", [128, 8], F32)
        cng = sb("cng", [128, 8], F32)
        qg = sb("qg", [128, 1], F32)
        kg = sb("kg", [128, 1], F32)
        sinke = sb("sinke", [128, 16], F32)
        router_f = sb("router_f", [128, 64], F32)
        cossin = sb("cossin", [128, 2 * 2048], BF16)
        cosT = cossin[:, 0:2048]
        sinT = cossin[:, 2048:4096]
        sq = [sb(f"sq{i}", [128, 512], BF16) for i in range(2)]
        rs = [sb(f"rs{i}", [128, 512], F32) for i in range(2)]
        t32 = [sb(f"t32_{i}", [128, 512], F32) for i in range(2)]
        gt = [sb(f"gt{i}", [128, 512], BF16) for i in range(2)]
        tt = [sb(f"tt{i}", [128, 512], BF16) for i in range(2)]
        TMP = sb("TMP", [128, 2560], F32)
        sml = sb("sml", [128, 64], F32)

        t_sq = [Tl(f"sq{i}") for i in range(2)]
        t_rs = [Tl(f"rs{i}") for i in range(2)]
        t_t32 = [Tl(f"t32{i}") for i in range(2)]
        t_gt = [Tl(f"gt{i}") for i in range(2)]
        t_tt = [Tl(f"tt{i}") for i in range(2)]
        t_par = Tl("par")
        t_lpar = Tl("lpar")
        t_mT = Tl("mT")
        t_X = [Tl(f"X{i}") for i in range(5)]
        t_H = [Tl(f"H{i}") for i in range(5)]
        TW = [[Tl(f"w{a}_{i}") for i in range(3)] for a in range(2)]
        t_p2 = Tl("m2p")
        TU = [[Tl(f"u{a}_{b}") for b in range(5)] for a in range(2)]
        rot = {"sq": 0, "rs": 0, "t32": 0, "gt": 0, "tt": 0}

        def nxt(name, arr, tls):
            i = rot[name] % len(arr)
            rot[name] += 1
            return arr[i], tls[i]

        k.dma("sp", ident[:, :], ident_d[:, :], writes=[t_par])
        k.dma("sp", cv[:, :], cv_d[:, :], writes=[t_par])
        k.dma("pool", perm_bf[:, :], perm_d[:, :], writes=[t_par])
        k.dma("pool", bd_bf[:, :], bd_d[:, :], writes=[t_par])
        k.dma("pool", mask_bf[:, :], mask_d[:, :], writes=[t_par])
        k.dma("pool", triu_bf[:, :], triu_d[:, :], writes=[t_par])
        k.dma("pool", ident_bf[:, :], ident_d[:, :], writes=[t_par])
        t_one = Tl("ones")
        k.op("dve", lambda e: e.memset(ones_bf[:, :], 1.0), writes=[t_one])
        k.op("dve", lambda e: e.memset(ones_f[:, :], 1.0), writes=[t_one])
        for b in range(5):
            t0, n, _ = (BLK_L + BLK_C)[b]
            k.dma("sp", xT[:, :, t0:t0 + n], xin[:, t0:t0 + n].rearrange("(c p) t -> p c t", p=128), writes=[t_X[b]])
        k.op("act", lambda e: e.activation(out=scv[:, :], in_=cv[:, :], func=AF.Silu), reads=[t_par], writes=[t_one])
        scv3 = scv[:, :].rearrange("p (c v) -> p c v", v=2)

        W0 = RW[:, 0:6144]
        W1s = RW[:, 6144:12288]
        slots = [W0, W1s]

        def layer_params(li):
            t_w = [TW[0][0], TW[1][0]]
            k.dma("sp", adab[:, :], ada_b[li], writes=[t_lpar])
            k.dma("sp", ng1[:, :], ng1_d[li], writes=[t_lpar])
            k.dma("sp", ng2[:, :], ng2_d[li], writes=[t_lpar])
            psm, t_psm = k.ps()
            src = ada_w[li].rearrange("(c p) n -> p c n", p=128)
            for i in range(12):
                wv = slots[i % 2][:, 0:4096].rearrange("p (c n) -> p c n", c=8)
                k.dma("pool", wv, src[:, :, i * 512:(i + 1) * 512], writes=[t_w[i % 2]])
                for jj in range(4):
                    j = 4 * i + jj
                    k.mm([t_w[i % 2], t_one], [t_psm],
                         [dict(out=psm[:, 2 * j:2 * j + 2], lhsT=wv[:, c, jj * 128:(jj + 1) * 128], rhs=scv3[:, c, :],
                               start=(c == 0), stop=(c == 7)) for c in range(8)])
            ps3 = psm[:, 0:96].rearrange("p (j v) -> p j v", v=2)
            for v in range(2):
                k.op("dve", lambda e, v=v: e.tensor_tensor(out=mT3[:, :, v], in0=ps3[:, :, v], in1=adab[:, :], op=ALU.add),
                     reads=[t_psm, t_lpar], writes=[t_mT])
            t16 = tmp16[:, :].rearrange("p (c v) -> p c v", v=2)
            for (Av, ng, off) in ((A1v, ng1, 8), (A2v, ng2, 32)):
                k.op("dve", lambda e, off=off: e.tensor_scalar(out=t16, in0=mT3[:, off:off + 8, :], scalar1=1.0, scalar2=None, op0=ALU.add),
                     reads=[t_mT], writes=[t_lpar])
                for v in range(2):
                    k.op("dve", lambda e, v=v, Av=Av, ng=ng: e.tensor_tensor(out=Av[:, :, v], in0=t16[:, :, v], in1=ng[:, :], op=ALU.mult),
                         reads=[t_lpar], writes=[t_lpar])
            k.barrier()

        SH1, G1, SH2, G2 = mT3[:, 0:8, :], mT3[:, 16:24, :], mT3[:, 24:32, :], mT3[:, 40:48, :]

        def make_h(Av, SHv, blks, moe_j=None, combT=None, t_comb=None):
            for (t0, n, v) in blks:
                b = t0 // 512
                rsb, t_r = nxt("rs", rs, t_rs)
                psm, t_ps = k.ps()
                for c in range(8):
                    sqb, t_s = nxt("sq", sq, t_sq)
                    k.op("act", lambda e, c=c, sqb=sqb: e.activation(out=sqb[:, :n], in_=xT[:, c, t0:t0 + n], func=AF.Square),
                         reads=[t_X[b]], writes=[t_s])
                    k.mm([t_s, t_one], [t_ps], [dict(out=psm[:, :n], lhsT=ones_bf[:, :], rhs=sqb[:, :n], start=(c == 0), stop=(c == 7))])
                k.op("act", lambda e: e.activation(out=rsb[:, :n], in_=psm[:, :n], func=AF.Ln, scale=1.0 / D, bias=EPS),
                     reads=[t_ps], writes=[t_r])
                k.op("act", lambda e: e.activation(out=rsb[:, :n], in_=rsb[:, :n], func=AF.Exp, scale=-0.5), reads=[t_r], writes=[t_r])
                if moe_j is not None:
                    pslg, t_pslg = k.ps()
                    nsb = n // 128
                    k.mm([t_one], [t_pslg], [dict(out=pslg[:, 0:8 * nsb], lhsT=zer_bf[:, :], rhs=zer_bf[:, 0:8 * nsb], start=True, stop=False,
                                                skip_group_check=True)])
                for c in range(8):
                    tb, t_t = nxt("t32", t32, t_t32)
                    k.op("dve", lambda e, c=c, tb=tb: e.scalar_tensor_tensor(out=tb[:, :n], in0=xT[:, c, t0:t0 + n], scalar=Av[:, c, v:v + 1],
                                                                           in1=rsb[:, :n], op0=ALU.mult, op1=ALU.mult),
                         reads=[t_X[b], t_r, t_lpar], writes=[t_t])
                    if moe_j is None:
                        k.op("act", lambda e, c=c, tb=tb: e.activation(out=hT[:, c, t0:t0 + n], in_=tb[:, :n], func=AF.Identity,
                                                                     bias=SHv[:, c, v:v + 1]),
                             reads=[t_t, t_mT], writes=[t_H[b]])
                    else:
                        k.op("act", lambda e, c=c, tb=tb: e.activation(out=tb[:, :n], in_=tb[:, :n], func=AF.Identity,
                                                                     bias=SHv[:, c, v:v + 1]),
                             reads=[t_mT], writes=[t_t])
                        k.op("act", lambda e, c=c, tb=tb: e.copy(out=hT[:, c, t0:t0 + n], in_=tb[:, :n]),
                             reads=[t_t], writes=[t_H[b]])
                        k.mm([t_t, t_lpar], [t_pslg],
                             [dict(out=pslg[:, 8 * s_:8 * s_ + 8], lhsT=tb[:, s_ * 128:(s_ + 1) * 128], rhs=router_f[:, 8 * c:8 * c + 8],
                                   start=False, stop=(c == 7), skip_group_check=True) for s_ in range(nsb)])
                if moe_j is not None:
                    for s_ in range(nsb):
                        lg = sml[:, 0:8]
                        mx = sml[:, 8:16]
                        ex = sml[:, 16:24]
                        msk = sml[:, 24:32]
                        nm1 = sml[:, 32:33]
                        den = sml[:, 33:34]
                        k.op("dve", lambda e: e.tensor_copy(out=lg, in_=pslg[:, 8 * s_:8 * s_ + 8]), reads=[t_pslg], writes=[t_sml])
                        k.op("dve", lambda e: e.max(out=mx, in_=lg), reads=[t_sml], writes=[t_sml])
                        k.op("dve", lambda e: e.tensor_scalar(out=nm1, in0=mx[:, 0:1], scalar1=-1.0, scalar2=None, op0=ALU.mult),
                             reads=[t_sml], writes=[t_sml])
                        k.op("act", lambda e: e.activation(out=ex, in_=lg, func=AF.Exp, bias=nm1), reads=[t_sml], writes=[t_sml])
                        k.op("dve", lambda e: e.tensor_scalar(out=msk, in0=lg, scalar1=mx[:, 1:2], scalar2=None, op0=ALU.is_ge),
                             reads=[t_sml], writes=[t_sml])
                        k.op("dve", lambda e: e.tensor_tensor(out=ex, in0=ex, in1=msk, op=ALU.mult), reads=[t_sml], writes=[t_sml])
                        k.op("dve", lambda e: e.reduce_sum(out=den, in_=ex, axis=AX.X), reads=[t_sml], writes=[t_sml])
                        k.op("dve", lambda e: e.reciprocal(out=den, in_=den), reads=[t_sml], writes=[t_sml])
                        k.op("dve", lambda e: e.tensor_scalar(out=ex, in0=ex, scalar1=den, scalar2=None, op0=ALU.mult),
                             reads=[t_sml], writes=[t_sml])
                        tbi = t0 // 128 + s_
                        k.op("dve", lambda e: e.tensor_copy(out=moe_j["comb_tm"][:, tbi, :], in_=ex), reads=[t_sml], writes=[moe_j["t_rt"]])
                        k.op("dve", lambda e: e.tensor_copy(out=moe_j["mask_tm"][:, tbi, :], in_=msk), reads=[t_sml], writes=[moe_j["t_rt"]])

        zer_bf = sb("zer_bf", [128, 128], BF16)
        k.op("dve", lambda e: e.memset(zer_bf[:, :], 0.0), writes=[t_one])
        t_sml = Tl("sml")

        def ffn(W1d, W3d, W2d, Gv, blks, tag, cb=None, t_cb=None, bar=True):
            NP = 14
            t_w = TW
            t_u = TU
            w1s = W1d.rearrange("(c p) n -> p c n", p=128)
            w3s = W3d.rearrange("(c p) n -> p c n", p=128)
            w2s = W2d.rearrange("(f p) n -> p f n", p=128)

            def views(s_):
                sl = slots[s_]
                return (sl[:, 0:2048].rearrange("p (c n) -> p c n", c=8), sl[:, 2048:4096].rearrange("p (c n) -> p c n", c=8),
                        sl[:, 4096:6144].rearrange("p (f n) -> p f n", f=2))

            def load(i):
                a, b_, c_ = views(i % 2)
                tw = t_w[i % 2]
                k.dma("pool", a, w1s[:, :, i * 256:(i + 1) * 256], writes=[tw[0]])
                k.dma("pool", b_, w3s[:, :, i * 256:(i + 1) * 256], writes=[tw[1]])
                k.dma("pool", c_, w2s[:, 2 * i:2 * i + 2, :], writes=[tw[2]])

            load(0)
            for i in range(NP):
                if i + 1 < NP:
                    load(i + 1)
                s_ = i % 2
                w1v, w3v, w2v = views(s_)
                tw = t_w[s_]
                uv = RA[:, s_ * 2 * NT:(s_ + 1) * 2 * NT].rearrange("p (f t) -> p f t", f=2)
                for fc in range(2):
                    for (t0, n, v) in blks:
                        b = t0 // 512
                        p1, t_p1 = k.ps()
                        p3, t_p3 = k.ps()
                        k.mm([tw[0], t_H[b]], [t_p1], [dict(out=p1[:, :n], lhsT=w1v[:, c, fc * 128:(fc + 1) * 128], rhs=hT[:, c, t0:t0 + n],
                                                          start=(c == 0), stop=(c == 7)) for c in range(8)])
                        k.mm([tw[1], t_H[b]], [t_p3], [dict(out=p3[:, :n], lhsT=w3v[:, c, fc * 128:(fc + 1) * 128], rhs=hT[:, c, t0:t0 + n],
                                                          start=(c == 0), stop=(c == 7)) for c in range(8)])
                        gb, t_g = nxt("gt", gt, t_gt)
                        k.op("act", lambda e: e.activation(out=gb[:, :n], in_=p1[:, :n], func=AF.Silu), reads=[t_p1], writes=[t_g])
                        if cb is None:
                            k.op("dve", lambda e: e.tensor_tensor(out=uv[:, fc, t0:t0 + n], in0=gb[:, :n], in1=p3[:, :n], op=ALU.mult),
                                 reads=[t_g, t_p3], writes=[t_u[s_][b]])
                        else:
                            tb_, t_t = nxt("tt", tt, t_tt)
                            k.op("dve", lambda e: e.tensor_tensor(out=tb_[:, :n], in0=gb[:, :n], in1=p3[:, :n], op=ALU.mult),
                                 reads=[t_g, t_p3], writes=[t_t])
                            k.op("pool", lambda e: e.tensor_tensor(out=uv[:, fc, t0:t0 + n], in0=tb_[:, :n], in1=cb[:, t0:t0 + n], op=ALU.mult),
                                 reads=[t_t, t_cb], writes=[t_u[s_][b]])
                for d in range(8):
                    for (t0, n, v) in blks:
                        b = t0 // 512
                        po, t_po = k.ps()
                        k.mm([tw[2], t_u[s_][b]], [t_po], [dict(out=po[:, :n], lhsT=w2v[:, fc, d * 128:(d + 1) * 128], rhs=uv[:, fc, t0:t0 + n],
                                                              start=(fc == 0), stop=(fc == 1)) for fc in range(2)])
                        k.op("dve", lambda e: e.scalar_tensor_tensor(out=xT[:, d, t0:t0 + n], in0=po[:, :n], scalar=Gv[:, d, v:v + 1],
                                                                    in1=xT[:, d, t0:t0 + n], op0=ALU.mult, op1=ALU.add),
                             reads=[t_po, t_mT, t_X[b]], writes=[t_X[b]])
            if bar:
                k.barrier()

        def proj_out(Wd_rows, yv, t_y, Gv, blks, tag):
            t_w = [TW[0][0], TW[1][0]]
            ws = Wd_rows.rearrange("(c p) n -> p c n", p=128)
            wv = [slots[i][:, 0:4096].rearrange("p (c n) -> p c n", c=4) for i in range(2)]
            for i in range(2):
                k.dma("pool", wv[i], ws[:, 4 * i:4 * i + 4, :], writes=[t_w[i]])
            for d in range(8):
                for (t0, n, v) in blks:
                    b = t0 // 512
                    po, t_po = k.ps()
                    k.mm([t_w[0], t_w[1], t_y[b]], [t_po],
                         [dict(out=po[:, :n], lhsT=wv[c // 4][:, c % 4, d * 128:(d + 1) * 128], rhs=yv[:, c, t0:t0 + n],
                               start=(c == 0), stop=(c == 7)) for c in range(8)])
                    k.op("dve", lambda e: e.scalar_tensor_tensor(out=xT[:, d, t0:t0 + n], in0=po[:, :n], scalar=Gv[:, d, v:v + 1],
                                                                in1=xT[:, d, t0:t0 + n], op0=ALU.mult, op1=ALU.add),
                         reads=[t_po, t_mT, t_X[b]], writes=[t_X[b]])
            k.barrier()

        def even_mixer(j, blks):
            yv = RA[:, :].rearrange("p (c t) -> p c t", c=8)
            t_y = [Tl(f"ya{j}_{b}") for b in range(5)]
            win = w_in[j].rearrange("(c p) n -> p c n", p=128)
            t_w = [TW[0][0], TW[1][0]]

            def wv1(s_):
                return slots[s_][:, 0:2048].rearrange("p (c n) -> p c n", c=8)
            k.dma("pool", wv1(0), win[:, :, 0:256], writes=[t_w[0]])
            for i in range(4):
                if i + 1 < 4:
                    k.dma("pool", wv1((i + 1) % 2), win[:, :, (i + 1) * 256:(i + 2) * 256], writes=[t_w[(i + 1) % 2]])
                for fc in range(2):
                    jc = 2 * i + fc
                    for (t0, n, v) in blks:
                        b = t0 // 512
                        p1, t_p1 = k.ps()
                        k.mm([t_w[i % 2], t_H[b]], [t_p1], [dict(out=p1[:, :n], lhsT=wv1(i % 2)[:, c, fc * 128:(fc + 1) * 128], rhs=hT[:, c, t0:t0 + n],
                                                              start=(c == 0), stop=(c == 7)) for c in range(8)])
                        k.op("act", lambda e: e.activation(out=yv[:, jc, t0:t0 + n], in_=p1[:, :n], func=AF.Gelu_apprx_tanh),
                             reads=[t_p1], writes=[t_y[b]])
            k.barrier()
            t_wv = [TW[0][0], TW[1][0]]
            wvv = [slots[i][:, 0:4096].rearrange("p (c n) -> p c n", c=8) for i in range(2)]
            for i in range(2):
                k.dma("pool", wvv[i], win[:, :, 1024 + i * 512:1024 + (i + 1) * 512], writes=[t_wv[i]])
            vgs = [slots[0][:, 4096:6144].bitcast(F32), slots[1][:, 4096:6144].bitcast(F32)]
            T2 = TMP[:, 0:1024]
            wsTv = TMP[:, 1024:1536].bitcast(BF16).rearrange("p (g q) -> p g q", g=8)
            bsb = TMP[:, 1536:2560]
            lgf = sml[:, 32:40]
            lbf = sml[:, 40:48]
            k.dma("sp", lgf, lngf_d[j], writes=[t_p2])
            k.dma("sp", lbf, lnbf_d[j], writes=[t_p2])
            k.dma("sp", bsb, bs_d[j:j + 1, :].partition_broadcast(128), writes=[t_p2])
            k.dma("pool", wsTv, wsT_d[j].rearrange("g q p -> q g p"), writes=[t_p2])
            t_T2 = Tl("T2")
            for gb_ in range(2):
                pw_, t_pw_ = k.ps()
                for gg in range(4):
                    g_ = gb_ * 4 + gg
                    k.mm([t_p2, t_one], [t_pw_], [dict(out=pw_[:, gg * 128:(gg + 1) * 128], lhsT=ones_bf[:, :], rhs=wsTv[:, g_, :], start=True, stop=True)])
                for gg in range(4):
                    g_ = gb_ * 4 + gg
                    k.op("dve", lambda e: e.scalar_tensor_tensor(out=T2[:, g_ * 128:(g_ + 1) * 128], in0=pw_[:, gg * 128:(gg + 1) * 128], scalar=lbf[:, g_:g_ + 1],
                                                                in1=bsb[:, g_ * 128:(g_ + 1) * 128], op0=ALU.mult, op1=ALU.add), reads=[t_pw_, t_p2], writes=[t_T2])
            t_vgs = [Tl("vg0"), Tl("vg1")]
            t_st = [Tl("st0"), Tl("st1")]
            vb2 = [t32[0][:, :].bitcast(BF16), t32[1][:, :].bitcast(BF16)]
            ntb = [tb for (t0, n, v) in blks for tb in range(t0 // 128, (t0 + n) // 128)]
            for it_, tb in enumerate(ntb):
                b = min(tb // 4, 4)
                tk = tb * 128
                par = it_ % 2
                vg, t_vg = vgs[par], t_vgs[par]
                pv = []
                for h_ in range(2):
                    p_, t_p = k.ps()
                    k.mm([t_wv[h_], t_H[b]], [t_p], [dict(out=p_[:, :], lhsT=hT[:, c, tk:tk + 128], rhs=wvv[h_][:, c, :],
                                                        start=(c == 0), stop=(c == 7)) for c in range(8)])
                    pv.append((p_, t_p))
                for h_ in range(2):
                    k.op("act", lambda e, h_=h_: e.activation(out=vg[:, h_ * 512:(h_ + 1) * 512], in_=pv[h_][0][:, :], func=AF.Gelu_apprx_tanh),
                         reads=[pv[h_][1]], writes=[t_vg])
                so = par * 16
                st = sml[:, so:so + 12].rearrange("p (a b) -> p a b", a=2)
                mv = sml[:, so + 12:so + 14]
                rstd = sml[:, so + 14:so + 15]
                nmr = sml[:, so + 15:so + 16]
                t_s_ = t_st[par]
                for h_ in range(2):
                    k.op("dve", lambda e, h_=h_: e.bn_stats(out=st[:, h_, :], in_=vg[:, h_ * 512:(h_ + 1) * 512]), reads=[t_vg], writes=[t_s_])
                k.op("dve", lambda e: e.bn_aggr(out=mv, in_=sml[:, so:so + 12]), reads=[t_s_], writes=[t_s_])
                k.op("act", lambda e: e.activation(out=rstd, in_=mv[:, 1:2], func=AF.Sqrt, bias=EPS), reads=[t_s_], writes=[t_s_])
                k.op("dve", lambda e: e.reciprocal(out=rstd, in_=rstd), reads=[t_s_], writes=[t_s_])
                k.op("dve", lambda e: e.scalar_tensor_tensor(out=nmr, in0=mv[:, 0:1], scalar=-1.0, in1=rstd, op0=ALU.mult, op1=ALU.mult),
                     reads=[t_s_], writes=[t_s_])
                vbf = vb2[par]
                t_v = t_t32[par]
                k.op("act", lambda e: e.activation(out=vbf, in_=vg, func=AF.Identity, scale=rstd, bias=nmr), reads=[t_s_, t_vg], writes=[t_v])
                for gb_ in range(2):
                    pg, t_pg = k.ps()
                    for gg in range(4):
                        g_ = gb_ * 4 + gg
                        k.mm([t_v, t_p2], [t_pg], [dict(out=pg[:, gg * 128:(gg + 1) * 128], lhsT=vbf[:, g_ * 128:(g_ + 1) * 128], rhs=wsTv[:, g_, :],
                                                      start=True, stop=True)])
                    tb_, t_t = nxt("rs", rs, t_rs)
                    for gg in range(4):
                        g_ = gb_ * 4 + gg
                        k.op("dve", lambda e: e.scalar_tensor_tensor(out=tb_[:, gg * 128:(gg + 1) * 128], in0=pg[:, gg * 128:(gg + 1) * 128], scalar=lgf[:, g_:g_ + 1],
                                                                    in1=T2[:, g_ * 128:(g_ + 1) * 128], op0=ALU.mult, op1=ALU.add),
                             reads=[t_pg, t_p2, t_T2], writes=[t_t])
                    yslice = yv[:, gb_ * 4:gb_ * 4 + 4, tk:tk + 128]
                    k.op("pool", lambda e: e.tensor_tensor(out=yslice, in0=yslice, in1=tb_[:, :].rearrange("p (g q) -> p g q", g=4), op=ALU.mult),
                         reads=[t_t], writes=[t_y[b]])
            k.barrier()
            proj_out(w_out[j, 0:1024, :], yv, t_y, G1, blks, f"m4a{j}")
            t_yb = [Tl(f"yb{j}_{b}") for b in range(5)]
            k.dma("sp", convw[:, :], convw_d[j], writes=[t_lpar])
            k.dma("sp", convb[:, :], convb_d[j], writes=[t_lpar])
            k.dma("sp", cng[:, :], cng_d[j], writes=[t_lpar])
            cw3 = convw[:, :].rearrange("p (c k) -> p c k", c=8)
            GW = 2078 + 286
            gbufs = [(slots[1][:, a_ * GW:a_ * GW + 2078], slots[1][:, a_ * GW + 2078:(a_ + 1) * GW]) for a_ in range(2)]
            t_gs = [Tl("g0"), Tl("g1")]
            Dg = TMP[:, 0:1984].bitcast(BF16).rearrange("p (k m) -> p k m", k=31)
            t_dg = Tl("dg")
            for a_ in range(2):
                k.op("pool", lambda e: e.memset(slots[1][:, a_ * GW:(a_ + 1) * GW], 0.0), writes=[t_gs[a_]])
            t_w3 = [TW[0][0], TW[0][1]]

            def wv3(s_):
                base = s_ * 2048
                return (slots[0][:, base:base + 1024].rearrange("p (c n) -> p c n", c=8),
                        slots[0][:, base + 1024:base + 2048].rearrange("p (c n) -> p c n", c=8))

            def load3(i):
                a_, g_ = wv3(i % 2)
                k.dma("pool", a_, win[:, :, 2048 + i * 128:2048 + (i + 1) * 128], writes=[t_w3[i % 2]])
                k.dma("pool", g_, win[:, :, 3072 + i * 128:3072 + (i + 1) * 128], writes=[t_w3[i % 2]], semt=t_w3[i % 2])

            def stage_proj(i):
                if i + 1 < 8:
                    load3(i + 1)
                a_, g_ = wv3(i % 2)
                gL, gC = gbufs[i % 2]
                for (t0, n, v) in blks:
                    b = t0 // 512
                    pa, t_pa = k.ps()
                    pg, t_pg = k.ps()
                    k.mm([t_w3[i % 2], t_H[b]], [t_pa], [dict(out=pa[:, :n], lhsT=a_[:, c, :], rhs=hT[:, c, t0:t0 + n], start=(c == 0), stop=(c == 7)) for c in range(8)])
                    k.mm([t_w3[i % 2], t_H[b]], [t_pg], [dict(out=pg[:, :n], lhsT=g_[:, c, :], rhs=hT[:, c, t0:t0 + n], start=(c == 0), stop=(c == 7)) for c in range(8)])
                    sg, t_sg = nxt("rs", rs, t_rs)
                    k.op("act", lambda e: e.activation(out=sg[:, :n], in_=pg[:, :n], func=AF.Sigmoid), reads=[t_pg], writes=[t_sg])
                    dst = gL[:, 15 + t0:15 + t0 + n] if v == 0 else gC[:, 15:15 + n]
                    k.op("dve", lambda e: e.tensor_tensor(out=dst, in0=sg[:, :n], in1=pa[:, :n], op=ALU.mult), reads=[t_sg, t_pa], writes=[t_gs[i % 2]])

            def stage_conv(i):
                gL, gC = gbufs[i % 2]
                for tap in range(31):
                    k.op("dve", lambda e: e.tensor_scalar(out=Dg[:, tap, :], in0=ident_bf[:, :], scalar1=cw3[:, i, tap:tap + 1], scalar2=None, op0=ALU.mult),
                         reads=[t_par, t_lpar], writes=[t_dg])
                for (t0, n, v) in blks:
                    b = t0 // 512
                    gbuf, o0 = (gL, t0) if v == 0 else (gC, 0)
                    pa_, t_pa_ = k.ps()
                    k.mm([t_dg, t_gs[i % 2]], [t_pa_], [dict(out=pa_[:, :n], lhsT=Dg[:, tap, :], rhs=gbuf[:, o0 + tap:o0 + tap + n], start=(tap == 0), stop=(tap == 30))
                                                        for tap in range(31)])
                    k.op("act", lambda e: e.activation(out=yv[:, i, t0:t0 + n], in_=pa_[:, :n], func=AF.Identity, bias=convb[:, i:i + 1]),
                         reads=[t_pa_, t_lpar], writes=[t_yb[b]])

            load3(0)
            stage_proj(0)
            for i in range(8):
                if i + 1 < 8:
                    stage_proj(i + 1)
                stage_conv(i)
            for (t0, n, v) in blks:
                b = t0 // 512
                rsb, t_r = nxt("rs", rs, t_rs)
                psm, t_ps = k.ps()
                for c in range(8):
                    sqb, t_s = nxt("sq", sq, t_sq)
                    k.op("act", lambda e: e.activation(out=sqb[:, :n], in_=yv[:, c, t0:t0 + n], func=AF.Square), reads=[t_yb[b]], writes=[t_s])
                    k.mm([t_s, t_one], [t_ps], [dict(out=psm[:, :n], lhsT=ones_bf[:, :], rhs=sqb[:, :n], start=(c == 0), stop=(c == 7))])
                k.op("act", lambda e: e.activation(out=rsb[:, :n], in_=psm[:, :n], func=AF.Ln, scale=1.0 / D, bias=EPS), reads=[t_ps], writes=[t_r])
                k.op("act", lambda e: e.activation(out=rsb[:, :n], in_=rsb[:, :n], func=AF.Exp, scale=-0.5), reads=[t_r], writes=[t_r])
                for c in range(8):
                    tb_, t_t = nxt("t32", t32, t_t32)
                    k.op("dve", lambda e: e.scalar_tensor_tensor(out=tb_[:, :n], in0=yv[:, c, t0:t0 + n], scalar=cng[:, c:c + 1], in1=rsb[:, :n],
                                                                op0=ALU.mult, op1=ALU.mult), reads=[t_yb[b], t_r, t_lpar], writes=[t_t])
                    k.op("act", lambda e: e.activation(out=yv[:, c, t0:t0 + n], in_=tb_[:, :n], func=AF.Silu), reads=[t_t], writes=[t_yb[b]])
            k.barrier()
            proj_out(w_out[j, 1024:2048, :], yv, t_yb, G1, blks, f"m4b{j}")


        I32 = mybir.dt.int32
        TWO_PI = 6.283185307179586

        def rope_tables():
            RAf = RA[:, 0:16384].bitcast(F32)
            y = RAf[:, 0:2048]
            yy = RAf[:, 2048:4096]
            kf = RAf[:, 4096:6144]
            ki = RAf[:, 6144:8192].bitcast(I32)
            fidx = sml[:, 40:41]
            invf = sml[:, 41:42]
            t_r = Tl("ropetmp")
            k.dma("sp", y, pos_d[:, :], writes=[t_r])
            k.dma("sp", fidx, fidx_d[:, :], writes=[t_r])
            k.op("act", lambda e: e.activation(out=invf, in_=fidx, func=AF.Exp, scale=-float(np.log(10000.0)) / 16.0), reads=[t_r], writes=[t_r])
            k.op("dve", lambda e: e.tensor_scalar(out=y, in0=y, scalar1=invf, scalar2=1.0 / TWO_PI, op0=ALU.mult, op1=ALU.mult), reads=[t_r], writes=[t_r])
            for shift, dst in ((0.0, sinT), (0.25, cosT)):
                k.op("dve", lambda e: e.tensor_scalar(out=yy, in0=y, scalar1=shift, scalar2=None, op0=ALU.add), reads=[t_r], writes=[t_r])
                k.op("dve", lambda e: e.tensor_copy(out=ki, in_=yy), reads=[t_r], writes=[t_r])
                k.op("dve", lambda e: e.tensor_copy(out=kf, in_=ki), reads=[t_r], writes=[t_r])
                k.op("dve", lambda e: e.tensor_tensor(out=yy, in0=yy, in1=kf, op=ALU.subtract), reads=[t_r], writes=[t_r])
                k.op("dve", lambda e: e.tensor_single_scalar(out=kf, in_=yy, scalar=0.5, op=ALU.is_gt), reads=[t_r], writes=[t_r])
                k.op("dve", lambda e: e.tensor_tensor(out=yy, in0=yy, in1=kf, op=ALU.subtract), reads=[t_r], writes=[t_r])
                k.op("dve", lambda e: e.tensor_single_scalar(out=kf, in_=yy, scalar=-0.5, op=ALU.is_lt), reads=[t_r], writes=[t_r])
                k.op("dve", lambda e: e.tensor_tensor(out=yy, in0=yy, in1=kf, op=ALU.add), reads=[t_r], writes=[t_r])
                k.op("act", lambda e: e.activation(out=dst, in_=yy, func=AF.Sin, scale=TWO_PI * (1.0 - 1e-6)), reads=[t_r], writes=[t_par])
            k.barrier()

        def attention(j, need_ctx):
            blks_q = BLK_L + (BLK_C if need_ctx else [])
            blks_a = BLK_L + BLK_C
            k.dma("sp", qg[:, :], qg_d[j], writes=[t_lpar])
            k.dma("sp", kg[:, :], kg_d[j], writes=[t_lpar])
            k.dma("sp", sinke[:, :], sink_d[j:j + 1, :].partition_broadcast(128), writes=[t_lpar])
            k.op("act", lambda e: e.activation(out=sinke[:, :], in_=sinke[:, :], func=AF.Exp), reads=[], writes=[t_lpar])
            k.npool = 6
            po, t_po = k.psum[6]
            pd, t_pd = k.psum[7]
            qT = RA[:, 0:2 * NT].rearrange("p (h t) -> p h t", h=2)
            kT = RA[:, 2 * NT:3 * NT]
            Vg = RA[:, 3 * NT:3 * NT + 1152].rearrange("p (b d) -> p b d", b=18)
            Pc = RA[:, 3 * NT + 1152:3 * NT + 1152 + 2 * NT].rearrange("p (b t) -> p b t", b=2)
            Pring = TMP[:, :].bitcast(BF16)
            NR = 8
            t_q = [[Tl(f"q{h}_{b}") for b in range(5)] for h in range(4)]
            t_k = [Tl(f"k{b}") for b in range(5)]
            t_v = [Tl(f"v{b}") for b in range(3)]
            t_pc = Tl("pc")
            t_pr = [Tl(f"pr{i}") for i in range(NR)]
            wsrc = wqkv[j].rearrange("(c p) n -> p c n", p=128)
            tq = [TW[0][0], TW[0][1]]
            tv = [TW[1][1], TW[1][2]]

            def wviews(s_):
                base = s_ * 3072
                return (slots[0][:, base:base + 2048].rearrange("p (c n) -> p c n", c=8),
                        slots[0][:, base + 2048:base + 3072].rearrange("p (c n) -> p c n", c=8),
                        slots[1][:, 2048 + s_ * 512:2048 + (s_ + 1) * 512].rearrange("p (c n) -> p c n", c=8))

            def loadw(g):
                a_, b_, c_ = wviews(g % 2)
                t_ = tq[g % 2]
                k.dma("pool", a_, wsrc[:, :, g * 256:(g + 1) * 256], writes=[t_])
                k.dma("pool", b_[:, :, 0:64], wsrc[:, :, 1024 + g * 64:1024 + (g + 1) * 64], writes=[t_])
                k.dma("pool", b_[:, :, 64:128], wsrc[:, :, 1024 + g * 64:1024 + (g + 1) * 64], writes=[t_])
                k.dma("pool", c_, wsrc[:, :, 1280 + g * 64:1280 + (g + 1) * 64], writes=[tv[g % 2]])
            wov = slots[1][:, 0:2048].rearrange("p (h n) -> p h n", h=2)
            t_wo = TW[1][0]

            def qk_chain(projitems, rd, n, gvec, dst, t_dsts, rope, t0):
                ps_ = k.ps()
                k.mm(rd, [ps_[1]], projitems(ps_[0]))
                yield
                sqb, t_s = nxt("sq", sq, t_sq)
                k.op("act", lambda e: e.activation(out=sqb[:, :n], in_=ps_[0][:, :n], func=AF.Square), reads=[ps_[1]], writes=[t_s])
                yield
                pss, t_pss = k.ps()
                k.mm([t_s, t_par], [t_pss], [dict(out=pss[:, :n], lhsT=bd_bf[:, :], rhs=sqb[:, :n], start=True, stop=True)])
                yield
                rsb, t_r = nxt("rs", rs, t_rs)
                k.op("act", lambda e: e.activation(out=rsb[:, :n], in_=pss[:, :n], func=AF.Ln, scale=1.0 / 64, bias=EPS), reads=[t_pss], writes=[t_r])
                yield
                k.op("act", lambda e: e.activation(out=rsb[:, :n], in_=rsb[:, :n], func=AF.Exp, scale=-0.5), reads=[t_r], writes=[t_r])
                yield
                qn, t_qn = nxt("t32", t32, t_t32)
                k.op("dve", lambda e: e.scalar_tensor_tensor(out=qn[:, :n], in0=ps_[0][:, :n], scalar=gvec[:, 0:1], in1=rsb[:, :n],
                                                            op0=ALU.mult, op1=ALU.mult), reads=[ps_[1], t_r, t_lpar], writes=[t_qn])
                yield
                if not rope:
                    k.op("act", lambda e: e.copy(out=dst, in_=qn[:, :n]), reads=[t_qn], writes=t_dsts)
                    return
                qb, t_qb = nxt("tt", tt, t_tt)
                k.op("pool", lambda e: e.tensor_copy(out=qb[:, :n], in_=qn[:, :n]), reads=[t_qn], writes=[t_qb])
                yield
                psr, t_psr = k.ps()
                k.mm([t_qb, t_par], [t_psr], [dict(out=psr[:, :n], lhsT=perm_bf[:, :], rhs=qb[:, :n], start=True, stop=True)])
                yield
                bb, t_bb = nxt("rs", rs, t_rs)
                k.op("dve", lambda e: e.tensor_tensor(out=bb[:, :n], in0=psr[:, :n], in1=sinT[:, t0:t0 + n], op=ALU.mult), reads=[t_psr, t_par], writes=[t_bb])
                k.op("dve", lambda e: e.tensor_tensor(out=qn[:, :n], in0=qn[:, :n], in1=cosT[:, t0:t0 + n], op=ALU.mult), reads=[t_par], writes=[t_qn])
                yield
                k.op("pool", lambda e: e.tensor_tensor(out=dst, in0=qn[:, :n], in1=bb[:, :n], op=ALU.add), reads=[t_qn, t_bb], writes=t_dsts)

            def lockstep(gens, width=2):
                for i0 in range(0, len(gens), width):
                    active = gens[i0:i0 + width]
                    while active:
                        alive = []
                        for g_ in active:
                            try:
                                next(g_)
                                alive.append(g_)
                            except StopIteration:
                                pass
                        active = alive

            loadw(0)
            for g in range(4):
                if g + 1 < 4:
                    loadw(g + 1)
                k.dma("pool", wov, wo_d[j][g * 256:(g + 1) * 256, :].rearrange("(h p) n -> p h n", p=128), writes=[t_wo])
                wq_, wk_, wv_ = wviews(g % 2)
                t_w = tq[g % 2]
                chains = []
                for (t0, n, v) in blks_a:
                    b = t0 // 512
                    chains.append(qk_chain(lambda pst, t0=t0, n=n: [dict(out=pst[:, :n], lhsT=wk_[:, c, :], rhs=hT[:, c, t0:t0 + n], start=(c == 0), stop=(c == 7)) for c in range(8)],
                                           [t_w, t_H[b]], n, kg, kT[:, t0:t0 + n], [t_k[b]], v == 0, t0))
                for pr in range(2):
                    for (t0, n, v) in blks_q:
                        b = t0 // 512
                        chains.append(qk_chain(lambda pst, t0=t0, n=n, pr=pr: [dict(out=pst[:, :n], lhsT=wq_[:, c, pr * 128:(pr + 1) * 128], rhs=hT[:, c, t0:t0 + n],
                                                                                 start=(c == 0), stop=(c == 7)) for c in range(8)],
                                               [t_w, t_H[b]], n, qg, qT[:, pr, t0:t0 + n], [t_q[2 * pr][b], t_q[2 * pr + 1][b]], v == 0, t0))
                lockstep(chains)
                for vb in range(3):
                    tbs = list(range(vb * 8, min(18, vb * 8 + 8)))
                    ps_ = k.ps()
                    for ii, tb in enumerate(tbs):
                        b = min(tb // 4, 4)
                        k.mm([tv[g % 2], t_H[b]], [ps_[1]], [dict(out=ps_[0][:, ii * 64:(ii + 1) * 64], lhsT=hT[:, c, tb * 128:(tb + 1) * 128], rhs=wv_[:, c, :],
                                                              start=(c == 0), stop=(c == 7)) for c in range(8)])
                    nb = len(tbs)
                    k.op("act", lambda e: e.copy(out=Vg[:, tbs[0]:tbs[0] + nb, :], in_=ps_[0][:, 0:nb * 64].rearrange("p (b d) -> p b d", b=nb)),
                         reads=[ps_[1]], writes=[t_v[vb]])
                for hh in range(4):
                    h = 4 * g + hh
                    pr = hh // 2
                    P0 = (hh % 2) * 64
                    P1 = P0 + 64
                    for kb in range(2):
                        for (t0, n, v) in blks_q:
                            b = t0 // 512
                            ps_ = k.ps()
                            k.mm([t_k[4], t_q[hh][b]], [ps_[1]], [dict(out=ps_[0][:, :n], lhsT=kT[P0:P1, 2048 + kb * 128:2048 + (kb + 1) * 128], rhs=qT[P0:P1, pr, t0:t0 + n],
                                                                    start=True, stop=True)])
                            k.op("act", lambda e: e.activation(out=Pc[:, kb, t0:t0 + n], in_=ps_[0][:, :n], func=AF.Exp, scale=0.125), reads=[ps_[1]], writes=[t_pc])
                    pinfo = {}

                    def pv(i):
                        col = (i % 4) * 128
                        srcs = [(Vg[:, 16 + kb, :], Pc[:, kb, i * 128:(i + 1) * 128], t_pc) for kb in range(2)]
                        for jb in (i - 1, i, i + 1):
                            if 0 <= jb <= 15:
                                pr_, q0_, t_ = pinfo[jb]
                                srcs.append((Vg[:, jb, :], pr_[:, i * 128 - q0_:i * 128 - q0_ + 128], t_))
                        rd = [t_v[0], t_v[1], t_v[2]] + [s_[2] for s_ in srcs]
                        k.mm(rd, [t_po], [dict(out=po[P0:P1, col:col + 128], lhsT=va, rhs=pa, start=(ii == 0), stop=(ii == len(srcs) - 1))
                                          for ii, (va, pa, _) in enumerate(srcs)])
                        k.mm(rd + [t_one], [t_pd], [dict(out=pd[P0:P1, col:col + 128], lhsT=ones_bf[:, 0:64], rhs=pa, start=(ii == 0), stop=(ii == len(srcs) - 1))
                                                    for ii, (va, pa, _) in enumerate(srcs)])
                        if i % 4 == 3:
                            m_ = i // 4
                            finish(m_ * 512, 512, m_)

                    def finish(t0, n, b):
                        dn, t_dn = nxt("rs", rs, t_rs)
                        k.op("act", lambda e: e.activation(out=dn[P0:P1, :n], in_=pd[P0:P1, :n], func=AF.Ln, bias=sinke[P0:P1, h:h + 1]), reads=[t_pd, t_lpar], writes=[t_dn])
                        k.op("act", lambda e: e.activation(out=dn[P0:P1, :n], in_=dn[P0:P1, :n], func=AF.Exp, scale=-1.0), reads=[t_dn], writes=[t_dn])
                        k.op("dve", lambda e: e.tensor_tensor(out=qT[P0:P1, pr, t0:t0 + n], in0=po[P0:P1, :n], in1=dn[P0:P1, :n], op=ALU.mult),
                             reads=[t_po, t_dn], writes=[t_q[hh][b]])

                    for jb in range(16):
                        q0 = max(0, 128 * (jb - 1))
                        q1 = min(NL, 128 * (jb + 2))
                        n = q1 - q0
                        mo = q0 - 128 * (jb - 1)
                        ps_ = k.ps()
                        qb_ = sorted(set([q0 // 512, (q1 - 1) // 512]))
                        k.mm([t_k[jb // 4], t_par] + [t_q[hh][b] for b in qb_], [ps_[1]],
                             [dict(out=ps_[0][:, :n], lhsT=kT[P0:P1, jb * 128:(jb + 1) * 128], rhs=qT[P0:P1, pr, q0:q1], start=True, stop=False),
                              dict(out=ps_[0][:, :n], lhsT=ident_bf[:, :], rhs=mask_bf[:, mo:mo + n], start=False, stop=True)])
                        ri = jb % NR
                        prt = Pring[:, ri * 384:(ri + 1) * 384]
                        k.op("act", lambda e: e.activation(out=prt[:, :n], in_=ps_[0][:, :n], func=AF.Exp, scale=0.125), reads=[ps_[1]], writes=[t_pr[ri]])
                        pinfo[jb] = (prt, q0, t_pr[ri])
                        if jb >= 4:
                            pv(jb - 4)
                    for i_ in range(12, 16):
                        pv(i_)
                    if need_ctx:
                        srcs = [(Vg[:, 16 + kb, :], Pc[:, kb, 2048:2304]) for kb in range(2)]
                        k.mm([t_v[2], t_pc], [t_po], [dict(out=po[P0:P1, 0:256], lhsT=va, rhs=pa, start=(ii == 0), stop=(ii == 1)) for ii, (va, pa) in enumerate(srcs)])
                        k.mm([t_pc, t_one], [t_pd], [dict(out=pd[P0:P1, 0:256], lhsT=ones_bf[:, 0:64], rhs=pa, start=(ii == 0), stop=(ii == 1)) for ii, (va, pa) in enumerate(srcs)])
                        finish(2048, 256, 4)
                for d in range(8):
                    for (t0, n, v) in blks_q:
                        b = t0 // 512
                        pw, t_pw = k.ps()
                        k.mm([t_wo] + [t_q[hh][b] for hh in range(4)], [t_pw],
                             [dict(out=pw[:, :n], lhsT=wov[:, pr, d * 128:(d + 1) * 128], rhs=qT[:, pr, t0:t0 + n], start=(pr == 0), stop=(pr == 1)) for pr in range(2)])
                        k.op("dve", lambda e: e.scalar_tensor_tensor(out=xT[:, d, t0:t0 + n], in0=pw[:, :n], scalar=G1[:, d, v:v + 1],
                                                                    in1=xT[:, d, t0:t0 + n], op0=ALU.mult, op1=ALU.add),
                             reads=[t_pw, t_mT, t_X[b]], writes=[t_X[b]])
                k.barrier()
            k.npool = 8

        def moe(j, blks):
            combT = RA[:, 4 * NT:6 * NT].bitcast(F32)
            cbs = [RA[:, 6 * NT:7 * NT], RA[:, 7 * NT:8 * NT]]
            t_comb = Tl("comb")
            t_cbs = [Tl("cb0"), Tl("cb1")]
            t_cm = Tl("cm")
            cm = TMP[0:8, 0:NT]
            k.dma("sp", router_f[:, :].rearrange("p (c e) -> p c e", c=8), router_d[j].rearrange("(c p) e -> p c e", p=128), writes=[t_lpar])
            make_h(A2v, SH2, blks, moe_j=j, combT=combT, t_comb=t_comb)
            k.barrier()
            for e_ in range(NE):
                k.op("dve", lambda e: e.tensor_scalar(out=cm, in0=combT[0:8, :], scalar1=ident[0:8, e_:e_ + 1], scalar2=None, op0=ALU.mult),
                     reads=[t_comb, t_par], writes=[t_cm])
                for (t0, n, v) in blks:
                    pc_, t_pc_ = k.ps()
                    k.mm([t_cm, t_one], [t_pc_], [dict(out=pc_[:, :n], lhsT=ones_f[0:8, :], rhs=cm[:, t0:t0 + n], start=True, stop=True)])
                    k.op("act", lambda e: e.copy(out=cbs[e_ % 2][:, t0:t0 + n], in_=pc_[:, :n]), reads=[t_pc_], writes=[t_cbs[e_ % 2]])
                ffn(mw1[j, e_], mw3[j, e_], mw2[j, e_], G2, blks, f"moe{j}_{e_}", cb=cbs[e_ % 2], t_cb=t_cbs[e_ % 2], bar=False)
            k.barrier()


        def moe_sparse(j, blks):
            ntok = sum(n for (_, n, _) in blks)
            ntb = ntok // 128
            NS = 768
            hg = RA[:, 0:6144].rearrange("p (c t) -> p c t", c=8)
            uvs = [RA[:, 6144 + a * 1536:6144 + (a + 1) * 1536].rearrange("p (f t) -> p f t", f=2) for a in range(2)]
            hbuf = [RA[:, 9216 + a * 1024:9216 + (a + 1) * 1024] for a in range(3)]
            Sg = [RA[:, 12288 + a * 384:12288 + (a + 1) * 384] for a in range(3)]
            STw = RA[:, 13440:16512].rearrange("p (a t) -> p a t", a=6)
            iota_f = RA[:, 16512:17280].bitcast(F32)
            pos_tm = RA[:, 17280:17568].bitcast(F32).rearrange("p (b e) -> p b e", e=8)
            comb_tm = RA[:, 17568:17856].bitcast(F32).rearrange("p (b e) -> p b e", e=8)
            mask_tm = RA[:, 17856:18000].rearrange("p (b e) -> p b e", e=8)
            cnt_i = RA[:, 18000:18016].bitcast(I32)
            slotid = RA[:, 18016:18052].bitcast(F32)
            CP = TMP[0:16, 0:NT]
            acc = RH[:, 0:12288].bitcast(F32).rearrange("p (c t) -> p c t", c=8)
            otm = RH[:, 12288:18432].rearrange("p (a d) -> p a d", a=6)
            t_rt = Tl("rt")
            t_mc = Tl("mc")
            t_cp = Tl("cp")
            t_hb = [Tl(f"hb{a}") for a in range(3)]
            t_sg = [Tl(f"sg{a}") for a in range(3)]
            t_hg = [Tl("hg0"), Tl("hg1")]
            t_acc = [Tl("acc0"), Tl("acc1")]
            t_otm = [Tl(f"otm{a}") for a in range(6)]
            t_stw = Tl("stw")
            t_hd = Tl("hd")
            k.dma("sp", router_f[:, :].rearrange("p (c e) -> p c e", c=8), router_d[j].rearrange("(c p) e -> p c e", p=128), writes=[t_lpar])
            k.dma("sp", iota_f, iota_d[:, :], writes=[t_mc])
            k.dma("sp", slotid, slotid_d[:, :], writes=[t_mc])
            make_h(A2v, SH2, blks, moe_j=dict(comb_tm=comb_tm, mask_tm=mask_tm, t_rt=t_rt))
            for tbi in range(ntb):
                pp, t_pp = k.ps()
                items = [dict(out=pp[:, 0:8], lhsT=ones_bf[:, :], rhs=mask_tm[:, b_, :], start=(b_ == 0), stop=False) for b_ in range(tbi)]
                items.append(dict(out=pp[:, 0:8], lhsT=triu_bf[:, :], rhs=mask_tm[:, tbi, :], start=(tbi == 0), stop=True))
                k.mm([t_rt, t_one, t_par], [t_pp], items)
                k.op("dve", lambda e: e.scalar_tensor_tensor(out=pos_tm[:, tbi, :], in0=pp[:, 0:8], scalar=1.0, in1=mask_tm[:, tbi, :], op0=ALU.add, op1=ALU.mult),
                     reads=[t_pp], writes=[t_rt])
                k.op("dve", lambda e: e.tensor_scalar(out=pos_tm[:, tbi, :], in0=pos_tm[:, tbi, :], scalar1=-1.0, scalar2=None, op0=ALU.add), writes=[t_rt])
                cp16 = sml[:, 48:64]
                k.op("dve", lambda e: e.tensor_copy(out=cp16[:, 0:8], in_=comb_tm[:, tbi, :]), reads=[t_rt], writes=[t_sml])
                k.op("dve", lambda e: e.tensor_copy(out=cp16[:, 8:16], in_=pos_tm[:, tbi, :]), reads=[t_rt], writes=[t_sml])
                pst, t_pst = k.ps()
                k.mm([t_sml, t_par], [t_pst], [dict(out=pst[0:16, 0:128], lhsT=cp16, rhs=ident[:, :], start=True, stop=True, is_transpose=True)])
                k.op("act", lambda e: e.copy(out=CP[0:16, tbi * 128:(tbi + 1) * 128], in_=pst[0:16, 0:128]), reads=[t_pst], writes=[t_cp])
            pcn, t_pcn = k.ps()
            k.mm([t_rt, t_one], [t_pcn], [dict(out=pcn[:, 0:8], lhsT=ones_bf[:, :], rhs=mask_tm[:, b_, :], start=(b_ == 0), stop=(b_ == ntb - 1)) for b_ in range(ntb)])
            t_cnt = Tl("cnt")
            k.op("dve", lambda e: e.tensor_copy(out=cnt_i, in_=pcn[:, 0:8]), reads=[t_pcn], writes=[t_cnt])
            for tb in range(ntb):
                b = min(tb // 4, 4)
                pt, t_pt = k.ps()
                ptb = pt[:, :].bitcast(BF16)
                k.mm([t_H[b], t_par], [t_pt], [dict(out=ptb[:, c * 128:(c + 1) * 128], lhsT=hT[:, c, tb * 128:(tb + 1) * 128], rhs=ident_bf[:, :],
                                                     start=True, stop=True, is_transpose=True) for c in range(8)])
                hb, t_h = hbuf[tb % 3], t_hb[tb % 3]
                k.op("act" if tb % 2 == 0 else "dve", (lambda e: e.copy(out=hb, in_=ptb[:, 0:1024])) if tb % 2 == 0 else (lambda e: e.tensor_copy(out=hb, in_=ptb[:, 0:1024])),
                     reads=[t_pt], writes=[t_h])
                k.dma("sp", h_tm_d[tb], hb, reads=[t_h], semt=t_hd)
            k.barrier()

            w_srcs = None

            def pass_body(e_, p_, blocks, nsb):
                spb = nsb // 2
                W1d, W3d, W2d = mw1[j, e_], mw3[j, e_], mw2[j, e_]
                w1s = W1d.rearrange("(c p) n -> p c n", p=128)
                w3s = W3d.rearrange("(c p) n -> p c n", p=128)
                w2s = W2d.rearrange("(f p) n -> p f n", p=128)

                def views(a):
                    sl = slots[a]
                    return (sl[:, 0:2048].rearrange("p (c n) -> p c n", c=8), sl[:, 2048:4096].rearrange("p (c n) -> p c n", c=8),
                            sl[:, 4096:6144].rearrange("p (f n) -> p f n", f=2))

                def load(i):
                    a, b_, c_ = views(i % 2)
                    tw = TW[i % 2]
                    k.dma("pool", a, w1s[:, :, i * 256:(i + 1) * 256], writes=[tw[0]])
                    k.dma("pool", b_, w3s[:, :, i * 256:(i + 1) * 256], writes=[tw[1]])
                    k.dma("pool", c_, w2s[:, 2 * i:2 * i + 2, :], writes=[tw[2]])
                load(0)
                hi = 0
                for bi, (s0, sn) in enumerate(blocks):
                    accs = [k.psum[c] for c in range(8)]
                    for tb in range(ntb):
                        hb, t_h = hbuf[hi % 3], t_hb[hi % 3]
                        sg, t_s = Sg[hi % 3], t_sg[hi % 3]
                        hi += 1
                        k.dma("sp", hb, h_tm_d[tb], writes=[t_h])
                        k.op("dve", lambda e: e.tensor_scalar(out=sg[:, 0:sn], in0=iota_f[:, 0:sn], scalar1=float(NS * p_ + s0), scalar2=pos_tm[:, tb, e_:e_ + 1],
                                                              op0=ALU.add, op1=ALU.is_equal), reads=[t_mc, t_rt], writes=[t_s])
                        for c in range(8):
                            k.mm([t_h, t_s], [accs[c][1]], [dict(out=accs[c][0][:, 0:sn], lhsT=hb[:, c * 128:(c + 1) * 128], rhs=sg[:, 0:sn], start=(tb == 0), stop=(tb == ntb - 1))])
                    for c in range(8):
                        if c % 2 == 0:
                            k.op("act", lambda e: e.copy(out=hg[:, c, s0:s0 + sn], in_=accs[c][0][:, 0:sn]), reads=[accs[c][1]], writes=[t_hg[bi]])
                        else:
                            k.op("dve", lambda e: e.tensor_copy(out=hg[:, c, s0:s0 + sn], in_=accs[c][0][:, 0:sn]), reads=[accs[c][1]], writes=[t_hg[bi]])
                for i in range(14):
                    if i + 1 < 14:
                        load(i + 1)
                    a = i % 2
                    w1v, w3v, w2v = views(a)
                    tw = TW[a]
                    uv = uvs[a]
                    for fc in range(2):
                        for bi, (s0, sn) in enumerate(blocks):
                            p1, t_p1 = k.ps()
                            p3, t_p3 = k.ps()
                            k.mm([tw[0], t_hg[bi]], [t_p1], [dict(out=p1[:, :sn], lhsT=w1v[:, c, fc * 128:(fc + 1) * 128], rhs=hg[:, c, s0:s0 + sn],
                                                                 start=(c == 0), stop=(c == 7)) for c in range(8)])
                            k.mm([tw[1], t_hg[bi]], [t_p3], [dict(out=p3[:, :sn], lhsT=w3v[:, c, fc * 128:(fc + 1) * 128], rhs=hg[:, c, s0:s0 + sn],
                                                                 start=(c == 0), stop=(c == 7)) for c in range(8)])
                            gb, t_g = nxt("gt", gt, t_gt)
                            k.op("act", lambda e: e.activation(out=gb[:, :sn], in_=p1[:, :sn], func=AF.Silu), reads=[t_p1], writes=[t_g])
                            k.op("dve", lambda e: e.tensor_tensor(out=uv[:, fc, s0:s0 + sn], in0=gb[:, :sn], in1=p3[:, :sn], op=ALU.mult),
                                 reads=[t_g, t_p3], writes=[TU[a][bi]])
                    for d in range(8):
                        for bi, (s0, sn) in enumerate(blocks):
                            po_, t_po_ = k.ps()
                            k.mm([tw[2], TU[a][bi]], [t_po_], [dict(out=po_[:, :sn], lhsT=w2v[:, fc, d * 128:(d + 1) * 128], rhs=uv[:, fc, s0:s0 + sn],
                                                                   start=(fc == 0), stop=(fc == 1)) for fc in range(2)])
                            if i == 0:
                                k.op("act", lambda e: e.copy(out=acc[:, d, s0:s0 + sn], in_=po_[:, :sn]), reads=[t_po_], writes=[t_acc[bi]])
                            else:
                                k.op("dve", lambda e: e.tensor_tensor(out=acc[:, d, s0:s0 + sn], in0=po_[:, :sn], in1=acc[:, d, s0:s0 + sn], op=ALU.add),
                                     reads=[t_po_], writes=[t_acc[bi]])
                for sb_ in range(nsb):
                    for half in range(2):
                        pt, t_pt = k.ps()
                        k.mm([t_acc[sb_ // spb], t_par], [t_pt],
                             [dict(out=pt[:, dd * 128:(dd + 1) * 128], lhsT=acc[:, half * 4 + dd, sb_ * 128:(sb_ + 1) * 128], rhs=ident[:, :],
                                   start=True, stop=True, is_transpose=True) for dd in range(4)])
                        if half == 0:
                            k.op("act", lambda e: e.copy(out=otm[:, sb_, 0:512], in_=pt[:, :]), reads=[t_pt], writes=[t_otm[sb_]])
                        else:
                            k.op("dve", lambda e: e.tensor_copy(out=otm[:, sb_, 512:1024], in_=pt[:, :]), reads=[t_pt], writes=[t_otm[sb_]])
                for (t0, n, v) in blks:
                    b = t0 // 512
                    cmt, t_c = nxt("t32", t32, t_t32)
                    k.op("dve", lambda e: e.tensor_scalar(out=cmt[0:16, :n], in0=CP[0:16, t0:t0 + n], scalar1=ident[0:16, e_:e_ + 1], scalar2=None, op0=ALU.mult),
                         reads=[t_cp, t_par], writes=[t_c])
                    pc_, t_pc_ = k.ps()
                    k.mm([t_c, t_one], [t_pc_], [dict(out=pc_[:, :n], lhsT=ones_f[0:16, :], rhs=cmt[0:16, :n], start=True, stop=True)])
                    cbb, t_cb = nxt("rs", rs, t_rs)
                    k.op("act", lambda e: e.copy(out=cbb[:, :n], in_=pc_[:, :n]), reads=[t_pc_], writes=[t_cb])
                    pmt, t_p = nxt("t32", t32, t_t32)
                    k.op("dve", lambda e: e.tensor_scalar(out=pmt[0:16, :n], in0=CP[0:16, t0:t0 + n], scalar1=ident[0:16, 8 + e_:9 + e_], scalar2=None, op0=ALU.mult),
                         reads=[t_cp, t_par], writes=[t_p])
                    pp_, t_pp_ = k.ps()
                    k.mm([t_p, t_one], [t_pp_], [dict(out=pp_[:, :n], lhsT=ones_f[0:16, :], rhs=pmt[0:16, :n], start=True, stop=True)])
                    for sb_ in range(nsb):
                        kk = 6 * p_ + sb_
                        k.op("dve", lambda e: e.scalar_tensor_tensor(out=STw[:, sb_, :n], in0=pp_[:, :n], scalar=slotid[:, kk:kk + 1], in1=cbb[:, :n],
                                                                    op0=ALU.is_equal, op1=ALU.mult), reads=[t_pp_, t_cb, t_mc], writes=[t_stw])
                    for d in range(8):
                        px, t_px = k.ps()
                        k.mm([t_stw] + t_otm[:nsb], [t_px], [dict(out=px[:, :n], lhsT=otm[:, sb_, d * 128:(d + 1) * 128], rhs=STw[:, sb_, :n],
                                                           start=(sb_ == 0), stop=(sb_ == nsb - 1)) for sb_ in range(nsb)])
                        k.op("dve", lambda e: e.scalar_tensor_tensor(out=xT[:, d, t0:t0 + n], in0=px[:, :n], scalar=G2[:, d, v:v + 1],
                                                                    in1=xT[:, d, t0:t0 + n], op0=ALU.mult, op1=ALU.add),
                             reads=[t_px, t_mT, t_X[b]], writes=[t_X[b]])

            npass = (ntok + NS - 1) // NS
            cnt_f = sml[:, 0:8]
            flg_f2 = TMP[:, 2304:2376]
            flg_f = flg_f2.rearrange("p (a e) -> p a e", e=8)
            flg_i = TMP[:, 2376:2448].bitcast(I32)
            t_flg = Tl("flg")
            k.op("dve", lambda e: e.tensor_copy(out=cnt_f, in_=cnt_i), reads=[t_cnt], writes=[t_sml])
            for p_ in range(3):
                k.op("dve", lambda e: e.tensor_single_scalar(out=flg_f[:, 2 * p_, :], in_=cnt_f, scalar=float(NS * p_ + 512), op=ALU.is_gt), reads=[t_sml], writes=[t_flg])
                k.op("dve", lambda e: e.tensor_single_scalar(out=flg_f[:, 2 * p_ + 1, :], in_=cnt_f, scalar=float(NS * p_), op=ALU.is_gt), reads=[t_sml], writes=[t_flg])
                k.op("dve", lambda e: e.tensor_tensor(out=flg_f[:, 2 * p_ + 1, :], in0=flg_f[:, 2 * p_ + 1, :], in1=flg_f[:, 2 * p_, :], op=ALU.subtract), writes=[t_flg])
            k.op("dve", lambda e: e.tensor_single_scalar(out=flg_f[:, 6, :], in_=cnt_f, scalar=float(NS), op=ALU.is_gt), reads=[t_sml], writes=[t_flg])
            k.op("dve", lambda e: e.tensor_single_scalar(out=flg_f[:, 7, :], in_=cnt_f, scalar=float(2 * NS), op=ALU.is_gt), reads=[t_sml], writes=[t_flg])
            k.op("dve", lambda e: e.tensor_tensor(out=flg_f[:, 8, :], in0=flg_f[:, 6, :], in1=flg_f[:, 0, :], op=ALU.subtract), writes=[t_flg])
            k.op("dve", lambda e: e.tensor_scalar(out=flg_f[:, 8, :], in0=flg_f[:, 8, :], scalar1=1.0, scalar2=None, op0=ALU.add), writes=[t_flg])
            k.op("dve", lambda e: e.tensor_copy(out=flg_i, in_=flg_f2), writes=[t_flg])
            regs = moe_regs
            variants = [([(0, 384), (384, 384)], 6), ([(0, 256), (256, 256)], 4)]

            def load_flag(row, e_):
                col = row * 8 + e_
                for r_ in regs:
                    en = {"Pool": "pool", "Activation": "act", "PE": "pe", "DVE": "dve", "SP": "sp"}[str(r_.engine).split(".")[-1]]
                    E = k.E[en]
                    k._need(E, k._deps([t_flg], []))
                    E.h.load(r_, flg_i[0:1, col:col + 1])

            def bump(deltas):
                for (en, h_, dv) in deltas:
                    k.E[en].h.sem_inc(h_, dv)

            def region(row, e_, body):
                load_flag(row, e_)
                k.region_begin()
                with nc.If_ne(regs, 0):
                    body()
                    deltas = k.region_end()
                with nc.Else():
                    bump(deltas)

            def run_pass(e_, p_):
                for vi, (blocks_v, nsb_v) in enumerate(variants):
                    region(2 * p_ + vi, e_, lambda: pass_body(e_, p_, blocks_v, nsb_v))

            def run_from(e_, p_):
                def body():
                    run_pass(e_, p_)
                    if p_ + 1 < npass:
                        run_from(e_, p_ + 1)
                region(5 + p_, e_, body)

            for e_ in range(NE):
                region(0, e_, lambda: pass_body(e_, 0, variants[0][0], variants[0][1]))

                def rest():
                    region(1, e_, lambda: pass_body(e_, 0, variants[1][0], variants[1][1]))
                    if npass > 1:
                        run_from(e_, 1)
                region(8, e_, rest)
            k.barrier()

        moe_regs = nc.alloc_registers("cnt")
        import os
        stop = int(os.environ.get("KSTOP", "99"))
        phase = 0
        for li in range(4):
            j = li // 2
            need_ctx = li < 3
            blks = BLK_L + (BLK_C if need_ctx else [])
            layer_params(li)
            if li % 2 == 0:
                make_h(A1v, SH1, blks)
                k.barrier()
                even_mixer(j, blks)
                phase += 1
                if phase >= stop:
                    break
                make_h(A2v, SH2, blks)
                k.barrier()
                ffn(ffw1[j], ffw3[j], ffw2[j], G2, blks, f"ff{j}")
                phase += 1
                if phase >= stop:
                    break
            else:
                if li == 1:
                    rope_tables()
                make_h(A1v, SH1, BLK_L + BLK_C)
                k.barrier()
                attention(j, need_ctx)
                phase += 1
                if phase >= stop:
                    break
                moe_sparse(j, blks)
                phase += 1
                if phase >= stop:
                    break

        k.barrier()
        t_out = Tl("out")
        for b in range(5):
            t0, n, _ = (BLK_L + BLK_C)[b]
            k.dma("sp", out_d[:, t0:t0 + n].rearrange("(c p) t -> p c t", p=128), xT[:, :, t0:t0 + n], reads=[t_X[b]], semt=t_out)
        k.E["sp"].h.wait_ge(t_out.dsem, t_out.dcnt)
    return nc


def _prep(inputs, b):
    f = np.float32
    x, ctx, c, c_ctx = inputs["x"], inputs["ctx"], inputs["c"], inputs["c_ctx"]
    m = {}
    m["xin"] = np.ascontiguousarray(np.concatenate([x[b], ctx[b]], axis=0).T)
    cvv = np.stack([c[b].reshape(8, 128).T, c_ctx.reshape(8, 128).T], axis=-1)
    m["cv"] = np.ascontiguousarray(cvv.reshape(128, 16))
    return m


def _shared(inputs):
    m = {}
    g = lambda a: np.ascontiguousarray(a, dtype=np.float32)
    m["ada_w"] = g(inputs["ada_w"])
    m["ada_b"] = g(inputs["ada_b"].reshape(4, 48, 128).transpose(0, 2, 1))
    m["ng1"] = g(inputs["norm_mix_g"].reshape(4, 8, 128).transpose(0, 2, 1))
    m["ng2"] = g(inputs["norm_ffn_g"].reshape(4, 8, 128).transpose(0, 2, 1))
    m["ev_w_in"] = g(inputs["ev_w_in"])
    m["ev_ln_g"] = g(inputs["ev_ln_g"].reshape(2, 8, 128).transpose(0, 2, 1))
    m["ev_ln_b"] = g(inputs["ev_ln_b"].reshape(2, 8, 128).transpose(0, 2, 1))
    m["ev_wsT"] = g(inputs["ev_ws"].transpose(0, 1, 3, 2))
    m["ev_bs"] = g(inputs["ev_bs"].reshape(2, 1024))
    m["ev_conv_w"] = g(inputs["ev_conv_w"].transpose(0, 2, 1).reshape(2, 8, 128, 31).transpose(0, 2, 1, 3).reshape(2, 128, 248))
    m["ev_conv_b"] = g(inputs["ev_conv_b"].reshape(2, 8, 128).transpose(0, 2, 1))
    m["ev_cnorm_g"] = g(inputs["ev_cnorm_g"].reshape(2, 8, 128).transpose(0, 2, 1))
    m["ev_w_out"] = g(inputs["ev_w_out"])
    m["od_w_qkv"] = g(inputs["od_w_qkv"])
    m["od_q_g"] = g(np.concatenate([inputs["od_q_g"], inputs["od_q_g"]], axis=1).reshape(2, 128, 1))
    m["od_k_g"] = g(np.concatenate([inputs["od_k_g"], inputs["od_k_g"]], axis=1).reshape(2, 128, 1))
    m["od_sink"] = g(inputs["od_sink"])
    m["od_w_o"] = g(inputs["od_w_o"])
    m["ff_w1"] = g(inputs["ff_w1"])
    m["ff_w3"] = g(inputs["ff_w3"])
    m["ff_w2"] = g(inputs["ff_w2"])
    m["moe_router"] = g(inputs["moe_router"])
    m["moe_w1"] = g(inputs["moe_w1"])
    m["moe_w3"] = g(inputs["moe_w3"])
    m["moe_w2"] = g(inputs["moe_w2"])
    m["c_ident"] = np.eye(128, dtype=np.float32)
    pm = np.zeros((64, 64), np.float32)
    for p in range(64):
        r = p % 32
        if r < 16:
            pm[p + 16, p] = -1.0
        else:
            pm[p - 16, p] = 1.0
    pm2 = np.zeros((128, 128), np.float32)
    pm2[0:64, 0:64] = pm
    pm2[64:128, 64:128] = pm
    m["c_perm"] = pm2
    bd = np.zeros((128, 128), np.float32)
    bd[0:64, 0:64] = 1.0
    bd[64:128, 64:128] = 1.0
    m["c_bd"] = bd
    kk = np.arange(128)[:, None]
    qq = np.arange(384)[None, :] - 128
    m["c_mask"] = np.where(np.abs(qq - kk) <= 128, 0.0, -240000.0).astype(np.float32)
    t = np.arange(2048)
    pos = np.zeros((64, 2048), np.float32)
    pos[0:32, :] = (t // 64)[None, :]
    pos[32:64, :] = (t % 64)[None, :]
    m["c_pos"] = np.concatenate([pos, pos], axis=0)
    m["c_fidx"] = (np.arange(128) % 16).astype(np.float32).reshape(128, 1)
    m["c_triu"] = (np.arange(128)[:, None] < np.arange(128)[None, :]).astype(np.float32)
    m["c_iota"] = np.tile(np.arange(384, dtype=np.float32)[None, :], (128, 1))
    m["c_slotid"] = (np.arange(128, dtype=np.float32)[:, None] + 128.0 * np.arange(18, dtype=np.float32)[None, :])
    return m


_NC_CACHE = {}


def kernel(**inputs):
    inputs = {k_: np.asarray(v) for k_, v in inputs.items()}
    ncores = 8
    if "nc" not in _NC_CACHE:
        _NC_CACHE["nc"] = build()
    nc = _NC_CACHE["nc"]
    shared = _shared(inputs)
    in_maps = []
    for b in range(ncores):
        m = dict(shared)
        m.update(_prep(inputs, b))
        in_maps.append(m)
    res = run_bass_kernel_spmd(nc, in_maps, core_ids=list(range(ncores)))
    outs = [np.asarray(r["out"]) for r in res.results]
    return np.stack([o[:, :NL].T for o in outs], axis=0).astype(np.float32)
```

```python
import numpy as np
import concourse.bass as bass
import concourse.mybir as mybir
from concourse.bass_utils import run_bass_kernel_spmd
from contextlib import ExitStack

F32 = mybir.dt.float32
BF16 = mybir.dt.bfloat16
AF = mybir.ActivationFunctionType
ALU = mybir.AluOpType
AX = mybir.AxisListType

NL, NCX, NT = 2048, 256, 2304
D, DFF, NE = 1024, 3584, 8
EPS = 1e-6
BLK_L = [(0, 512, 0), (512, 512, 0), (1024, 512, 0), (1536, 512, 0)]
BLK_C = [(2048, 256, 1)]
SEMLIM = 10 ** 9


class Tl:
    __slots__ = ("name", "w", "r", "dsem", "dcnt")

    def __init__(s, name):
        s.name = name
        s.w = None
        s.r = {}
        s.dsem = None
        s.dcnt = 0


class Eng:
    def __init__(s, name, h):
        s.name, s.h, s.sem, s.cnt, s.waited = name, h, None, 0, {}


class K:
    def __init__(s, nc, es):
        s.nc, s.es = nc, es
        s.E = {"pe": Eng("pe", nc.tensor), "act": Eng("act", nc.scalar), "dve": Eng("dve", nc.vector),
               "pool": Eng("pool", nc.gpsimd), "sp": Eng("sp", nc.sync)}
        s.nsem = 0
        for e in s.E.values():
            e.sem = s.newsem(e.name)
        s.dsems = {}
        s.psum = []
        s.psi = 0
        s._snaps = []
        s.npool = 8

    def newsem(s, name):
        s.nsem += 1
        return s.es.enter_context(s.nc.semaphore(f"{name}_{s.nsem}"))

    def _need(s, E, deps, embed=False):
        todo = []
        for num, (h, v) in deps.items():
            if E.waited.get(num, 0) < v:
                todo.append((h, v))
                E.waited[num] = v
        last = None
        if embed and todo:
            last = todo.pop()
        for (h, v) in todo:
            E.h.wait_ge(h, v)
        return last

    @staticmethod
    def _deps(reads, writes):
        d = {}

        def add(rec):
            h, v = rec
            if h.num not in d or d[h.num][1] < v:
                d[h.num] = rec
        for t in reads:
            if t.w:
                add(t.w)
        for t in writes:
            if t.w:
                add(t.w)
            for rec in t.r.values():
                add(rec)
        return d

    @staticmethod
    def _mark(rec, reads, writes):
        for t in reads:
            t.r[rec[0].num] = rec
        for t in writes:
            t.w = rec
            t.r = {}

    def _rot(s, E):
        if E.cnt >= SEMLIM:
            E.sem = s.newsem(E.name)
            E.cnt = 0

    def op(s, en, fn, reads=(), writes=()):
        E = s.E[en]
        last = s._need(E, s._deps(reads, writes), embed=True)
        ins = fn(E.h)
        if last is not None:
            ins._wait_ge(last[0], last[1])
        E.cnt += 1
        ins.then_inc(E.sem, 1)
        s._mark((E.sem, E.cnt), reads, writes)
        s._rot(E)

    def mm(s, reads, writes, items):
        E = s.E["pe"]
        last = s._need(E, s._deps(reads, writes), embed=True)
        ins = None
        for it in items:
            ins = E.h.matmul(**it)
            if last is not None:
                ins._wait_ge(last[0], last[1])
                last = None
        E.cnt += 1
        ins.then_inc(E.sem, 1)
        s._mark((E.sem, E.cnt), reads, writes)
        s._rot(E)

    def dma(s, q, out, in_, reads=(), writes=(), semt=None):
        E = s.E[q]
        s._need(E, s._deps(reads, writes))
        ins = E.h.dma_start(out=out, in_=in_)
        t = semt if semt is not None else writes[0]
        if t.dsem is None:
            t.dsem = s.newsem("d" + t.name)
        t.dcnt += 16
        ins.then_inc(t.dsem, 16)
        s.dsems[t.dsem.num] = (t.dsem, t.dcnt)
        s._mark((t.dsem, t.dcnt), reads, writes)

    def barrier(s, engines=("pe", "act", "dve", "pool", "sp"), own=False):
        for en in engines:
            E = s.E[en]
            d = {}
            for F in s.E.values():
                if (own or F is not E) and F.cnt > 0:
                    d[F.sem.num] = (F.sem, F.cnt)
            d.update(s.dsems)
            s._need(E, d)

    def region_begin(s):
        for n, E in s.E.items():
            d = {}
            if E.cnt > 0:
                d[E.sem.num] = (E.sem, E.cnt)
            if n == "sp":
                d.update(s.dsems)
            s._need(E, d)
        s._snaps.append(({n: (E.sem, E.cnt) for n, E in s.E.items()}, dict(s.dsems), {n: dict(E.waited) for n, E in s.E.items()}))

    def region_end(s):
        e0, d0, w0 = s._snaps.pop()
        deltas = []
        for n, E in s.E.items():
            assert E.sem.num == e0[n][0].num, "semaphore rotated inside region"
            if E.cnt > e0[n][1]:
                deltas.append((n, E.sem, E.cnt - e0[n][1]))
        for num, (h, v) in s.dsems.items():
            v0 = d0[num][1] if num in d0 else 0
            if v > v0:
                deltas.append(("sp", h, v - v0))
        for n, E in s.E.items():
            E.waited = w0[n]
        return deltas

    def ps(s):
        p = s.psum[s.psi % s.npool]
        s.psi += 1
        return p


def build():
    nc = bass.Bass("TRN2", target_bir_lowering=False)

    def din(name, shape):
        return nc.dram_tensor(name, list(shape), F32, kind="ExternalInput").ap()

    xin = din("xin", [1024, NT])
    cv_d = din("cv", [128, 16])
    ada_w = din("ada_w", [4, 1024, 6144])
    ada_b = din("ada_b", [4, 128, 48])
    ng1_d = din("ng1", [4, 128, 8])
    ng2_d = din("ng2", [4, 128, 8])
    w_in = din("ev_w_in", [2, 1024, 4096])
    lngf_d = din("ev_ln_g", [2, 128, 8])
    lnbf_d = din("ev_ln_b", [2, 128, 8])
    wsT_d = din("ev_wsT", [2, 8, 128, 128])
    bs_d = din("ev_bs", [2, 1024])
    convw_d = din("ev_conv_w", [2, 128, 248])
    convb_d = din("ev_conv_b", [2, 128, 8])
    cng_d = din("ev_cnorm_g", [2, 128, 8])
    w_out = din("ev_w_out", [2, 2048, 1024])
    wqkv = din("od_w_qkv", [2, 1024, 1536])
    qg_d = din("od_q_g", [2, 128, 1])
    kg_d = din("od_k_g", [2, 128, 1])
    sink_d = din("od_sink", [2, 16])
    wo_d = din("od_w_o", [2, 1024, 1024])
    ffw1 = din("ff_w1", [2, 1024, DFF])
    ffw3 = din("ff_w3", [2, 1024, DFF])
    ffw2 = din("ff_w2", [2, DFF, 1024])
    router_d = din("moe_router", [2, 1024, 8])
    mw1 = din("moe_w1", [2, 8, 1024, DFF])
    mw3 = din("moe_w3", [2, 8, 1024, DFF])
    mw2 = din("moe_w2", [2, 8, DFF, 1024])
    ident_d = din("c_ident", [128, 128])
    perm_d = din("c_perm", [128, 128])
    bd_d = din("c_bd", [128, 128])
    mask_d = din("c_mask", [128, 384])
    pos_d = din("c_pos", [128, 2048])
    fidx_d = din("c_fidx", [128, 1])
    triu_d = din("c_triu", [128, 128])
    iota_d = din("c_iota", [128, 384])
    slotid_d = din("c_slotid", [128, 18])
    h_tm_d = nc.dram_tensor("h_tm_scratch", [18, 128, 1024], BF16, kind="Internal").ap()
    out_d = nc.dram_tensor("out", [1024, NT], F32, kind="ExternalOutput").ap()

    with ExitStack() as es:
        k = K(nc, es)

        def sb(name, shape, dt):
            return es.enter_context(nc.sbuf_tensor("sb_" + name, list(shape), dt))

        RX = sb("RX", [128, 8 * NT], F32)
        RH = sb("RH", [128, 8 * NT], BF16)
        RA = sb("RA", [128, 8 * NT], BF16)
        RW = sb("RW", [128, 12288], BF16)
        xT = RX[:, :].rearrange("p (c t) -> p c t", c=8)
        hT = RH[:, :].rearrange("p (c t) -> p c t", c=8)
        for i in range(8):
            k.psum.append((es.enter_context(nc.psum_tensor(f"ps{i}", [128, 512], F32)), Tl(f"ps{i}")))

        ident = sb("ident", [128, 128], F32)
        ones_bf = sb("ones_bf", [128, 128], BF16)
        ones_f = sb("ones_f", [16, 128], F32)
        triu_bf = sb("triu_bf", [128, 128], BF16)
        ident_bf = sb("ident_bf", [128, 128], BF16)
        perm_bf = sb("perm_bf", [128, 128], BF16)
        bd_bf = sb("bd_bf", [128, 128], BF16)
        mask_bf = sb("mask_bf", [128, 384], BF16)
        cv = sb("cv", [128, 16], F32)
        scv = sb("scv", [128, 16], BF16)
        adab = sb("adab", [128, 48], F32)
        mT = sb("mT", [128, 96], F32)
        mT3 = mT[:, :].rearrange("p (j v) -> p j v", v=2)
        A1 = sb("A1", [128, 16], F32)
        A2 = sb("A2", [128, 16], F32)
        A1v = A1[:, :].rearrange("p (c v) -> p c v", v=2)
        A2v = A2[:, :].rearrange("p (c v) -> p c v", v=2)
        ng1 = sb("ng1", [128, 8], F32)
        ng2 = sb("ng2", [128, 8], F32)
        tmp16 = sb("tmp16", [128, 16], F32)
        convw = sb("convw", [128, 248], F32)
        convb = sb("convb", [128, 8], F32)
        cng = sb("cng", [128, 8], F32)
        qg = sb("qg", [128, 1], F32)
        kg = sb("kg", [128, 1], F32)
        sinke = sb("sinke", [128, 16], F32)
        router_f = sb("router_f", [128, 64], F32)
        cossin = sb("cossin", [128, 2 * 2048], BF16)
        cosT = cossin[:, 0:2048]
        sinT = cossin[:, 2048:4096]
        sq = [sb(f"sq{i}", [128, 512], BF16) for i in range(2)]
        rs = [sb(f"rs{i}", [128, 512], F32) for i in range(2)]
        t32 = [sb(f"t32_{i}", [128, 512], F32) for i in range(2)]
        gt = [sb(f"gt{i}", [128, 512], BF16) for i in range(2)]
        tt = [sb(f"tt{i}", [128, 512], BF16) for i in range(2)]
        TMP = sb("TMP", [128, 2560], F32)
        sml = sb("sml", [128, 64], F32)

        t_sq = [Tl(f"sq{i}") for i in range(2)]
        t_rs = [Tl(f"rs{i}") for i in range(2)]
        t_t32 = [Tl(f"t32{i}") for i in range(2)]
        t_gt = [Tl(f"gt{i}") for i in range(2)]
        t_tt = [Tl(f"tt{i}") for i in range(2)]
        t_par = Tl("par")
        t_lpar = Tl("lpar")
        t_mT = Tl("mT")
        t_X = [Tl(f"X{i}") for i in range(5)]
        t_H = [Tl(f"H{i}") for i in range(5)]
        TW = [[Tl(f"w{a}_{i}") for i in range(3)] for a in range(2)]
        t_p2 = Tl("m2p")
        TU = [[Tl(f"u{a}_{b}") for b in range(5)] for a in range(2)]
        rot = {"sq": 0, "rs": 0, "t32": 0, "gt": 0, "tt": 0}

        def nxt(name, arr, tls):
            i = rot[name] % len(arr)
            rot[name] += 1
            return arr[i], tls[i]

        k.dma("sp", ident[:, :], ident_d[:, :], writes=[t_par])
        k.dma("sp", cv[:, :], cv_d[:, :], writes=[t_par])
        k.dma("pool", perm_bf[:, :], perm_d[:, :], writes=[t_par])
        k.dma("pool", bd_bf[:, :], bd_d[:, :], writes=[t_par])
        k.dma("pool", mask_bf[:, :], mask_d[:, :], writes=[t_par])
        k.dma("pool", triu_bf[:, :], triu_d[:, :], writes=[t_par])
        k.dma("pool", ident_bf[:, :], ident_d[:, :], writes=[t_par])
        t_one = Tl("ones")
        k.op("dve", lambda e: e.memset(ones_bf[:, :], 1.0), writes=[t_one])
        k.op("dve", lambda e: e.memset(ones_f[:, :], 1.0), writes=[t_one])
        for b in range(5):
            t0, n, _ = (BLK_L + BLK_C)[b]
            k.dma("sp", xT[:, :, t0:t0 + n], xin[:, t0:t0 + n].rearrange("(c p) t -> p c t", p=128), writes=[t_X[b]])
        k.op("act", lambda e: e.activation(out=scv[:, :], in_=cv[:, :], func=AF.Silu), reads=[t_par], writes=[t_one])
        scv3 = scv[:, :].rearrange("p (c v) -> p c v", v=2)

        W0 = RW[:, 0:6144]
        W1s = RW[:, 6144:12288]
        slots = [W0, W1s]

        def layer_params(li):
            t_w = [TW[0][0], TW[1][0]]
            k.dma("sp", adab[:, :], ada_b[li], writes=[t_lpar])
            k.dma("sp", ng1[:, :], ng1_d[li], writes=[t_lpar])
            k.dma("sp", ng2[:, :], ng2_d[li], writes=[t_lpar])
            psm, t_psm = k.ps()
            src = ada_w[li].rearrange("(c p) n -> p c n", p=128)
            for i in range(12):
                wv = slots[i % 2][:, 0:4096].rearrange("p (c n) -> p c n", c=8)
                k.dma("pool", wv, src[:, :, i * 512:(i + 1) * 512], writes=[t_w[i % 2]])
                for jj in range(4):
                    j = 4 * i + jj
                    k.mm([t_w[i % 2], t_one], [t_psm],
                         [dict(out=psm[:, 2 * j:2 * j + 2], lhsT=wv[:, c, jj * 128:(jj + 1) * 128], rhs=scv3[:, c, :],
                               start=(c == 0), stop=(c == 7)) for c in range(8)])
            ps3 = psm[:, 0:96].rearrange("p (j v) -> p j v", v=2)
            for v in range(2):
                k.op("dve", lambda e, v=v: e.tensor_tensor(out=mT3[:, :, v], in0=ps3[:, :, v], in1=adab[:, :], op=ALU.add),
                     reads=[t_psm, t_lpar], writes=[t_mT])
            t16 = tmp16[:, :].rearrange("p (c v) -> p c v", v=2)
            for (Av, ng, off) in ((A1v, ng1, 8), (A2v, ng2, 32)):
                k.op("dve", lambda e, off=off: e.tensor_scalar(out=t16, in0=mT3[:, off:off + 8, :], scalar1=1.0, scalar2=None, op0=ALU.add),
                     reads=[t_mT], writes=[t_lpar])
                for v in range(2):
                    k.op("dve", lambda e, v=v, Av=Av, ng=ng: e.tensor_tensor(out=Av[:, :, v], in0=t16[:, :, v], in1=ng[:, :], op=ALU.mult),
                         reads=[t_lpar], writes=[t_lpar])
            k.barrier()

        SH1, G1, SH2, G2 = mT3[:, 0:8, :], mT3[:, 16:24, :], mT3[:, 24:32, :], mT3[:, 40:48, :]

        def make_h(Av, SHv, blks, moe_j=None, combT=None, t_comb=None):
            for (t0, n, v) in blks:
                b = t0 // 512
                rsb, t_r = nxt("rs", rs, t_rs)
                psm, t_ps = k.ps()
                for c in range(8):
                    sqb, t_s = nxt("sq", sq, t_sq)
                    k.op("act", lambda e, c=c, sqb=sqb: e.activation(out=sqb[:, :n], in_=xT[:, c, t0:t0 + n], func=AF.Square),
                         reads=[t_X[b]], writes=[t_s])
                    k.mm([t_s, t_one], [t_ps], [dict(out=psm[:, :n], lhsT=ones_bf[:, :], rhs=sqb[:, :n], start=(c == 0), stop=(c == 7))])
                k.op("act", lambda e: e.activation(out=rsb[:, :n], in_=psm[:, :n], func=AF.Ln, scale=1.0 / D, bias=EPS),
                     reads=[t_ps], writes=[t_r])
                k.op("act", lambda e: e.activation(out=rsb[:, :n], in_=rsb[:, :n], func=AF.Exp, scale=-0.5), reads=[t_r], writes=[t_r])
                if moe_j is not None:
                    pslg, t_pslg = k.ps()
                    nsb = n // 128
                    k.mm([t_one], [t_pslg], [dict(out=pslg[:, 0:8 * nsb], lhsT=zer_bf[:, :], rhs=zer_bf[:, 0:8 * nsb], start=True, stop=False,
                                                skip_group_check=True)])
                for c in range(8):
                    tb, t_t = nxt("t32", t32, t_t32)
                    k.op("dve", lambda e, c=c, tb=tb: e.scalar_tensor_tensor(out=tb[:, :n], in0=xT[:, c, t0:t0 + n], scalar=Av[:, c, v:v + 1],
                                                                           in1=rsb[:, :n], op0=ALU.mult, op1=ALU.mult),
                         reads=[t_X[b], t_r, t_lpar], writes=[t_t])
                    if moe_j is None:
                        k.op("act", lambda e, c=c, tb=tb: e.activation(out=hT[:, c, t0:t0 + n], in_=tb[:, :n], func=AF.Identity,
                                                                     bias=SHv[:, c, v:v + 1]),
                             reads=[t_t, t_mT], writes=[t_H[b]])
                    else:
                        k.op("act", lambda e, c=c, tb=tb: e.activation(out=tb[:, :n], in_=tb[:, :n], func=AF.Identity,
                                                                     bias=SHv[:, c, v:v + 1]),
                             reads=[t_mT], writes=[t_t])
                        k.op("act", lambda e, c=c, tb=tb: e.copy(out=hT[:, c, t0:t0 + n], in_=tb[:, :n]),
                             reads=[t_t], writes=[t_H[b]])
                        k.mm([t_t, t_lpar], [t_pslg],
                             [dict(out=pslg[:, 8 * s_:8 * s_ + 8], lhsT=tb[:, s_ * 128:(s_ + 1) * 128], rhs=router_f[:, 8 * c:8 * c + 8],
                                   start=False, stop=(c == 7), skip_group_check=True) for s_ in range(nsb)])
                if moe_j is not None:
                    for s_ in range(nsb):
                        lg = sml[:, 0:8]
                        mx = sml[:, 8:16]
                        ex = sml[:, 16:24]
                        msk = sml[:, 24:32]
                        nm1 = sml[:, 32:33]
                        den = sml[:, 33:34]
                        k.op("dve", lambda e: e.tensor_copy(out=lg, in_=pslg[:, 8 * s_:8 * s_ + 8]), reads=[t_pslg], writes=[t_sml])
                        k.op("dve", lambda e: e.max(out=mx, in_=lg), reads=[t_sml], writes=[t_sml])
                        k.op("dve", lambda e: e.tensor_scalar(out=nm1, in0=mx[:, 0:1], scalar1=-1.0, scalar2=None, op0=ALU.mult),
                             reads=[t_sml], writes=[t_sml])
                        k.op("act", lambda e: e.activation(out=ex, in_=lg, func=AF.Exp, bias=nm1), reads=[t_sml], writes=[t_sml])
                        k.op("dve", lambda e: e.tensor_scalar(out=msk, in0=lg, scalar1=mx[:, 1:2], scalar2=None, op0=ALU.is_ge),
                             reads=[t_sml], writes=[t_sml])
                        k.op("dve", lambda e: e.tensor_tensor(out=ex, in0=ex, in1=msk, op=ALU.mult), reads=[t_sml], writes=[t_sml])
                        k.op("dve", lambda e: e.reduce_sum(out=den, in_=ex, axis=AX.X), reads=[t_sml], writes=[t_sml])
                        k.op("dve", lambda e: e.reciprocal(out=den, in_=den), reads=[t_sml], writes=[t_sml])
                        k.op("dve", lambda e: e.tensor_scalar(out=ex, in0=ex, scalar1=den, scalar2=None, op0=ALU.mult),
                             reads=[t_sml], writes=[t_sml])
                        tbi = t0 // 128 + s_
                        k.op("dve", lambda e: e.tensor_copy(out=moe_j["comb_tm"][:, tbi, :], in_=ex), reads=[t_sml], writes=[moe_j["t_rt"]])
                        k.op("dve", lambda e: e.tensor_copy(out=moe_j["mask_tm"][:, tbi, :], in_=msk), reads=[t_sml], writes=[moe_j["t_rt"]])

        zer_bf = sb("zer_bf", [128, 128], BF16)
        k.op("dve", lambda e: e.memset(zer_bf[:, :], 0.0), writes=[t_one])
        t_sml = Tl("sml")

        def ffn(W1d, W3d, W2d, Gv, blks, tag, cb=None, t_cb=None, bar=True):
            NP = 14
            t_w = TW
            t_u = TU
            w1s = W1d.rearrange("(c p) n -> p c n", p=128)
            w3s = W3d.rearrange("(c p) n -> p c n", p=128)
            w2s = W2d.rearrange("(f p) n -> p f n", p=128)

            def views(s_):
                sl = slots[s_]
                return (sl[:, 0:2048].rearrange("p (c n) -> p c n", c=8), sl[:, 2048:4096].rearrange("p (c n) -> p c n", c=8),
                        sl[:, 4096:6144].rearrange("p (f n) -> p f n", f=2))

            def load(i):
                a, b_, c_ = views(i % 2)
                tw = t_w[i % 2]
                k.dma("pool", a, w1s[:, :, i * 256:(i + 1) * 256], writes=[tw[0]])
                k.dma("pool", b_, w3s[:, :, i * 256:(i + 1) * 256], writes=[tw[1]])
                k.dma("pool", c_, w2s[:, 2 * i:2 * i + 2, :], writes=[tw[2]])

            load(0)
            for i in range(NP):
                if i + 1 < NP:
                    load(i + 1)
                s_ = i % 2
                w1v, w3v, w2v = views(s_)
                tw = t_w[s_]
                uv = RA[:, s_ * 2 * NT:(s_ + 1) * 2 * NT].rearrange("p (f t) -> p f t", f=2)
                for fc in range(2):
                    for (t0, n, v) in blks:
                        b = t0 // 512
                        p1, t_p1 = k.ps()
                        p3, t_p3 = k.ps()
                        k.mm([tw[0], t_H[b]], [t_p1], [dict(out=p1[:, :n], lhsT=w1v[:, c, fc * 128:(fc + 1) * 128], rhs=hT[:, c, t0:t0 + n],
                                                          start=(c == 0), stop=(c == 7)) for c in range(8)])
                        k.mm([tw[1], t_H[b]], [t_p3], [dict(out=p3[:, :n], lhsT=w3v[:, c, fc * 128:(fc + 1) * 128], rhs=hT[:, c, t0:t0 + n],
                                                          start=(c == 0), stop=(c == 7)) for c in range(8)])
                        gb, t_g = nxt("gt", gt, t_gt)
                        k.op("act", lambda e: e.activation(out=gb[:, :n], in_=p1[:, :n], func=AF.Silu), reads=[t_p1], writes=[t_g])
                        if cb is None:
                            k.op("dve", lambda e: e.tensor_tensor(out=uv[:, fc, t0:t0 + n], in0=gb[:, :n], in1=p3[:, :n], op=ALU.mult),
                                 reads=[t_g, t_p3], writes=[t_u[s_][b]])
                        else:
                            tb_, t_t = nxt("tt", tt, t_tt)
                            k.op("dve", lambda e: e.tensor_tensor(out=tb_[:, :n], in0=gb[:, :n], in1=p3[:, :n], op=ALU.mult),
                                 reads=[t_g, t_p3], writes=[t_t])
                            k.op("pool", lambda e: e.tensor_tensor(out=uv[:, fc, t0:t0 + n], in0=tb_[:, :n], in1=cb[:, t0:t0 + n], op=ALU.mult),
                                 reads=[t_t, t_cb], writes=[t_u[s_][b]])
                for d in range(8):
                    for (t0, n, v) in blks:
                        b = t0 // 512
                        po, t_po = k.ps()
                        k.mm([tw[2], t_u[s_][b]], [t_po], [dict(out=po[:, :n], lhsT=w2v[:, fc, d * 128:(d + 1) * 128], rhs=uv[:, fc, t0:t0 + n],
                                                              start=(fc == 0), stop=(fc == 1)) for fc in range(2)])
                        k.op("dve", lambda e: e.scalar_tensor_tensor(out=xT[:, d, t0:t0 + n], in0=po[:, :n], scalar=Gv[:, d, v:v + 1],
                                                                    in1=xT[:, d, t0:t0 + n], op0=ALU.mult, op1=ALU.add),
                             reads=[t_po, t_mT, t_X[b]], writes=[t_X[b]])
            if bar:
                k.barrier()

        def proj_out(Wd_rows, yv, t_y, Gv, blks, tag):
            t_w = [TW[0][0], TW[1][0]]
            ws = Wd_rows.rearrange("(c p) n -> p c n", p=128)
            wv = [slots[i][:, 0:4096].rearrange("p (c n) -> p c n", c=4) for i in range(2)]
            for i in range(2):
                k.dma("pool", wv[i], ws[:, 4 * i:4 * i + 4, :], writes=[t_w[i]])
            for d in range(8):
                for (t0, n, v) in blks:
                    b = t0 // 512
                    po, t_po = k.ps()
                    k.mm([t_w[0], t_w[1], t_y[b]], [t_po],
                         [dict(out=po[:, :n], lhsT=wv[c // 4][:, c % 4, d * 128:(d + 1) * 128], rhs=yv[:, c, t0:t0 + n],
                               start=(c == 0), stop=(c == 7)) for c in range(8)])
                    k.op("dve", lambda e: e.scalar_tensor_tensor(out=xT[:, d, t0:t0 + n], in0=po[:, :n], scalar=Gv[:, d, v:v + 1],
                                                                in1=xT[:, d, t0:t0 + n], op0=ALU.mult, op1=ALU.add),
                         reads=[t_po, t_mT, t_X[b]], writes=[t_X[b]])
            k.barrier()

        def even_mixer(j, blks):
            yv = RA[:, :].rearrange("p (c t) -> p c t", c=8)
            t_y = [Tl(f"ya{j}_{b}") for b in range(5)]
            win = w_in[j].rearrange("(c p) n -> p c n", p=128)
            t_w = [TW[0][0], TW[1][0]]

            def wv1(s_):
                return slots[s_][:, 0:2048].rearrange("p (c n) -> p c n", c=8)
            k.dma("pool", wv1(0), win[:, :, 0:256], writes=[t_w[0]])
            for i in range(4):
                if i + 1 < 4:
                    k.dma("pool", wv1((i + 1) % 2), win[:, :, (i + 1) * 256:(i + 2) * 256], writes=[t_w[(i + 1) % 2]])
                for fc in range(2):
                    jc = 2 * i + fc
                    for (t0, n, v) in blks:
                        b = t0 // 512
                        p1, t_p1 = k.ps()
                        k.mm([t_w[i % 2], t_H[b]], [t_p1], [dict(out=p1[:, :n], lhsT=wv1(i % 2)[:, c, fc * 128:(fc + 1) * 128], rhs=hT[:, c, t0:t0 + n],
                                                              start=(c == 0), stop=(c == 7)) for c in range(8)])
                        k.op("act", lambda e: e.activation(out=yv[:, jc, t0:t0 + n], in_=p1[:, :n], func=AF.Gelu_apprx_tanh),
                             reads=[t_p1], writes=[t_y[b]])
            k.barrier()
            t_wv = [TW[0][0], TW[1][0]]
            wvv = [slots[i][:, 0:4096].rearrange("p (c n) -> p c n", c=8) for i in range(2)]
            for i in range(2):
                k.dma("pool", wvv[i], win[:, :, 1024 + i * 512:1024 + (i + 1) * 512], writes=[t_wv[i]])
            vgs = [slots[0][:, 4096:6144].bitcast(F32), slots[1][:, 4096:6144].bitcast(F32)]
            T2 = TMP[:, 0:1024]
            wsTv = TMP[:, 1024:1536].bitcast(BF16).rearrange("p (g q) -> p g q", g=8)
            bsb = TMP[:, 1536:2560]
            lgf = sml[:, 32:40]
            lbf = sml[:, 40:48]
            k.dma("sp", lgf, lngf_d[j], writes=[t_p2])
            k.dma("sp", lbf, lnbf_d[j], writes=[t_p2])
            k.dma("sp", bsb, bs_d[j:j + 1, :].partition_broadcast(128), writes=[t_p2])
            k.dma("pool", wsTv, wsT_d[j].rearrange("g q p -> q g p"), writes=[t_p2])
            t_T2 = Tl("T2")
            for gb_ in range(2):
                pw_, t_pw_ = k.ps()
                for gg in range(4):
                    g_ = gb_ * 4 + gg
                    k.mm([t_p2, t_one], [t_pw_], [dict(out=pw_[:, gg * 128:(gg + 1) * 128], lhsT=ones_bf[:, :], rhs=wsTv[:, g_, :], start=True, stop=True)])
                for gg in range(4):
                    g_ = gb_ * 4 + gg
                    k.op("dve", lambda e: e.scalar_tensor_tensor(out=T2[:, g_ * 128:(g_ + 1) * 128], in0=pw_[:, gg * 128:(gg + 1) * 128], scalar=lbf[:, g_:g_ + 1],
                                                                in1=bsb[:, g_ * 128:(g_ + 1) * 128], op0=ALU.mult, op1=ALU.add), reads=[t_pw_, t_p2], writes=[t_T2])
            t_vgs = [Tl("vg0"), Tl("vg1")]
            t_st = [Tl("st0"), Tl("st1")]
            vb2 = [t32[0][:, :].bitcast(BF16), t32[1][:, :].bitcast(BF16)]
            ntb = [tb for (t0, n, v) in blks for tb in range(t0 // 128, (t0 + n) // 128)]
            for it_, tb in enumerate(ntb):
                b = min(tb // 4, 4)
                tk = tb * 128
                par = it_ % 2
                vg, t_vg = vgs[par], t_vgs[par]
                pv = []
                for h_ in range(2):
                    p_, t_p = k.ps()
                    k.mm([t_wv[h_], t_H[b]], [t_p], [dict(out=p_[:, :], lhsT=hT[:, c, tk:tk + 128], rhs=wvv[h_][:, c, :],
                                                        start=(c == 0), stop=(c == 7)) for c in range(8)])
                    pv.append((p_, t_p))
                for h_ in range(2):
                    k.op("act", lambda e, h_=h_: e.activation(out=vg[:, h_ * 512:(h_ + 1) * 512], in_=pv[h_][0][:, :], func=AF.Gelu_apprx_tanh),
                         reads=[pv[h_][1]], writes=[t_vg])
                so = par * 16
                st = sml[:, so:so + 12].rearrange("p (a b) -> p a b", a=2)
                mv = sml[:, so + 12:so + 14]
                rstd = sml[:, so + 14:so + 15]
                nmr = sml[:, so + 15:so + 16]
                t_s_ = t_st[par]
                for h_ in range(2):
                    k.op("dve", lambda e, h_=h_: e.bn_stats(out=st[:, h_, :], in_=vg[:, h_ * 512:(h_ + 1) * 512]), reads=[t_vg], writes=[t_s_])
                k.op("dve", lambda e: e.bn_aggr(out=mv, in_=sml[:, so:so + 12]), reads=[t_s_], writes=[t_s_])
                k.op("act", lambda e: e.activation(out=rstd, in_=mv[:, 1:2], func=AF.Sqrt, bias=EPS), reads=[t_s_], writes=[t_s_])
                k.op("dve", lambda e: e.reciprocal(out=rstd, in_=rstd), reads=[t_s_], writes=[t_s_])
                k.op("dve", lambda e: e.scalar_tensor_tensor(out=nmr, in0=mv[:, 0:1], scalar=-1.0, in1=rstd, op0=ALU.mult, op1=ALU.mult),
                     reads=[t_s_], writes=[t_s_])
                vbf = vb2[par]
                t_v = t_t32[par]
                k.op("act", lambda e: e.activation(out=vbf, in_=vg, func=AF.Identity, scale=rstd, bias=nmr), reads=[t_s_, t_vg], writes=[t_v])
                for gb_ in range(2):
                    pg, t_pg = k.ps()
                    for gg in range(4):
                        g_ = gb_ * 4 + gg
                        k.mm([t_v, t_p2], [t_pg], [dict(out=pg[:, gg * 128:(gg + 1) * 128], lhsT=vbf[:, g_ * 128:(g_ + 1) * 128], rhs=wsTv[:, g_, :],
                                                      start=True, stop=True)])
                    tb_, t_t = nxt("rs", rs, t_rs)
                    for gg in range(4):
                        g_ = gb_ * 4 + gg
                        k.op("dve", lambda e: e.scalar_tensor_tensor(out=tb_[:, gg * 128:(gg + 1) * 128], in0=pg[:, gg * 128:(gg + 1) * 128], scalar=lgf[:, g_:g_ + 1],
                                                                    in1=T2[:, g_ * 128:(g_ + 1) * 128], op0=ALU.mult, op1=ALU.add),
                             reads=[t_pg, t_p2, t_T2], writes=[t_t])
                    yslice = yv[:, gb_ * 4:gb_ * 4 + 4, tk:tk + 128]
                    k.op("pool", lambda e: e.tensor_tensor(out=yslice, in0=yslice, in1=tb_[:, :].rearrange("p (g q) -> p g q", g=4), op=ALU.mult),
                         reads=[t_t], writes=[t_y[b]])
            k.barrier()
            proj_out(w_out[j, 0:1024, :], yv, t_y, G1, blks, f"m4a{j}")
            t_yb = [Tl(f"yb{j}_{b}") for b in range(5)]
            k.dma("sp", convw[:, :], convw_d[j], writes=[t_lpar])
            k.dma("sp", convb[:, :], convb_d[j], writes=[t_lpar])
            k.dma("sp", cng[:, :], cng_d[j], writes=[t_lpar])
            cw3 = convw[:, :].rearrange("p (c k) -> p c k", c=8)
            GW = 2078 + 286
            gbufs = [(slots[1][:, a_ * GW:a_ * GW + 2078], slots[1][:, a_ * GW + 2078:(a_ + 1) * GW]) for a_ in range(2)]
            t_gs = [Tl("g0"), Tl("g1")]
            Dg = TMP[:, 0:1984].bitcast(BF16).rearrange("p (k m) -> p k m", k=31)
            t_dg = Tl("dg")
            for a_ in range(2):
                k.op("pool", lambda e: e.memset(slots[1][:, a_ * GW:(a_ + 1) * GW], 0.0), writes=[t_gs[a_]])
            t_w3 = [TW[0][0], TW[0][1]]

            def wv3(s_):
                base = s_ * 2048
                return (slots[0][:, base:base + 1024].rearrange("p (c n) -> p c n", c=8),
                        slots[0][:, base + 1024:base + 2048].rearrange("p (c n) -> p c n", c=8))

            def load3(i):
                a_, g_ = wv3(i % 2)
                k.dma("pool", a_, win[:, :, 2048 + i * 128:2048 + (i + 1) * 128], writes=[t_w3[i % 2]])
                k.dma("pool", g_, win[:, :, 3072 + i * 128:3072 + (i + 1) * 128], writes=[t_w3[i % 2]], semt=t_w3[i % 2])

            def stage_proj(i):
                if i + 1 < 8:
                    load3(i + 1)
                a_, g_ = wv3(i % 2)
                gL, gC = gbufs[i % 2]
                for (t0, n, v) in blks:
                    b = t0 // 512
                    pa, t_pa = k.ps()
                    pg, t_pg = k.ps()
                    k.mm([t_w3[i % 2], t_H[b]], [t_pa], [dict(out=pa[:, :n], lhsT=a_[:, c, :], rhs=hT[:, c, t0:t0 + n], start=(c == 0), stop=(c == 7)) for c in range(8)])
                    k.mm([t_w3[i % 2], t_H[b]], [t_pg], [dict(out=pg[:, :n], lhsT=g_[:, c, :], rhs=hT[:, c, t0:t0 + n], start=(c == 0), stop=(c == 7)) for c in range(8)])
                    sg, t_sg = nxt("rs", rs, t_rs)
                    k.op("act", lambda e: e.activation(out=sg[:, :n], in_=pg[:, :n], func=AF.Sigmoid), reads=[t_pg], writes=[t_sg])
                    dst = gL[:, 15 + t0:15 + t0 + n] if v == 0 else gC[:, 15:15 + n]
                    k.op("dve", lambda e: e.tensor_tensor(out=dst, in0=sg[:, :n], in1=pa[:, :n], op=ALU.mult), reads=[t_sg, t_pa], writes=[t_gs[i % 2]])

            def stage_conv(i):
                gL, gC = gbufs[i % 2]
                for tap in range(31):
                    k.op("dve", lambda e: e.tensor_scalar(out=Dg[:, tap, :], in0=ident_bf[:, :], scalar1=cw3[:, i, tap:tap + 1], scalar2=None, op0=ALU.mult),
                         reads=[t_par, t_lpar], writes=[t_dg])
                for (t0, n, v) in blks:
                    b = t0 // 512
                    gbuf, o0 = (gL, t0) if v == 0 else (gC, 0)
                    pa_, t_pa_ = k.ps()
                    k.mm([t_dg, t_gs[i % 2]], [t_pa_], [dict(out=pa_[:, :n], lhsT=Dg[:, tap, :], rhs=gbuf[:, o0 + tap:o0 + tap + n], start=(tap == 0), stop=(tap == 30))
                                                        for tap in range(31)])
                    k.op("act", lambda e: e.activation(out=yv[:, i, t0:t0 + n], in_=pa_[:, :n], func=AF.Identity, bias=convb[:, i:i + 1]),
                         reads=[t_pa_, t_lpar], writes=[t_yb[b]])

            load3(0)
            stage_proj(0)
            for i in range(8):
                if i + 1 < 8:
                    stage_proj(i + 1)
                stage_conv(i)
            for (t0, n, v) in blks:
                b = t0 // 512
                rsb, t_r = nxt("rs", rs, t_rs)
                psm, t_ps = k.ps()
                for c in range(8):
                    sqb, t_s = nxt("sq", sq, t_sq)
                    k.op("act", lambda e: e.activation(out=sqb[:, :n], in_=yv[:, c, t0:t0 + n], func=AF.Square), reads=[t_yb[b]], writes=[t_s])
                    k.mm([t_s, t_one], [t_ps], [dict(out=psm[:, :n], lhsT=ones_bf[:, :], rhs=sqb[:, :n], start=(c == 0), stop=(c == 7))])
                k.op("act", lambda e: e.activation(out=rsb[:, :n], in_=psm[:, :n], func=AF.Ln, scale=1.0 / D, bias=EPS), reads=[t_ps], writes=[t_r])
                k.op("act", lambda e: e.activation(out=rsb[:, :n], in_=rsb[:, :n], func=AF.Exp, scale=-0.5), reads=[t_r], writes=[t_r])
                for c in range(8):
                    tb_, t_t = nxt("t32", t32, t_t32)
                    k.op("dve", lambda e: e.scalar_tensor_tensor(out=tb_[:, :n], in0=yv[:, c, t0:t0 + n], scalar=cng[:, c:c + 1], in1=rsb[:, :n],
                                                                op0=ALU.mult, op1=ALU.mult), reads=[t_yb[b], t_r, t_lpar], writes=[t_t])
                    k.op("act", lambda e: e.activation(out=yv[:, c, t0:t0 + n], in_=tb_[:, :n], func=AF.Silu), reads=[t_t], writes=[t_yb[b]])
            k.barrier()
            proj_out(w_out[j, 1024:2048, :], yv, t_yb, G1, blks, f"m4b{j}")


        I32 = mybir.dt.int32
        TWO_PI = 6.283185307179586

        def rope_tables():
            RAf = RA[:, 0:16384].bitcast(F32)
            y = RAf[:, 0:2048]
            yy = RAf[:, 2048:4096]
            kf = RAf[:, 4096:6144]
            ki = RAf[:, 6144:8192].bitcast(I32)
            fidx = sml[:, 40:41]
            invf = sml[:, 41:42]
            t_r = Tl("ropetmp")
            k.dma("sp", y, pos_d[:, :], writes=[t_r])
            k.dma("sp", fidx, fidx_d[:, :], writes=[t_r])
            k.op("act", lambda e: e.activation(out=invf, in_=fidx, func=AF.Exp, scale=-float(np.log(10000.0)) / 16.0), reads=[t_r], writes=[t_r])
            k.op("dve", lambda e: e.tensor_scalar(out=y, in0=y, scalar1=invf, scalar2=1.0 / TWO_PI, op0=ALU.mult, op1=ALU.mult), reads=[t_r], writes=[t_r])
            for shift, dst in ((0.0, sinT), (0.25, cosT)):
                k.op("dve", lambda e: e.tensor_scalar(out=yy, in0=y, scalar1=shift, scalar2=None, op0=ALU.add), reads=[t_r], writes=[t_r])
                k.op("dve", lambda e: e.tensor_copy(out=ki, in_=yy), reads=[t_r], writes=[t_r])
                k.op("dve", lambda e: e.tensor_copy(out=kf, in_=ki), reads=[t_r], writes=[t_r])
                k.op("dve", lambda e: e.tensor_tensor(out=yy, in0=yy, in1=kf, op=ALU.subtract), reads=[t_r], writes=[t_r])
                k.op("dve", lambda e: e.tensor_single_scalar(out=kf, in_=yy, scalar=0.5, op=ALU.is_gt), reads=[t_r], writes=[t_r])
                k.op("dve", lambda e: e.tensor_tensor(out=yy, in0=yy, in1=kf, op=ALU.subtract), reads=[t_r], writes=[t_r])
                k.op("dve", lambda e: e.tensor_single_scalar(out=kf, in_=yy, scalar=-0.5, op=ALU.is_lt), reads=[t_r], writes=[t_r])
                k.op("dve", lambda e: e.tensor_tensor(out=yy, in0=yy, in1=kf, op=ALU.add), reads=[t_r], writes=[t_r])
                k.op("act", lambda e: e.activation(out=dst, in_=yy, func=AF.Sin, scale=TWO_PI * (1.0 - 1e-6)), reads=[t_r], writes=[t_par])
            k.barrier()

        def attention(j, need_ctx):
            blks_q = BLK_L + (BLK_C if need_ctx else [])
            blks_a = BLK_L + BLK_C
            k.dma("sp", qg[:, :], qg_d[j], writes=[t_lpar])
            k.dma("sp", kg[:, :], kg_d[j], writes=[t_lpar])
            k.dma("sp", sinke[:, :], sink_d[j:j + 1, :].partition_broadcast(128), writes=[t_lpar])
            k.op("act", lambda e: e.activation(out=sinke[:, :], in_=sinke[:, :], func=AF.Exp), reads=[], writes=[t_lpar])
            k.npool = 6
            po, t_po = k.psum[6]
            pd, t_pd = k.psum[7]
            qT = RA[:, 0:2 * NT].rearrange("p (h t) -> p h t", h=2)
            kT = RA[:, 2 * NT:3 * NT]
            Vg = RA[:, 3 * NT:3 * NT + 1152].rearrange("p (b d) -> p b d", b=18)
            Pc = RA[:, 3 * NT + 1152:3 * NT + 1152 + 2 * NT].rearrange("p (b t) -> p b t", b=2)
            Pring = TMP[:, :].bitcast(BF16)
            NR = 8
            t_q = [[Tl(f"q{h}_{b}") for b in range(5)] for h in range(4)]
            t_k = [Tl(f"k{b}") for b in range(5)]
            t_v = [Tl(f"v{b}") for b in range(3)]
            t_pc = Tl("pc")
            t_pr = [Tl(f"pr{i}") for i in range(NR)]
            wsrc = wqkv[j].rearrange("(c p) n -> p c n", p=128)
            tq = [TW[0][0], TW[0][1]]
            tv = [TW[1][1], TW[1][2]]

            def wviews(s_):
                base = s_ * 3072
                return (slots[0][:, base:base + 2048].rearrange("p (c n) -> p c n", c=8),
                        slots[0][:, base + 2048:base + 3072].rearrange("p (c n) -> p c n", c=8),
                        slots[1][:, 2048 + s_ * 512:2048 + (s_ + 1) * 512].rearrange("p (c n) -> p c n", c=8))

            def loadw(g):
                a_, b_, c_ = wviews(g % 2)
                t_ = tq[g % 2]
                k.dma("pool", a_, wsrc[:, :, g * 256:(g + 1) * 256], writes=[t_])
                k.dma("pool", b_[:, :, 0:64], wsrc[:, :, 1024 + g * 64:1024 + (g + 1) * 64], writes=[t_])
                k.dma("pool", b_[:, :, 64:128], wsrc[:, :, 1024 + g * 64:1024 + (g + 1) * 64], writes=[t_])
                k.dma("pool", c_, wsrc[:, :, 1280 + g * 64:1280 + (g + 1) * 64], writes=[tv[g % 2]])
            wov = slots[1][:, 0:2048].rearrange("p (h n) -> p h n", h=2)
            t_wo = TW[1][0]

            def qk_chain(projitems, rd, n, gvec, dst, t_dsts, rope, t0):
                ps_ = k.ps()
                k.mm(rd, [ps_[1]], projitems(ps_[0]))
                yield
                sqb, t_s = nxt("sq", sq, t_sq)
                k.op("act", lambda e: e.activation(out=sqb[:, :n], in_=ps_[0][:, :n], func=AF.Square), reads=[ps_[1]], writes=[t_s])
                yield
                pss, t_pss = k.ps()
                k.mm([t_s, t_par], [t_pss], [dict(out=pss[:, :n], lhsT=bd_bf[:, :], rhs=sqb[:, :n], start=True, stop=True)])
                yield
                rsb, t_r = nxt("rs", rs, t_rs)
                k.op("act", lambda e: e.activation(out=rsb[:, :n], in_=pss[:, :n], func=AF.Ln, scale=1.0 / 64, bias=EPS), reads=[t_pss], writes=[t_r])
                yield
                k.op("act", lambda e: e.activation(out=rsb[:, :n], in_=rsb[:, :n], func=AF.Exp, scale=-0.5), reads=[t_r], writes=[t_r])
                yield
                qn, t_qn = nxt("t32", t32, t_t32)
                k.op("dve", lambda e: e.scalar_tensor_tensor(out=qn[:, :n], in0=ps_[0][:, :n], scalar=gvec[:, 0:1], in1=rsb[:, :n],
                                                            op0=ALU.mult, op1=ALU.mult), reads=[ps_[1], t_r, t_lpar], writes=[t_qn])
                yield
                if not rope:
                    k.op("act", lambda e: e.copy(out=dst, in_=qn[:, :n]), reads=[t_qn], writes=t_dsts)
                    return
                qb, t_qb = nxt("tt", tt, t_tt)
                k.op("act", lambda e: e.copy(out=qb[:, :n], in_=qn[:, :n]), reads=[t_qn], writes=[t_qb])
                yield
                psr, t_psr = k.ps()
                k.mm([t_qb, t_par], [t_psr], [dict(out=psr[:, :n], lhsT=perm_bf[:, :], rhs=qb[:, :n], start=True, stop=True)])
                yield
                bb, t_bb = nxt("rs", rs, t_rs)
                k.op("dve", lambda e: e.tensor_tensor(out=bb[:, :n], in0=psr[:, :n], in1=sinT[:, t0:t0 + n], op=ALU.mult), reads=[t_psr, t_par], writes=[t_bb])
                k.op("dve", lambda e: e.tensor_tensor(out=qn[:, :n], in0=qn[:, :n], in1=cosT[:, t0:t0 + n], op=ALU.mult), reads=[t_par], writes=[t_qn])
                yield
                k.op("pool", lambda e: e.tensor_tensor(out=dst, in0=qn[:, :n], in1=bb[:, :n], op=ALU.add), reads=[t_qn, t_bb], writes=t_dsts)

            def lockstep(gens, width=2):
                for i0 in range(0, len(gens), width):
                    active = gens[i0:i0 + width]
                    while active:
                        alive = []
                        for g_ in active:
                            try:
                                next(g_)
                                alive.append(g_)
                            except StopIteration:
                                pass
                        active = alive

            loadw(0)
            for g in range(4):
                if g + 1 < 4:
                    loadw(g + 1)
                k.dma("pool", wov, wo_d[j][g * 256:(g + 1) * 256, :].rearrange("(h p) n -> p h n", p=128), writes=[t_wo])
                wq_, wk_, wv_ = wviews(g % 2)
                t_w = tq[g % 2]
                chains = []
                for (t0, n, v) in blks_a:
                    b = t0 // 512
                    chains.append(qk_chain(lambda pst, t0=t0, n=n: [dict(out=pst[:, :n], lhsT=wk_[:, c, :], rhs=hT[:, c, t0:t0 + n], start=(c == 0), stop=(c == 7)) for c in range(8)],
                                           [t_w, t_H[b]], n, kg, kT[:, t0:t0 + n], [t_k[b]], v == 0, t0))
                for pr in range(2):
                    for (t0, n, v) in blks_q:
                        b = t0 // 512
                        chains.append(qk_chain(lambda pst, t0=t0, n=n, pr=pr: [dict(out=pst[:, :n], lhsT=wq_[:, c, pr * 128:(pr + 1) * 128], rhs=hT[:, c, t0:t0 + n],
                                                                                 start=(c == 0), stop=(c == 7)) for c in range(8)],
                                               [t_w, t_H[b]], n, qg, qT[:, pr, t0:t0 + n], [t_q[2 * pr][b], t_q[2 * pr + 1][b]], v == 0, t0))
                lockstep(chains)
                for vb in range(3):
                    tbs = list(range(vb * 8, min(18, vb * 8 + 8)))
                    ps_ = k.ps()
                    for ii, tb in enumerate(tbs):
                        b = min(tb // 4, 4)
                        k.mm([tv[g % 2], t_H[b]], [ps_[1]], [dict(out=ps_[0][:, ii * 64:(ii + 1) * 64], lhsT=hT[:, c, tb * 128:(tb + 1) * 128], rhs=wv_[:, c, :],
                                                              start=(c == 0), stop=(c == 7)) for c in range(8)])
                    nb = len(tbs)
                    k.op("act", lambda e: e.copy(out=Vg[:, tbs[0]:tbs[0] + nb, :], in_=ps_[0][:, 0:nb * 64].rearrange("p (b d) -> p b d", b=nb)),
                         reads=[ps_[1]], writes=[t_v[vb]])
                for hh in range(4):
                    h = 4 * g + hh
                    pr = hh // 2
                    P0 = (hh % 2) * 64
                    P1 = P0 + 64
                    for kb in range(2):
                        for (t0, n, v) in blks_q:
                            b = t0 // 512
                            ps_ = k.ps()
                            k.mm([t_k[4], t_q[hh][b]], [ps_[1]], [dict(out=ps_[0][:, :n], lhsT=kT[P0:P1, 2048 + kb * 128:2048 + (kb + 1) * 128], rhs=qT[P0:P1, pr, t0:t0 + n],
                                                                    start=True, stop=True)])
                            k.op("act", lambda e: e.activation(out=Pc[:, kb, t0:t0 + n], in_=ps_[0][:, :n], func=AF.Exp, scale=0.125), reads=[ps_[1]], writes=[t_pc])
                    pinfo = {}

                    def pv(i):
                        col = (i % 4) * 128
                        srcs = [(Vg[:, 16 + kb, :], Pc[:, kb, i * 128:(i + 1) * 128], t_pc) for kb in range(2)]
                        for jb in (i - 1, i, i + 1):
                            if 0 <= jb <= 15:
                                pr_, q0_, t_ = pinfo[jb]
                                srcs.append((Vg[:, jb, :], pr_[:, i * 128 - q0_:i * 128 - q0_ + 128], t_))
                        rd = [t_v[0], t_v[1], t_v[2]] + [s_[2] for s_ in srcs]
                        k.mm(rd, [t_po], [dict(out=po[P0:P1, col:col + 128], lhsT=va, rhs=pa, start=(ii == 0), stop=(ii == len(srcs) - 1))
                                          for ii, (va, pa, _) in enumerate(srcs)])
                        k.mm(rd + [t_one], [t_pd], [dict(out=pd[P0:P1, col:col + 128], lhsT=ones_bf[:, 0:64], rhs=pa, start=(ii == 0), stop=(ii == len(srcs) - 1))
                                                    for ii, (va, pa, _) in enumerate(srcs)])
                        if i % 4 == 3:
                            m_ = i // 4
                            finish(m_ * 512, 512, m_)

                    def finish(t0, n, b):
                        dn, t_dn = nxt("rs", rs, t_rs)
                        k.op("act", lambda e: e.activation(out=dn[P0:P1, :n], in_=pd[P0:P1, :n], func=AF.Ln, bias=sinke[P0:P1, h:h + 1]), reads=[t_pd, t_lpar], writes=[t_dn])
                        k.op("act", lambda e: e.activation(out=dn[P0:P1, :n], in_=dn[P0:P1, :n], func=AF.Exp, scale=-1.0), reads=[t_dn], writes=[t_dn])
                        k.op("dve", lambda e: e.tensor_tensor(out=qT[P0:P1, pr, t0:t0 + n], in0=po[P0:P1, :n], in1=dn[P0:P1, :n], op=ALU.mult),
                             reads=[t_po, t_dn], writes=[t_q[hh][b]])

                    for jb in range(16):
                        q0 = max(0, 128 * (jb - 1))
                        q1 = min(NL, 128 * (jb + 2))
                        n = q1 - q0
                        mo = q0 - 128 * (jb - 1)
                        ps_ = k.ps()
                        qb_ = sorted(set([q0 // 512, (q1 - 1) // 512]))
                        k.mm([t_k[jb // 4], t_par] + [t_q[hh][b] for b in qb_], [ps_[1]],
                             [dict(out=ps_[0][:, :n], lhsT=kT[P0:P1, jb * 128:(jb + 1) * 128], rhs=qT[P0:P1, pr, q0:q1], start=True, stop=False),
                              dict(out=ps_[0][:, :n], lhsT=ident_bf[:, :], rhs=mask_bf[:, mo:mo + n], start=False, stop=True)])
                        ri = jb % NR
                        prt = Pring[:, ri * 384:(ri + 1) * 384]
                        k.op("act", lambda e: e.activation(out=prt[:, :n], in_=ps_[0][:, :n], func=AF.Exp, scale=0.125), reads=[ps_[1]], writes=[t_pr[ri]])
                        pinfo[jb] = (prt, q0, t_pr[ri])
                        if jb >= 4:
                            pv(jb - 4)
                    for i_ in range(12, 16):
                        pv(i_)
                    if need_ctx:
                        srcs = [(Vg[:, 16 + kb, :], Pc[:, kb, 2048:2304]) for kb in range(2)]
                        k.mm([t_v[2], t_pc], [t_po], [dict(out=po[P0:P1, 0:256], lhsT=va, rhs=pa, start=(ii == 0), stop=(ii == 1)) for ii, (va, pa) in enumerate(srcs)])
                        k.mm([t_pc, t_one], [t_pd], [dict(out=pd[P0:P1, 0:256], lhsT=ones_bf[:, 0:64], rhs=pa, start=(ii == 0), stop=(ii == 1)) for ii, (va, pa) in enumerate(srcs)])
                        finish(2048, 256, 4)
                for d in range(8):
                    for (t0, n, v) in blks_q:
                        b = t0 // 512
                        pw, t_pw = k.ps()
                        k.mm([t_wo] + [t_q[hh][b] for hh in range(4)], [t_pw],
                             [dict(out=pw[:, :n], lhsT=wov[:, pr, d * 128:(d + 1) * 128], rhs=qT[:, pr, t0:t0 + n], start=(pr == 0), stop=(pr == 1)) for pr in range(2)])
                        k.op("dve", lambda e: e.scalar_tensor_tensor(out=xT[:, d, t0:t0 + n], in0=pw[:, :n], scalar=G1[:, d, v:v + 1],
                                                                    in1=xT[:, d, t0:t0 + n], op0=ALU.mult, op1=ALU.add),
                             reads=[t_pw, t_mT, t_X[b]], writes=[t_X[b]])
                k.barrier()
            k.npool = 8

        def moe(j, blks):
            combT = RA[:, 4 * NT:6 * NT].bitcast(F32)
            cbs = [RA[:, 6 * NT:7 * NT], RA[:, 7 * NT:8 * NT]]
            t_comb = Tl("comb")
            t_cbs = [Tl("cb0"), Tl("cb1")]
            t_cm = Tl("cm")
            cm = TMP[0:8, 0:NT]
            k.dma("sp", router_f[:, :].rearrange("p (c e) -> p c e", c=8), router_d[j].rearrange("(c p) e -> p c e", p=128), writes=[t_lpar])
            make_h(A2v, SH2, blks, moe_j=j, combT=combT, t_comb=t_comb)
            k.barrier()
            for e_ in range(NE):
                k.op("dve", lambda e: e.tensor_scalar(out=cm, in0=combT[0:8, :], scalar1=ident[0:8, e_:e_ + 1], scalar2=None, op0=ALU.mult),
                     reads=[t_comb, t_par], writes=[t_cm])
                for (t0, n, v) in blks:
                    pc_, t_pc_ = k.ps()
                    k.mm([t_cm, t_one], [t_pc_], [dict(out=pc_[:, :n], lhsT=ones_f[0:8, :], rhs=cm[:, t0:t0 + n], start=True, stop=True)])
                    k.op("act", lambda e: e.copy(out=cbs[e_ % 2][:, t0:t0 + n], in_=pc_[:, :n]), reads=[t_pc_], writes=[t_cbs[e_ % 2]])
                ffn(mw1[j, e_], mw3[j, e_], mw2[j, e_], G2, blks, f"moe{j}_{e_}", cb=cbs[e_ % 2], t_cb=t_cbs[e_ % 2], bar=False)
            k.barrier()


        def moe_sparse(j, blks):
            ntok = sum(n for (_, n, _) in blks)
            ntb = ntok // 128
            NS = 768
            hg = RA[:, 0:6144].rearrange("p (c t) -> p c t", c=8)
            uvs = [RA[:, 6144 + a * 1536:6144 + (a + 1) * 1536].rearrange("p (f t) -> p f t", f=2) for a in range(2)]
            hbuf = [RA[:, 9216 + a * 1024:9216 + (a + 1) * 1024] for a in range(3)]
            Sg = [RA[:, 12288 + a * 384:12288 + (a + 1) * 384] for a in range(3)]
            STw = RA[:, 13440:16512].rearrange("p (a t) -> p a t", a=6)
            iota_f = RA[:, 16512:17280].bitcast(F32)
            pos_tm = RA[:, 17280:17568].bitcast(F32).rearrange("p (b e) -> p b e", e=8)
            comb_tm = RA[:, 17568:17856].bitcast(F32).rearrange("p (b e) -> p b e", e=8)
            mask_tm = RA[:, 17856:18000].rearrange("p (b e) -> p b e", e=8)
            cnt_i = RA[:, 18000:18016].bitcast(I32)
            slotid = RA[:, 18016:18052].bitcast(F32)
            CP = TMP[0:16, 0:NT]
            acc = RH[:, 0:12288].bitcast(F32).rearrange("p (c t) -> p c t", c=8)
            otm = RH[:, 12288:18432].rearrange("p (a d) -> p a d", a=6)
            t_rt = Tl("rt")
            t_mc = Tl("mc")
            t_cp = Tl("cp")
            t_hb = [Tl(f"hb{a}") for a in range(3)]
            t_sg = [Tl(f"sg{a}") for a in range(3)]
            t_hg = [Tl("hg0"), Tl("hg1")]
            t_acc = [Tl("acc0"), Tl("acc1")]
            t_otm = [Tl(f"otm{a}") for a in range(6)]
            t_stw = Tl("stw")
            t_hd = Tl("hd")
            k.dma("sp", router_f[:, :].rearrange("p (c e) -> p c e", c=8), router_d[j].rearrange("(c p) e -> p c e", p=128), writes=[t_lpar])
            k.dma("sp", iota_f, iota_d[:, :], writes=[t_mc])
            k.dma("sp", slotid, slotid_d[:, :], writes=[t_mc])
            make_h(A2v, SH2, blks, moe_j=dict(comb_tm=comb_tm, mask_tm=mask_tm, t_rt=t_rt))
            for tbi in range(ntb):
                pp, t_pp = k.ps()
                items = [dict(out=pp[:, 0:8], lhsT=ones_bf[:, :], rhs=mask_tm[:, b_, :], start=(b_ == 0), stop=False) for b_ in range(tbi)]
                items.append(dict(out=pp[:, 0:8], lhsT=triu_bf[:, :], rhs=mask_tm[:, tbi, :], start=(tbi == 0), stop=True))
                k.mm([t_rt, t_one, t_par], [t_pp], items)
                k.op("dve", lambda e: e.scalar_tensor_tensor(out=pos_tm[:, tbi, :], in0=pp[:, 0:8], scalar=1.0, in1=mask_tm[:, tbi, :], op0=ALU.add, op1=ALU.mult),
                     reads=[t_pp], writes=[t_rt])
                k.op("dve", lambda e: e.tensor_scalar(out=pos_tm[:, tbi, :], in0=pos_tm[:, tbi, :], scalar1=-1.0, scalar2=None, op0=ALU.add), writes=[t_rt])
                cp16 = sml[:, 48:64]
                k.op("dve", lambda e: e.tensor_copy(out=cp16[:, 0:8], in_=comb_tm[:, tbi, :]), reads=[t_rt], writes=[t_sml])
                k.op("dve", lambda e: e.tensor_copy(out=cp16[:, 8:16], in_=pos_tm[:, tbi, :]), reads=[t_rt], writes=[t_sml])
                pst, t_pst = k.ps()
                k.mm([t_sml, t_par], [t_pst], [dict(out=pst[0:16, 0:128], lhsT=cp16, rhs=ident[:, :], start=True, stop=True, is_transpose=True)])
                k.op("act", lambda e: e.copy(out=CP[0:16, tbi * 128:(tbi + 1) * 128], in_=pst[0:16, 0:128]), reads=[t_pst], writes=[t_cp])
            pcn, t_pcn = k.ps()
            k.mm([t_rt, t_one], [t_pcn], [dict(out=pcn[:, 0:8], lhsT=ones_bf[:, :], rhs=mask_tm[:, b_, :], start=(b_ == 0), stop=(b_ == ntb - 1)) for b_ in range(ntb)])
            t_cnt = Tl("cnt")
            k.op("dve", lambda e: e.tensor_copy(out=cnt_i, in_=pcn[:, 0:8]), reads=[t_pcn], writes=[t_cnt])
            for tb in range(ntb):
                b = min(tb // 4, 4)
                pt, t_pt = k.ps()
                ptb = pt[:, :].bitcast(BF16)
                k.mm([t_H[b], t_par], [t_pt], [dict(out=ptb[:, c * 128:(c + 1) * 128], lhsT=hT[:, c, tb * 128:(tb + 1) * 128], rhs=ident_bf[:, :],
                                                     start=True, stop=True, is_transpose=True) for c in range(8)])
                hb, t_h = hbuf[tb % 3], t_hb[tb % 3]
                k.op("act" if tb % 2 == 0 else "dve", (lambda e: e.copy(out=hb, in_=ptb[:, 0:1024])) if tb % 2 == 0 else (lambda e: e.tensor_copy(out=hb, in_=ptb[:, 0:1024])),
                     reads=[t_pt], writes=[t_h])
                k.dma("sp", h_tm_d[tb], hb, reads=[t_h], semt=t_hd)
            k.barrier()

            w_srcs = None

            def pass_body(e_, p_, blocks, nsb):
                spb = nsb // 2
                W1d, W3d, W2d = mw1[j, e_], mw3[j, e_], mw2[j, e_]
                w1s = W1d.rearrange("(c p) n -> p c n", p=128)
                w3s = W3d.rearrange("(c p) n -> p c n", p=128)
                w2s = W2d.rearrange("(f p) n -> p f n", p=128)

                def views(a):
                    sl = slots[a]
                    return (sl[:, 0:2048].rearrange("p (c n) -> p c n", c=8), sl[:, 2048:4096].rearrange("p (c n) -> p c n", c=8),
                            sl[:, 4096:6144].rearrange("p (f n) -> p f n", f=2))

                def load(i):
                    a, b_, c_ = views(i % 2)
                    tw = TW[i % 2]
                    k.dma("pool", a, w1s[:, :, i * 256:(i + 1) * 256], writes=[tw[0]])
                    k.dma("pool", b_, w3s[:, :, i * 256:(i + 1) * 256], writes=[tw[1]])
                    k.dma("pool", c_, w2s[:, 2 * i:2 * i + 2, :], writes=[tw[2]])
                load(0)
                hi = 0
                for bi, (s0, sn) in enumerate(blocks):
                    accs = [k.psum[c] for c in range(8)]
                    for tb in range(ntb):
                        hb, t_h = hbuf[hi % 3], t_hb[hi % 3]
                        sg, t_s = Sg[hi % 3], t_sg[hi % 3]
                        hi += 1
                        k.dma("sp", hb, h_tm_d[tb], writes=[t_h])
                        k.op("dve", lambda e: e.tensor_scalar(out=sg[:, 0:sn], in0=iota_f[:, 0:sn], scalar1=float(NS * p_ + s0), scalar2=pos_tm[:, tb, e_:e_ + 1],
                                                              op0=ALU.add, op1=ALU.is_equal), reads=[t_mc, t_rt], writes=[t_s])
                        for c in range(8):
                            k.mm([t_h, t_s], [accs[c][1]], [dict(out=accs[c][0][:, 0:sn], lhsT=hb[:, c * 128:(c + 1) * 128], rhs=sg[:, 0:sn], start=(tb == 0), stop=(tb == ntb - 1))])
                    for c in range(8):
                        if c % 2 == 0:
                            k.op("act", lambda e: e.copy(out=hg[:, c, s0:s0 + sn], in_=accs[c][0][:, 0:sn]), reads=[accs[c][1]], writes=[t_hg[bi]])
                        else:
                            k.op("dve", lambda e: e.tensor_copy(out=hg[:, c, s0:s0 + sn], in_=accs[c][0][:, 0:sn]), reads=[accs[c][1]], writes=[t_hg[bi]])
                for i in range(14):
                    if i + 1 < 14:
                        load(i + 1)
                    a = i % 2
                    w1v, w3v, w2v = views(a)
                    tw = TW[a]
                    uv = uvs[a]
                    for fc in range(2):
                        for bi, (s0, sn) in enumerate(blocks):
                            p1, t_p1 = k.ps()
                            p3, t_p3 = k.ps()
                            k.mm([tw[0], t_hg[bi]], [t_p1], [dict(out=p1[:, :sn], lhsT=w1v[:, c, fc * 128:(fc + 1) * 128], rhs=hg[:, c, s0:s0 + sn],
                                                                 start=(c == 0), stop=(c == 7)) for c in range(8)])
                            k.mm([tw[1], t_hg[bi]], [t_p3], [dict(out=p3[:, :sn], lhsT=w3v[:, c, fc * 128:(fc + 1) * 128], rhs=hg[:, c, s0:s0 + sn],
                                                                 start=(c == 0), stop=(c == 7)) for c in range(8)])
                            gb, t_g = nxt("gt", gt, t_gt)
                            k.op("act", lambda e: e.activation(out=gb[:, :sn], in_=p1[:, :sn], func=AF.Silu), reads=[t_p1], writes=[t_g])
                            k.op("dve", lambda e: e.tensor_tensor(out=uv[:, fc, s0:s0 + sn], in0=gb[:, :sn], in1=p3[:, :sn], op=ALU.mult),
                                 reads=[t_g, t_p3], writes=[TU[a][bi]])
                    for d in range(8):
                        for bi, (s0, sn) in enumerate(blocks):
                            po_, t_po_ = k.ps()
                            k.mm([tw[2], TU[a][bi]], [t_po_], [dict(out=po_[:, :sn], lhsT=w2v[:, fc, d * 128:(d + 1) * 128], rhs=uv[:, fc, s0:s0 + sn],
                                                                   start=(fc == 0), stop=(fc == 1)) for fc in range(2)])
                            if i == 0:
                                k.op("act", lambda e: e.copy(out=acc[:, d, s0:s0 + sn], in_=po_[:, :sn]), reads=[t_po_], writes=[t_acc[bi]])
                            else:
                                k.op("dve", lambda e: e.tensor_tensor(out=acc[:, d, s0:s0 + sn], in0=po_[:, :sn], in1=acc[:, d, s0:s0 + sn], op=ALU.add),
                                     reads=[t_po_], writes=[t_acc[bi]])
                for sb_ in range(nsb):
                    for half in range(2):
                        pt, t_pt = k.ps()
                        k.mm([t_acc[sb_ // spb], t_par], [t_pt],
                             [dict(out=pt[:, dd * 128:(dd + 1) * 128], lhsT=acc[:, half * 4 + dd, sb_ * 128:(sb_ + 1) * 128], rhs=ident[:, :],
                                   start=True, stop=True, is_transpose=True) for dd in range(4)])
                        if half == 0:
                            k.op("act", lambda e: e.copy(out=otm[:, sb_, 0:512], in_=pt[:, :]), reads=[t_pt], writes=[t_otm[sb_]])
                        else:
                            k.op("dve", lambda e: e.tensor_copy(out=otm[:, sb_, 512:1024], in_=pt[:, :]), reads=[t_pt], writes=[t_otm[sb_]])
                for (t0, n, v) in blks:
                    b = t0 // 512
                    cmt, t_c = nxt("t32", t32, t_t32)
                    k.op("dve", lambda e: e.tensor_scalar(out=cmt[0:16, :n], in0=CP[0:16, t0:t0 + n], scalar1=ident[0:16, e_:e_ + 1], scalar2=None, op0=ALU.mult),
                         reads=[t_cp, t_par], writes=[t_c])
                    pc_, t_pc_ = k.ps()
                    k.mm([t_c, t_one], [t_pc_], [dict(out=pc_[:, :n], lhsT=ones_f[0:16, :], rhs=cmt[0:16, :n], start=True, stop=True)])
                    cbb, t_cb = nxt("rs", rs, t_rs)
                    k.op("act", lambda e: e.copy(out=cbb[:, :n], in_=pc_[:, :n]), reads=[t_pc_], writes=[t_cb])
                    pmt, t_p = nxt("t32", t32, t_t32)
                    k.op("dve", lambda e: e.tensor_scalar(out=pmt[0:16, :n], in0=CP[0:16, t0:t0 + n], scalar1=ident[0:16, 8 + e_:9 + e_], scalar2=None, op0=ALU.mult),
                         reads=[t_cp, t_par], writes=[t_p])
                    pp_, t_pp_ = k.ps()
                    k.mm([t_p, t_one], [t_pp_], [dict(out=pp_[:, :n], lhsT=ones_f[0:16, :], rhs=pmt[0:16, :n], start=True, stop=True)])
                    for sb_ in range(nsb):
                        kk = 6 * p_ + sb_
                        k.op("dve", lambda e: e.scalar_tensor_tensor(out=STw[:, sb_, :n], in0=pp_[:, :n], scalar=slotid[:, kk:kk + 1], in1=cbb[:, :n],
                                                                    op0=ALU.is_equal, op1=ALU.mult), reads=[t_pp_, t_cb, t_mc], writes=[t_stw])
                    for d in range(8):
                        px, t_px = k.ps()
                        k.mm([t_stw] + t_otm[:nsb], [t_px], [dict(out=px[:, :n], lhsT=otm[:, sb_, d * 128:(d + 1) * 128], rhs=STw[:, sb_, :n],
                                                           start=(sb_ == 0), stop=(sb_ == nsb - 1)) for sb_ in range(nsb)])
                        k.op("dve", lambda e: e.scalar_tensor_tensor(out=xT[:, d, t0:t0 + n], in0=px[:, :n], scalar=G2[:, d, v:v + 1],
                                                                    in1=xT[:, d, t0:t0 + n], op0=ALU.mult, op1=ALU.add),
                             reads=[t_px, t_mT, t_X[b]], writes=[t_X[b]])

            npass = (ntok + NS - 1) // NS
            cnt_f = sml[:, 0:8]
            flg_f2 = TMP[:, 2304:2376]
            flg_f = flg_f2.rearrange("p (a e) -> p a e", e=8)
            flg_i = TMP[:, 2376:2448].bitcast(I32)
            t_flg = Tl("flg")
            k.op("dve", lambda e: e.tensor_copy(out=cnt_f, in_=cnt_i), reads=[t_cnt], writes=[t_sml])
            for p_ in range(3):
                k.op("dve", lambda e: e.tensor_single_scalar(out=flg_f[:, 2 * p_, :], in_=cnt_f, scalar=float(NS * p_ + 512), op=ALU.is_gt), reads=[t_sml], writes=[t_flg])
                k.op("dve", lambda e: e.tensor_single_scalar(out=flg_f[:, 2 * p_ + 1, :], in_=cnt_f, scalar=float(NS * p_), op=ALU.is_gt), reads=[t_sml], writes=[t_flg])
                k.op("dve", lambda e: e.tensor_tensor(out=flg_f[:, 2 * p_ + 1, :], in0=flg_f[:, 2 * p_ + 1, :], in1=flg_f[:, 2 * p_, :], op=ALU.subtract), writes=[t_flg])
            k.op("dve", lambda e: e.tensor_single_scalar(out=flg_f[:, 6, :], in_=cnt_f, scalar=float(NS), op=ALU.is_gt), reads=[t_sml], writes=[t_flg])
            k.op("dve", lambda e: e.tensor_single_scalar(out=flg_f[:, 7, :], in_=cnt_f, scalar=float(2 * NS), op=ALU.is_gt), reads=[t_sml], writes=[t_flg])
            k.op("dve", lambda e: e.tensor_tensor(out=flg_f[:, 8, :], in0=flg_f[:, 6, :], in1=flg_f[:, 0, :], op=ALU.subtract), writes=[t_flg])
            k.op("dve", lambda e: e.tensor_scalar(out=flg_f[:, 8, :], in0=flg_f[:, 8, :], scalar1=1.0, scalar2=None, op0=ALU.add), writes=[t_flg])
            k.op("dve", lambda e: e.tensor_copy(out=flg_i, in_=flg_f2), writes=[t_flg])
            regs = moe_regs
            variants = [([(0, 384), (384, 384)], 6), ([(0, 256), (256, 256)], 4)]

            def load_flag(row, e_):
                col = row * 8 + e_
                for r_ in regs:
                    en = {"Pool": "pool", "Activation": "act", "PE": "pe", "DVE": "dve", "SP": "sp"}[str(r_.engine).split(".")[-1]]
                    E = k.E[en]
                    k._need(E, k._deps([t_flg], []))
                    E.h.load(r_, flg_i[0:1, col:col + 1])

            def bump(deltas):
                for (en, h_, dv) in deltas:
                    k.E[en].h.sem_inc(h_, dv)

            def region(row, e_, body):
                load_flag(row, e_)
                k.region_begin()
                with nc.If_ne(regs, 0):
                    body()
                    deltas = k.region_end()
                with nc.Else():
                    bump(deltas)

            def run_pass(e_, p_):
                for vi, (blocks_v, nsb_v) in enumerate(variants):
                    region(2 * p_ + vi, e_, lambda: pass_body(e_, p_, blocks_v, nsb_v))

            def run_from(e_, p_):
                def body():
                    run_pass(e_, p_)
                    if p_ + 1 < npass:
                        run_from(e_, p_ + 1)
                region(5 + p_, e_, body)

            for e_ in range(NE):
                region(0, e_, lambda: pass_body(e_, 0, variants[0][0], variants[0][1]))

                def rest():
                    region(1, e_, lambda: pass_body(e_, 0, variants[1][0], variants[1][1]))
                    if npass > 1:
                        run_from(e_, 1)
                region(8, e_, rest)
            k.barrier()

        moe_regs = nc.alloc_registers("cnt")
        import os
        stop = int(os.environ.get("KSTOP", "99"))
        phase = 0
        for li in range(4):
            j = li // 2
            need_ctx = li < 3
            blks = BLK_L + (BLK_C if need_ctx else [])
            layer_params(li)
            if li % 2 == 0:
                make_h(A1v, SH1, blks)
                k.barrier()
                even_mixer(j, blks)
                phase += 1
                if phase >= stop:
                    break
                make_h(A2v, SH2, blks)
                k.barrier()
                ffn(ffw1[j], ffw3[j], ffw2[j], G2, blks, f"ff{j}")
                phase += 1
                if phase >= stop:
                    break
            else:
                if li == 1:
                    rope_tables()
                make_h(A1v, SH1, BLK_L + BLK_C)
                k.barrier()
                attention(j, need_ctx)
                phase += 1
                if phase >= stop:
                    break
                moe_sparse(j, blks)
                phase += 1
                if phase >= stop:
                    break

        k.barrier()
        t_out = Tl("out")
        for b in range(5):
            t0, n, _ = (BLK_L + BLK_C)[b]
            k.dma("sp", out_d[:, t0:t0 + n].rearrange("(c p) t -> p c t", p=128), xT[:, :, t0:t0 + n], reads=[t_X[b]], semt=t_out)
        k.E["sp"].h.wait_ge(t_out.dsem, t_out.dcnt)
    return nc


def _prep(inputs, b):
    f = np.float32
    x, ctx, c, c_ctx = inputs["x"], inputs["ctx"], inputs["c"], inputs["c_ctx"]
    m = {}
    m["xin"] = np.ascontiguousarray(np.concatenate([x[b], ctx[b]], axis=0).T)
    cvv = np.stack([c[b].reshape(8, 128).T, c_ctx.reshape(8, 128).T], axis=-1)
    m["cv"] = np.ascontiguousarray(cvv.reshape(128, 16))
    return m


def _shared(inputs):
    m = {}
    g = lambda a: np.ascontiguousarray(a, dtype=np.float32)
    m["ada_w"] = g(inputs["ada_w"])
    m["ada_b"] = g(inputs["ada_b"].reshape(4, 48, 128).transpose(0, 2, 1))
    m["ng1"] = g(inputs["norm_mix_g"].reshape(4, 8, 128).transpose(0, 2, 1))
    m["ng2"] = g(inputs["norm_ffn_g"].reshape(4, 8, 128).transpose(0, 2, 1))
    m["ev_w_in"] = g(inputs["ev_w_in"])
    m["ev_ln_g"] = g(inputs["ev_ln_g"].reshape(2, 8, 128).transpose(0, 2, 1))
    m["ev_ln_b"] = g(inputs["ev_ln_b"].reshape(2, 8, 128).transpose(0, 2, 1))
    m["ev_wsT"] = g(inputs["ev_ws"].transpose(0, 1, 3, 2))
    m["ev_bs"] = g(inputs["ev_bs"].reshape(2, 1024))
    m["ev_conv_w"] = g(inputs["ev_conv_w"].transpose(0, 2, 1).reshape(2, 8, 128, 31).transpose(0, 2, 1, 3).reshape(2, 128, 248))
    m["ev_conv_b"] = g(inputs["ev_conv_b"].reshape(2, 8, 128).transpose(0, 2, 1))
    m["ev_cnorm_g"] = g(inputs["ev_cnorm_g"].reshape(2, 8, 128).transpose(0, 2, 1))
    m["ev_w_out"] = g(inputs["ev_w_out"])
    m["od_w_qkv"] = g(inputs["od_w_qkv"])
    m["od_q_g"] = g(np.concatenate([inputs["od_q_g"], inputs["od_q_g"]], axis=1).reshape(2, 128, 1))
    m["od_k_g"] = g(np.concatenate([inputs["od_k_g"], inputs["od_k_g"]], axis=1).reshape(2, 128, 1))
    m["od_sink"] = g(inputs["od_sink"])
    m["od_w_o"] = g(inputs["od_w_o"])
    m["ff_w1"] = g(inputs["ff_w1"])
    m["ff_w3"] = g(inputs["ff_w3"])
    m["ff_w2"] = g(inputs["ff_w2"])
    m["moe_router"] = g(inputs["moe_router"])
    m["moe_w1"] = g(inputs["moe_w1"])
    m["moe_w3"] = g(inputs["moe_w3"])
    m["moe_w2"] = g(inputs["moe_w2"])
    m["c_ident"] = np.eye(128, dtype=np.float32)
    pm = np.zeros((64, 64), np.float32)
    for p in range(64):
        r = p % 32
        if r < 16:
            pm[p + 16, p] = -1.0
        else:
            pm[p - 16, p] = 1.0
    pm2 = np.zeros((128, 128), np.float32)
    pm2[0:64, 0:64] = pm
    pm2[64:128, 64:128] = pm
    m["c_perm"] = pm2
    bd = np.zeros((128, 128), np.float32)
    bd[0:64, 0:64] = 1.0
    bd[64:128, 64:128] = 1.0
    m["c_bd"] = bd
    kk = np.arange(128)[:, None]
    qq = np.arange(384)[None, :] - 128
    m["c_mask"] = np.where(np.abs(qq - kk) <= 128, 0.0, -240000.0).astype(np.float32)
    t = np.arange(2048)
    pos = np.zeros((64, 2048), np.float32)
    pos[0:32, :] = (t // 64)[None, :]
    pos[32:64, :] = (t % 64)[None, :]
    m["c_pos"] = np.concatenate([pos, pos], axis=0)
    m["c_fidx"] = (np.arange(128) % 16).astype(np.float32).reshape(128, 1)
    m["c_triu"] = (np.arange(128)[:, None] < np.arange(128)[None, :]).astype(np.float32)
    m["c_iota"] = np.tile(np.arange(384, dtype=np.float32)[None, :], (128, 1))
    m["c_slotid"] = (np.arange(128, dtype=np.float32)[:, None] + 128.0 * np.arange(18, dtype=np.float32)[None, :])
    return m


_NC_CACHE = {}


def kernel(**inputs):
    inputs = {k_: np.asarray(v) for k_, v in inputs.items()}
    ncores = 8
    if "nc" not in _NC_CACHE:
        _NC_CACHE["nc"] = build()
    nc = _NC_CACHE["nc"]
    shared = _shared(inputs)
    in_maps = []
    for b in range(ncores):
        m = dict(shared)
        m.update(_prep(inputs, b))
        in_maps.append(m)
    res = run_bass_kernel_spmd(nc, in_maps, core_ids=list(range(ncores)))
    outs = [np.asarray(r["out"]) for r in res.results]
    return np.stack([o[:, :NL].T for o in outs], axis=0).astype(np.float32)
```

```python
import numpy as np
import concourse.bass as bass
import concourse.mybir as mybir
from concourse.bass_utils import run_bass_kernel_spmd
from contextlib import ExitStack

F32 = mybir.dt.float32
BF16 = mybir.dt.bfloat16
AF = mybir.ActivationFunctionType
ALU = mybir.AluOpType
AX = mybir.AxisListType

NL, NCX, NT = 2048, 256, 2304
D, DFF, NE = 1024, 3584, 8
EPS = 1e-6
BLK_L = [(0, 512, 0), (512, 512, 0), (1024, 512, 0), (1536, 512, 0)]
BLK_C = [(2048, 256, 1)]
SEMLIM = 10 ** 9


class Tl:
    __slots__ = ("name", "w", "r", "dsem", "dcnt")

    def __init__(s, name):
        s.name = name
        s.w = None
        s.r = {}
        s.dsem = None
        s.dcnt = 0


class Eng:
    def __init__(s, name, h):
        s.name, s.h, s.sem, s.cnt, s.waited = name, h, None, 0, {}


class K:
    def __init__(s, nc, es):
        s.nc, s.es = nc, es
        s.E = {"pe": Eng("pe", nc.tensor), "act": Eng("act", nc.scalar), "dve": Eng("dve", nc.vector),
               "pool": Eng("pool", nc.gpsimd), "sp": Eng("sp", nc.sync)}
        s.nsem = 0
        for e in s.E.values():
            e.sem = s.newsem(e.name)
        s.dsems = {}
        s.psum = []
        s.psi = 0
        s._snaps = []
        s.npool = 8

    def newsem(s, name):
        s.nsem += 1
        return s.es.enter_context(s.nc.semaphore(f"{name}_{s.nsem}"))

    def _need(s, E, deps, embed=False):
        todo = []
        for num, (h, v) in deps.items():
            if E.waited.get(num, 0) < v:
                todo.append((h, v))
                E.waited[num] = v
        last = None
        if embed and todo:
            last = todo.pop()
        for (h, v) in todo:
            E.h.wait_ge(h, v)
        return last

    @staticmethod
    def _deps(reads, writes):
        d = {}

        def add(rec):
            h, v = rec
            if h.num not in d or d[h.num][1] < v:
                d[h.num] = rec
        for t in reads:
            if t.w:
                add(t.w)
        for t in writes:
            if t.w:
                add(t.w)
            for rec in t.r.values():
                add(rec)
        return d

    @staticmethod
    def _mark(rec, reads, writes):
        for t in reads:
            t.r[rec[0].num] = rec
        for t in writes:
            t.w = rec
            t.r = {}

    def _rot(s, E):
        if E.cnt >= SEMLIM:
            E.sem = s.newsem(E.name)
            E.cnt = 0

    def op(s, en, fn, reads=(), writes=()):
        E = s.E[en]
        last = s._need(E, s._deps(reads, writes), embed=True)
        ins = fn(E.h)
        if last is not None:
            ins._wait_ge(last[0], last[1])
        E.cnt += 1
        ins.then_inc(E.sem, 1)
        s._mark((E.sem, E.cnt), reads, writes)
        s._rot(E)

    def mm(s, reads, writes, items):
        E = s.E["pe"]
        last = s._need(E, s._deps(reads, writes), embed=True)
        ins = None
        for it in items:
            ins = E.h.matmul(**it)
            if last is not None:
                ins._wait_ge(last[0], last[1])
                last = None
        E.cnt += 1
        ins.then_inc(E.sem, 1)
        s._mark((E.sem, E.cnt), reads, writes)
        s._rot(E)

    def dma(s, q, out, in_, reads=(), writes=(), semt=None):
        E = s.E[q]
        s._need(E, s._deps(reads, writes))
        ins = E.h.dma_start(out=out, in_=in_)
        t = semt if semt is not None else writes[0]
        if t.dsem is None:
            t.dsem = s.newsem("d" + t.name)
        t.dcnt += 16
        ins.then_inc(t.dsem, 16)
        s.dsems[t.dsem.num] = (t.dsem, t.dcnt)
        s._mark((t.dsem, t.dcnt), reads, writes)

    def barrier(s, engines=("pe", "act", "dve", "pool", "sp"), own=False):
        for en in engines:
            E = s.E[en]
            d = {}
            for F in s.E.values():
                if (own or F is not E) and F.cnt > 0:
                    d[F.sem.num] = (F.sem, F.cnt)
            d.update(s.dsems)
            s._need(E, d)

    def region_begin(s):
        for n, E in s.E.items():
            d = {}
            if E.cnt > 0:
                d[E.sem.num] = (E.sem, E.cnt)
            if n == "sp":
                d.update(s.dsems)
            s._need(E, d)
        s._snaps.append(({n: (E.sem, E.cnt) for n, E in s.E.items()}, dict(s.dsems), {n: dict(E.waited) for n, E in s.E.items()}))

    def region_end(s):
        e0, d0, w0 = s._snaps.pop()
        deltas = []
        for n, E in s.E.items():
            assert E.sem.num == e0[n][0].num, "semaphore rotated inside region"
            if E.cnt > e0[n][1]:
                deltas.append((n, E.sem, E.cnt - e0[n][1]))
        for num, (h, v) in s.dsems.items():
            v0 = d0[num][1] if num in d0 else 0
            if v > v0:
                deltas.append(("sp", h, v - v0))
        for n, E in s.E.items():
            E.waited = w0[n]
        return deltas

    def ps(s):
        p = s.psum[s.psi % s.npool]
        s.psi += 1
        return p


def build():
    nc = bass.Bass("TRN2", target_bir_lowering=False)

    def din(name, shape):
        return nc.dram_tensor(name, list(shape), F32, kind="ExternalInput").ap()

    xin = din("xin", [1024, NT])
    cv_d = din("cv", [128, 16])
    ada_w = din("ada_w", [4, 1024, 6144])
    ada_b = din("ada_b", [4, 128, 48])
    ng1_d = din("ng1", [4, 128, 8])
    ng2_d = din("ng2", [4, 128, 8])
    w_in = din("ev_w_in", [2, 1024, 4096])
    lngf_d = din("ev_ln_g", [2, 128, 8])
    lnbf_d = din("ev_ln_b", [2, 128, 8])
    wsT_d = din("ev_wsT", [2, 8, 128, 128])
    bs_d = din("ev_bs", [2, 1024])
    convw_d = din("ev_conv_w", [2, 128, 248])
    convb_d = din("ev_conv_b", [2, 128, 8])
    cng_d = din("ev_cnorm_g", [2, 128, 8])
    w_out = din("ev_w_out", [2, 2048, 1024])
    wqkv = din("od_w_qkv", [2, 1024, 1536])
    qg_d = din("od_q_g", [2, 128, 1])
    kg_d = din("od_k_g", [2, 128, 1])
    sink_d = din("od_sink", [2, 16])
    wo_d = din("od_w_o", [2, 1024, 1024])
    ffw1 = din("ff_w1", [2, 1024, DFF])
    ffw3 = din("ff_w3", [2, 1024, DFF])
    ffw2 = din("ff_w2", [2, DFF, 1024])
    router_d = din("moe_router", [2, 1024, 8])
    mw1 = din("moe_w1", [2, 8, 1024, DFF])
    mw3 = din("moe_w3", [2, 8, 1024, DFF])
    mw2 = din("moe_w2", [2, 8, DFF, 1024])
    ident_d = din("c_ident", [128, 128])
    perm_d = din("c_perm", [128, 128])
    bd_d = din("c_bd", [128, 128])
    mask_d = din("c_mask", [128, 384])
    pos_d = din("c_pos", [128, 2048])
    fidx_d = din("c_fidx", [128, 1])
    triu_d = din("c_triu", [128, 128])
    iota_d = din("c_iota", [128, 384])
    slotid_d = din("c_slotid", [128, 18])
    h_tm_d = nc.dram_tensor("h_tm_scratch", [18, 128, 1024], BF16, kind="Internal").ap()
    out_d = nc.dram_tensor("out", [1024, NT], F32, kind="ExternalOutput").ap()

    with ExitStack() as es:
        k = K(nc, es)

        def sb(name, shape, dt):
            return es.enter_context(nc.sbuf_tensor("sb_" + name, list(shape), dt))

        RX = sb("RX", [128, 8 * NT], F32)
        RH = sb("RH", [128, 8 * NT], BF16)
        RA = sb("RA", [128, 8 * NT], BF16)
        RW = sb("RW", [128, 12288], BF16)
        xT = RX[:, :].rearrange("p (c t) -> p c t", c=8)
        hT = RH[:, :].rearrange("p (c t) -> p c t", c=8)
        for i in range(8):
            k.psum.append((es.enter_context(nc.psum_tensor(f"ps{i}", [128, 512], F32)), Tl(f"ps{i}")))

        ident = sb("ident", [128, 128], F32)
        ones_bf = sb("ones_bf", [128, 128], BF16)
        ones_f = sb("ones_f", [16, 128], F32)
        triu_bf = sb("triu_bf", [128, 128], BF16)
        ident_bf = sb("ident_bf", [128, 128], BF16)
        perm_bf = sb("perm_bf", [128, 128], BF16)
        bd_bf = sb("bd_bf", [128, 128], BF16)
        mask_bf = sb("mask_bf", [128, 384], BF16)
        cv = sb("cv", [128, 16], F32)
        scv = sb("scv", [128, 16], BF16)
        adab = sb("adab", [128, 48], F32)
        mT = sb("mT", [128, 96], F32)
        mT3 = mT[:, :].rearrange("p (j v) -> p j v", v=2)
        A1 = sb("A1", [128, 16], F32)
        A2 = sb("A2", [128, 16], F32)
        A1v = A1[:, :].rearrange("p (c v) -> p c v", v=2)
        A2v = A2[:, :].rearrange("p (c v) -> p c v", v=2)
        ng1 = sb("ng1", [128, 8], F32)
        ng2 = sb("ng2", [128, 8], F32)
        tmp16 = sb("tmp16", [128, 16], F32)
        convw = sb("convw", [128, 248], F32)
        convb = sb("convb", [128, 8], F32)
        cng = sb("cng", [128, 8], F32)
        qg = sb("qg", [128, 1], F32)
        kg = sb("kg", [128, 1], F32)
        sinke = sb("sinke", [128, 16], F32)
        router_f = sb("router_f", [128, 64], F32)
        cossin = sb("cossin", [128, 2 * 2048], BF16)
        cosT = cossin[:, 0:2048]
        sinT = cossin[:, 2048:4096]
        sq = [sb(f"sq{i}", [128, 512], BF16) for i in range(2)]
        rs = [sb(f"rs{i}", [128, 512], F32) for i in range(2)]
        t32 = [sb(f"t32_{i}", [128, 512], F32) for i in range(2)]
        gt = [sb(f"gt{i}", [128, 512], BF16) for i in range(2)]
        tt = [sb(f"tt{i}", [128, 512], BF16) for i in range(2)]
        TMP = sb("TMP", [128, 2560], F32)
        sml = sb("sml", [128, 64], F32)

        t_sq = [Tl(f"sq{i}") for i in range(2)]
        t_rs = [Tl(f"rs{i}") for i in range(2)]
        t_t32 = [Tl(f"t32{i}") for i in range(2)]
        t_gt = [Tl(f"gt{i}") for i in range(2)]
        t_tt = [Tl(f"tt{i}") for i in range(2)]
        t_par = Tl("par")
        t_lpar = Tl("lpar")
        t_mT = Tl("mT")
        t_X = [Tl(f"X{i}") for i in range(5)]
        t_H = [Tl(f"H{i}") for i in range(5)]
        TW = [[Tl(f"w{a}_{i}") for i in range(3)] for a in range(2)]
        t_p2 = Tl("m2p")
        TU = [[Tl(f"u{a}_{b}") for b in range(5)] for a in range(2)]
        rot = {"sq": 0, "rs": 0, "t32": 0, "gt": 0, "tt": 0}

        def nxt(name, arr, tls):
            i = rot[name] % len(arr)
            rot[name] += 1
            return arr[i], tls[i]

        k.dma("sp", ident[:, :], ident_d[:, :], writes=[t_par])
        k.dma("sp", cv[:, :], cv_d[:, :], writes=[t_par])
        k.dma("pool", perm_bf[:, :], perm_d[:, :], writes=[t_par])
        k.dma("pool", bd_bf[:, :], bd_d[:, :], writes=[t_par])
        k.dma("pool", mask_bf[:, :], mask_d[:, :], writes=[t_par])
        k.dma("pool", triu_bf[:, :], triu_d[:, :], writes=[t_par])
        k.dma("pool", ident_bf[:, :], ident_d[:, :], writes=[t_par])
        t_one = Tl("ones")
        k.op("dve", lambda e: e.memset(ones_bf[:, :], 1.0), writes=[t_one])
        k.op("dve", lambda e: e.memset(ones_f[:, :], 1.0), writes=[t_one])
        for b in range(5):
            t0, n, _ = (BLK_L + BLK_C)[b]
            k.dma("sp", xT[:, :, t0:t0 + n], xin[:, t0:t0 + n].rearrange("(c p) t -> p c t", p=128), writes=[t_X[b]])
        k.op("act", lambda e: e.activation(out=scv[:, :], in_=cv[:, :], func=AF.Silu), reads=[t_par], writes=[t_one])
        scv3 = scv[:, :].rearrange("p (c v) -> p c v", v=2)

        W0 = RW[:, 0:6144]
        W1s = RW[:, 6144:12288]
        slots = [W0, W1s]

        def layer_params(li):
            t_w = [TW[0][0], TW[1][0]]
            k.dma("sp", adab[:, :], ada_b[li], writes=[t_lpar])
            k.dma("sp", ng1[:, :], ng1_d[li], writes=[t_lpar])
            k.dma("sp", ng2[:, :], ng2_d[li], writes=[t_lpar])
            psm, t_psm = k.ps()
            src = ada_w[li].rearrange("(c p) n -> p c n", p=128)
            for i in range(12):
                wv = slots[i % 2][:, 0:4096].rearrange("p (c n) -> p c n", c=8)
                k.dma("pool", wv, src[:, :, i * 512:(i + 1) * 512], writes=[t_w[i % 2]])
                for jj in range(4):
                    j = 4 * i + jj
                    k.mm([t_w[i % 2], t_one], [t_psm],
                         [dict(out=psm[:, 2 * j:2 * j + 2], lhsT=wv[:, c, jj * 128:(jj + 1) * 128], rhs=scv3[:, c, :],
                               start=(c == 0), stop=(c == 7)) for c in range(8)])
            ps3 = psm[:, 0:96].rearrange("p (j v) -> p j v", v=2)
            for v in range(2):
                k.op("dve", lambda e, v=v: e.tensor_tensor(out=mT3[:, :, v], in0=ps3[:, :, v], in1=adab[:, :], op=ALU.add),
                     reads=[t_psm, t_lpar], writes=[t_mT])
            t16 = tmp16[:, :].rearrange("p (c v) -> p c v", v=2)
            for (Av, ng, off) in ((A1v, ng1, 8), (A2v, ng2, 32)):
                k.op("dve", lambda e, off=off: e.tensor_scalar(out=t16, in0=mT3[:, off:off + 8, :], scalar1=1.0, scalar2=None, op0=ALU.add),
                     reads=[t_mT], writes=[t_lpar])
                for v in range(2):
                    k.op("dve", lambda e, v=v, Av=Av, ng=ng: e.tensor_tensor(out=Av[:, :, v], in0=t16[:, :, v], in1=ng[:, :], op=ALU.mult),
                         reads=[t_lpar], writes=[t_lpar])
            k.barrier()

        SH1, G1, SH2, G2 = mT3[:, 0:8, :], mT3[:, 16:24, :], mT3[:, 24:32, :], mT3[:, 40:48, :]

        def make_h(Av, SHv, blks, moe_j=None, combT=None, t_comb=None):
            for (t0, n, v) in blks:
                b = t0 // 512
                rsb, t_r = nxt("rs", rs, t_rs)
                psm, t_ps = k.ps()
                for c in range(8):
                    sqb, t_s = nxt("sq", sq, t_sq)
                    k.op("act", lambda e, c=c, sqb=sqb: e.activation(out=sqb[:, :n], in_=xT[:, c, t0:t0 + n], func=AF.Square),
                         reads=[t_X[b]], writes=[t_s])
                    k.mm([t_s, t_one], [t_ps], [dict(out=psm[:, :n], lhsT=ones_bf[:, :], rhs=sqb[:, :n], start=(c == 0), stop=(c == 7))])
                k.op("act", lambda e: e.activation(out=rsb[:, :n], in_=psm[:, :n], func=AF.Ln, scale=1.0 / D, bias=EPS),
                     reads=[t_ps], writes=[t_r])
                k.op("act", lambda e: e.activation(out=rsb[:, :n], in_=rsb[:, :n], func=AF.Exp, scale=-0.5), reads=[t_r], writes=[t_r])
                if moe_j is not None:
                    pslg, t_pslg = k.ps()
                    nsb = n // 128
                    k.mm([t_one], [t_pslg], [dict(out=pslg[:, 0:8 * nsb], lhsT=zer_bf[:, :], rhs=zer_bf[:, 0:8 * nsb], start=True, stop=False,
                                                skip_group_check=True)])
                for c in range(8):
                    tb, t_t = nxt("t32", t32, t_t32)
                    k.op("dve", lambda e, c=c, tb=tb: e.scalar_tensor_tensor(out=tb[:, :n], in0=xT[:, c, t0:t0 + n], scalar=Av[:, c, v:v + 1],
                                                                           in1=rsb[:, :n], op0=ALU.mult, op1=ALU.mult),
                         reads=[t_X[b], t_r, t_lpar], writes=[t_t])
                    if moe_j is None:
                        k.op("act", lambda e, c=c, tb=tb: e.activation(out=hT[:, c, t0:t0 + n], in_=tb[:, :n], func=AF.Identity,
                                                                     bias=SHv[:, c, v:v + 1]),
                             reads=[t_t, t_mT], writes=[t_H[b]])
                    else:
                        k.op("act", lambda e, c=c, tb=tb: e.activation(out=tb[:, :n], in_=tb[:, :n], func=AF.Identity,
                                                                     bias=SHv[:, c, v:v + 1]),
                             reads=[t_mT], writes=[t_t])
                        k.op("act", lambda e, c=c, tb=tb: e.copy(out=hT[:, c, t0:t0 + n], in_=tb[:, :n]),
                             reads=[t_t], writes=[t_H[b]])
                        k.mm([t_t, t_lpar], [t_pslg],
                             [dict(out=pslg[:, 8 * s_:8 * s_ + 8], lhsT=tb[:, s_ * 128:(s_ + 1) * 128], rhs=router_f[:, 8 * c:8 * c + 8],
                                   start=False, stop=(c == 7), skip_group_check=True) for s_ in range(nsb)])
                if moe_j is not None:
                    for s_ in range(nsb):
                        lg = sml[:, 0:8]
                        mx = sml[:, 8:16]
                        ex = sml[:, 16:24]
                        msk = sml[:, 24:32]
                        nm1 = sml[:, 32:33]
                        den = sml[:, 33:34]
                        k.op("dve", lambda e: e.tensor_copy(out=lg, in_=pslg[:, 8 * s_:8 * s_ + 8]), reads=[t_pslg], writes=[t_sml])
                        k.op("dve", lambda e: e.max(out=mx, in_=lg), reads=[t_sml], writes=[t_sml])
                        k.op("dve", lambda e: e.tensor_scalar(out=nm1, in0=mx[:, 0:1], scalar1=-1.0, scalar2=None, op0=ALU.mult),
                             reads=[t_sml], writes=[t_sml])
                        k.op("act", lambda e: e.activation(out=ex, in_=lg, func=AF.Exp, bias=nm1), reads=[t_sml], writes=[t_sml])
                        k.op("dve", lambda e: e.tensor_scalar(out=msk, in0=lg, scalar1=mx[:, 1:2], scalar2=None, op0=ALU.is_ge),
                             reads=[t_sml], writes=[t_sml])
                        k.op("dve", lambda e: e.tensor_tensor(out=ex, in0=ex, in1=msk, op=ALU.mult), reads=[t_sml], writes=[t_sml])
                        k.op("dve", lambda e: e.reduce_sum(out=den, in_=ex, axis=AX.X), reads=[t_sml], writes=[t_sml])
                        k.op("dve", lambda e: e.reciprocal(out=den, in_=den), reads=[t_sml], writes=[t_sml])
                        k.op("dve", lambda e: e.tensor_scalar(out=ex, in0=ex, scalar1=den, scalar2=None, op0=ALU.mult),
                             reads=[t_sml], writes=[t_sml])
                        tbi = t0 // 128 + s_
                        k.op("dve", lambda e: e.tensor_copy(out=moe_j["comb_tm"][:, tbi, :], in_=ex), reads=[t_sml], writes=[moe_j["t_rt"]])
                        k.op("dve", lambda e: e.tensor_copy(out=moe_j["mask_tm"][:, tbi, :], in_=msk), reads=[t_sml], writes=[moe_j["t_rt"]])

        zer_bf = sb("zer_bf", [128, 128], BF16)
        k.op("dve", lambda e: e.memset(zer_bf[:, :], 0.0), writes=[t_one])
        t_sml = Tl("sml")

        def ffn(W1d, W3d, W2d, Gv, blks, tag, cb=None, t_cb=None, bar=True):
            NP = 14
            t_w = TW
            t_u = TU
            w1s = W1d.rearrange("(c p) n -> p c n", p=128)
            w3s = W3d.rearrange("(c p) n -> p c n", p=128)
            w2s = W2d.rearrange("(f p) n -> p f n", p=128)

            def views(s_):
                sl = slots[s_]
                return (sl[:, 0:2048].rearrange("p (c n) -> p c n", c=8), sl[:, 2048:4096].rearrange("p (c n) -> p c n", c=8),
                        sl[:, 4096:6144].rearrange("p (f n) -> p f n", f=2))

            def load(i):
                a, b_, c_ = views(i % 2)
                tw = t_w[i % 2]
                k.dma("pool", a, w1s[:, :, i * 256:(i + 1) * 256], writes=[tw[0]])
                k.dma("pool", b_, w3s[:, :, i * 256:(i + 1) * 256], writes=[tw[1]])
                k.dma("pool", c_, w2s[:, 2 * i:2 * i + 2, :], writes=[tw[2]])

            load(0)
            for i in range(NP):
                if i + 1 < NP:
                    load(i + 1)
                s_ = i % 2
                w1v, w3v, w2v = views(s_)
                tw = t_w[s_]
                uv = RA[:, s_ * 2 * NT:(s_ + 1) * 2 * NT].rearrange("p (f t) -> p f t", f=2)
                for fc in range(2):
                    for (t0, n, v) in blks:
                        b = t0 // 512
                        p1, t_p1 = k.ps()
                        p3, t_p3 = k.ps()
                        k.mm([tw[0], t_H[b]], [t_p1], [dict(out=p1[:, :n], lhsT=w1v[:, c, fc * 128:(fc + 1) * 128], rhs=hT[:, c, t0:t0 + n],
                                                          start=(c == 0), stop=(c == 7)) for c in range(8)])
                        k.mm([tw[1], t_H[b]], [t_p3], [dict(out=p3[:, :n], lhsT=w3v[:, c, fc * 128:(fc + 1) * 128], rhs=hT[:, c, t0:t0 + n],
                                                          start=(c == 0), stop=(c == 7)) for c in range(8)])
                        gb, t_g = nxt("gt", gt, t_gt)
                        k.op("act", lambda e: e.activation(out=gb[:, :n], in_=p1[:, :n], func=AF.Silu), reads=[t_p1], writes=[t_g])
                        if cb is None:
                            k.op("dve", lambda e: e.tensor_tensor(out=uv[:, fc, t0:t0 + n], in0=gb[:, :n], in1=p3[:, :n], op=ALU.mult),
                                 reads=[t_g, t_p3], writes=[t_u[s_][b]])
                        else:
                            tb_, t_t = nxt("tt", tt, t_tt)
                            k.op("dve", lambda e: e.tensor_tensor(out=tb_[:, :n], in0=gb[:, :n], in1=p3[:, :n], op=ALU.mult),
                                 reads=[t_g, t_p3], writes=[t_t])
                            k.op("pool", lambda e: e.tensor_tensor(out=uv[:, fc, t0:t0 + n], in0=tb_[:, :n], in1=cb[:, t0:t0 + n], op=ALU.mult),
                                 reads=[t_t, t_cb], writes=[t_u[s_][b]])
                for d in range(8):
                    for (t0, n, v) in blks:
                        b = t0 // 512
                        po, t_po = k.ps()
                        k.mm([tw[2], t_u[s_][b]], [t_po], [dict(out=po[:, :n], lhsT=w2v[:, fc, d * 128:(d + 1) * 128], rhs=uv[:, fc, t0:t0 + n],
                                                              start=(fc == 0), stop=(fc == 1)) for fc in range(2)])
                        k.op("dve", lambda e: e.scalar_tensor_tensor(out=xT[:, d, t0:t0 + n], in0=po[:, :n], scalar=Gv[:, d, v:v + 1],
                                                                    in1=xT[:, d, t0:t0 + n], op0=ALU.mult, op1=ALU.add),
                             reads=[t_po, t_mT, t_X[b]], writes=[t_X[b]])
            if bar:
                k.barrier()

        def proj_out(Wd_rows, yv, t_y, Gv, blks, tag):
            t_w = [TW[0][0], TW[1][0]]
            ws = Wd_rows.rearrange("(c p) n -> p c n", p=128)
            wv = [slots[i][:, 0:4096].rearrange("p (c n) -> p c n", c=4) for i in range(2)]
            for i in range(2):
                k.dma("pool", wv[i], ws[:, 4 * i:4 * i + 4, :], writes=[t_w[i]])
            for d in range(8):
                for (t0, n, v) in blks:
                    b = t0 // 512
                    po, t_po = k.ps()
                    k.mm([t_w[0], t_w[1], t_y[b]], [t_po],
                         [dict(out=po[:, :n], lhsT=wv[c // 4][:, c % 4, d * 128:(d + 1) * 128], rhs=yv[:, c, t0:t0 + n],
                               start=(c == 0), stop=(c == 7)) for c in range(8)])
                    k.op("dve", lambda e: e.scalar_tensor_tensor(out=xT[:, d, t0:t0 + n], in0=po[:, :n], scalar=Gv[:, d, v:v + 1],
                                                                in1=xT[:, d, t0:t0 + n], op0=ALU.mult, op1=ALU.add),
                         reads=[t_po, t_mT, t_X[b]], writes=[t_X[b]])
            k.barrier()

        def even_mixer(j, blks):
            yv = RA[:, :].rearrange("p (c t) -> p c t", c=8)
            t_y = [Tl(f"ya{j}_{b}") for b in range(5)]
            win = w_in[j].rearrange("(c p) n -> p c n", p=128)
            t_w = [TW[0][0], TW[1][0]]

            def wv1(s_):
                return slots[s_][:, 0:2048].rearrange("p (c n) -> p c n", c=8)
            k.dma("pool", wv1(0), win[:, :, 0:256], writes=[t_w[0]])
            for i in range(4):
                if i + 1 < 4:
                    k.dma("pool", wv1((i + 1) % 2), win[:, :, (i + 1) * 256:(i + 2) * 256], writes=[t_w[(i + 1) % 2]])
                for fc in range(2):
                    jc = 2 * i + fc
                    for (t0, n, v) in blks:
                        b = t0 // 512
                        p1, t_p1 = k.ps()
                        k.mm([t_w[i % 2], t_H[b]], [t_p1], [dict(out=p1[:, :n], lhsT=wv1(i % 2)[:, c, fc * 128:(fc + 1) * 128], rhs=hT[:, c, t0:t0 + n],
                                                              start=(c == 0), stop=(c == 7)) for c in range(8)])
                        k.op("act", lambda e: e.activation(out=yv[:, jc, t0:t0 + n], in_=p1[:, :n], func=AF.Gelu_apprx_tanh),
                             reads=[t_p1], writes=[t_y[b]])
            k.barrier()
            t_wv = [TW[0][0], TW[1][0]]
            wvv = [slots[i][:, 0:4096].rearrange("p (c n) -> p c n", c=8) for i in range(2)]
            for i in range(2):
                k.dma("pool", wvv[i], win[:, :, 1024 + i * 512:1024 + (i + 1) * 512], writes=[t_wv[i]])
            vgs = [slots[0][:, 4096:6144].bitcast(F32), slots[1][:, 4096:6144].bitcast(F32)]
            T2 = TMP[:, 0:1024]
            wsTv = TMP[:, 1024:1536].bitcast(BF16).rearrange("p (g q) -> p g q", g=8)
            bsb = TMP[:, 1536:2560]
            lgf = sml[:, 32:40]
            lbf = sml[:, 40:48]
            k.dma("sp", lgf, lngf_d[j], writes=[t_p2])
            k.dma("sp", lbf, lnbf_d[j], writes=[t_p2])
            k.dma("sp", bsb, bs_d[j:j + 1, :].partition_broadcast(128), writes=[t_p2])
            k.dma("pool", wsTv, wsT_d[j].rearrange("g q p -> q g p"), writes=[t_p2])
            t_T2 = Tl("T2")
            for gb_ in range(2):
                pw_, t_pw_ = k.ps()
                for gg in range(4):
                    g_ = gb_ * 4 + gg
                    k.mm([t_p2, t_one], [t_pw_], [dict(out=pw_[:, gg * 128:(gg + 1) * 128], lhsT=ones_bf[:, :], rhs=wsTv[:, g_, :], start=True, stop=True)])
                for gg in range(4):
                    g_ = gb_ * 4 + gg
                    k.op("dve", lambda e: e.scalar_tensor_tensor(out=T2[:, g_ * 128:(g_ + 1) * 128], in0=pw_[:, gg * 128:(gg + 1) * 128], scalar=lbf[:, g_:g_ + 1],
                                                                in1=bsb[:, g_ * 128:(g_ + 1) * 128], op0=ALU.mult, op1=ALU.add), reads=[t_pw_, t_p2], writes=[t_T2])
            t_vgs = [Tl("vg0"), Tl("vg1")]
            t_st = [Tl("st0"), Tl("st1")]
            vb2 = [t32[0][:, :].bitcast(BF16), t32[1][:, :].bitcast(BF16)]
            ntb = [tb for (t0, n, v) in blks for tb in range(t0 // 128, (t0 + n) // 128)]
            for it_, tb in enumerate(ntb):
                b = min(tb // 4, 4)
                tk = tb * 128
                par = it_ % 2
                vg, t_vg = vgs[par], t_vgs[par]
                pv = []
                for h_ in range(2):
                    p_, t_p = k.ps()
                    k.mm([t_wv[h_], t_H[b]], [t_p], [dict(out=p_[:, :], lhsT=hT[:, c, tk:tk + 128], rhs=wvv[h_][:, c, :],
                                                        start=(c == 0), stop=(c == 7)) for c in range(8)])
                    pv.append((p_, t_p))
                for h_ in range(2):
                    k.op("act", lambda e, h_=h_: e.activation(out=vg[:, h_ * 512:(h_ + 1) * 512], in_=pv[h_][0][:, :], func=AF.Gelu_apprx_tanh),
                         reads=[pv[h_][1]], writes=[t_vg])
                so = par * 16
                st = sml[:, so:so + 12].rearrange("p (a b) -> p a b", a=2)
                mv = sml[:, so + 12:so + 14]
                rstd = sml[:, so + 14:so + 15]
                nmr = sml[:, so + 15:so + 16]
                t_s_ = t_st[par]
                for h_ in range(2):
                    k.op("dve", lambda e, h_=h_: e.bn_stats(out=st[:, h_, :], in_=vg[:, h_ * 512:(h_ + 1) * 512]), reads=[t_vg], writes=[t_s_])
                k.op("dve", lambda e: e.bn_aggr(out=mv, in_=sml[:, so:so + 12]), reads=[t_s_], writes=[t_s_])
                k.op("act", lambda e: e.activation(out=rstd, in_=mv[:, 1:2], func=AF.Sqrt, bias=EPS), reads=[t_s_], writes=[t_s_])
                k.op("dve", lambda e: e.reciprocal(out=rstd, in_=rstd), reads=[t_s_], writes=[t_s_])
                k.op("dve", lambda e: e.scalar_tensor_tensor(out=nmr, in0=mv[:, 0:1], scalar=-1.0, in1=rstd, op0=ALU.mult, op1=ALU.mult),
                     reads=[t_s_], writes=[t_s_])
                vbf = vb2[par]
                t_v = t_t32[par]
                k.op("act", lambda e: e.activation(out=vbf, in_=vg, func=AF.Identity, scale=rstd, bias=nmr), reads=[t_s_, t_vg], writes=[t_v])
                for gb_ in range(2):
                    pg, t_pg = k.ps()
                    for gg in range(4):
                        g_ = gb_ * 4 + gg
                        k.mm([t_v, t_p2], [t_pg], [dict(out=pg[:, gg * 128:(gg + 1) * 128], lhsT=vbf[:, g_ * 128:(g_ + 1) * 128], rhs=wsTv[:, g_, :],
                                                      start=True, stop=True)])
                    tb_, t_t = nxt("rs", rs, t_rs)
                    for gg in range(4):
                        g_ = gb_ * 4 + gg
                        k.op("dve", lambda e: e.scalar_tensor_tensor(out=tb_[:, gg * 128:(gg + 1) * 128], in0=pg[:, gg * 128:(gg + 1) * 128], scalar=lgf[:, g_:g_ + 1],
                                                                    in1=T2[:, g_ * 128:(g_ + 1) * 128], op0=ALU.mult, op1=ALU.add),
                             reads=[t_pg, t_p2, t_T2], writes=[t_t])
                    yslice = yv[:, gb_ * 4:gb_ * 4 + 4, tk:tk + 128]
                    k.op("pool", lambda e: e.tensor_tensor(out=yslice, in0=yslice, in1=tb_[:, :].rearrange("p (g q) -> p g q", g=4), op=ALU.mult),
                         reads=[t_t], writes=[t_y[b]])
            k.barrier()
            proj_out(w_out[j, 0:1024, :], yv, t_y, G1, blks, f"m4a{j}")
            t_yb = [Tl(f"yb{j}_{b}") for b in range(5)]
            k.dma("sp", convw[:, :], convw_d[j], writes=[t_lpar])
            k.dma("sp", convb[:, :], convb_d[j], writes=[t_lpar])
            k.dma("sp", cng[:, :], cng_d[j], writes=[t_lpar])
            cw3 = convw[:, :].rearrange("p (c k) -> p c k", c=8)
            GW = 2078 + 286
            gbufs = [(slots[1][:, a_ * GW:a_ * GW + 2078], slots[1][:, a_ * GW + 2078:(a_ + 1) * GW]) for a_ in range(2)]
            t_gs = [Tl("g0"), Tl("g1")]
            Dg = TMP[:, 0:1984].bitcast(BF16).rearrange("p (k m) -> p k m", k=31)
            t_dg = Tl("dg")
            for a_ in range(2):
                k.op("pool", lambda e: e.memset(slots[1][:, a_ * GW:(a_ + 1) * GW], 0.0), writes=[t_gs[a_]])
            t_w3 = [TW[0][0], TW[0][1]]

            def wv3(s_):
                base = s_ * 2048
                return (slots[0][:, base:base + 1024].rearrange("p (c n) -> p c n", c=8),
                        slots[0][:, base + 1024:base + 2048].rearrange("p (c n) -> p c n", c=8))

            def load3(i):
                a_, g_ = wv3(i % 2)
                k.dma("pool", a_, win[:, :, 2048 + i * 128:2048 + (i + 1) * 128], writes=[t_w3[i % 2]])
                k.dma("pool", g_, win[:, :, 3072 + i * 128:3072 + (i + 1) * 128], writes=[t_w3[i % 2]], semt=t_w3[i % 2])

            def stage_proj(i):
                if i + 1 < 8:
                    load3(i + 1)
                a_, g_ = wv3(i % 2)
                gL, gC = gbufs[i % 2]
                for (t0, n, v) in blks:
                    b = t0 // 512
                    pa, t_pa = k.ps()
                    pg, t_pg = k.ps()
                    k.mm([t_w3[i % 2], t_H[b]], [t_pa], [dict(out=pa[:, :n], lhsT=a_[:, c, :], rhs=hT[:, c, t0:t0 + n], start=(c == 0), stop=(c == 7)) for c in range(8)])
                    k.mm([t_w3[i % 2], t_H[b]], [t_pg], [dict(out=pg[:, :n], lhsT=g_[:, c, :], rhs=hT[:, c, t0:t0 + n], start=(c == 0), stop=(c == 7)) for c in range(8)])
                    sg, t_sg = nxt("rs", rs, t_rs)
                    k.op("act", lambda e: e.activation(out=sg[:, :n], in_=pg[:, :n], func=AF.Sigmoid), reads=[t_pg], writes=[t_sg])
                    dst = gL[:, 15 + t0:15 + t0 + n] if v == 0 else gC[:, 15:15 + n]
                    k.op("dve", lambda e: e.tensor_tensor(out=dst, in0=sg[:, :n], in1=pa[:, :n], op=ALU.mult), reads=[t_sg, t_pa], writes=[t_gs[i % 2]])

            def stage_conv(i):
                gL, gC = gbufs[i % 2]
                for tap in range(31):
                    k.op("dve", lambda e: e.tensor_scalar(out=Dg[:, tap, :], in0=ident_bf[:, :], scalar1=cw3[:, i, tap:tap + 1], scalar2=None, op0=ALU.mult),
                         reads=[t_par, t_lpar], writes=[t_dg])
                for (t0, n, v) in blks:
                    b = t0 // 512
                    gbuf, o0 = (gL, t0) if v == 0 else (gC, 0)
                    pa_, t_pa_ = k.ps()
                    k.mm([t_dg, t_gs[i % 2]], [t_pa_], [dict(out=pa_[:, :n], lhsT=Dg[:, tap, :], rhs=gbuf[:, o0 + tap:o0 + tap + n], start=(tap == 0), stop=(tap == 30))
                                                        for tap in range(31)])
                    k.op("act", lambda e: e.activation(out=yv[:, i, t0:t0 + n], in_=pa_[:, :n], func=AF.Identity, bias=convb[:, i:i + 1]),
                         reads=[t_pa_, t_lpar], writes=[t_yb[b]])

            load3(0)
            stage_proj(0)
            for i in range(8):
                if i + 1 < 8:
                    stage_proj(i + 1)
                stage_conv(i)
            for (t0, n, v) in blks:
                b = t0 // 512
                rsb, t_r = nxt("rs", rs, t_rs)
                psm, t_ps = k.ps()
                for c in range(8):
                    sqb, t_s = nxt("sq", sq, t_sq)
                    k.op("act", lambda e: e.activation(out=sqb[:, :n], in_=yv[:, c, t0:t0 + n], func=AF.Square), reads=[t_yb[b]], writes=[t_s])
                    k.mm([t_s, t_one], [t_ps], [dict(out=psm[:, :n], lhsT=ones_bf[:, :], rhs=sqb[:, :n], start=(c == 0), stop=(c == 7))])
                k.op("act", lambda e: e.activation(out=rsb[:, :n], in_=psm[:, :n], func=AF.Ln, scale=1.0 / D, bias=EPS), reads=[t_ps], writes=[t_r])
                k.op("act", lambda e: e.activation(out=rsb[:, :n], in_=rsb[:, :n], func=AF.Exp, scale=-0.5), reads=[t_r], writes=[t_r])
                for c in range(8):
                    tb_, t_t = nxt("t32", t32, t_t32)
                    k.op("dve", lambda e: e.scalar_tensor_tensor(out=tb_[:, :n], in0=yv[:, c, t0:t0 + n], scalar=cng[:, c:c + 1], in1=rsb[:, :n],
                                                                op0=ALU.mult, op1=ALU.mult), reads=[t_yb[b], t_r, t_lpar], writes=[t_t])
                    k.op("act", lambda e: e.activation(out=yv[:, c, t0:t0 + n], in_=tb_[:, :n], func=AF.Silu), reads=[t_t], writes=[t_yb[b]])
            k.barrier()
            proj_out(w_out[j, 1024:2048, :], yv, t_yb, G1, blks, f"m4b{j}")


        I32 = mybir.dt.int32
        TWO_PI = 6.283185307179586

        def rope_tables():
            RAf = RA[:, 0:16384].bitcast(F32)
            y = RAf[:, 0:2048]
            yy = RAf[:, 2048:4096]
            kf = RAf[:, 4096:6144]
            ki = RAf[:, 6144:8192].bitcast(I32)
            fidx = sml[:, 40:41]
            invf = sml[:, 41:42]
            t_r = Tl("ropetmp")
            k.dma("sp", y, pos_d[:, :], writes=[t_r])
            k.dma("sp", fidx, fidx_d[:, :], writes=[t_r])
            k.op("act", lambda e: e.activation(out=invf, in_=fidx, func=AF.Exp, scale=-float(np.log(10000.0)) / 16.0), reads=[t_r], writes=[t_r])
            k.op("dve", lambda e: e.tensor_scalar(out=y, in0=y, scalar1=invf, scalar2=1.0 / TWO_PI, op0=ALU.mult, op1=ALU.mult), reads=[t_r], writes=[t_r])
            for shift, dst in ((0.0, sinT), (0.25, cosT)):
                k.op("dve", lambda e: e.tensor_scalar(out=yy, in0=y, scalar1=shift, scalar2=None, op0=ALU.add), reads=[t_r], writes=[t_r])
                k.op("dve", lambda e: e.tensor_copy(out=ki, in_=yy), reads=[t_r], writes=[t_r])
                k.op("dve", lambda e: e.tensor_copy(out=kf, in_=ki), reads=[t_r], writes=[t_r])
                k.op("dve", lambda e: e.tensor_tensor(out=yy, in0=yy, in1=kf, op=ALU.subtract), reads=[t_r], writes=[t_r])
                k.op("dve", lambda e: e.tensor_single_scalar(out=kf, in_=yy, scalar=0.5, op=ALU.is_gt), reads=[t_r], writes=[t_r])
                k.op("dve", lambda e: e.tensor_tensor(out=yy, in0=yy, in1=kf, op=ALU.subtract), reads=[t_r], writes=[t_r])
                k.op("dve", lambda e: e.tensor_single_scalar(out=kf, in_=yy, scalar=-0.5, op=ALU.is_lt), reads=[t_r], writes=[t_r])
                k.op("dve", lambda e: e.tensor_tensor(out=yy, in0=yy, in1=kf, op=ALU.add), reads=[t_r], writes=[t_r])
                k.op("act", lambda e: e.activation(out=dst, in_=yy, func=AF.Sin, scale=TWO_PI * (1.0 - 1e-6)), reads=[t_r], writes=[t_par])
            k.barrier()

        def attention(j, need_ctx):
            blks_q = BLK_L + (BLK_C if need_ctx else [])
            blks_a = BLK_L + BLK_C
            k.dma("sp", qg[:, :], qg_d[j], writes=[t_lpar])
            k.dma("sp", kg[:, :], kg_d[j], writes=[t_lpar])
            k.dma("sp", sinke[:, :], sink_d[j:j + 1, :].partition_broadcast(128), writes=[t_lpar])
            k.op("act", lambda e: e.activation(out=sinke[:, :], in_=sinke[:, :], func=AF.Exp), reads=[], writes=[t_lpar])
            k.npool = 6
            po, t_po = k.psum[6]
            pd, t_pd = k.psum[7]
            qT = RA[:, 0:2 * NT].rearrange("p (h t) -> p h t", h=2)
            kT = RA[:, 2 * NT:3 * NT]
            Vg = RA[:, 3 * NT:3 * NT + 1152].rearrange("p (b d) -> p b d", b=18)
            Pc = RA[:, 3 * NT + 1152:3 * NT + 1152 + 2 * NT].rearrange("p (b t) -> p b t", b=2)
            Pring = TMP[:, :].bitcast(BF16)
            NR = 8
            t_q = [[Tl(f"q{h}_{b}") for b in range(5)] for h in range(4)]
            t_k = [Tl(f"k{b}") for b in range(5)]
            t_v = [Tl(f"v{b}") for b in range(3)]
            t_pc = Tl("pc")
            t_pr = [Tl(f"pr{i}") for i in range(NR)]
            wsrc = wqkv[j].rearrange("(c p) n -> p c n", p=128)
            tq = [TW[0][0], TW[0][1]]
            tv = [TW[1][1], TW[1][2]]

            def wviews(s_):
                base = s_ * 3072
                return (slots[0][:, base:base + 2048].rearrange("p (c n) -> p c n", c=8),
                        slots[0][:, base + 2048:base + 3072].rearrange("p (c n) -> p c n", c=8),
                        slots[1][:, 2048 + s_ * 512:2048 + (s_ + 1) * 512].rearrange("p (c n) -> p c n", c=8))

            def loadw(g):
                a_, b_, c_ = wviews(g % 2)
                t_ = tq[g % 2]
                k.dma("pool", a_, wsrc[:, :, g * 256:(g + 1) * 256], writes=[t_])
                k.dma("pool", b_[:, :, 0:64], wsrc[:, :, 1024 + g * 64:1024 + (g + 1) * 64], writes=[t_])
                k.dma("pool", b_[:, :, 64:128], wsrc[:, :, 1024 + g * 64:1024 + (g + 1) * 64], writes=[t_])
                k.dma("pool", c_, wsrc[:, :, 1280 + g * 64:1280 + (g + 1) * 64], writes=[tv[g % 2]])
            wov = slots[1][:, 0:2048].rearrange("p (h n) -> p h n", h=2)
            t_wo = TW[1][0]

            def qk_chain(projitems, rd, n, gvec, dst, t_dsts, rope, t0):
                ps_ = k.ps()
                k.mm(rd, [ps_[1]], projitems(ps_[0]))
                yield
                sqb, t_s = nxt("sq", sq, t_sq)
                k.op("act", lambda e: e.activation(out=sqb[:, :n], in_=ps_[0][:, :n], func=AF.Square), reads=[ps_[1]], writes=[t_s])
                yield
                pss, t_pss = k.ps()
                k.mm([t_s, t_par], [t_pss], [dict(out=pss[:, :n], lhsT=bd_bf[:, :], rhs=sqb[:, :n], start=True, stop=True)])
                yield
                rsb, t_r = nxt("rs", rs, t_rs)
                k.op("act", lambda e: e.activation(out=rsb[:, :n], in_=pss[:, :n], func=AF.Ln, scale=1.0 / 64, bias=EPS), reads=[t_pss], writes=[t_r])
                yield
                k.op("act", lambda e: e.activation(out=rsb[:, :n], in_=rsb[:, :n], func=AF.Exp, scale=-0.5), reads=[t_r], writes=[t_r])
                yield
                qn, t_qn = nxt("t32", t32, t_t32)
                k.op("dve", lambda e: e.scalar_tensor_tensor(out=qn[:, :n], in0=ps_[0][:, :n], scalar=gvec[:, 0:1], in1=rsb[:, :n],
                                                            op0=ALU.mult, op1=ALU.mult), reads=[ps_[1], t_r, t_lpar], writes=[t_qn])
                yield
                if not rope:
                    k.op("act", lambda e: e.copy(out=dst, in_=qn[:, :n]), reads=[t_qn], writes=t_dsts)
                    return
                qb, t_qb = nxt("tt", tt, t_tt)
                k.op("act", lambda e: e.copy(out=qb[:, :n], in_=qn[:, :n]), reads=[t_qn], writes=[t_qb])
                yield
                psr, t_psr = k.ps()
                k.mm([t_qb, t_par], [t_psr], [dict(out=psr[:, :n], lhsT=perm_bf[:, :], rhs=qb[:, :n], start=True, stop=True)])
                yield
                bb, t_bb = nxt("rs", rs, t_rs)
                k.op("dve", lambda e: e.tensor_tensor(out=bb[:, :n], in0=psr[:, :n], in1=sinT[:, t0:t0 + n], op=ALU.mult), reads=[t_psr, t_par], writes=[t_bb])
                k.op("dve", lambda e: e.tensor_tensor(out=qn[:, :n], in0=qn[:, :n], in1=cosT[:, t0:t0 + n], op=ALU.mult), reads=[t_par], writes=[t_qn])
                yield
                k.op("dve", lambda e: e.tensor_tensor(out=dst, in0=qn[:, :n], in1=bb[:, :n], op=ALU.add), reads=[t_qn, t_bb], writes=t_dsts)

            def lockstep(gens, width=2):
                for i0 in range(0, len(gens), width):
                    active = gens[i0:i0 + width]
                    while active:
                        alive = []
                        for g_ in active:
                            try:
                                next(g_)
                                alive.append(g_)
                            except StopIteration:
                                pass
                        active = alive

            loadw(0)
            for g in range(4):
                if g + 1 < 4:
                    loadw(g + 1)
                k.dma("pool", wov, wo_d[j][g * 256:(g + 1) * 256, :].rearrange("(h p) n -> p h n", p=128), writes=[t_wo])
                wq_, wk_, wv_ = wviews(g % 2)
                t_w = tq[g % 2]
                chains = []
                for (t0, n, v) in blks_a:
                    b = t0 // 512
                    chains.append(qk_chain(lambda pst, t0=t0, n=n: [dict(out=pst[:, :n], lhsT=wk_[:, c, :], rhs=hT[:, c, t0:t0 + n], start=(c == 0), stop=(c == 7)) for c in range(8)],
                                           [t_w, t_H[b]], n, kg, kT[:, t0:t0 + n], [t_k[b]], v == 0, t0))
                for pr in range(2):
                    for (t0, n, v) in blks_q:
                        b = t0 // 512
                        chains.append(qk_chain(lambda pst, t0=t0, n=n, pr=pr: [dict(out=pst[:, :n], lhsT=wq_[:, c, pr * 128:(pr + 1) * 128], rhs=hT[:, c, t0:t0 + n],
                                                                                 start=(c == 0), stop=(c == 7)) for c in range(8)],
                                               [t_w, t_H[b]], n, qg, qT[:, pr, t0:t0 + n], [t_q[2 * pr][b], t_q[2 * pr + 1][b]], v == 0, t0))
                lockstep(chains)
                for vb in range(3):
                    tbs = list(range(vb * 8, min(18, vb * 8 + 8)))
                    ps_ = k.ps()
                    for ii, tb in enumerate(tbs):
                        b = min(tb // 4, 4)
                        k.mm([tv[g % 2], t_H[b]], [ps_[1]], [dict(out=ps_[0][:, ii * 64:(ii + 1) * 64], lhsT=hT[:, c, tb * 128:(tb + 1) * 128], rhs=wv_[:, c, :],
                                                              start=(c == 0), stop=(c == 7)) for c in range(8)])
                    nb = len(tbs)
                    k.op("act", lambda e: e.copy(out=Vg[:, tbs[0]:tbs[0] + nb, :], in_=ps_[0][:, 0:nb * 64].rearrange("p (b d) -> p b d", b=nb)),
                         reads=[ps_[1]], writes=[t_v[vb]])
                for hh in range(4):
                    h = 4 * g + hh
                    pr = hh // 2
                    P0 = (hh % 2) * 64
                    P1 = P0 + 64
                    for kb in range(2):
                        for (t0, n, v) in blks_q:
                            b = t0 // 512
                            ps_ = k.ps()
                            k.mm([t_k[4], t_q[hh][b]], [ps_[1]], [dict(out=ps_[0][:, :n], lhsT=kT[P0:P1, 2048 + kb * 128:2048 + (kb + 1) * 128], rhs=qT[P0:P1, pr, t0:t0 + n],
                                                                    start=True, stop=True)])
                            k.op("act", lambda e: e.activation(out=Pc[:, kb, t0:t0 + n], in_=ps_[0][:, :n], func=AF.Exp, scale=0.125), reads=[ps_[1]], writes=[t_pc])
                    pinfo = {}

                    def pv(i):
                        col = (i % 4) * 128
                        srcs = [(Vg[:, 16 + kb, :], Pc[:, kb, i * 128:(i + 1) * 128], t_pc) for kb in range(2)]
                        for jb in (i - 1, i, i + 1):
                            if 0 <= jb <= 15:
                                pr_, q0_, t_ = pinfo[jb]
                                srcs.append((Vg[:, jb, :], pr_[:, i * 128 - q0_:i * 128 - q0_ + 128], t_))
                        rd = [t_v[0], t_v[1], t_v[2]] + [s_[2] for s_ in srcs]
                        k.mm(rd, [t_po], [dict(out=po[P0:P1, col:col + 128], lhsT=va, rhs=pa, start=(ii == 0), stop=(ii == len(srcs) - 1))
                                          for ii, (va, pa, _) in enumerate(srcs)])
                        k.mm(rd + [t_one], [t_pd], [dict(out=pd[P0:P1, col:col + 128], lhsT=ones_bf[:, 0:64], rhs=pa, start=(ii == 0), stop=(ii == len(srcs) - 1))
                                                    for ii, (va, pa, _) in enumerate(srcs)])
                        if i % 4 == 3:
                            m_ = i // 4
                            finish(m_ * 512, 512, m_)

                    def finish(t0, n, b):
                        dn, t_dn = nxt("rs", rs, t_rs)
                        k.op("act", lambda e: e.activation(out=dn[P0:P1, :n], in_=pd[P0:P1, :n], func=AF.Ln, bias=sinke[P0:P1, h:h + 1]), reads=[t_pd, t_lpar], writes=[t_dn])
                        k.op("act", lambda e: e.activation(out=dn[P0:P1, :n], in_=dn[P0:P1, :n], func=AF.Exp, scale=-1.0), reads=[t_dn], writes=[t_dn])
                        k.op("dve", lambda e: e.tensor_tensor(out=qT[P0:P1, pr, t0:t0 + n], in0=po[P0:P1, :n], in1=dn[P0:P1, :n], op=ALU.mult),
                             reads=[t_po, t_dn], writes=[t_q[hh][b]])

                    for jb in range(16):
                        q0 = max(0, 128 * (jb - 1))
                        q1 = min(NL, 128 * (jb + 2))
                        n = q1 - q0
                        mo = q0 - 128 * (jb - 1)
                        ps_ = k.ps()
                        qb_ = sorted(set([q0 // 512, (q1 - 1) // 512]))
                        k.mm([t_k[jb // 4], t_par] + [t_q[hh][b] for b in qb_], [ps_[1]],
                             [dict(out=ps_[0][:, :n], lhsT=kT[P0:P1, jb * 128:(jb + 1) * 128], rhs=qT[P0:P1, pr, q0:q1], start=True, stop=False),
                              dict(out=ps_[0][:, :n], lhsT=ident_bf[:, :], rhs=mask_bf[:, mo:mo + n], start=False, stop=True)])
                        ri = jb % NR
                        prt = Pring[:, ri * 384:(ri + 1) * 384]
                        k.op("act", lambda e: e.activation(out=prt[:, :n], in_=ps_[0][:, :n], func=AF.Exp, scale=0.125), reads=[ps_[1]], writes=[t_pr[ri]])
                        pinfo[jb] = (prt, q0, t_pr[ri])
                        if jb >= 4:
                            pv(jb - 4)
                    for i_ in range(12, 16):
                        pv(i_)
                    if need_ctx:
                        srcs = [(Vg[:, 16 + kb, :], Pc[:, kb, 2048:2304]) for kb in range(2)]
                        k.mm([t_v[2], t_pc], [t_po], [dict(out=po[P0:P1, 0:256], lhsT=va, rhs=pa, start=(ii == 0), stop=(ii == 1)) for ii, (va, pa) in enumerate(srcs)])
                        k.mm([t_pc, t_one], [t_pd], [dict(out=pd[P0:P1, 0:256], lhsT=ones_bf[:, 0:64], rhs=pa, start=(ii == 0), stop=(ii == 1)) for ii, (va, pa) in enumerate(srcs)])
                        finish(2048, 256, 4)
                for d in range(8):
                    for (t0, n, v) in blks_q:
                        b = t0 // 512
                        pw, t_pw = k.ps()
                        k.mm([t_wo] + [t_q[hh][b] for hh in range(4)], [t_pw],
                             [dict(out=pw[:, :n], lhsT=wov[:, pr, d * 128:(d + 1) * 128], rhs=qT[:, pr, t0:t0 + n], start=(pr == 0), stop=(pr == 1)) for pr in range(2)])
                        k.op("dve", lambda e: e.scalar_tensor_tensor(out=xT[:, d, t0:t0 + n], in0=pw[:, :n], scalar=G1[:, d, v:v + 1],
                                                                    in1=xT[:, d, t0:t0 + n], op0=ALU.mult, op1=ALU.add),
                             reads=[t_pw, t_mT, t_X[b]], writes=[t_X[b]])
                k.barrier()
            k.npool = 8

        def moe(j, blks):
            combT = RA[:, 4 * NT:6 * NT].bitcast(F32)
            cbs = [RA[:, 6 * NT:7 * NT], RA[:, 7 * NT:8 * NT]]
            t_comb = Tl("comb")
            t_cbs = [Tl("cb0"), Tl("cb1")]
            t_cm = Tl("cm")
            cm = TMP[0:8, 0:NT]
            k.dma("sp", router_f[:, :].rearrange("p (c e) -> p c e", c=8), router_d[j].rearrange("(c p) e -> p c e", p=128), writes=[t_lpar])
            make_h(A2v, SH2, blks, moe_j=j, combT=combT, t_comb=t_comb)
            k.barrier()
            for e_ in range(NE):
                k.op("dve", lambda e: e.tensor_scalar(out=cm, in0=combT[0:8, :], scalar1=ident[0:8, e_:e_ + 1], scalar2=None, op0=ALU.mult),
                     reads=[t_comb, t_par], writes=[t_cm])
                for (t0, n, v) in blks:
                    pc_, t_pc_ = k.ps()
                    k.mm([t_cm, t_one], [t_pc_], [dict(out=pc_[:, :n], lhsT=ones_f[0:8, :], rhs=cm[:, t0:t0 + n], start=True, stop=True)])
                    k.op("act", lambda e: e.copy(out=cbs[e_ % 2][:, t0:t0 + n], in_=pc_[:, :n]), reads=[t_pc_], writes=[t_cbs[e_ % 2]])
                ffn(mw1[j, e_], mw3[j, e_], mw2[j, e_], G2, blks, f"moe{j}_{e_}", cb=cbs[e_ % 2], t_cb=t_cbs[e_ % 2], bar=False)
            k.barrier()


        def moe_sparse(j, blks):
            ntok = sum(n for (_, n, _) in blks)
            ntb = ntok // 128
            NS = 768
            hg = RA[:, 0:6144].rearrange("p (c t) -> p c t", c=8)
            uvs = [RA[:, 6144 + a * 1536:6144 + (a + 1) * 1536].rearrange("p (f t) -> p f t", f=2) for a in range(2)]
            hbuf = [RA[:, 9216 + a * 1024:9216 + (a + 1) * 1024] for a in range(3)]
            Sg = [RA[:, 12288 + a * 384:12288 + (a + 1) * 384] for a in range(3)]
            STw = RA[:, 13440:16512].rearrange("p (a t) -> p a t", a=6)
            iota_f = RA[:, 16512:17280].bitcast(F32)
            pos_tm = RA[:, 17280:17568].bitcast(F32).rearrange("p (b e) -> p b e", e=8)
            comb_tm = RA[:, 17568:17856].bitcast(F32).rearrange("p (b e) -> p b e", e=8)
            mask_tm = RA[:, 17856:18000].rearrange("p (b e) -> p b e", e=8)
            cnt_i = RA[:, 18000:18016].bitcast(I32)
            slotid = RA[:, 18016:18052].bitcast(F32)
            CP = TMP[0:16, 0:NT]
            acc = RH[:, 0:12288].bitcast(F32).rearrange("p (c t) -> p c t", c=8)
            otm = RH[:, 12288:18432].rearrange("p (a d) -> p a d", a=6)
            t_rt = Tl("rt")
            t_mc = Tl("mc")
            t_cp = Tl("cp")
            t_hb = [Tl(f"hb{a}") for a in range(3)]
            t_sg = [Tl(f"sg{a}") for a in range(3)]
            t_hg = [Tl("hg0"), Tl("hg1")]
            t_acc = [Tl("acc0"), Tl("acc1")]
            t_otm = [Tl(f"otm{a}") for a in range(6)]
            t_stw = Tl("stw")
            t_hd = Tl("hd")
            k.dma("sp", router_f[:, :].rearrange("p (c e) -> p c e", c=8), router_d[j].rearrange("(c p) e -> p c e", p=128), writes=[t_lpar])
            k.dma("sp", iota_f, iota_d[:, :], writes=[t_mc])
            k.dma("sp", slotid, slotid_d[:, :], writes=[t_mc])
            make_h(A2v, SH2, blks, moe_j=dict(comb_tm=comb_tm, mask_tm=mask_tm, t_rt=t_rt))
            for tbi in range(ntb):
                pp, t_pp = k.ps()
                items = [dict(out=pp[:, 0:8], lhsT=ones_bf[:, :], rhs=mask_tm[:, b_, :], start=(b_ == 0), stop=False) for b_ in range(tbi)]
                items.append(dict(out=pp[:, 0:8], lhsT=triu_bf[:, :], rhs=mask_tm[:, tbi, :], start=(tbi == 0), stop=True))
                k.mm([t_rt, t_one, t_par], [t_pp], items)
                k.op("dve", lambda e: e.scalar_tensor_tensor(out=pos_tm[:, tbi, :], in0=pp[:, 0:8], scalar=1.0, in1=mask_tm[:, tbi, :], op0=ALU.add, op1=ALU.mult),
                     reads=[t_pp], writes=[t_rt])
                k.op("dve", lambda e: e.tensor_scalar(out=pos_tm[:, tbi, :], in0=pos_tm[:, tbi, :], scalar1=-1.0, scalar2=None, op0=ALU.add), writes=[t_rt])
                cp16 = sml[:, 48:64]
                k.op("dve", lambda e: e.tensor_copy(out=cp16[:, 0:8], in_=comb_tm[:, tbi, :]), reads=[t_rt], writes=[t_sml])
                k.op("dve", lambda e: e.tensor_copy(out=cp16[:, 8:16], in_=pos_tm[:, tbi, :]), reads=[t_rt], writes=[t_sml])
                pst, t_pst = k.ps()
                k.mm([t_sml, t_par], [t_pst], [dict(out=pst[0:16, 0:128], lhsT=cp16, rhs=ident[:, :], start=True, stop=True, is_transpose=True)])
                k.op("act", lambda e: e.copy(out=CP[0:16, tbi * 128:(tbi + 1) * 128], in_=pst[0:16, 0:128]), reads=[t_pst], writes=[t_cp])
            pcn, t_pcn = k.ps()
            k.mm([t_rt, t_one], [t_pcn], [dict(out=pcn[:, 0:8], lhsT=ones_bf[:, :], rhs=mask_tm[:, b_, :], start=(b_ == 0), stop=(b_ == ntb - 1)) for b_ in range(ntb)])
            t_cnt = Tl("cnt")
            k.op("dve", lambda e: e.tensor_copy(out=cnt_i, in_=pcn[:, 0:8]), reads=[t_pcn], writes=[t_cnt])
            for tb in range(ntb):
                b = min(tb // 4, 4)
                pt, t_pt = k.ps()
                ptb = pt[:, :].bitcast(BF16)
                k.mm([t_H[b], t_par], [t_pt], [dict(out=ptb[:, c * 128:(c + 1) * 128], lhsT=hT[:, c, tb * 128:(tb + 1) * 128], rhs=ident_bf[:, :],
                                                     start=True, stop=True, is_transpose=True) for c in range(8)])
                hb, t_h = hbuf[tb % 3], t_hb[tb % 3]
                k.op("act" if tb % 2 == 0 else "dve", (lambda e: e.copy(out=hb, in_=ptb[:, 0:1024])) if tb % 2 == 0 else (lambda e: e.tensor_copy(out=hb, in_=ptb[:, 0:1024])),
                     reads=[t_pt], writes=[t_h])
                k.dma("sp", h_tm_d[tb], hb, reads=[t_h], semt=t_hd)
            k.barrier()

            w_srcs = None

            def pass_body(e_, p_, blocks, nsb):
                spb = nsb // 2
                W1d, W3d, W2d = mw1[j, e_], mw3[j, e_], mw2[j, e_]
                w1s = W1d.rearrange("(c p) n -> p c n", p=128)
                w3s = W3d.rearrange("(c p) n -> p c n", p=128)
                w2s = W2d.rearrange("(f p) n -> p f n", p=128)

                def views(a):
                    sl = slots[a]
                    return (sl[:, 0:2048].rearrange("p (c n) -> p c n", c=8), sl[:, 2048:4096].rearrange("p (c n) -> p c n", c=8),
                            sl[:, 4096:6144].rearrange("p (f n) -> p f n", f=2))

                def load(i):
                    a, b_, c_ = views(i % 2)
                    tw = TW[i % 2]
                    k.dma("pool", a, w1s[:, :, i * 256:(i + 1) * 256], writes=[tw[0]])
                    k.dma("pool", b_, w3s[:, :, i * 256:(i + 1) * 256], writes=[tw[1]])
                    k.dma("pool", c_, w2s[:, 2 * i:2 * i + 2, :], writes=[tw[2]])
                load(0)
                hi = 0
                for bi, (s0, sn) in enumerate(blocks):
                    accs = [k.psum[c] for c in range(8)]
                    for tb in range(ntb):
                        hb, t_h = hbuf[hi % 3], t_hb[hi % 3]
                        sg, t_s = Sg[hi % 3], t_sg[hi % 3]
                        hi += 1
                        k.dma("sp", hb, h_tm_d[tb], writes=[t_h])
                        k.op("dve", lambda e: e.tensor_scalar(out=sg[:, 0:sn], in0=iota_f[:, 0:sn], scalar1=float(NS * p_ + s0), scalar2=pos_tm[:, tb, e_:e_ + 1],
                                                              op0=ALU.add, op1=ALU.is_equal), reads=[t_mc, t_rt], writes=[t_s])
                        for c in range(8):
                            k.mm([t_h, t_s], [accs[c][1]], [dict(out=accs[c][0][:, 0:sn], lhsT=hb[:, c * 128:(c + 1) * 128], rhs=sg[:, 0:sn], start=(tb == 0), stop=(tb == ntb - 1))])
                    for c in range(8):
                        if c % 2 == 0:
                            k.op("act", lambda e: e.copy(out=hg[:, c, s0:s0 + sn], in_=accs[c][0][:, 0:sn]), reads=[accs[c][1]], writes=[t_hg[bi]])
                        else:
                            k.op("dve", lambda e: e.tensor_copy(out=hg[:, c, s0:s0 + sn], in_=accs[c][0][:, 0:sn]), reads=[accs[c][1]], writes=[t_hg[bi]])
                for i in range(14):
                    if i + 1 < 14:
                        load(i + 1)
                    a = i % 2
                    w1v, w3v, w2v = views(a)
                    tw = TW[a]
                    uv = uvs[a]
                    for fc in range(2):
                        for bi, (s0, sn) in enumerate(blocks):
                            p1, t_p1 = k.ps()
                            p3, t_p3 = k.ps()
                            k.mm([tw[0], t_hg[bi]], [t_p1], [dict(out=p1[:, :sn], lhsT=w1v[:, c, fc * 128:(fc + 1) * 128], rhs=hg[:, c, s0:s0 + sn],
                                                                 start=(c == 0), stop=(c == 7)) for c in range(8)])
                            k.mm([tw[1], t_hg[bi]], [t_p3], [dict(out=p3[:, :sn], lhsT=w3v[:, c, fc * 128:(fc + 1) * 128], rhs=hg[:, c, s0:s0 + sn],
                                                                 start=(c == 0), stop=(c == 7)) for c in range(8)])
                            gb, t_g = nxt("gt", gt, t_gt)
                            k.op("act", lambda e: e.activation(out=gb[:, :sn], in_=p1[:, :sn], func=AF.Silu), reads=[t_p1], writes=[t_g])
                            k.op("dve", lambda e: e.tensor_tensor(out=uv[:, fc, s0:s0 + sn], in0=gb[:, :sn], in1=p3[:, :sn], op=ALU.mult),
                                 reads=[t_g, t_p3], writes=[TU[a][bi]])
                    for d in range(8):
                        for bi, (s0, sn) in enumerate(blocks):
                            po_, t_po_ = k.ps()
                            k.mm([tw[2], TU[a][bi]], [t_po_], [dict(out=po_[:, :sn], lhsT=w2v[:, fc, d * 128:(d + 1) * 128], rhs=uv[:, fc, s0:s0 + sn],
                                                                   start=(fc == 0), stop=(fc == 1)) for fc in range(2)])
                            if i == 0:
                                k.op("act", lambda e: e.copy(out=acc[:, d, s0:s0 + sn], in_=po_[:, :sn]), reads=[t_po_], writes=[t_acc[bi]])
                            else:
                                k.op("dve", lambda e: e.tensor_tensor(out=acc[:, d, s0:s0 + sn], in0=po_[:, :sn], in1=acc[:, d, s0:s0 + sn], op=ALU.add),
                                     reads=[t_po_], writes=[t_acc[bi]])
                for sb_ in range(nsb):
                    for half in range(2):
                        pt, t_pt = k.ps()
                        k.mm([t_acc[sb_ // spb], t_par], [t_pt],
                             [dict(out=pt[:, dd * 128:(dd + 1) * 128], lhsT=acc[:, half * 4 + dd, sb_ * 128:(sb_ + 1) * 128], rhs=ident[:, :],
                                   start=True, stop=True, is_transpose=True) for dd in range(4)])
                        if half == 0:
                            k.op("act", lambda e: e.copy(out=otm[:, sb_, 0:512], in_=pt[:, :]), reads=[t_pt], writes=[t_otm[sb_]])
                        else:
                            k.op("dve", lambda e: e.tensor_copy(out=otm[:, sb_, 512:1024], in_=pt[:, :]), reads=[t_pt], writes=[t_otm[sb_]])
                for (t0, n, v) in blks:
                    b = t0 // 512
                    cmt, t_c = nxt("t32", t32, t_t32)
                    k.op("dve", lambda e: e.tensor_scalar(out=cmt[0:16, :n], in0=CP[0:16, t0:t0 + n], scalar1=ident[0:16, e_:e_ + 1], scalar2=None, op0=ALU.mult),
                         reads=[t_cp, t_par], writes=[t_c])
                    pc_, t_pc_ = k.ps()
                    k.mm([t_c, t_one], [t_pc_], [dict(out=pc_[:, :n], lhsT=ones_f[0:16, :], rhs=cmt[0:16, :n], start=True, stop=True)])
                    cbb, t_cb = nxt("rs", rs, t_rs)
                    k.op("act", lambda e: e.copy(out=cbb[:, :n], in_=pc_[:, :n]), reads=[t_pc_], writes=[t_cb])
                    pmt, t_p = nxt("t32", t32, t_t32)
                    k.op("dve", lambda e: e.tensor_scalar(out=pmt[0:16, :n], in0=CP[0:16, t0:t0 + n], scalar1=ident[0:16, 8 + e_:9 + e_], scalar2=None, op0=ALU.mult),
                         reads=[t_cp, t_par], writes=[t_p])
                    pp_, t_pp_ = k.ps()
                    k.mm([t_p, t_one], [t_pp_], [dict(out=pp_[:, :n], lhsT=ones_f[0:16, :], rhs=pmt[0:16, :n], start=True, stop=True)])
                    for sb_ in range(nsb):
                        kk = 6 * p_ + sb_
                        k.op("dve", lambda e: e.scalar_tensor_tensor(out=STw[:, sb_, :n], in0=pp_[:, :n], scalar=slotid[:, kk:kk + 1], in1=cbb[:, :n],
                                                                    op0=ALU.is_equal, op1=ALU.mult), reads=[t_pp_, t_cb, t_mc], writes=[t_stw])
                    for d in range(8):
                        px, t_px = k.ps()
                        k.mm([t_stw] + t_otm[:nsb], [t_px], [dict(out=px[:, :n], lhsT=otm[:, sb_, d * 128:(d + 1) * 128], rhs=STw[:, sb_, :n],
                                                           start=(sb_ == 0), stop=(sb_ == nsb - 1)) for sb_ in range(nsb)])
                        k.op("dve", lambda e: e.scalar_tensor_tensor(out=xT[:, d, t0:t0 + n], in0=px[:, :n], scalar=G2[:, d, v:v + 1],
                                                                    in1=xT[:, d, t0:t0 + n], op0=ALU.mult, op1=ALU.add),
                             reads=[t_px, t_mT, t_X[b]], writes=[t_X[b]])

            npass = (ntok + NS - 1) // NS
            cnt_f = sml[:, 0:8]
            flg_f2 = TMP[:, 2304:2376]
            flg_f = flg_f2.rearrange("p (a e) -> p a e", e=8)
            flg_i = TMP[:, 2376:2448].bitcast(I32)
            t_flg = Tl("flg")
            k.op("dve", lambda e: e.tensor_copy(out=cnt_f, in_=cnt_i), reads=[t_cnt], writes=[t_sml])
            for p_ in range(3):
                k.op("dve", lambda e: e.tensor_single_scalar(out=flg_f[:, 2 * p_, :], in_=cnt_f, scalar=float(NS * p_ + 512), op=ALU.is_gt), reads=[t_sml], writes=[t_flg])
                k.op("dve", lambda e: e.tensor_single_scalar(out=flg_f[:, 2 * p_ + 1, :], in_=cnt_f, scalar=float(NS * p_), op=ALU.is_gt), reads=[t_sml], writes=[t_flg])
                k.op("dve", lambda e: e.tensor_tensor(out=flg_f[:, 2 * p_ + 1, :], in0=flg_f[:, 2 * p_ + 1, :], in1=flg_f[:, 2 * p_, :], op=ALU.subtract), writes=[t_flg])
            k.op("dve", lambda e: e.tensor_single_scalar(out=flg_f[:, 6, :], in_=cnt_f, scalar=float(NS), op=ALU.is_gt), reads=[t_sml], writes=[t_flg])
            k.op("dve", lambda e: e.tensor_single_scalar(out=flg_f[:, 7, :], in_=cnt_f, scalar=float(2 * NS), op=ALU.is_gt), reads=[t_sml], writes=[t_flg])
            k.op("dve", lambda e: e.tensor_tensor(out=flg_f[:, 8, :], in0=flg_f[:, 6, :], in1=flg_f[:, 0, :], op=ALU.subtract), writes=[t_flg])
            k.op("dve", lambda e: e.tensor_scalar(out=flg_f[:, 8, :], in0=flg_f[:, 8, :], scalar1=1.0, scalar2=None, op0=ALU.add), writes=[t_flg])
            k.op("dve", lambda e: e.tensor_copy(out=flg_i, in_=flg_f2), writes=[t_flg])
            regs = moe_regs
            variants = [([(0, 384), (384, 384)], 6), ([(0, 256), (256, 256)], 4)]

            def load_flag(row, e_):
                col = row * 8 + e_
                for r_ in regs:
                    en = {"Pool": "pool", "Activation": "act", "PE": "pe", "DVE": "dve", "SP": "sp"}[str(r_.engine).split(".")[-1]]
                    E = k.E[en]
                    k._need(E, k._deps([t_flg], []))
                    E.h.load(r_, flg_i[0:1, col:col + 1])

            def bump(deltas):
                for (en, h_, dv) in deltas:
                    k.E[en].h.sem_inc(h_, dv)

            def region(row, e_, body):
                load_flag(row, e_)
                k.region_begin()
                with nc.If_ne(regs, 0):
                    body()
                    deltas = k.region_end()
                with nc.Else():
                    bump(deltas)

            def run_pass(e_, p_):
                for vi, (blocks_v, nsb_v) in enumerate(variants):
                    region(2 * p_ + vi, e_, lambda: pass_body(e_, p_, blocks_v, nsb_v))

            def run_from(e_, p_):
                def body():
                    run_pass(e_, p_)
                    if p_ + 1 < npass:
                        run_from(e_, p_ + 1)
                region(5 + p_, e_, body)

            for e_ in range(NE):
                region(0, e_, lambda: pass_body(e_, 0, variants[0][0], variants[0][1]))

                def rest():
                    region(1, e_, lambda: pass_body(e_, 0, variants[1][0], variants[1][1]))
                    if npass > 1:
                        run_from(e_, 1)
                region(8, e_, rest)
            k.barrier()

        moe_regs = nc.alloc_registers("cnt")
        import os
        stop = int(os.environ.get("KSTOP", "99"))
        phase = 0
        for li in range(4):
            j = li // 2
            need_ctx = li < 3
            blks = BLK_L + (BLK_C if need_ctx else [])
            layer_params(li)
            if li % 2 == 0:
                make_h(A1v, SH1, blks)
                k.barrier()
                even_mixer(j, blks)
                phase += 1
                if phase >= stop:
                    break
                make_h(A2v, SH2, blks)
                k.barrier()
                ffn(ffw1[j], ffw3[j], ffw2[j], G2, blks, f"ff{j}")
                phase += 1
                if phase >= stop:
                    break
            else:
                if li == 1:
                    rope_tables()
                make_h(A1v, SH1, BLK_L + BLK_C)
                k.barrier()
                attention(j, need_ctx)
                phase += 1
                if phase >= stop:
                    break
                moe_sparse(j, blks)
                phase += 1
                if phase >= stop:
                    break

        k.barrier()
        t_out = Tl("out")
        for b in range(5):
            t0, n, _ = (BLK_L + BLK_C)[b]
            k.dma("sp", out_d[:, t0:t0 + n].rearrange("(c p) t -> p c t", p=128), xT[:, :, t0:t0 + n], reads=[t_X[b]], semt=t_out)
        k.E["sp"].h.wait_ge(t_out.dsem, t_out.dcnt)
    return nc


def _prep(inputs, b):
    f = np.float32
    x, ctx, c, c_ctx = inputs["x"], inputs["ctx"], inputs["c"], inputs["c_ctx"]
    m = {}
    m["xin"] = np.ascontiguousarray(np.concatenate([x[b], ctx[b]], axis=0).T)
    cvv = np.stack([c[b].reshape(8, 128).T, c_ctx.reshape(8, 128).T], axis=-1)
    m["cv"] = np.ascontiguousarray(cvv.reshape(128, 16))
    return m


def _shared(inputs):
    m = {}
    g = lambda a: np.ascontiguousarray(a, dtype=np.float32)
    m["ada_w"] = g(inputs["ada_w"])
    m["ada_b"] = g(inputs["ada_b"].reshape(4, 48, 128).transpose(0, 2, 1))
    m["ng1"] = g(inputs["norm_mix_g"].reshape(4, 8, 128).transpose(0, 2, 1))
    m["ng2"] = g(inputs["norm_ffn_g"].reshape(4, 8, 128).transpose(0, 2, 1))
    m["ev_w_in"] = g(inputs["ev_w_in"])
    m["ev_ln_g"] = g(inputs["ev_ln_g"].reshape(2, 8, 128).transpose(0, 2, 1))
    m["ev_ln_b"] = g(inputs["ev_ln_b"].reshape(2, 8, 128).transpose(0, 2, 1))
    m["ev_wsT"] = g(inputs["ev_ws"].transpose(0, 1, 3, 2))
    m["ev_bs"] = g(inputs["ev_bs"].reshape(2, 1024))
    m["ev_conv_w"] = g(inputs["ev_conv_w"].transpose(0, 2, 1).reshape(2, 8, 128, 31).transpose(0, 2, 1, 3).reshape(2, 128, 248))
    m["ev_conv_b"] = g(inputs["ev_conv_b"].reshape(2, 8, 128).transpose(0, 2, 1))
    m["ev_cnorm_g"] = g(inputs["ev_cnorm_g"].reshape(2, 8, 128).transpose(0, 2, 1))
    m["ev_w_out"] = g(inputs["ev_w_out"])
    m["od_w_qkv"] = g(inputs["od_w_qkv"])
    m["od_q_g"] = g(np.concatenate([inputs["od_q_g"], inputs["od_q_g"]], axis=1).reshape(2, 128, 1))
    m["od_k_g"] = g(np.concatenate([inputs["od_k_g"], inputs["od_k_g"]], axis=1).reshape(2, 128, 1))
    m["od_sink"] = g(inputs["od_sink"])
    m["od_w_o"] = g(inputs["od_w_o"])
    m["ff_w1"] = g(inputs["ff_w1"])
    m["ff_w3"] = g(inputs["ff_w3"])
    m["ff_w2"] = g(inputs["ff_w2"])
    m["moe_router"] = g(inputs["moe_router"])
    m["moe_w1"] = g(inputs["moe_w1"])
    m["moe_w3"] = g(inputs["moe_w3"])
    m["moe_w2"] = g(inputs["moe_w2"])
    m["c_ident"] = np.eye(128, dtype=np.float32)
    pm = np.zeros((64, 64), np.float32)
    for p in range(64):
        r = p % 32
        if r < 16:
            pm[p + 16, p] = -1.0
        else:
            pm[p - 16, p] = 1.0
    pm2 = np.zeros((128, 128), np.float32)
    pm2[0:64, 0:64] = pm
    pm2[64:128, 64:128] = pm
    m["c_perm"] = pm2
    bd = np.zeros((128, 128), np.float32)
    bd[0:64, 0:64] = 1.0
    bd[64:128, 64:128] = 1.0
    m["c_bd"] = bd
    kk = np.arange(128)[:, None]
    qq = np.arange(384)[None, :] - 128
    m["c_mask"] = np.where(np.abs(qq - kk) <= 128, 0.0, -240000.0).astype(np.float32)
    t = np.arange(2048)
    pos = np.zeros((64, 2048), np.float32)
    pos[0:32, :] = (t // 64)[None, :]
    pos[32:64, :] = (t % 64)[None, :]
    m["c_pos"] = np.concatenate([pos, pos], axis=0)
    m["c_fidx"] = (np.arange(128) % 16).astype(np.float32).reshape(128, 1)
    m["c_triu"] = (np.arange(128)[:, None] < np.arange(128)[None, :]).astype(np.float32)
    m["c_iota"] = np.tile(np.arange(384, dtype=np.float32)[None, :], (128, 1))
    m["c_slotid"] = (np.arange(128, dtype=np.float32)[:, None] + 128.0 * np.arange(18, dtype=np.float32)[None, :])
    return m


_NC_CACHE = {}


def kernel(**inputs):
    inputs = {k_: np.asarray(v) for k_, v in inputs.items()}
    ncores = 8
    if "nc" not in _NC_CACHE:
        _NC_CACHE["nc"] = build()
    nc = _NC_CACHE["nc"]
    shared = _shared(inputs)
    in_maps = []
    for b in range(ncores):
        m = dict(shared)
        m.update(_prep(inputs, b))
        in_maps.append(m)
    res = run_bass_kernel_spmd(nc, in_maps, core_ids=list(range(ncores)))
    outs = [np.asarray(r["out"]) for r in res.results]
    return np.stack([o[:, :NL].T for o in outs], axis=0).astype(np.float32)
```
